# Optimizing a Trainium2 kernel written in Bass

```python
import math
import jax, jax.numpy as jnp
from jax import lax
import numpy as np

D_MODEL = 1024
BATCH = 4
SEQ = 8192
DEPTH = 2

GRID_W = 64
CTX_LEN = 256
CHUNK = 128
Q_BLOCK = 128
EPS = 1e-6
ROPE_BASE = 10000.0
ML_HEADS = 4
ML_DQK = D_MODEL // (2 * ML_HEADS)
ML_DV = D_MODEL // (2 * ML_HEADS)
RET_HEADS = 4
RET_DQK = D_MODEL // (2 * RET_HEADS)
RET_DV = D_MODEL // (2 * RET_HEADS)
DA_HEADS = 8
DA_DHEAD = D_MODEL // (2 * DA_HEADS)
DA_DV = 2 * DA_DHEAD
N_GROUPS = 4
EXPERTS_PER_GROUP = 8
N_EXPERTS = N_GROUPS * EXPERTS_PER_GROUP
TOP_K = 2
D_EXPERT = D_MODEL // 2
MOE_BLOCK = 256
N_EVEN = (DEPTH + 1) // 2
N_ODD = DEPTH // 2
EVEN_SPLITS = (ML_HEADS * ML_DQK, ML_HEADS * ML_DQK, ML_HEADS * ML_DV, ML_HEADS * ML_DV,
               2 * ML_HEADS, 2 * ML_HEADS,
               RET_HEADS * RET_DQK, RET_HEADS * RET_DQK, RET_HEADS * RET_DV, RET_HEADS * RET_DV)
EVEN_IN = sum(EVEN_SPLITS)
EVEN_SPLIT_IDX = tuple(int(s) for s in np.cumsum(EVEN_SPLITS)[:-1])
EVEN_MIX_W = ML_HEADS * ML_DV + RET_HEADS * RET_DV
ODD_IN = 4 * DA_HEADS * DA_DHEAD + DA_HEADS * DA_DV
ODD_MIX_W = DA_HEADS * DA_DV

kernel_name = 'hybrid_mlstm_retention_diffattn_hmoe_dit'

F32 = jnp.float32


def rms_norm(x, g):
    xf = x.astype(F32)
    y = xf * lax.rsqrt(jnp.mean(xf * xf, -1, keepdims=True) + EPS)
    return (y * g.astype(F32)).astype(x.dtype)


def adaln(h, g, shift, scale):
    return rms_norm(h, g) * (1 + scale) + shift


def head_layer_norm(x, g):
    xf = x.astype(F32)
    mu = jnp.mean(xf, -1, keepdims=True)
    var = jnp.mean(jnp.square(xf - mu), -1, keepdims=True)
    return ((xf - mu) * lax.rsqrt(var + EPS) * g[:, None, :].astype(F32)).astype(x.dtype)


def head_rms_norm(x, g):
    xf = x.astype(F32)
    y = xf * lax.rsqrt(jnp.mean(xf * xf, -1, keepdims=True) + EPS)
    return (y * g[:, None, :].astype(F32)).astype(x.dtype)


def modulation(cond, w, b):
    m = jax.nn.silu(cond) @ w + b
    return m.reshape(*cond.shape[:-1], 6, D_MODEL)


def rope_angles(pos, n_freq):
    inv = ROPE_BASE ** (-jnp.arange(n_freq, dtype=F32) / n_freq)
    return pos.astype(F32)[:, None] * inv[None, :]


def apply_rotary(x, ang):
    x1, x2 = jnp.split(x.astype(F32), 2, axis=-1)
    cos, sin = jnp.cos(ang), jnp.sin(ang)
    return jnp.concatenate([x1 * cos - x2 * sin, x1 * sin + x2 * cos], -1).astype(x.dtype)


def rope_2d(x, rows, cols):
    half = x.shape[-1] // 2
    nf = half // 2
    return jnp.concatenate([apply_rotary(x[..., :half], rope_angles(rows, nf)),
                            apply_rotary(x[..., half:], rope_angles(cols, nf))], -1)


def to_chunks(a):
    B, H, T = a.shape[:3]
    return jnp.moveaxis(a.reshape(B, H, T // CHUNK, CHUNK, *a.shape[3:]), 2, 0)


def from_chunks(a):
    a = jnp.moveaxis(a, 0, 2)
    return a.reshape(a.shape[0], a.shape[1], -1, a.shape[-1])


def mlstm_scan(q, k, v, ig, lf, state):
    xs = tuple(to_chunks(a.astype(F32)) for a in (q, k, v, ig, lf))
    causal = jnp.tril(jnp.ones((CHUNK, CHUNK), bool))

    def step(carry, inp):
        C, n, m = carry
        qc, kc, vc, ic, fc = inp
        b = jnp.cumsum(fc, -1)
        dlog = jnp.where(causal, b[..., :, None] - b[..., None, :] + ic[..., None, :], -jnp.inf)
        inter = b + m[..., None]
        m_t = jnp.maximum(inter, jnp.max(dlog, -1))
        s = jnp.einsum('bhtk,bhsk->bhts', qc, kc) * jnp.exp(dlog - m_t[..., None])
        w_inter = jnp.exp(inter - m_t)
        numer = jnp.einsum('bhts,bhsv->bhtv', s, vc) + w_inter[..., None] * jnp.einsum('bhtk,bhvk->bhtv', qc, C)
        denom = jnp.sum(s, -1) + w_inter * jnp.einsum('bhtk,bhk->bht', qc, n)
        h = numer / jnp.maximum(jnp.abs(denom), jnp.exp(-m_t))[..., None]
        b_last = b[..., -1]
        w_log = b_last[..., None] - b + ic
        m_new = jnp.maximum(b_last + m, jnp.max(w_log, -1))
        decay = jnp.exp(b_last + m - m_new)
        w = jnp.exp(w_log - m_new[..., None])
        C = decay[..., None, None] * C + jnp.einsum('bhsv,bhsk->bhvk', vc * w[..., None], kc)
        n = decay[..., None] * n + jnp.einsum('bhs,bhsk->bhk', w, kc)
        return (C, n, m_new), h

    state, h = lax.scan(step, state, xs)
    return from_chunks(h).astype(q.dtype), state


def retention_scan(q, k, v, log_gamma, R):
    xs = tuple(to_chunks(a.astype(F32)) for a in (q, k, v))
    lg = log_gamma.astype(F32)
    idx = jnp.arange(CHUNK, dtype=F32)
    causal = jnp.tril(jnp.ones((CHUNK, CHUNK), bool))
    dist = jnp.maximum(idx[:, None] - idx[None, :], 0.0)
    intra = jnp.where(causal, jnp.exp(lg[:, None, None] * dist), 0.0)
    q_decay = jnp.exp(lg[:, None] * (idx + 1.0))
    k_decay = jnp.exp(lg[:, None] * (CHUNK - 1.0 - idx))
    chunk_decay = jnp.exp(lg * CHUNK)

    def step(R, inp):
        qc, kc, vc = inp
        s = jnp.einsum('bhtk,bhsk->bhts', qc, kc) * intra
        o = jnp.einsum('bhts,bhsv->bhtv', s, vc) + q_decay[..., None] * jnp.einsum('bhtk,bhkv->bhtv', qc, R)
        R = chunk_decay[:, None, None] * R + jnp.einsum('bhsk,bhsv->bhkv', kc * k_decay[..., None], vc)
        return R, o

    R, o = lax.scan(step, R, xs)
    return from_chunks(o).astype(q.dtype), R


def mlstm_retention_mixer(u_ctx, u_lat, w_in, gate_b, ml_g, ret_ld, ret_g, w_out):
    B, L, _ = u_ctx.shape
    S = u_lat.shape[1]

    def project(u, pos):
        T = u.shape[1]
        mq, mk, mv, mo, mi, mf, rq, rk, rv, rg = jnp.split(u @ w_in, EVEN_SPLIT_IDX, axis=-1)
        heads = lambda a, H: a.reshape(B, T, H, -1).transpose(0, 2, 1, 3)
        ang = rope_angles(pos, RET_DQK // 2)
        ig = (mi.reshape(B, T, 2, ML_HEADS) + gate_b[:, 0]).astype(F32).transpose(2, 0, 3, 1)
        lf = jax.nn.log_sigmoid((mf.reshape(B, T, 2, ML_HEADS) + gate_b[:, 1]).astype(F32)).transpose(2, 0, 3, 1)
        return dict(mq=heads(mq, ML_HEADS), mk=heads(mk, ML_HEADS) * (ML_DQK ** -0.5), mv=heads(mv, ML_HEADS),
                    mo=jax.nn.sigmoid(heads(mo, ML_HEADS)), mi=ig, mf=lf,
                    rq=apply_rotary(heads(rq, RET_HEADS), ang),
                    rk=apply_rotary(heads(rk, RET_HEADS), ang) * (RET_DQK ** -0.5),
                    rv=heads(rv, RET_HEADS), rg=rg)

    pc = project(u_ctx, jnp.arange(L))
    pl = project(u_lat, L + jnp.arange(S))
    ml_c = ml_l = ret_c = ret_l = 0.0
    for d in range(2):
        fl = (lambda a: a) if d == 0 else (lambda a: jnp.flip(a, 2))
        st0 = (jnp.zeros((B, ML_HEADS, ML_DV, ML_DQK), F32), jnp.zeros((B, ML_HEADS, ML_DQK), F32),
               jnp.zeros((B, ML_HEADS), F32))
        hc, st = mlstm_scan(fl(pc['mq']), fl(pc['mk']), fl(pc['mv']), fl(pc['mi'][d]), fl(pc['mf'][d]), st0)
        hl, _ = mlstm_scan(fl(pl['mq']), fl(pl['mk']), fl(pl['mv']), fl(pl['mi'][d]), fl(pl['mf'][d]), st)
        ml_c = ml_c + fl(hc)
        ml_l = ml_l + fl(hl)
        R0 = jnp.zeros((B, RET_HEADS, RET_DQK, RET_DV), F32)
        oc, R = retention_scan(fl(pc['rq']), fl(pc['rk']), fl(pc['rv']), ret_ld[d], R0)
        ol, _ = retention_scan(fl(pl['rq']), fl(pl['rk']), fl(pl['rv']), ret_ld[d], R)
        ret_c = ret_c + fl(oc)
        ret_l = ret_l + fl(ol)

    def merge(p, ml, ret):
        T = ml.shape[2]
        ml = head_layer_norm(p['mo'] * ml, ml_g).transpose(0, 2, 1, 3).reshape(B, T, -1)
        ret = jax.nn.silu(p['rg']) * head_layer_norm(ret, ret_g).transpose(0, 2, 1, 3).reshape(B, T, -1)
        return jnp.concatenate([ml, ret], -1) @ w_out

    return merge(pc, ml_c, ret_c), merge(pl, ml_l, ret_l)


def diff_attend(q, k, v, lam):
    B, H, _, T, d = q.shape
    nb = T // Q_BLOCK
    qb = jnp.moveaxis(q.reshape(B, H, 2, nb, Q_BLOCK, d), 3, 0)
    scale = d ** -0.5

    def block(qi):
        s = jnp.einsum('bhmqd,bhmkd->bhmqk', qi, k).astype(F32) * scale
        p = jax.nn.softmax(s, -1)
        a = p[:, :, 0] - lam * p[:, :, 1]
        return jnp.einsum('bhqk,bhkv->bhqv', a.astype(v.dtype), v)

    o = lax.map(block, qb)
    return jnp.moveaxis(o, 0, 2).reshape(B, H, T, -1)


def diff_attention_mixer(u_ctx, u_lat, rows, cols, w_in, lam_p, norm_g, w_out, lam_init, with_ctx):
    B = u_ctx.shape[0]

    def project(u):
        T = u.shape[1]
        q, k, v = jnp.split(u @ w_in, [2 * DA_HEADS * DA_DHEAD, 4 * DA_HEADS * DA_DHEAD], axis=-1)
        q = q.reshape(B, T, DA_HEADS, 2, DA_DHEAD).transpose(0, 2, 3, 1, 4)
        k = k.reshape(B, T, DA_HEADS, 2, DA_DHEAD).transpose(0, 2, 3, 1, 4)
        v = v.reshape(B, T, DA_HEADS, DA_DV).transpose(0, 2, 1, 3)
        return q, k, v

    qc, kc, vc = project(u_ctx)
    ql, kl, vl = project(u_lat)
    ql = rope_2d(ql, rows, cols)
    kl = rope_2d(kl, rows, cols)
    lp = lam_p.astype(F32)
    lam = jnp.exp(jnp.sum(lp[0] * lp[1])) - jnp.exp(jnp.sum(lp[2] * lp[3])) + lam_init

    def finish(o):
        T = o.shape[2]
        o = head_rms_norm(o, norm_g) * (1.0 - lam_init)
        return o.transpose(0, 2, 1, 3).reshape(B, T, -1) @ w_out

    y_lat = finish(diff_attend(ql, jnp.concatenate([kl, kc], 3), jnp.concatenate([vl, vc], 2), lam))
    y_ctx = finish(diff_attend(qc, kc, vc, lam)) if with_ctx else None
    return y_ctx, y_lat


def routed_experts(x, e, g, w1, w3, w2):
    N, D = x.shape
    A = e.shape[0]
    nblk = -(-A // MOE_BLOCK) + N_EXPERTS
    P = nblk * MOE_BLOCK
    tok = jnp.repeat(jnp.arange(N, dtype=jnp.int32), TOP_K)
    order = jnp.argsort(e)
    e_s = e[order]
    counts = jnp.bincount(e, length=N_EXPERTS)
    padded = (counts + MOE_BLOCK - 1) // MOE_BLOCK * MOE_BLOCK
    start = jnp.cumsum(counts) - counts
    pend = jnp.cumsum(padded)
    dest = (pend - padded)[e_s] + jnp.arange(A) - start[e_s]
    buf_tok = jnp.full((P,), N, jnp.int32).at[dest].set(tok[order])
    buf_g = jnp.zeros((P,), x.dtype).at[dest].set(g[order].astype(x.dtype))
    blk_expert = jnp.clip(jnp.searchsorted(pend, jnp.arange(nblk) * MOE_BLOCK, side='right'), 0, N_EXPERTS - 1)
    xp = jnp.concatenate([x, jnp.zeros((1, D), x.dtype)], 0)
    xb = xp[buf_tok].reshape(nblk, MOE_BLOCK, D)

    def expert_block(args):
        xi, ei = args
        return (jax.nn.silu(xi @ w1[ei]) * (xi @ w3[ei])) @ w2[ei]

    yb = lax.map(expert_block, (xb, blk_expert)).reshape(P, D)
    out = jnp.zeros((N + 1, D), x.dtype).at[buf_tok].add(yb * buf_g[:, None])
    return out[:N]


def hmoe(x, wg, bg, we, be, w1, w3, w2):
    N = x.shape[0]
    pg = jax.nn.softmax((x @ wg).astype(F32) + bg.astype(F32), -1)
    pg_top, grp = lax.top_k(pg, 1)
    le = ((x @ we).astype(F32) + be.astype(F32)).reshape(N, N_GROUPS, EXPERTS_PER_GROUP)
    le = jnp.take_along_axis(le, grp[:, :, None], 1)[:, 0]
    pe_top, e_in = lax.top_k(jax.nn.softmax(le, -1), TOP_K)
    gate = pg_top * pe_top / jnp.sum(pe_top, -1, keepdims=True)
    expert = grp * EXPERTS_PER_GROUP + e_in
    return routed_experts(x, expert.reshape(-1).astype(jnp.int32), gate.reshape(-1), w1, w3, w2)


def setup_inputs(seed: int = 0) -> dict:
    key = jax.random.key(seed)
    ks = iter(jax.random.split(key, 32))
    nrm = lambda shape, s: jax.random.normal(next(ks), shape, F32) * s
    D = D_MODEL
    x = nrm((BATCH, SEQ, D), 1.0)
    c = nrm((BATCH, D), 1.0)
    ctx = nrm((BATCH, CTX_LEN, D), 1.0)
    c_ctx = nrm((D,), 1.0)
    w_mod = nrm((DEPTH, D, 6 * D), 0.5 * D ** -0.5)
    b_mod = nrm((DEPTH, 6 * D), 0.02)
    norm1_g = 1.0 + nrm((DEPTH, D), 0.05)
    norm2_g = 1.0 + nrm((DEPTH, D), 0.05)
    w_in_even = nrm((N_EVEN, D, EVEN_IN), D ** -0.5)
    gate_base = jnp.stack([jnp.zeros((ML_HEADS,), F32), jnp.linspace(3.0, 6.0, ML_HEADS)])
    ml_gate_b = gate_base[None, None] + nrm((N_EVEN, 2, 2, ML_HEADS), 0.1)
    ml_norm_g = 1.0 + nrm((N_EVEN, ML_HEADS, ML_DV), 0.05)
    base_ld = jnp.log1p(-(2.0 ** (-jnp.linspace(5.0, 12.0, RET_HEADS))))
    ret_log_decay = base_ld[None, None, :] * jnp.exp(nrm((N_EVEN, 2, RET_HEADS), 0.1))
    ret_norm_g = 1.0 + nrm((N_EVEN, RET_HEADS, RET_DV), 0.05)
    w_out_even = nrm((N_EVEN, EVEN_MIX_W, D), EVEN_MIX_W ** -0.5)
    w_in_odd = nrm((N_ODD, D, ODD_IN), D ** -0.5)
    da_lambda = nrm((N_ODD, 4, DA_DHEAD), 0.1)
    da_norm_g = 1.0 + nrm((N_ODD, DA_HEADS, DA_DV), 0.05)
    w_out_odd = nrm((N_ODD, ODD_MIX_W, D), ODD_MIX_W ** -0.5)
    router_g_w = nrm((DEPTH, D, N_GROUPS), D ** -0.5)
    router_g_b = nrm((DEPTH, N_GROUPS), 0.01)
    router_e_w = nrm((DEPTH, D, N_EXPERTS), D ** -0.5)
    router_e_b = nrm((DEPTH, N_EXPERTS), 0.01)
    w1 = nrm((DEPTH, N_EXPERTS, D, D_EXPERT), D ** -0.5)
    w3 = nrm((DEPTH, N_EXPERTS, D, D_EXPERT), D ** -0.5)
    w2 = nrm((DEPTH, N_EXPERTS, D_EXPERT, D), D_EXPERT ** -0.5)
    final_norm_g = 1.0 + nrm((D,), 0.05)
    return {'x': x, 'c': c, 'ctx': ctx, 'c_ctx': c_ctx, 'w_mod': w_mod, 'b_mod': b_mod,
            'norm1_g': norm1_g, 'norm2_g': norm2_g, 'w_in_even': w_in_even, 'ml_gate_b': ml_gate_b,
            'ml_norm_g': ml_norm_g, 'ret_log_decay': ret_log_decay, 'ret_norm_g': ret_norm_g,
            'w_out_even': w_out_even, 'w_in_odd': w_in_odd, 'da_lambda': da_lambda, 'da_norm_g': da_norm_g,
            'w_out_odd': w_out_odd, 'router_g_w': router_g_w, 'router_g_b': router_g_b,
            'router_e_w': router_e_w, 'router_e_b': router_e_b, 'w1': w1, 'w3': w3, 'w2': w2,
            'final_norm_g': final_norm_g}


def reference(x, c, ctx, c_ctx, w_mod, b_mod, norm1_g, norm2_g, w_in_even, ml_gate_b, ml_norm_g,
              ret_log_decay, ret_norm_g, w_out_even, w_in_odd, da_lambda, da_norm_g, w_out_odd,
              router_g_w, router_g_b, router_e_w, router_e_b, w1, w3, w2, final_norm_g):
    B, S, D = x.shape
    L = ctx.shape[1]
    t = jnp.arange(S)
    rows, cols = t // GRID_W, t % GRID_W
    h_lat, h_ctx = x, ctx
    for l in range(DEPTH):
        last = l == DEPTH - 1
        j = l // 2
        m_lat = modulation(c, w_mod[l], b_mod[l])[:, :, None, :]
        m_ctx = modulation(c_ctx, w_mod[l], b_mod[l])
        u_lat = adaln(h_lat, norm1_g[l], m_lat[:, 0], m_lat[:, 1])
        u_ctx = adaln(h_ctx, norm1_g[l], m_ctx[0], m_ctx[1])
        if l % 2 == 0:
            y_ctx, y_lat = mlstm_retention_mixer(u_ctx, u_lat, w_in_even[j], ml_gate_b[j], ml_norm_g[j],
                                                 ret_log_decay[j], ret_norm_g[j], w_out_even[j])
        else:
            lam_init = 0.8 - 0.6 * math.exp(-0.3 * l)
            y_ctx, y_lat = diff_attention_mixer(u_ctx, u_lat, rows, cols, w_in_odd[j], da_lambda[j],
                                                da_norm_g[j], w_out_odd[j], lam_init, not last)
        h_lat = h_lat + m_lat[:, 2] * y_lat
        v_lat = adaln(h_lat, norm2_g[l], m_lat[:, 3], m_lat[:, 4])
        moe_w = (router_g_w[l], router_g_b[l], router_e_w[l], router_e_b[l], w1[l], w3[l], w2[l])
        if last:
            h_lat = h_lat + m_lat[:, 5] * hmoe(v_lat.reshape(-1, D), *moe_w).reshape(B, S, D)
        else:
            h_ctx = h_ctx + m_ctx[2] * y_ctx
            v_ctx = adaln(h_ctx, norm2_g[l], m_ctx[3], m_ctx[4])
            y = hmoe(jnp.concatenate([v_ctx.reshape(-1, D), v_lat.reshape(-1, D)], 0), *moe_w)
            n_ctx = B * L
            h_ctx = h_ctx + m_ctx[5] * y[:n_ctx].reshape(B, L, D)
            h_lat = h_lat + m_lat[:, 5] * y[n_ctx:].reshape(B, S, D)
    return rms_norm(h_lat, final_norm_g)
```

```python
import math
import contextlib
import numpy as np
import ml_dtypes
import concourse.bass as bass
import concourse.mybir as mybir


F32 = mybir.dt.float32
BF16 = mybir.dt.bfloat16
I32 = mybir.dt.int32
U32 = mybir.dt.uint32
AF = mybir.ActivationFunctionType
ALU = mybir.AluOpType
AX = mybir.AxisListType


class Buf:
    __slots__ = ("name", "w", "r")

    def __init__(self, name=""):
        self.name = name
        self.w = None
        self.r = {}


class Sync:
    def __init__(self, nc, n_dma_sems=48, same_engine_wait=True):
        self.nc = nc
        self.eng = {"pe": nc.tensor, "act": nc.scalar, "dve": nc.vector, "pool": nc.gpsimd, "sp": nc.sync}
        self.sem = {}
        self.cnt = {}
        for k in ["pe", "act", "dve", "pool"]:
            self.sem[k] = nc.alloc_semaphore(name="s_" + k)
            self.cnt[k] = 0
        self.dsem = [nc.alloc_semaphore(name="s_dma%d" % i) for i in range(n_dma_sems)]
        self.dcnt = [0] * n_dma_sems
        self.dnext = 0
        self.waited = {k: {} for k in self.eng}
        self.same_engine_wait = same_engine_wait
        self.n_inst = 0

    def _semh(self, key):
        return self.sem[key] if isinstance(key, str) else self.dsem[key]

    def _wait(self, e, key, val):
        if val <= 0:
            return
        w = self.waited[e]
        if w.get(key, 0) >= val:
            return
        if key == e and not self.same_engine_wait:
            return
        if key == e == "pe":
            return
        self.eng[e].wait_ge(self._semh(key), val)
        w[key] = val

    def _deps(self, e, reads, writes):
        for b in reads:
            if b.w is not None:
                self._wait(e, *b.w)
        for b in writes:
            if b.w is not None:
                self._wait(e, *b.w)
            for k, v in b.r.items():
                self._wait(e, k, v)

    def _record(self, ev, reads, writes):
        k, v = ev
        for b in reads:
            if b.r.get(k, 0) < v:
                b.r[k] = v
        for b in writes:
            b.w = ev
            b.r = {}

    def op(self, e, fn, reads=(), writes=()):
        self._deps(e, reads, writes)
        ins = fn(self.eng[e])
        self.cnt[e] += 1
        ins.then_inc(self.sem[e], 1)
        self._record((e, self.cnt[e]), reads, writes)
        self.n_inst += 1
        return ins

    def dma(self, q, out, in_, reads=(), writes=(), indirect=None, **kw):
        self._deps(q, reads, writes)
        k = self.dnext
        self.dnext = (self.dnext + 1) % len(self.dsem)
        self._wait(q, k, 16 * self.dcnt[k])
        if indirect is not None:
            ins = self.eng[q].indirect_dma_start(out, indirect.get("out_offset"), in_, indirect.get("in_offset"), **kw)
        else:
            ins = self.eng[q].dma_start(out=out, in_=in_, **kw)
        self.dcnt[k] += 1
        ins.then_inc(self.dsem[k], 16)
        self._record((k, 16 * self.dcnt[k]), reads, writes)
        self.n_inst += 1
        return ins

    def collective(self, kind, ins, outs, groups, reads=(), writes=()):
        self._deps("pool", reads, writes)
        if "cc" not in self.sem:
            self.sem["cc"] = self.nc.alloc_semaphore(name="s_cc")
            self.cnt["cc"] = 0
        ins_ = self.nc.gpsimd.collective_compute(kind, ALU.bypass, replica_groups=groups, ins=ins, outs=outs)
        self.cnt["cc"] += 1
        ins_.then_inc(self.sem["cc"], 1)
        self._record(("cc", self.cnt["cc"]), reads, writes)
        self.n_inst += 1
        return ins_

    def barrier(self):
        for e in self.eng:
            for k in self.sem:
                self._wait(e, k, self.cnt[k])
            for k in range(len(self.dsem)):
                self._wait(e, k, 16 * self.dcnt[k])

    def touch(self, e, reads=(), writes=()):
        self._deps(e, reads, writes)

    def finish(self, bufs, e="sp"):
        for b in bufs:
            if b.w is not None:
                self._wait(e, *b.w)


D = 1024
EPS = 1e-6
XOFF = 524288


class Ctx:
    def __init__(self, nc):
        self.nc = nc
        self.S = Sync(nc)
        self.stk = [contextlib.ExitStack()]
        self.PB = [nc.alloc_psum_tensor("pb%d" % i, [128, 512], F32) for i in range(7)]
        self.PBb = [Buf() for _ in range(7)]
        self.PT = nc.alloc_psum_tensor("pt_bf", [128, 1024], BF16)
        self.b_PT = Buf()
        self.nreg = 0

    def din(self, name, shape, dt=F32):
        return self.nc.dram_tensor(name, list(shape), dt, kind="ExternalInput").ap()

    def dout(self, name, shape, dt=F32):
        return self.nc.dram_tensor(name, list(shape), dt, kind="ExternalOutput").ap()

    def dscr(self, name, shape, dt=F32):
        return self.nc.dram_tensor(name, list(shape), dt).ap()

    def sb(self, name, shape, dt=F32):
        self.nreg += 1
        return self.stk[-1].enter_context(self.nc.sbuf_tensor("%s_u%d" % (name, self.nreg), list(shape), dt))

    def phase_begin(self):
        self.stk.append(contextlib.ExitStack())

    def phase_end(self):
        self.S.barrier()
        self.stk.pop().close()

    def load_const(self, name, shape, dt=F32):
        d = self.din(name, shape, dt)
        t = self.sb(name + "_s", shape, dt)
        b = Buf()
        self.S.dma("sp", t[:], d, writes=[b])
        return t, b

    def load_bcast(self, name, n, dram_ap=None):
        d = dram_ap if dram_ap is not None else self.din(name, [1, n])
        t = self.sb(name + "_s", [128, n])
        b = Buf()
        self.S.dma("sp", t[:], d.partition_broadcast(128), writes=[b])
        return t, b


def common_consts():
    c = {}
    c["ident_bf"] = np.eye(128, dtype=np.float32).astype(ml_dtypes.bfloat16)
    c["ident_f"] = np.eye(128, dtype=np.float32)
    c["ones_f"] = np.ones((128, 128), np.float32)
    s = np.arange(128)
    c["slt"] = (s[:, None] < s[None, :]).astype(np.float32)
    c["blkstart"] = (256.0 * s).astype(np.float32)[:, None]
    c["pcol"] = (1.0 * s).astype(np.float32)[:, None]
    c["thr"] = np.tile((256.0 * np.arange(34)).astype(np.float32)[None, :], (128, 1))
    return c


def emit_silu_bcast(cx, cT_aps, ones_f, b_ones):
    S = cx.S
    n = len(cT_aps)
    cT_s = cx.sb("cT_s", [128, n, 8]); b_cT = Buf()
    for i, a in enumerate(cT_aps):
        S.dma("sp", cT_s[:, i, :], a, writes=[b_cT])
    sc = cx.sb("sc", [128, n, 8]); b_sc = Buf()
    S.op("act", lambda e: e.activation(out=sc[:], in_=cT_s[:], func=AF.Silu), reads=[b_cT], writes=[b_sc])
    scb = cx.sb("scb", [128, n, 8, 128]); b_scb = Buf()
    for w in range(n):
        for j in range(8):
            S.op("dve", lambda e, w=w, j=j: e.tensor_scalar(out=scb[:, w, j, :], in0=ones_f[:], scalar1=sc[:, w, j:j + 1],
                                                           scalar2=None, op0=ALU.mult),
                 reads=[b_ones, b_sc], writes=[b_scb])
    return scb, b_scb


def emit_mod(cx, scb, b_scb, nvec, wmod_ap, bmod_s, b_bmod, ncols, dest_fn, b_dest, wm_s, b_wm):
    S = cx.S
    wmod_v = wmod_ap.rearrange("(j p) n -> p j n", p=128)
    k = 0
    for cc in range(ncols // 512):
        wb = cc % 2
        S.dma("sp", wm_s[wb][:], wmod_v[:, :, cc * 512:(cc + 1) * 512], writes=[b_wm[wb]])
        for w in range(nvec):
            pbi = k % 7; k += 1
            for j in range(8):
                S.op("pe", lambda e, w=w, j=j, wb=wb, pbi=pbi: e.matmul(cx.PB[pbi][:], lhsT=scb[:, w, j, :], rhs=wm_s[wb][:, j, :],
                                                                        start=(j == 0), stop=(j == 7)),
                     reads=[b_scb, b_wm[wb]], writes=[cx.PBb[pbi]])
            S.op("dve", lambda e, w=w, pbi=pbi, cc=cc: e.tensor_tensor(out=dest_fn(w, cc), in0=cx.PB[pbi][:],
                                                                      in1=bmod_s[:, cc * 512:(cc + 1) * 512], op=ALU.add),
                 reads=[cx.PBb[pbi], b_bmod], writes=[b_dest])


class NormBufs:
    def __init__(self, cx, tag=""):
        self.junk = cx.sb("junk" + tag, [128, D], BF16); self.b_junk = Buf()
        self.ss = cx.sb("ss" + tag, [128, 2]); self.b_ss = [Buf(), Buf()]
        self.rstd = cx.sb("rstd" + tag, [128, 2]); self.b_rstd = [Buf(), Buf()]
        self.tmp32 = cx.sb("tmp32" + tag, [128, D]); self.b_tmp32 = Buf()
        self.k = 0


def emit_adaln(cx, nb, x_ap, b_x, A_ap, Bsh_ap, b_mod, out_ap, b_out):
    S = cx.S
    xp = nb.k % 2; nb.k += 1
    ss = nb.ss[:, xp:xp + 1]; rstd = nb.rstd[:, xp:xp + 1]
    S.op("dve", lambda e: e.memset(ss, 0.0), writes=[nb.b_ss[xp]])
    S.op("act", lambda e: e.activation(out=nb.junk[:], in_=x_ap, func=AF.Square, accum_out=ss),
         reads=[b_x], writes=[nb.b_junk, nb.b_ss[xp]])
    S.op("dve", lambda e: e.tensor_scalar(out=rstd, in0=ss, scalar1=1.0 / D, scalar2=EPS, op0=ALU.mult, op1=ALU.add),
         reads=[nb.b_ss[xp]], writes=[nb.b_rstd[xp]])
    S.op("act", lambda e: e.activation(out=rstd, in_=rstd, func=AF.Sqrt), reads=[nb.b_rstd[xp]], writes=[nb.b_rstd[xp]])
    S.op("dve", lambda e: e.reciprocal(out=rstd, in_=rstd), reads=[nb.b_rstd[xp]], writes=[nb.b_rstd[xp]])
    if Bsh_ap is None:
        S.op("dve", lambda e: e.scalar_tensor_tensor(out=out_ap, in0=x_ap, scalar=rstd, in1=A_ap, op0=ALU.mult, op1=ALU.mult),
             reads=[b_x, nb.b_rstd[xp], b_mod], writes=[b_out])
    else:
        S.op("dve", lambda e: e.scalar_tensor_tensor(out=nb.tmp32[:], in0=x_ap, scalar=rstd, in1=A_ap, op0=ALU.mult, op1=ALU.mult),
             reads=[b_x, nb.b_rstd[xp], b_mod], writes=[nb.b_tmp32])
        S.op("dve", lambda e: e.tensor_tensor(out=out_ap, in0=nb.tmp32[:], in1=Bsh_ap, op=ALU.add),
             reads=[nb.b_tmp32, b_mod], writes=[b_out])


def emit_transpose(cx, src_fn, b_src, nblk, ident_bf, b_ident, dst_ap, b_dst):
    S = cx.S
    for j in range(nblk):
        S.op("pe", lambda e, j=j: e.transpose(cx.PT[:, j * 128:(j + 1) * 128], src_fn(j), ident_bf[:]),
             reads=[b_src, b_ident], writes=[cx.b_PT])
    S.op("act", lambda e: e.activation(out=dst_ap, in_=cx.PT[:, 0:nblk * 128].rearrange("p (j t) -> p j t", j=nblk), func=AF.Copy),
         reads=[cx.b_PT], writes=[b_dst])


class MoE:
    def __init__(self, cx, ntile, Wr_d, br_d, w1_d, w3_d, w2_d, consts, tag=""):
        self.cx = cx
        self.nt = ntile
        self.A = 2 * ntile * 128
        self.nblk = (self.A + 255) // 256 + 32
        self.c = consts
        self.tag = tag
        self.w1_d, self.w3_d, self.w2_d = w1_d, w3_d, w2_d
        self.Wr_d, self.br_d = Wr_d, br_d
        nr = self.nblk * 256
        self.xbuf = cx.dscr("xbuf" + tag, [nr, D], BF16); self.b_xbuf = Buf()
        self.ybuf = cx.dscr("ybuf" + tag, [nr, D], F32); self.b_ybuf = Buf()
        self.W1b = cx.dscr("W1b" + tag, [32 * 128, 4096], BF16); self.b_W1b = Buf()
        self.W3b = cx.dscr("W3b" + tag, [32 * 128, 4096], BF16); self.b_W3b = Buf()
        self.W2b = cx.dscr("W2b" + tag, [32 * 128, 4096], BF16); self.b_W2b = Buf()

    def precast(self):
        S = self.cx.S
        for (src, dst, b, j) in [(self.w1_d, self.W1b, self.b_W1b, 8), (self.w3_d, self.W3b, self.b_W3b, 8), (self.w2_d, self.W2b, self.b_W2b, 4)]:
            sv = src.rearrange("e (p j) n -> e p (j n)", j=j)
            for e_ in range(32):
                S.dma("pool", dst[e_ * 128:(e_ + 1) * 128, :].rearrange("p (a b) -> p a b", a=2), sv[e_].rearrange("p (a b) -> p a b", a=2), writes=[b])

    def alloc_persistent(self):
        cx = self.cx
        t = self.tag
        self.OH = cx.sb("OH" + t, [128, self.nt, 2, 32]); self.b_OH = [Buf() for _ in range(self.nt)]
        self.gate = cx.sb("gate" + t, [128, self.nt, 2]); self.b_gate = [Buf() for _ in range(self.nt)]
        self.dest = cx.sb("dest" + t, [128, self.nt, 2], I32); self.b_dest = [Buf() for _ in range(self.nt)]
        self.idxw = cx.sb("idxw" + t, [128, 128], I32); self.b_idxw = Buf()
        self.Wr_s = cx.sb("Wr_s" + t, [128, 8, 36], BF16); self.b_Wr = Buf()
        for j in range(8):
            cx.S.dma("pool", self.Wr_s[:, j, :], self.Wr_d[j * 128:(j + 1) * 128, :], writes=[self.b_Wr])
        self.br_s, self.b_br = cx.load_bcast("br" + t, 36, self.br_d)
        self.lg = cx.sb("lg" + t, [128, 36]); self.b_lg = Buf()
        self.sm = cx.sb("rsm" + t, [128, 64]); self.b_sm = Buf()

    def route_tile(self, ti, vT_fn, b_vT):
        cx, S = self.cx, self.cx.S
        pbi = 6
        PBt, bPB = cx.PB[pbi], cx.PBb[pbi]
        for j in range(8):
            S.op("pe", lambda e, j=j: e.matmul(PBt[:, 0:36], lhsT=vT_fn(j), rhs=self.Wr_s[:, j, :], start=(j == 0), stop=(j == 7)),
                 reads=[b_vT, self.b_Wr], writes=[bPB])
        lg, sm = self.lg, self.sm
        b_lg, b_sm = self.b_lg, self.b_sm
        S.op("dve", lambda e: e.tensor_tensor(out=lg[:], in0=PBt[:, 0:36], in1=self.br_s[:], op=ALU.add), reads=[bPB, self.b_br], writes=[b_lg])
        S.op("dve", lambda e: e.tensor_reduce(out=sm[:, 0:1], in_=lg[:, 0:4], axis=AX.X, op=ALU.max), reads=[b_lg], writes=[b_sm])
        S.op("dve", lambda e: e.tensor_scalar(out=sm[:, 1:2], in0=sm[:, 0:1], scalar1=-1.0, scalar2=None, op0=ALU.mult), reads=[b_sm], writes=[b_sm])
        S.op("dve", lambda e: e.memset(sm[:, 2:3], 0.0), reads=[b_sm], writes=[b_sm])
        S.op("act", lambda e: e.activation(out=sm[:, 44:48], in_=lg[:, 0:4], func=AF.Exp, bias=sm[:, 1:2], accum_out=sm[:, 2:3]),
             reads=[b_lg, b_sm], writes=[b_sm])
        S.op("dve", lambda e: e.reciprocal(out=sm[:, 3:4], in_=sm[:, 2:3]), reads=[b_sm], writes=[b_sm])
        S.op("dve", lambda e: e.tensor_scalar(out=sm[:, 4:8], in0=lg[:, 0:4], scalar1=sm[:, 0:1], scalar2=None, op0=ALU.is_equal),
             reads=[b_lg, b_sm], writes=[b_sm])
        S.op("dve", lambda e: e.tensor_scalar(out=sm[:, 8:16], in0=lg[:, 4:12], scalar1=sm[:, 4:5], scalar2=None, op0=ALU.mult),
             reads=[b_lg, b_sm], writes=[b_sm])
        for g in range(1, 4):
            S.op("dve", lambda e, g=g: e.scalar_tensor_tensor(out=sm[:, 8:16], in0=lg[:, 4 + 8 * g:12 + 8 * g], scalar=sm[:, 4 + g:5 + g],
                                                             in1=sm[:, 8:16], op0=ALU.mult, op1=ALU.add),
                 reads=[b_lg, b_sm], writes=[b_sm])
        S.op("dve", lambda e: e.max(out=sm[:, 16:24], in_=sm[:, 8:16]), reads=[b_sm], writes=[b_sm])
        S.op("dve", lambda e: e.tensor_scalar(out=sm[:, 24:32], in0=sm[:, 8:16], scalar1=sm[:, 16:17], scalar2=None, op0=ALU.is_equal),
             reads=[b_sm], writes=[b_sm])
        S.op("dve", lambda e: e.tensor_scalar(out=sm[:, 32:40], in0=sm[:, 8:16], scalar1=sm[:, 17:18], scalar2=None, op0=ALU.is_equal),
             reads=[b_sm], writes=[b_sm])
        S.op("dve", lambda e: e.tensor_tensor(out=sm[:, 40:41], in0=sm[:, 17:18], in1=sm[:, 16:17], op=ALU.subtract), reads=[b_sm], writes=[b_sm])
        S.op("act", lambda e: e.activation(out=sm[:, 41:42], in_=sm[:, 40:41], func=AF.Exp), reads=[b_sm], writes=[b_sm])
        S.op("dve", lambda e: e.tensor_scalar(out=sm[:, 42:43], in0=sm[:, 41:42], scalar1=1.0, scalar2=None, op0=ALU.add), reads=[b_sm], writes=[b_sm])
        S.op("dve", lambda e: e.reciprocal(out=sm[:, 42:43], in_=sm[:, 42:43]), reads=[b_sm], writes=[b_sm])
        S.op("dve", lambda e: e.tensor_tensor(out=sm[:, 43:44], in0=sm[:, 41:42], in1=sm[:, 42:43], op=ALU.mult), reads=[b_sm], writes=[b_sm])
        S.op("dve", lambda e: e.tensor_scalar(out=self.gate[:, ti, :], in0=sm[:, 42:44], scalar1=sm[:, 3:4], scalar2=None, op0=ALU.mult),
             reads=[b_sm], writes=[self.b_gate[ti]])
        for k in range(2):
            for g in range(4):
                S.op("dve", lambda e, k=k, g=g: e.tensor_scalar(out=self.OH[:, ti, k, g * 8:(g + 1) * 8], in0=sm[:, 24 + 8 * k:32 + 8 * k],
                                                               scalar1=sm[:, 4 + g:5 + g], scalar2=None, op0=ALU.mult),
                     reads=[b_sm], writes=[self.b_OH[ti]])

    def plan(self):
        cx, S = self.cx, self.cx.S
        t = self.tag
        ones_f, b_ones = self.c["ones_f"]
        ident_f, b_identf = self.c["ident_f"]
        blkstart, b_blk = self.c["blkstart"]
        PBt, bPB = cx.PB[0], cx.PBb[0]
        for ti in range(self.nt):
            S.op("pe", lambda e, ti=ti: e.matmul(PBt[:, 0:64], lhsT=ones_f[:], rhs=self.OH[:, ti, :, :].rearrange("p k e -> p (k e)"),
                                                 start=(ti == 0), stop=(ti == self.nt - 1)),
                 reads=[b_ones, self.b_OH[ti]], writes=[bPB])
        pl = cx.sb("pl" + t, [128, 8, 32]); b_pl = Buf()
        S.op("dve", lambda e: e.tensor_copy(out=pl[:, 7, :], in_=PBt[:, 0:32]), reads=[bPB], writes=[b_pl])
        S.op("dve", lambda e: e.tensor_tensor(out=pl[:, 0, :], in0=pl[:, 7, :], in1=PBt[:, 32:64], op=ALU.add), reads=[bPB, b_pl], writes=[b_pl])
        thr, b_thr = self.c["thr"]
        cmp3 = cx.sb("cmp3" + t, [128, 32, 34]); b_cmp3 = Buf()
        S.op("dve", lambda e: e.tensor_tensor(out=cmp3[:], in0=pl[:, 0, :].unsqueeze(2).broadcast_to([128, 32, 34]),
                                              in1=thr[:].unsqueeze(1).broadcast_to([128, 32, 34]), op=ALU.is_gt),
             reads=[b_pl, b_thr], writes=[b_cmp3])
        S.op("dve", lambda e: e.tensor_reduce(out=pl[:, 1, :], in_=cmp3[:], axis=AX.X, op=ALU.add), reads=[b_cmp3], writes=[b_pl])
        S.op("dve", lambda e: e.tensor_scalar(out=pl[:, 3, :], in0=pl[:, 1, :], scalar1=256.0, scalar2=None, op0=ALU.mult), reads=[b_pl], writes=[b_pl])
        S.op("dve", lambda e: e.memset(pl[:, 5, :], 0.0), reads=[b_pl], writes=[b_pl])
        S.op("dve", lambda e: e.tensor_tensor_scan(out=pl[:, 4, :], data0=pl[:, 3, :], data1=pl[:, 5, :], initial=0.0, op0=ALU.add, op1=ALU.add),
             reads=[b_pl], writes=[b_pl])
        self.run = cx.sb("run" + t, [128, 32]); self.b_run = Buf()
        S.op("dve", lambda e: e.tensor_tensor(out=self.run[:], in0=pl[:, 4, :], in1=pl[:, 3, :], op=ALU.subtract), reads=[b_pl], writes=[self.b_run])
        S.op("dve", lambda e: e.tensor_scalar(out=pl[:, 6, :], in0=pl[:, 4, :], scalar1=blkstart[:, 0:1], scalar2=None, op0=ALU.is_le),
             reads=[b_pl, b_blk], writes=[b_pl])
        be = cx.sb("be" + t, [128, 1]); b_be = Buf()
        S.op("dve", lambda e: e.tensor_reduce(out=be[:], in_=pl[:, 6, :], axis=AX.X, op=ALU.add), reads=[b_pl], writes=[b_be])
        S.op("dve", lambda e: e.tensor_scalar(out=be[:], in0=be[:], scalar1=31.0, scalar2=128.0, op0=ALU.min, op1=ALU.mult), reads=[b_be], writes=[b_be])
        bbc = cx.sb("bbc" + t, [128, 128]); b_bbc = Buf()
        pcol, b_pcol = self.c["pcol"]
        S.op("dve", lambda e: e.tensor_scalar(out=bbc[:], in0=ones_f[:], scalar1=be[:, 0:1], scalar2=None, op0=ALU.mult), reads=[b_be, b_ones], writes=[b_bbc])
        PB1, bPB1 = cx.PB[1], cx.PBb[1]
        S.op("pe", lambda e: e.matmul(PB1[:, 0:128], lhsT=bbc[:], rhs=ident_f[:], start=True, stop=True), reads=[b_bbc, b_identf], writes=[bPB1])
        idxf = cx.sb("idxf" + t, [128, 128]); b_idxf = Buf()
        S.op("dve", lambda e: e.tensor_scalar(out=idxf[:], in0=PB1[:, 0:128], scalar1=pcol[:, 0:1], scalar2=None, op0=ALU.add), reads=[bPB1, b_pcol], writes=[b_idxf])
        S.op("dve", lambda e: e.tensor_copy(out=self.idxw[:], in_=idxf[:]), reads=[b_idxf], writes=[self.b_idxw])
        self.d1 = cx.sb("d1" + t, [128, 32]); self.b_d1 = Buf()
        self.destf = cx.sb("destf" + t, [128, 2]); self.b_destf = Buf()

    def dispatch_tile(self, ti, v_ap, b_v, do_scatter=True):
        cx, S = self.cx, self.cx.S
        ones_f, b_ones = self.c["ones_f"]
        slt, b_slt = self.c["slt"]
        for k in range(2):
            pa, ba = cx.PB[2 + k], cx.PBb[2 + k]
            pt_, bt_ = cx.PB[4 + k], cx.PBb[4 + k]
            S.op("pe", lambda e, k=k, pa=pa: e.matmul(pa[:, 0:32], lhsT=slt[:], rhs=self.OH[:, ti, k, :], start=True, stop=True),
                 reads=[b_slt, self.b_OH[ti]], writes=[ba])
            S.op("pe", lambda e, k=k, pt_=pt_: e.matmul(pt_[:, 0:32], lhsT=ones_f[:], rhs=self.OH[:, ti, k, :], start=True, stop=True),
                 reads=[b_ones, self.b_OH[ti]], writes=[bt_])
            S.op("dve", lambda e, pa=pa: e.tensor_tensor(out=self.d1[:], in0=pa[:, 0:32], in1=self.run[:], op=ALU.add),
                 reads=[ba, self.b_run], writes=[self.b_d1])
            S.op("dve", lambda e, k=k: e.tensor_tensor(out=self.d1[:], in0=self.d1[:], in1=self.OH[:, ti, k, :], op=ALU.mult),
                 reads=[self.b_d1, self.b_OH[ti]], writes=[self.b_d1])
            S.op("dve", lambda e, k=k: e.tensor_reduce(out=self.destf[:, k:k + 1], in_=self.d1[:], axis=AX.X, op=ALU.add),
                 reads=[self.b_d1], writes=[self.b_destf])
            S.op("dve", lambda e, k=k: e.tensor_copy(out=self.dest[:, ti, k:k + 1], in_=self.destf[:, k:k + 1]),
                 reads=[self.b_destf], writes=[self.b_dest[ti]])
            S.op("dve", lambda e, pt_=pt_: e.tensor_tensor(out=self.run[:], in0=self.run[:], in1=pt_[:, 0:32], op=ALU.add),
                 reads=[bt_, self.b_run], writes=[self.b_run])
            if do_scatter:
              S.dma("pool", self.xbuf, v_ap, reads=[b_v, self.b_dest[ti]], writes=[self.b_xbuf],
                  indirect={"out_offset": bass.IndirectOffsetOnAxis(ap=self.dest[:, ti, k:k + 1], axis=0)})

    def experts(self):
        cx, S, nc = self.cx, self.cx.S, self.cx.nc
        t = self.tag
        ident_bf, b_identbf = self.c["ident_bf"]
        w1_s = [cx.sb("w1_s%d%s" % (i, t), [128, 8, 512], BF16) for i in range(2)]
        w3_s = [cx.sb("w3_s%d%s" % (i, t), [128, 8, 512], BF16) for i in range(2)]
        w2_s = [cx.sb("w2_s%d%s" % (i, t), [128, 4, 1024], BF16) for i in range(2)]
        b_w1 = [Buf(), Buf()]; b_w3 = [Buf(), Buf()]; b_w2 = [Buf(), Buf()]
        xb = [cx.sb("xb%d%s" % (i, t), [128, D], BF16) for i in range(2)]; b_xb = [Buf(), Buf()]
        xT = [cx.sb("xT%d%s" % (i, t), [128, 8, 128], BF16) for i in range(2)]; b_xT = [Buf(), Buf()]
        s1 = cx.sb("s1" + t, [128, 512]); b_s1 = Buf()
        hh = [cx.sb("hh%d%s" % (i, t), [128, 512], BF16) for i in range(2)]; b_hh = [Buf(), Buf()]
        hhT = [cx.sb("hhT%d%s" % (i, t), [128, 4, 128], BF16) for i in range(2)]; b_hhT = [Buf(), Buf()]
        yst = [cx.sb("yst%d%s" % (i, t), [128, D]) for i in range(2)]; b_yst = [Buf(), Buf()]
        u = 0
        for i in range(self.nblk):
            p = i % 2
            off = bass.IndirectOffsetOnAxis(ap=self.idxw[:, i:i + 1], axis=0)
            S.dma("pool", w1_s[p][:].rearrange("p j n -> p (j n)"), self.W1b, reads=[self.b_idxw, self.b_W1b], writes=[b_w1[p]], indirect={"in_offset": off})
            S.dma("pool", w3_s[p][:].rearrange("p j n -> p (j n)"), self.W3b, reads=[self.b_idxw, self.b_W3b], writes=[b_w3[p]], indirect={"in_offset": off})
            S.dma("pool", w2_s[p][:].rearrange("p j n -> p (j n)"), self.W2b, reads=[self.b_idxw, self.b_W2b], writes=[b_w2[p]], indirect={"in_offset": off})
            for sub in range(2):
                q = u % 2; u += 1
                row0 = (2 * i + sub) * 128
                S.dma("sp", xb[q][:], self.xbuf[row0:row0 + 128, :], reads=[self.b_xbuf], writes=[b_xb[q]])
                emit_transpose(cx, lambda j, q=q: xb[q][:].rearrange("t (p j) -> t j p", j=8)[:, j, :], b_xb[q], 8, ident_bf, b_identbf, xT[q][:], b_xT[q])
                pa, ba = cx.PB[0 + q], cx.PBb[0 + q]
                pb_, bb = cx.PB[2 + q], cx.PBb[2 + q]
                for j in range(8):
                    S.op("pe", lambda e, j=j, q=q, p=p, pa=pa: e.matmul(pa[:], lhsT=xT[q][:, j, :], rhs=w1_s[p][:, j, :], start=(j == 0), stop=(j == 7)),
                         reads=[b_xT[q], b_w1[p]], writes=[ba])
                for j in range(8):
                    S.op("pe", lambda e, j=j, q=q, p=p, pb_=pb_: e.matmul(pb_[:], lhsT=xT[q][:, j, :], rhs=w3_s[p][:, j, :], start=(j == 0), stop=(j == 7)),
                         reads=[b_xT[q], b_w3[p]], writes=[bb])
                S.op("act", lambda e, pa=pa: e.activation(out=s1[:], in_=pa[:], func=AF.Silu), reads=[ba], writes=[b_s1])
                S.op("dve", lambda e, q=q, pb_=pb_: e.tensor_tensor(out=hh[q][:], in0=s1[:], in1=pb_[:], op=ALU.mult), reads=[b_s1, bb], writes=[b_hh[q]])
                emit_transpose(cx, lambda j, q=q: hh[q][:].rearrange("t (p j) -> t j p", j=4)[:, j, :], b_hh[q], 4, ident_bf, b_identbf, hhT[q][:], b_hhT[q])
                for half in range(2):
                    pc, bc = cx.PB[4 + half], cx.PBb[4 + half]
                    for j in range(4):
                        S.op("pe", lambda e, j=j, q=q, p=p, pc=pc, half=half: e.matmul(pc[:], lhsT=hhT[q][:, j, :],
                                                                                        rhs=w2_s[p][:, j, half * 512:(half + 1) * 512],
                                                                                        start=(j == 0), stop=(j == 3)),
                             reads=[b_hhT[q], b_w2[p]], writes=[bc])
                    if half == 0:
                        S.op("act", lambda e, q=q, pc=pc: e.activation(out=yst[q][:, 0:512], in_=pc[:], func=AF.Copy), reads=[bc], writes=[b_yst[q]])
                    else:
                        S.op("dve", lambda e, q=q, pc=pc: e.tensor_copy(out=yst[q][:, 512:1024], in_=pc[:]), reads=[bc], writes=[b_yst[q]])
                S.dma("sp", self.ybuf[row0:row0 + 128, :], yst[q][:], reads=[b_yst[q]], writes=[self.b_ybuf])

    def gather_tile(self, ti, y0_ap, y1_ap, b_y0, b_y1):
        S = self.cx.S
        S.dma("pool", y0_ap, self.ybuf, reads=[self.b_ybuf, self.b_dest[ti]], writes=[b_y0],
              indirect={"in_offset": bass.IndirectOffsetOnAxis(ap=self.dest[:, ti, 0:1], axis=0)})
        S.dma("pool", y1_ap, self.ybuf, reads=[self.b_ybuf, self.b_dest[ti]], writes=[b_y1],
              indirect={"in_offset": bass.IndirectOffsetOnAxis(ap=self.dest[:, ti, 1:2], axis=0)})


D = 1024
NT = 66
NG = 22
RS = 128 ** -0.5
EPS = 1e-6


def host_consts_l1():
    c = {}
    c["ident_bf"] = np.eye(128, dtype=np.float32).astype(ml_dtypes.bfloat16)
    c["ident_f"] = np.eye(128, dtype=np.float32)
    s = np.arange(128)
    mf = (s[:, None] <= s[None, :]).astype(np.float32)
    mb = (s[:, None] >= s[None, :]).astype(np.float32)
    c["mask_f32"] = np.stack([mf, mb], 1)
    c["mask_bf"] = c["mask_f32"].astype(ml_dtypes.bfloat16)
    c["ones_f"] = np.ones((128, 128), np.float32)
    pos = np.arange(128, dtype=np.float32)
    c["poscol"] = np.stack([127.0 - pos, pos, -(127.0 - pos), -pos], 1).astype(np.float32)
    T = NT * 128
    inv = (10000.0 ** (-np.arange(64, dtype=np.float32) / 64)).astype(np.float32)
    ang = (np.arange(T, dtype=np.float32)[:, None] * inv[None, :]).astype(np.float32)
    cos = np.cos(ang).astype(np.float32).T
    sin = np.sin(ang).astype(np.float32).T
    c["ropecos"] = np.ascontiguousarray(np.concatenate([cos, cos], 0))
    c["ropesin"] = np.ascontiguousarray(np.concatenate([-sin, sin], 0))
    return c


def build_l1(cx, io):
    nc = cx.nc
    S = cx.S
    cx.phase_begin()

    def din(name, shape, dt=F32):
        return io[name]

    xin = din("xin", [NT * 128, D])
    cT = din("cT", [128, 8])
    cctxT = din("cctxT", [128, 8])
    wmod = din("wmod", [D, 2048])
    bmod = din("bmod", [1, 2048])
    g1 = din("g1", [1, D])
    Wfm = din("Wfm", [D, 1536])
    Wtm = din("Wtm", [D, 1032])
    gbias = din("gbias", [1, 8])
    rld = din("rld", [1, 4])
    mlg = din("mlg", [1, 256])
    retg = din("retg", [1, 256])
    ident_bf_d = din("ident_bf", [128, 128], BF16)
    ident_f_d = din("ident_f", [128, 128])
    mask_f32_d = din("mask_f32", [128, 2, 128])
    mask_bf_d = din("mask_bf", [128, 2, 128], BF16)
    ones_d = din("ones_f", [128, 128])
    poscol_d = din("poscol", [128, 4])
    ropecos_d = din("ropecos", [128, NT * 128])
    ropesin_d = din("ropesin", [128, NT * 128])
    merged = io["merged"]

    FM = nc.dram_tensor("FM", [NT, 128, 1024], BF16).ap()
    TM = nc.dram_tensor("TM", [NT, 128, 1024], BF16).ap()
    OG = nc.dram_tensor("OG", [NT, 128, 512], BF16).ap()
    HS = nc.dram_tensor("HS", [2, NT, 128, 512], F32).ap()

    sb = cx.sb
    phase_begin = cx.phase_begin
    phase_end = cx.phase_end

    ident_bf = sb("ident_bf_s", [128, 128], BF16); b_identbf = Buf()
    ident_f = sb("ident_f_s", [128, 128]); b_identf = Buf()
    mask_f32 = sb("mask_f32_s", [128, 2, 128]); b_maskf = Buf()
    mask_bf = sb("mask_bf_s", [128, 2, 128], BF16); b_maskbf = Buf()
    ones_f = sb("ones_s", [128, 128]); b_ones = Buf()
    poscol = sb("poscol_s", [128, 4]); b_poscol = Buf()
    S.dma("sp", ident_bf[:], ident_bf_d, writes=[b_identbf])
    S.dma("sp", ident_f[:], ident_f_d, writes=[b_identf])
    S.dma("sp", mask_f32[:], mask_f32_d, writes=[b_maskf])
    S.dma("sp", mask_bf[:], mask_bf_d, writes=[b_maskbf])
    S.dma("sp", ones_f[:], ones_d, writes=[b_ones])
    S.dma("sp", poscol[:], poscol_d, writes=[b_poscol])

    Gall = sb("Gall", [128, 8, NT]); b_G = Buf()
    phase_begin()
    Wfm_s = sb("Wfm_s", [128, 8, 1536], BF16); b_wfm = Buf()
    Wtm_s = sb("Wtm_s", [128, 8, 1032], BF16); b_wtm = Buf()
    for j in range(8):
        S.dma("pool", Wfm_s[:, j, :], Wfm[j * 128:(j + 1) * 128, :], writes=[b_wfm])
        S.dma("pool", Wtm_s[:, j, :], Wtm[j * 128:(j + 1) * 128, :], writes=[b_wtm])

    PB = cx.PB
    PBb = cx.PBb

    cT_s = sb("cT_s", [128, 2, 8]); b_cT = Buf()
    S.dma("sp", cT_s[:, 0, :], cT, writes=[b_cT])
    S.dma("sp", cT_s[:, 1, :], cctxT, writes=[b_cT])
    sc = sb("sc", [128, 2, 8]); b_sc = Buf()
    S.op("act", lambda e: e.activation(out=sc[:], in_=cT_s[:], func=AF.Silu), reads=[b_cT], writes=[b_sc])
    scb = sb("scb", [128, 2, 8, 128]); b_scb = Buf()
    for w in range(2):
        for j in range(8):
            S.op("dve", lambda e, w=w, j=j: e.tensor_scalar(out=scb[:, w, j, :], in0=ones_f[:], scalar1=sc[:, w, j:j + 1],
                                                           scalar2=None, op0=ALU.mult),
                 reads=[b_ones, b_sc], writes=[b_scb])
    bmod_s = sb("bmod_s", [128, 2048]); b_bmod = Buf()
    S.dma("sp", bmod_s[:], bmod.partition_broadcast(128), writes=[b_bmod])
    g1_s = sb("g1_s", [128, D]); b_g1 = Buf()
    S.dma("sp", g1_s[:], g1.partition_broadcast(128), writes=[b_g1])
    modS = sb("modS", [128, 2, D]); modA = sb("modA", [128, 2, D]); b_mod = Buf()
    wm_s = [sb("wm_s%d" % i, [128, 8, 512]) for i in range(2)]
    b_wm = [Buf(), Buf()]
    wmod_v = wmod.rearrange("(j p) n -> p j n", p=128)
    for cc in range(4):
        wb = cc % 2
        S.dma("sp", wm_s[wb][:], wmod_v[:, :, cc * 512:(cc + 1) * 512], writes=[b_wm[wb]])
        for w in range(2):
            pbi = (cc * 2 + w) % 7
            for j in range(8):
                S.op("pe", lambda e, w=w, j=j, wb=wb, pbi=pbi: e.matmul(PB[pbi][:], lhsT=scb[:, w, j, :], rhs=wm_s[wb][:, j, :],
                                                                        start=(j == 0), stop=(j == 7)),
                     reads=[b_scb, b_wm[wb]], writes=[PBb[pbi]])
            half = cc % 2
            if cc < 2:
                S.op("dve", lambda e, w=w, pbi=pbi, cc=cc, half=half: e.tensor_tensor(
                    out=modS[:, w, half * 512:(half + 1) * 512], in0=PB[pbi][:], in1=bmod_s[:, cc * 512:(cc + 1) * 512], op=ALU.add),
                    reads=[PBb[pbi], b_bmod], writes=[b_mod])
            else:
                S.op("dve", lambda e, w=w, pbi=pbi, cc=cc, half=half: e.tensor_tensor(
                    out=modA[:, w, half * 512:(half + 1) * 512], in0=PB[pbi][:], in1=bmod_s[:, cc * 512:(cc + 1) * 512], op=ALU.add),
                    reads=[PBb[pbi], b_bmod], writes=[b_mod])
                S.op("dve", lambda e, w=w, half=half: e.scalar_tensor_tensor(
                    out=modA[:, w, half * 512:(half + 1) * 512], in0=modA[:, w, half * 512:(half + 1) * 512], scalar=1.0,
                    in1=g1_s[:, half * 512:(half + 1) * 512], op0=ALU.add, op1=ALU.mult),
                    reads=[b_mod, b_g1], writes=[b_mod])

    xt = [sb("xt%d" % i, [128, D]) for i in range(2)]; b_xt = [Buf(), Buf()]
    junk = sb("junk", [128, D], BF16); b_junk = Buf()
    ss = sb("ss", [128, 2]); b_ss = [Buf(), Buf()]
    rstd = sb("rstd", [128, 2]); b_rstd = [Buf(), Buf()]
    tmp32 = sb("tmp32", [128, D]); b_tmp32 = Buf()
    u_bf = [sb("u_bf%d" % i, [128, D], BF16) for i in range(2)]; b_u = [Buf(), Buf()]
    uT = [sb("uT%d" % i, [128, 8, 384], BF16) for i in range(2)]
    b_uT = [[Buf() for _ in range(3)] for _ in range(2)]
    FMst = [sb("FMst%d" % i, [128, 3, 8, 128], BF16) for i in range(2)]
    b_FMst = [[Buf() for _ in range(8)] for _ in range(2)]
    TMst = [sb("TMst%d" % i, [128, 3, 1024], BF16) for i in range(2)]
    b_TMst = [[Buf() for _ in range(3)] for _ in range(2)]
    OGst = [sb("OGst%d" % i, [128, 3, 512], BF16) for i in range(2)]
    b_OGst = [[Buf() for _ in range(3)] for _ in range(2)]
    rc = [sb("rc%d" % i, [128, 384]) for i in range(2)]; rsn = [sb("rsn%d" % i, [128, 384]) for i in range(2)]
    b_rope = [Buf(), Buf()]
    t1 = sb("t1", [128, 384]); t2 = sb("t2", [128, 384]); b_t1 = Buf(); b_t2 = Buf()
    PT = cx.PT
    b_PT = cx.b_PT
    b_FM_d = [Buf() for _ in range(NT)]
    b_TM_d = [Buf() for _ in range(NT)]
    b_OG_d = [Buf() for _ in range(NT)]
    tmi = 0
    fmi = 0
    for g in range(NG):
        p = g % 2
        S.dma("sp", rc[p][:], ropecos_d[:, g * 384:(g + 1) * 384], writes=[b_rope[p]])
        S.dma("sp", rsn[p][:], ropesin_d[:, g * 384:(g + 1) * 384], writes=[b_rope[p]])
        for ti in range(3):
            c = g * 3 + ti
            w = 1 if c < 2 else 0
            xp = c % 2
            S.dma("sp", xt[xp][:], xin[c * 128:(c + 1) * 128, :], writes=[b_xt[xp]])
            S.op("dve", lambda e, xp=xp: e.memset(ss[:, xp:xp + 1], 0.0), writes=[b_ss[xp]])
            S.op("act", lambda e, xp=xp: e.activation(out=junk[:], in_=xt[xp][:], func=AF.Square, accum_out=ss[:, xp:xp + 1]),
                 reads=[b_xt[xp]], writes=[b_junk, b_ss[xp]])
            S.op("dve", lambda e, xp=xp: e.tensor_scalar(out=rstd[:, xp:xp + 1], in0=ss[:, xp:xp + 1], scalar1=1.0 / D, scalar2=EPS,
                                                        op0=ALU.mult, op1=ALU.add), reads=[b_ss[xp]], writes=[b_rstd[xp]])
            S.op("act", lambda e, xp=xp: e.activation(out=rstd[:, xp:xp + 1], in_=rstd[:, xp:xp + 1], func=AF.Sqrt),
                 reads=[b_rstd[xp]], writes=[b_rstd[xp]])
            S.op("dve", lambda e, xp=xp: e.reciprocal(out=rstd[:, xp:xp + 1], in_=rstd[:, xp:xp + 1]), reads=[b_rstd[xp]], writes=[b_rstd[xp]])
            S.op("dve", lambda e, xp=xp, w=w: e.scalar_tensor_tensor(out=tmp32[:], in0=xt[xp][:], scalar=rstd[:, xp:xp + 1],
                                                                    in1=modA[:, w, :], op0=ALU.mult, op1=ALU.mult),
                 reads=[b_xt[xp], b_rstd[xp], b_mod], writes=[b_tmp32])
            S.op("dve", lambda e, xp=xp, w=w: e.tensor_tensor(out=u_bf[xp][:], in0=tmp32[:], in1=modS[:, w, :], op=ALU.add),
                 reads=[b_tmp32, b_mod], writes=[b_u[xp]])
            for j in range(8):
                S.op("pe", lambda e, xp=xp, j=j: e.transpose(PT[:, j * 128:(j + 1) * 128], u_bf[xp][:, j * 128:(j + 1) * 128], ident_bf[:]),
                     reads=[b_u[xp], b_identbf], writes=[b_PT])
            S.op("act", lambda e, p=p, ti=ti: e.activation(out=uT[p][:, :, ti * 128:(ti + 1) * 128],
                                                          in_=PT[:].rearrange("p (j t) -> p j t", j=8), func=AF.Copy),
                 reads=[b_PT], writes=[b_uT[p][ti]])
            for (c0, c1) in [(0, 512), (512, 1024), (1024, 1032)]:
                pbi = tmi % 4; tmi += 1
                for j in range(8):
                    S.op("pe", lambda e, p=p, ti=ti, j=j, c0=c0, c1=c1, pbi=pbi: e.matmul(
                        PB[pbi][:, 0:c1 - c0], lhsT=uT[p][:, j, ti * 128:(ti + 1) * 128], rhs=Wtm_s[:, j, c0:c1],
                        start=(j == 0), stop=(j == 7)), reads=[b_uT[p][ti], b_wtm], writes=[PBb[pbi]])
                if c0 == 0:
                    S.op("act", lambda e, p=p, ti=ti, pbi=pbi: e.activation(out=TMst[p][:, ti, 512:1024], in_=PB[pbi][:], func=AF.Copy),
                         reads=[PBb[pbi]], writes=[b_TMst[p][ti]])
                elif c0 == 512:
                    S.op("act", lambda e, p=p, ti=ti, pbi=pbi: e.activation(out=OGst[p][:, ti, :], in_=PB[pbi][:], func=AF.Copy),
                         reads=[PBb[pbi]], writes=[b_OGst[p][ti]])
                else:
                    S.op("dve", lambda e, c=c, pbi=pbi: e.tensor_copy(out=Gall[:, :, c], in_=PB[pbi][:, 0:8]),
                         reads=[PBb[pbi]], writes=[b_G])
        def fm_mm(cb, pbi):
            for j in range(8):
                S.op("pe", lambda e, j=j: e.matmul(PB[pbi][:, 0:384], lhsT=Wfm_s[:, j, cb * 128:(cb + 1) * 128], rhs=uT[p][:, j, :],
                                                   start=(j == 0), stop=(j == 7)),
                     reads=[b_wfm] + b_uT[p], writes=[PBb[pbi]])
        for cb in range(4):
            pbi = 4 + fmi % 3; fmi += 1
            fm_mm(cb, pbi)
            sc_ = 1.0 if cb < 2 else RS
            S.op("act", lambda e, cb=cb, pbi=pbi, sc_=sc_: e.activation(
                out=FMst[p][:, :, cb, :], in_=PB[pbi][:, 0:384].rearrange("p (c t) -> p c t", c=3), func=AF.Copy, scale=sc_),
                reads=[PBb[pbi]], writes=[b_FMst[p][cb]])
        for qk in range(2):
            for h in range(2):
                cb_raw = 4 + qk * 4 + h
                cb_sw = 4 + qk * 4 + 2 + h
                pa = 4 + fmi % 3; fmi += 1
                fm_mm(cb_raw, pa)
                pb_ = 4 + fmi % 3; fmi += 1
                fm_mm(cb_sw, pb_)
                sc_ = 1.0 if qk == 0 else RS
                S.op("dve", lambda e, pa=pa, sc_=sc_: e.scalar_tensor_tensor(out=t1[:], in0=PB[pa][:, 0:384], scalar=sc_, in1=rc[p][:],
                                                                            op0=ALU.mult, op1=ALU.mult),
                     reads=[PBb[pa], b_rope[p]], writes=[b_t1])
                S.op("dve", lambda e, pb_=pb_, sc_=sc_: e.scalar_tensor_tensor(out=t2[:], in0=PB[pb_][:, 0:384], scalar=sc_, in1=rsn[p][:],
                                                                              op0=ALU.mult, op1=ALU.mult),
                     reads=[PBb[pb_], b_rope[p]], writes=[b_t2])
                a = 4 + qk * 2 + h
                S.op("dve", lambda e, a=a: e.tensor_tensor(out=FMst[p][:, :, a, :], in0=t1[:].rearrange("p (c t) -> p c t", c=3),
                                                           in1=t2[:].rearrange("p (c t) -> p c t", c=3), op=ALU.add),
                     reads=[b_t1, b_t2], writes=[b_FMst[p][a]])
        for ti in range(3):
            for di, a in enumerate([2, 3, 6, 7]):
                S.op("pe", lambda e, ti=ti, a=a, di=di: e.transpose(PT[:, di * 128:(di + 1) * 128], FMst[p][:, ti, a, :], ident_bf[:]),
                     reads=[b_FMst[p][a], b_identbf], writes=[b_PT])
            S.op("act", lambda e, ti=ti: e.activation(out=TMst[p][:, ti, 0:512], in_=PT[:, 0:512], func=AF.Copy),
                 reads=[b_PT], writes=[b_TMst[p][ti]])
        c0 = g * 3
        S.dma("sp", FM[c0:c0 + 3].rearrange("c p n -> p c n"), FMst[p][:].rearrange("p c a t -> p c (a t)"),
              reads=b_FMst[p], writes=b_FM_d[c0:c0 + 3])
        S.dma("sp", TM[c0:c0 + 3].rearrange("c p n -> p c n"), TMst[p][:], reads=b_TMst[p], writes=b_TM_d[c0:c0 + 3])
        S.dma("sp", OG[c0:c0 + 3].rearrange("c p n -> p c n"), OGst[p][:], reads=b_OGst[p], writes=b_OG_d[c0:c0 + 3])

    phase_end()
    wml = sb("wml", [128, 4, NT]); b_wml = Buf()
    flo = sb("flo", [128, 4, NT]); b_flo = Buf()
    decb = sb("decb", [128, 4, NT]); b_decb = Buf()
    wret = sb("wret", [128, 4]); rho = sb("rho", [128, 4]); dret = sb("dret", [128, 4]); b_retc = Buf()
    phase_begin()
    gb_s = sb("gb_s", [128, 8]); b_gb = Buf()
    S.dma("sp", gb_s[:], gbias.partition_broadcast(128), writes=[b_gb])
    Gi = sb("Gi", [128, 4, NT]); b_Gi = Buf()
    nlf = sb("nlf", [128, 4, NT]); b_nlf = Buf()
    for k in range(4):
        S.op("dve", lambda e, k=k: e.tensor_scalar(out=Gi[:, k, :], in0=Gall[:, k, :], scalar1=gb_s[:, k:k + 1], scalar2=None, op0=ALU.add),
             reads=[b_G, b_gb], writes=[b_Gi])
        S.op("dve", lambda e, k=k: e.tensor_scalar(out=nlf[:, k, :], in0=Gall[:, 4 + k, :], scalar1=gb_s[:, 4 + k:5 + k], scalar2=None, op0=ALU.add),
             reads=[b_G, b_gb], writes=[b_nlf])
    S.op("act", lambda e: e.activation(out=nlf[:], in_=nlf[:], func=AF.Exp, scale=-1.0), reads=[b_nlf], writes=[b_nlf])
    S.op("act", lambda e: e.activation(out=nlf[:], in_=nlf[:], func=AF.Ln, bias=1.0), reads=[b_nlf], writes=[b_nlf])
    nb = sb("nb", [128, 4, NT]); b_nb = Buf()
    nbL = sb("nbL", [128, 4, NT]); b_nbL = Buf()
    for d in range(2):
        S.op("pe", lambda e, d=d: e.matmul(PB[d][:, 0:2 * NT], lhsT=mask_f32[:, d, :], rhs=nlf[:, 2 * d:2 * d + 2, :].rearrange("p a c -> p (a c)"),
                                           start=True, stop=True), reads=[b_maskf, b_nlf], writes=[PBb[d]])
        S.op("dve", lambda e, d=d: e.tensor_copy(out=nb[:, 2 * d:2 * d + 2, :].rearrange("p a c -> p (a c)"), in_=PB[d][:, 0:2 * NT]),
             reads=[PBb[d]], writes=[b_nb])
    S.op("pe", lambda e: e.matmul(PB[2][:, 0:4 * NT], lhsT=ones_f[:], rhs=nlf[:].rearrange("p a c -> p (a c)"), start=True, stop=True),
         reads=[b_ones, b_nlf], writes=[PBb[2]])
    S.op("dve", lambda e: e.tensor_copy(out=nbL[:].rearrange("p a c -> p (a c)"), in_=PB[2][:, 0:4 * NT]), reads=[PBb[2]], writes=[b_nbL])
    av = sb("av", [128, 4, NT]); b_av = Buf()
    S.op("dve", lambda e: e.tensor_tensor(out=av[:], in0=Gi[:], in1=nb[:], op=ALU.add), reads=[b_Gi, b_nb], writes=[b_av])
    avf = av[:].rearrange("p a c -> p (a c)")
    Acol = sb("Acol", [128, 3]); b_Acol = Buf()
    S.op("dve", lambda e: e.memset(Acol[:], 0.0), writes=[b_Acol])
    pieces = [(0, 128), (128, 256), (256, 264)]
    for pi, (a0, a1) in enumerate(pieces):
        m = a1 - a0
        S.op("pe", lambda e, a0=a0, a1=a1, m=m, pi=pi: e.matmul(PB[3 + pi][0:m, 0:128], lhsT=avf[:, a0:a1], rhs=ident_f[:], start=True, stop=True),
             reads=[b_av, b_identf], writes=[PBb[3 + pi]])
        S.op("dve", lambda e, m=m, pi=pi: e.tensor_reduce(out=Acol[0:m, pi:pi + 1], in_=PB[3 + pi][0:m, 0:128], axis=AX.X, op=ALU.max),
             reads=[PBb[3 + pi]], writes=[b_Acol])
    Arow = sb("Arow", [1, 4, NT]); b_Arow = Buf()
    for pi, (a0, a1) in enumerate(pieces):
        m = a1 - a0
        S.op("pe", lambda e, a0=a0, a1=a1, m=m, pi=pi: e.matmul(PB[6][0:1, a0:a1], lhsT=Acol[0:128, pi:pi + 1], rhs=ident_f[:, 0:m],
                                                                start=True, stop=True), reads=[b_Acol, b_identf], writes=[PBb[6]])
    S.op("dve", lambda e: e.tensor_copy(out=Arow[:].rearrange("p a c -> p (a c)"), in_=PB[6][0:1, 0:4 * NT]), reads=[PBb[6]], writes=[b_Arow])
    MLrow = sb("MLrow", [1, 4, NT]); b_MLrow = Buf()
    dargrow = sb("dargrow", [1, 4, NT]); b_darg = Buf()
    mstate = sb("mstate", [1, 4]); b_ms = [Buf(), Buf()]
    b_MLd = [Buf(), Buf()]; b_dargd = [Buf(), Buf()]
    S.op("dve", lambda e: e.memset(mstate[:], 0.0), writes=b_ms)
    order = [list(range(NT)), [1, 0] + list(range(NT - 1, 1, -1))]
    engs = ["dve", "dve"]
    for i in range(NT):
        for d in range(2):
            c = order[d][i]
            en = engs[d]
            sl = slice(2 * d, 2 * d + 2)
            S.op(en, lambda e, c=c, sl=sl: e.tensor_tensor(out=MLrow[:, sl, c], in0=mstate[:, sl], in1=Arow[:, sl, c], op=ALU.max),
                 reads=[b_ms[d], b_Arow], writes=[b_MLd[d]])
            S.op(en, lambda e, c=c, sl=sl: e.tensor_tensor(out=dargrow[:, sl, c], in0=mstate[:, sl], in1=MLrow[:, sl, c], op=ALU.subtract),
                 reads=[b_ms[d], b_MLd[d]], writes=[b_dargd[d]])
            S.op(en, lambda e, c=c, sl=sl: e.tensor_tensor(out=mstate[:, sl], in0=MLrow[:, sl, c], in1=nbL[0:1, sl, c], op=ALU.subtract),
                 reads=[b_nbL, b_MLd[d]], writes=[b_ms[d]])
    MLb = sb("MLb", [128, 4, NT]); b_MLb = Buf()
    S.op("pe", lambda e: e.matmul(PB[0][:, 0:4 * NT], lhsT=ones_f[0:1, :], rhs=MLrow[:].rearrange("p a c -> p (a c)"), start=True, stop=True),
         reads=[b_ones] + b_MLd + b_dargd, writes=[PBb[0]])
    S.op("dve", lambda e: e.tensor_copy(out=MLb[:].rearrange("p a c -> p (a c)"), in_=PB[0][:, 0:4 * NT]), reads=[PBb[0]], writes=[b_MLb])
    S.op("pe", lambda e: e.matmul(PB[1][:, 0:4 * NT], lhsT=ones_f[0:1, :], rhs=dargrow[:].rearrange("p a c -> p (a c)"), start=True, stop=True),
         reads=[b_ones] + b_MLd + b_dargd, writes=[PBb[1]])
    S.op("act", lambda e: e.activation(out=decb[:].rearrange("p a c -> p (a c)"), in_=PB[1][:, 0:4 * NT], func=AF.Exp), reads=[PBb[1]], writes=[b_decb])
    S.op("dve", lambda e: e.tensor_tensor(out=wml[:], in0=av[:], in1=MLb[:], op=ALU.subtract), reads=[b_av, b_MLb], writes=[b_wml])
    S.op("act", lambda e: e.activation(out=wml[:], in_=wml[:], func=AF.Exp), reads=[b_wml], writes=[b_wml])
    S.op("dve", lambda e: e.tensor_tensor(out=flo[:], in0=nb[:], in1=MLb[:], op=ALU.subtract), reads=[b_nb, b_MLb], writes=[b_flo])
    S.op("act", lambda e: e.activation(out=flo[:], in_=flo[:], func=AF.Exp), reads=[b_flo], writes=[b_flo])
    lg = sb("lg", [128, 4]); b_lg = Buf()
    S.dma("sp", lg[:], rld.partition_broadcast(128), writes=[b_lg])
    for d in range(2):
        for h in range(2):
            x = 2 * d + h
            S.op("act", lambda e, x=x, d=d: e.activation(out=wret[:, x:x + 1], in_=poscol[:, d:d + 1], func=AF.Exp, scale=lg[:, x:x + 1]),
                 reads=[b_poscol, b_lg], writes=[b_retc])
            S.op("act", lambda e, x=x, d=d: e.activation(out=rho[:, x:x + 1], in_=poscol[:, 2 + d:3 + d], func=AF.Exp, scale=lg[:, x:x + 1]),
                 reads=[b_poscol, b_lg], writes=[b_retc])
    S.op("act", lambda e: e.activation(out=dret[:], in_=lg[:], func=AF.Exp, scale=128.0), reads=[b_lg], writes=[b_retc])

    phase_end()
    phase_begin()
    FMl = [[sb("FMl%d%d" % (d, i), [128, 8, 128], BF16) for i in range(2)] for d in range(2)]
    TMl = [[sb("TMl%d%d" % (d, i), [128, 8, 128], BF16) for i in range(2)] for d in range(2)]
    b_FMl = [[Buf() for _ in range(2)] for _ in range(2)]
    b_TMl = [[Buf() for _ in range(2)] for _ in range(2)]
    va = [sb("va%d" % i, [128, 132], BF16) for i in range(4)]; b_va = [Buf() for _ in range(4)]
    sm = [sb("sm%d" % i, [128, 128], BF16) for i in range(4)]; b_sm = [Buf() for _ in range(4)]
    CT32 = sb("CT32", [128, 8, 132]); CTd32 = sb("CTd32", [128, 8, 132]); CTbf = sb("CTbf", [128, 8, 132], BF16)
    b_CT32 = [Buf() for _ in range(8)]; b_CTd = [Buf() for _ in range(8)]; b_CTbf = [Buf() for _ in range(8)]
    S.op("dve", lambda e: e.memset(CTd32[:], 0.0), writes=b_CTd)
    S.op("dve", lambda e: e.memset(CTbf[:], 0.0), writes=b_CTbf)
    HSst = [[sb("HSst%d%d" % (d, i), [128, 512]) for i in range(2)] for d in range(2)]
    b_HSst = [[Buf() for _ in range(2)] for _ in range(2)]
    den = sb("den", [128, 4]); b_den = [Buf() for _ in range(4)]
    b_HS_d = [[Buf() for _ in range(NT)] for _ in range(2)]
    u = 0
    for i in range(NT):
        for d in range(2):
            c = order[d][i]
            cn = order[d][i + 1] if i + 1 < NT else c
            lp = i % 2
            S.dma("sp", FMl[d][lp][:].rearrange("p a t -> p (a t)"), FM[c], reads=[b_FM_d[c]], writes=[b_FMl[d][lp]])
            S.dma("sp", TMl[d][lp][:].rearrange("p a t -> p (a t)"), TM[c], reads=[b_TM_d[c]], writes=[b_TMl[d][lp]])
            hp = i % 2
            for typ in range(2):
                for h in range(2):
                    ch = typ * 4 + h * 2 + d
                    x = 2 * d + h
                    qT = FMl[d][lp][:, typ * 4 + h, :]
                    kT = FMl[d][lp][:, typ * 4 + 2 + h, :]
                    kk = TMl[d][lp][:, typ * 2 + h, :]
                    vv = TMl[d][lp][:, 4 + typ * 2 + h, :]
                    vi = u % 4
                    pS = PB[u % 2]; bS = PBb[u % 2]
                    pO = PB[2 + u % 2]; bO = PBb[2 + u % 2]
                    pC = PB[4 + u % 2]; bC = PBb[4 + u % 2]
                    u += 1
                    if typ == 0:
                        wcol = wml[:, x, c:c + 1]; wb_ = b_wml
                        dn = decb[:, x, cn:cn + 1]; db_ = b_decb
                    else:
                        wcol = wret[:, x:x + 1]; wb_ = b_retc
                        dn = dret[:, x:x + 1]; db_ = b_retc
                    S.op("dve", lambda e, vi=vi, vv=vv, wcol=wcol: e.tensor_scalar(out=va[vi][:, 0:128], in0=vv, scalar1=wcol, scalar2=None, op0=ALU.mult),
                         reads=[b_TMl[d][lp], wb_], writes=[b_va[vi]])
                    S.op("dve", lambda e, vi=vi, wcol=wcol: e.tensor_copy(out=va[vi][:, 128:129], in_=wcol), reads=[wb_], writes=[b_va[vi]])
                    S.op("pe", lambda e, pS=pS, kT=kT, qT=qT: e.matmul(pS[:, 0:128], lhsT=kT, rhs=qT, start=True, stop=True),
                         reads=[b_FMl[d][lp]], writes=[bS])
                    S.op("dve", lambda e, vi=vi, pS=pS, d=d: e.tensor_tensor(out=sm[vi][:], in0=pS[:, 0:128], in1=mask_bf[:, d, :], op=ALU.mult),
                         reads=[bS, b_maskbf], writes=[b_sm[vi]])
                    S.op("pe", lambda e, pO=pO, vi=vi: e.matmul(pO[:, 0:129], lhsT=sm[vi][:], rhs=va[vi][:, 0:129], start=True, stop=False),
                         reads=[b_sm[vi], b_va[vi]], writes=[bO])
                    S.op("pe", lambda e, pO=pO, qT=qT, ch=ch: e.matmul(pO[:, 0:129], lhsT=qT, rhs=CTbf[:, ch, 0:129], start=False, stop=True),
                         reads=[b_FMl[d][lp], b_CTbf[ch]], writes=[bO])
                    S.op("pe", lambda e, pC=pC, kk=kk, vi=vi: e.matmul(pC[:, 0:129], lhsT=kk, rhs=va[vi][:, 0:129], start=True, stop=True),
                         reads=[b_TMl[d][lp], b_va[vi]], writes=[bC])
                    S.op("dve", lambda e, pC=pC, ch=ch: e.tensor_tensor(out=CT32[:, ch, 0:129], in0=pC[:, 0:129], in1=CTd32[:, ch, 0:129], op=ALU.add),
                         reads=[bC, b_CTd[ch]], writes=[b_CT32[ch]])
                    S.op("act", lambda e, ch=ch, dn=dn: e.activation(out=CTd32[:, ch, 0:129], in_=CT32[:, ch, 0:129], func=AF.Copy, scale=dn),
                         reads=[b_CT32[ch], db_], writes=[b_CTd[ch]])
                    S.op("act", lambda e, ch=ch, dn=dn: e.activation(out=CTbf[:, ch, 0:129], in_=CT32[:, ch, 0:129], func=AF.Copy, scale=dn),
                         reads=[b_CT32[ch], db_], writes=[b_CTbf[ch]])
                    oc = (typ * 2 + h) * 128
                    if typ == 0:
                        S.op("act", lambda e, pO=pO, vi=vi: e.activation(out=den[:, vi:vi + 1], in_=pO[:, 128:129], func=AF.Abs),
                             reads=[bO], writes=[b_den[vi]])
                        S.op("dve", lambda e, vi=vi, x=x, c=c: e.tensor_scalar(out=den[:, vi:vi + 1], in0=den[:, vi:vi + 1], scalar1=flo[:, x, c:c + 1],
                                                                               scalar2=None, op0=ALU.max),
                             reads=[b_den[vi], b_flo], writes=[b_den[vi]])
                        S.op("dve", lambda e, vi=vi: e.reciprocal(out=den[:, vi:vi + 1], in_=den[:, vi:vi + 1]), reads=[b_den[vi]], writes=[b_den[vi]])
                        S.op("act", lambda e, pO=pO, vi=vi, oc=oc: e.activation(out=HSst[d][hp][:, oc:oc + 128], in_=pO[:, 0:128], func=AF.Copy,
                                                                               scale=den[:, vi:vi + 1]),
                             reads=[bO, b_den[vi]], writes=[b_HSst[d][hp]])
                    else:
                        S.op("act", lambda e, pO=pO, x=x, oc=oc: e.activation(out=HSst[d][hp][:, oc:oc + 128], in_=pO[:, 0:128], func=AF.Copy,
                                                                             scale=rho[:, x:x + 1]),
                             reads=[bO, b_retc], writes=[b_HSst[d][hp]])
            S.dma("sp", HS[d, c], HSst[d][hp][:], reads=[b_HSst[d][hp]], writes=[b_HS_d[d][c]])

    phase_end()
    phase_begin()
    gml = sb("gml", [128, 256]); gret = sb("gret", [128, 256]); b_gm = Buf()
    S.dma("sp", gml[:], mlg.partition_broadcast(128), writes=[b_gm])
    S.dma("sp", gret[:], retg.partition_broadcast(128), writes=[b_gm])
    h0 = [sb("h0_%d" % i, [128, 512]) for i in range(2)]; h1 = [sb("h1_%d" % i, [128, 512]) for i in range(2)]
    og = [sb("og%d" % i, [128, 512], BF16) for i in range(2)]
    b_h0 = [Buf(), Buf()]; b_h1 = [Buf(), Buf()]; b_og = [Buf(), Buf()]
    hz = sb("hz", [128, 512]); b_hz = Buf()
    sg = sb("sg", [128, 512]); b_sg = Buf()
    st6 = sb("st6", [128, 4, 6]); mv2 = sb("mv2", [128, 4, 2]); b_st = Buf(); b_mv = Buf()
    rs4 = sb("rs4", [128, 4]); b_rs4 = Buf()
    mo_st = [sb("mo_st%d" % i, [128, 512], BF16) for i in range(2)]; b_most = [Buf(), Buf()]
    b_out = Buf()
    for c in range(NT):
        p = c % 2
        S.dma("sp", h0[p][:], HS[0, c], reads=[b_HS_d[0][c]], writes=[b_h0[p]])
        S.dma("sp", h1[p][:], HS[1, c], reads=[b_HS_d[1][c]], writes=[b_h1[p]])
        S.dma("sp", og[p][:], OG[c], reads=[b_OG_d[c]], writes=[b_og[p]])
        S.op("dve", lambda e, p=p: e.tensor_tensor(out=hz[:], in0=h0[p][:], in1=h1[p][:], op=ALU.add), reads=[b_h0[p], b_h1[p]], writes=[b_hz])
        S.op("act", lambda e, p=p: e.activation(out=sg[:, 0:256], in_=og[p][:, 0:256], func=AF.Sigmoid), reads=[b_og[p]], writes=[b_sg])
        S.op("act", lambda e, p=p: e.activation(out=sg[:, 256:512], in_=og[p][:, 256:512], func=AF.Silu), reads=[b_og[p]], writes=[b_sg])
        S.op("dve", lambda e: e.tensor_tensor(out=hz[:, 0:256], in0=hz[:, 0:256], in1=sg[:, 0:256], op=ALU.mult), reads=[b_hz, b_sg], writes=[b_hz])
        for k in range(4):
            S.op("dve", lambda e, k=k: e.bn_stats(out=st6[:, k, :], in_=hz[:, k * 128:(k + 1) * 128]), reads=[b_hz], writes=[b_st])
            S.op("dve", lambda e, k=k: e.bn_aggr(out=mv2[:, k, :], in_=st6[:, k, :]), reads=[b_st], writes=[b_mv])
        S.op("dve", lambda e: e.tensor_scalar(out=rs4[:], in0=mv2[:, :, 1], scalar1=EPS, scalar2=None, op0=ALU.add),
             reads=[b_mv], writes=[b_rs4])
        S.op("act", lambda e: e.activation(out=rs4[:], in_=rs4[:], func=AF.Sqrt), reads=[b_rs4], writes=[b_rs4])
        S.op("dve", lambda e: e.reciprocal(out=rs4[:], in_=rs4[:]), reads=[b_rs4], writes=[b_rs4])
        for k in range(4):
            S.op("dve", lambda e, k=k: e.tensor_scalar(out=hz[:, k * 128:(k + 1) * 128], in0=hz[:, k * 128:(k + 1) * 128],
                                                      scalar1=mv2[:, k, 0:1], scalar2=rs4[:, k:k + 1], op0=ALU.subtract, op1=ALU.mult),
                 reads=[b_hz, b_mv, b_rs4], writes=[b_hz])
        S.op("dve", lambda e, p=p: e.tensor_tensor(out=mo_st[p][:, 0:256], in0=hz[:, 0:256], in1=gml[:], op=ALU.mult),
             reads=[b_hz, b_gm], writes=[b_most[p]])
        S.op("dve", lambda e: e.tensor_tensor(out=hz[:, 256:512], in0=hz[:, 256:512], in1=gret[:], op=ALU.mult), reads=[b_hz, b_gm], writes=[b_hz])
        S.op("dve", lambda e, p=p: e.tensor_tensor(out=mo_st[p][:, 256:512], in0=hz[:, 256:512], in1=sg[:, 256:512], op=ALU.mult),
             reads=[b_hz, b_sg], writes=[b_most[p]])
        S.dma("sp", merged[c * 128:(c + 1) * 128, :], mo_st[p][:], reads=[b_most[p]], writes=[b_out])
    phase_end()
    phase_end()
    return b_out


def l1_inputs(I, b, g, consts):
    d = dict(consts)
    d["xin"] = np.ascontiguousarray(np.concatenate([I["ctx"][b], I["x"][b]], 0))
    d["cT"] = np.ascontiguousarray(I["c"][b].reshape(8, 128).T)
    d["cctxT"] = np.ascontiguousarray(I["c_ctx"].reshape(8, 128).T)
    d["wmod"] = np.ascontiguousarray(I["w_mod"][0][:, 0:2048])
    d["bmod"] = np.ascontiguousarray(I["b_mod"][0][None, 0:2048])
    d["g1"] = np.ascontiguousarray(I["norm1_g"][0][None, :])
    W = I["w_in_even"][0]
    sp = np.cumsum([0, 512, 512, 512, 512, 8, 8, 512, 512, 512, 512])
    mq, mk, mv, mo, mi, mf, rq, rk, rv, rg = [W[:, sp[i]:sp[i + 1]] for i in range(10)]
    hs = [2 * g, 2 * g + 1]
    def hcols(M, h): return M[:, h * 128:(h + 1) * 128]
    def swp(M): return np.concatenate([M[:, 64:128], M[:, 0:64]], 1)
    fm = [hcols(mq, h) for h in hs] + [hcols(mk, h) for h in hs] + [hcols(rq, h) for h in hs] + [swp(hcols(rq, h)) for h in hs] \
        + [hcols(rk, h) for h in hs] + [swp(hcols(rk, h)) for h in hs]
    d["Wfm"] = np.ascontiguousarray(np.concatenate(fm, 1))
    gi = [mi[:, dd * 4 + h:dd * 4 + h + 1] for dd in range(2) for h in hs]
    gf = [mf[:, dd * 4 + h:dd * 4 + h + 1] for dd in range(2) for h in hs]
    tm = [hcols(mv, h) for h in hs] + [hcols(rv, h) for h in hs] + [hcols(mo, h) for h in hs] + [hcols(rg, h) for h in hs] + gi + gf
    d["Wtm"] = np.ascontiguousarray(np.concatenate(tm, 1))
    gb = I["ml_gate_b"][0]
    d["gbias"] = np.array([[gb[dd, 0, h] for dd in range(2) for h in hs] + [gb[dd, 1, h] for dd in range(2) for h in hs]], np.float32)
    d["rld"] = np.array([[I["ret_log_decay"][0][dd, h] for dd in range(2) for h in hs]], np.float32)
    d["mlg"] = np.ascontiguousarray(I["ml_norm_g"][0][hs].reshape(1, 256))
    d["retg"] = np.ascontiguousarray(I["ret_norm_g"][0][hs].reshape(1, 256))
    return d


NT2 = 33
GROUPS2 = [[0]] + [[1 + 4 * g + i for i in range(4)] for g in range(8)]


def host_consts_l2(half):
    c = common_consts()
    T = NT2 * 128
    inv = (10000.0 ** (-np.arange(16, dtype=np.float32) / 16)).astype(np.float32)
    t = np.arange(half * 4096, (half + 1) * 4096)
    rows = (t // 64).astype(np.float32); cols = (t % 64).astype(np.float32)
    ar = (rows[:, None] * inv[None, :]).astype(np.float32)
    ac = (cols[:, None] * inv[None, :]).astype(np.float32)
    cos64 = np.concatenate([np.cos(ar), np.cos(ar), np.cos(ac), np.cos(ac)], 1).astype(np.float32)
    sin64 = np.concatenate([-np.sin(ar), np.sin(ar), -np.sin(ac), np.sin(ac)], 1).astype(np.float32)
    cosT = np.ones((128, T), np.float32); sinT = np.zeros((128, T), np.float32)
    cosT[:, 128:] = np.concatenate([cos64, cos64], 1).T
    sinT[:, 128:] = np.concatenate([sin64, sin64], 1).T
    c["rc2"] = np.ascontiguousarray(cosT); c["rs2"] = np.ascontiguousarray(sinT)
    return c


def build_l2(cx, io, debug=None):
    nc = cx.nc
    S = cx.S
    cx.phase_begin()
    xin = io["xin"]
    MG = io["MG"]; b_MG = io["b_MG"]
    idxm_d = io["idxm"]
    cT = io["cT"]; cctxT = io["cctxT"]
    wmod = io["wmod"]; bmod = io["bmod"]
    g2_0 = io["g2_0"]; g1_1 = io["g1_1"]
    w_out = io["w_out"]
    Wr = io["Wr"]; br = io["br"]
    w1 = io["w1"]; w3 = io["w3"]; w2 = io["w2"]
    Wq = io["Wq"]; Wk = io["Wk"]; Wv = io["Wv"]
    rc2_d = io["rc2"]; rs2_d = io["rs2"]
    hout = io["H2"]
    QT = io["QTs"]
    KT = io["KTo"]
    Vo = io["Vown"]
    H1 = cx.dscr("H1", [NT2, 128, D]); b_H1 = [Buf() for _ in range(NT2)]
    Vs = cx.dscr("Vs", [NT2, 128, D], BF16); b_Vs = [Buf() for _ in range(NT2)]
    UT = cx.dscr("UT", [NT2, 128, 8, 128], BF16); b_UT = [Buf() for _ in range(NT2)]

    def lc(name, shape, dt=F32):
        t = cx.sb(name + "_2s", shape, dt); b = Buf()
        S.dma("sp", t[:], io[name], writes=[b])
        return t, b
    idxm, b_idxm = lc("idxm", [128, NT2, 2], I32)
    ident_bf = lc("ident_bf", [128, 128], BF16)
    ident_f = lc("ident_f", [128, 128])
    ones_f = lc("ones_f", [128, 128])
    slt = lc("slt", [128, 128])
    blkstart = lc("blkstart", [128, 1])
    pcol = lc("pcol", [128, 1])
    thr = lc("thr", [128, 34])
    consts = {"thr": thr, "ident_bf": ident_bf, "ident_f": ident_f, "ones_f": ones_f, "slt": slt, "blkstart": blkstart, "pcol": pcol}
    moe = MoE(cx, NT2, Wr, br, w1, w3, w2, consts, tag="A")
    moe.alloc_persistent()
    moe.precast()

    mods = cx.sb("mods", [128, 2, 6, D]); b_mods = Buf()
    cx.phase_begin()
    scb, b_scb = emit_silu_bcast(cx, [cT, cctxT], ones_f[0], ones_f[1])
    bmod_s, b_bmod = cx.load_bcast("bmod2", 6144, bmod)
    g2_s, b_g2 = cx.load_bcast("g2_0", D, g2_0)
    g1n_s, b_g1n = cx.load_bcast("g1_1", D, g1_1)
    wm_s = [cx.sb("wm_s%d" % i, [128, 8, 512]) for i in range(2)]; b_wm = [Buf(), Buf()]
    emit_mod(cx, scb, b_scb, 2, wmod, bmod_s, b_bmod, 6144,
             lambda w, cc: mods[:, w, cc // 2, (cc % 2) * 512:(cc % 2 + 1) * 512], b_mods, wm_s, b_wm)
    for w in range(2):
        for slot, gt, bg in [(2, g2_s, b_g2), (5, g1n_s, b_g1n)]:
            S.op("dve", lambda e, w=w, slot=slot, gt=gt: e.scalar_tensor_tensor(out=mods[:, w, slot, :], in0=mods[:, w, slot, :], scalar=1.0,
                                                                               in1=gt[:], op0=ALU.add, op1=ALU.mult),
                 reads=[b_mods, bg], writes=[b_mods])
    cx.phase_end()

    cx.phase_begin()
    wo_s = cx.sb("wo_s", [128, 8, D], BF16); b_wo = Buf()
    for j in range(8):
        S.dma("pool", wo_s[:, j, :], w_out[j * 128:(j + 1) * 128, :], writes=[b_wo])
    nb = NormBufs(cx)
    ht = [cx.sb("ht%d" % i, [128, D]) for i in range(2)]; b_ht = [Buf(), Buf()]
    mt = [cx.sb("mt%d" % i, [128, D], BF16) for i in range(2)]; b_mt = [Buf(), Buf()]
    mT = [cx.sb("mT%d" % i, [128, 8, 128], BF16) for i in range(2)]; b_mT = [Buf(), Buf()]
    vt = [cx.sb("vt%d" % i, [128, D], BF16) for i in range(2)]; b_vt = [Buf(), Buf()]
    vT = [cx.sb("vT%d" % i, [128, 8, 128], BF16) for i in range(2)]; b_vT = [Buf(), Buf()]
    ytmp = cx.sb("ytmp", [128, D]); b_ytmp = Buf()
    for ti in range(NT2):
        p = ti % 2
        w = 1 if ti == 0 else 0
        S.dma("sp", ht[p][:], xin[ti * 128:(ti + 1) * 128, :], writes=[b_ht[p]])
        for r in range(2):
            S.dma("pool", mt[p][:, r * 512:(r + 1) * 512], MG, reads=[b_MG, b_idxm], writes=[b_mt[p]],
                  indirect={"in_offset": bass.IndirectOffsetOnAxis(ap=idxm[:, ti, r:r + 1], axis=0)})
        cmap = [(0, 0), (0, 1), (1, 0), (1, 1), (0, 2), (0, 3), (1, 2), (1, 3)]
        emit_transpose(cx, lambda j, p=p: mt[p][:, cmap[j][0] * 512 + cmap[j][1] * 128: cmap[j][0] * 512 + (cmap[j][1] + 1) * 128],
                       b_mt[p], 8, ident_bf[0], ident_bf[1], mT[p][:], b_mT[p])
        for half in range(2):
            pc, bc = cx.PB[half], cx.PBb[half]
            for j in range(8):
                S.op("pe", lambda e, j=j, p=p, pc=pc, half=half: e.matmul(pc[:], lhsT=mT[p][:, j, :], rhs=wo_s[:, j, half * 512:(half + 1) * 512],
                                                                          start=(j == 0), stop=(j == 7)),
                     reads=[b_mT[p], b_wo], writes=[bc])
            sl = slice(half * 512, (half + 1) * 512)
            S.op("dve", lambda e, pc=pc, w=w, sl=sl: e.tensor_tensor(out=ytmp[:, sl], in0=pc[:], in1=mods[:, w, 0, sl], op=ALU.mult),
                 reads=[bc, b_mods], writes=[b_ytmp])
            S.op("dve", lambda e, p=p, sl=sl: e.tensor_tensor(out=ht[p][:, sl], in0=ht[p][:, sl], in1=ytmp[:, sl], op=ALU.add),
                 reads=[b_ytmp, b_ht[p]], writes=[b_ht[p]])
        S.dma("sp", H1[ti], ht[p][:], reads=[b_ht[p]], writes=[b_H1[ti]])
        emit_adaln(cx, nb, ht[p][:], b_ht[p], mods[:, w, 2, :], mods[:, w, 1, :], b_mods, vt[p][:], b_vt[p])
        S.dma("sp", Vs[ti], vt[p][:], reads=[b_vt[p]], writes=[b_Vs[ti]])
        emit_transpose(cx, lambda j, p=p: vt[p][:, j * 128:(j + 1) * 128], b_vt[p], 8, ident_bf[0], ident_bf[1], vT[p][:], b_vT[p])
        moe.route_tile(ti, lambda j, p=p: vT[p][:, j, :], b_vT[p])
    cx.phase_end()

    cx.phase_begin()
    moe.plan()
    vl = [cx.sb("vl%d" % i, [128, D], BF16) for i in range(2)]; b_vl = [Buf(), Buf()]
    for ti in range(NT2):
        p = ti % 2
        S.dma("sp", vl[p][:], Vs[ti], reads=[b_Vs[ti]], writes=[b_vl[p]])
        moe.dispatch_tile(ti, vl[p][:], b_vl[p], do_scatter=(debug != "plan"))
    if debug == "plan":
        dbg_dest = cx.dout("dbg_dest", [128, NT2 * 2], I32)
        dbg_idxw = cx.dout("dbg_idxw", [128, 128], I32)
        dbg_gate = cx.dout("dbg_gate", [128, NT2 * 2])
        dbg_oh = cx.dout("dbg_oh", [128, NT2 * 64])
        b_dbg = Buf()
        S.dma("sp", dbg_dest, moe.dest[:].rearrange("p t k -> p (t k)"), reads=moe.b_dest, writes=[b_dbg])
        S.dma("sp", dbg_idxw, moe.idxw[:], reads=[moe.b_idxw], writes=[b_dbg])
        S.dma("sp", dbg_gate, moe.gate[:].rearrange("p t k -> p (t k)"), reads=moe.b_gate, writes=[b_dbg])
        S.dma("sp", dbg_oh, moe.OH[:].rearrange("p t k e -> p (t k e)"), reads=moe.b_OH, writes=[b_dbg])
        cx.phase_end()
        return None
    moe.experts()
    cx.phase_end()

    cx.phase_begin()
    nb = NormBufs(cx, "d")
    h1t = [cx.sb("h1t%d" % i, [128, D]) for i in range(2)]; b_h1t = [Buf(), Buf()]
    y0 = [cx.sb("y0_%d" % i, [128, D]) for i in range(2)]; b_y0 = [Buf(), Buf()]
    y1 = [cx.sb("y1_%d" % i, [128, D]) for i in range(2)]; b_y1 = [Buf(), Buf()]
    ut = [cx.sb("ut%d" % i, [128, D], BF16) for i in range(2)]; b_ut = [Buf(), Buf()]
    uTt = [cx.sb("uTt%d" % i, [128, 8, 128], BF16) for i in range(2)]; b_uTt = [Buf(), Buf()]
    b_hout = Buf()
    for ti in range(NT2):
        p = ti % 2
        w = 1 if ti == 0 else 0
        S.dma("sp", h1t[p][:], H1[ti], reads=[b_H1[ti]], writes=[b_h1t[p]])
        moe.gather_tile(ti, y0[p][:], y1[p][:], b_y0[p], b_y1[p])
        S.op("dve", lambda e, p=p, ti=ti: e.tensor_scalar(out=y0[p][:], in0=y0[p][:], scalar1=moe.gate[:, ti, 0:1], scalar2=None, op0=ALU.mult),
             reads=[b_y0[p], moe.b_gate[ti]], writes=[b_y0[p]])
        S.op("dve", lambda e, p=p, ti=ti: e.scalar_tensor_tensor(out=y0[p][:], in0=y1[p][:], scalar=moe.gate[:, ti, 1:2], in1=y0[p][:],
                                                                op0=ALU.mult, op1=ALU.add),
             reads=[b_y0[p], b_y1[p], moe.b_gate[ti]], writes=[b_y0[p]])
        S.op("dve", lambda e, p=p, w=w: e.tensor_tensor(out=y0[p][:], in0=y0[p][:], in1=mods[:, w, 3, :], op=ALU.mult),
             reads=[b_y0[p], b_mods], writes=[b_y0[p]])
        S.op("dve", lambda e, p=p: e.tensor_tensor(out=h1t[p][:], in0=h1t[p][:], in1=y0[p][:], op=ALU.add),
             reads=[b_y0[p], b_h1t[p]], writes=[b_h1t[p]])
        S.dma("sp", hout[ti * 128:(ti + 1) * 128, :], h1t[p][:], reads=[b_h1t[p]], writes=[b_hout])
        emit_adaln(cx, nb, h1t[p][:], b_h1t[p], mods[:, w, 5, :], mods[:, w, 4, :], b_mods, ut[p][:], b_ut[p])
        emit_transpose(cx, lambda j, p=p: ut[p][:, j * 128:(j + 1) * 128], b_ut[p], 8, ident_bf[0], ident_bf[1], uTt[p][:], b_uTt[p])
        S.dma("sp", UT[ti], uTt[p][:], reads=[b_uTt[p]], writes=[b_UT[ti]])
    cx.phase_end()

    cx.phase_begin()
    Wq_s = cx.sb("Wq_s", [128, 8, 2048], BF16); Wk_s = cx.sb("Wk_s", [128, 8, 2048], BF16); Wv_s = cx.sb("Wv_s", [128, 8, D], BF16)
    b_Wq = Buf(); b_Wk = Buf(); b_Wv = Buf()
    for j in range(8):
        S.dma("pool", Wq_s[:, j, :], Wq[j * 128:(j + 1) * 128, :], writes=[b_Wq])
        S.dma("pool", Wk_s[:, j, :], Wk[j * 128:(j + 1) * 128, :], writes=[b_Wk])
        S.dma("pool", Wv_s[:, j, :], Wv[j * 128:(j + 1) * 128, :], writes=[b_Wv])
    uTg = [cx.sb("uTg%d" % i, [128, 4, 8, 128], BF16) for i in range(2)]; b_uTg = [Buf(), Buf()]
    rc = [cx.sb("rc%d" % i, [128, 512]) for i in range(2)]; rsn = [cx.sb("rsn%d" % i, [128, 512]) for i in range(2)]; b_rope = [Buf(), Buf()]
    t1 = cx.sb("t1", [128, 512]); t2 = cx.sb("t2", [128, 512]); b_t1 = Buf(); b_t2 = Buf()
    qkst = [cx.sb("qkst%d" % i, [128, 512], BF16) for i in range(4)]; b_qkst = [Buf() for _ in range(4)]
    vst = [cx.sb("vst%d" % i, [128, D], BF16) for i in range(2)]; b_vst = [Buf(), Buf()]
    b_QT = Buf(); b_KT = Buf(); b_Vo = Buf()
    si = 0; vi = 0; fi = 0
    for gi, tiles in enumerate(GROUPS2):
        p = gi % 2
        n = len(tiles) * 128
        t0 = tiles[0]
        S.dma("sp", uTg[p][:, 0:len(tiles)], UT[t0:t0 + len(tiles)].rearrange("c p j t -> p c j t"),
              reads=b_UT[t0:t0 + len(tiles)], writes=[b_uTg[p]])
        S.dma("sp", rc[p][:, 0:n], rc2_d[:, t0 * 128:t0 * 128 + n], writes=[b_rope[p]])
        S.dma("sp", rsn[p][:, 0:n], rs2_d[:, t0 * 128:t0 * 128 + n], writes=[b_rope[p]])
        for ci, ti in enumerate(tiles):
            vq = vi % 2; vi += 1
            for half in range(2):
                pc, bc = cx.PB[half], cx.PBb[half]
                for j in range(8):
                    S.op("pe", lambda e, j=j, ci=ci, pc=pc, half=half: e.matmul(pc[:], lhsT=uTg[p][:, ci, j, :], rhs=Wv_s[:, j, half * 512:(half + 1) * 512],
                                                                                start=(j == 0), stop=(j == 7)),
                         reads=[b_uTg[p], b_Wv], writes=[bc])
                S.op("act", lambda e, vq=vq, pc=pc, half=half: e.activation(out=vst[vq][:, half * 512:(half + 1) * 512], in_=pc[:], func=AF.Copy),
                     reads=[bc], writes=[b_vst[vq]])
            S.dma("sp", Vo[ti * 128:(ti + 1) * 128, :], vst[vq][:], reads=[b_vst[vq]], writes=[b_Vo])
        for qk in range(2):
            if qk == 0 and gi == 0:
                continue
            Ws, bW = (Wq_s, b_Wq) if qk == 0 else (Wk_s, b_Wk)
            sc_ = 0.125 if qk == 0 else 1.0
            for h in range(8):
                pa = 2 + fi % 4; fi += 1
                pb_ = 2 + fi % 4; fi += 1
                for (pp, cb) in [(pa, h), (pb_, 8 + h)]:
                    for j in range(8):
                        S.op("pe", lambda e, j=j, pp=pp, cb=cb: e.matmul(cx.PB[pp][:, 0:n], lhsT=Ws[:, j, cb * 128:(cb + 1) * 128],
                                                                         rhs=uTg[p][:, 0:len(tiles), j, :],
                                                                         start=(j == 0), stop=(j == 7)),
                             reads=[bW, b_uTg[p]], writes=[cx.PBb[pp]])
                S.op("dve", lambda e, pa=pa: e.scalar_tensor_tensor(out=t1[:, 0:n], in0=cx.PB[pa][:, 0:n], scalar=sc_, in1=rc[p][:, 0:n],
                                                                    op0=ALU.mult, op1=ALU.mult), reads=[cx.PBb[pa], b_rope[p]], writes=[b_t1])
                S.op("dve", lambda e, pb_=pb_: e.scalar_tensor_tensor(out=t2[:, 0:n], in0=cx.PB[pb_][:, 0:n], scalar=sc_, in1=rsn[p][:, 0:n],
                                                                      op0=ALU.mult, op1=ALU.mult), reads=[cx.PBb[pb_], b_rope[p]], writes=[b_t2])
                sq = si % 4; si += 1
                S.op("dve", lambda e, sq=sq: e.tensor_tensor(out=qkst[sq][:, 0:n], in0=t1[:, 0:n], in1=t2[:, 0:n], op=ALU.add),
                     reads=[b_t1, b_t2], writes=[b_qkst[sq]])
                if qk == 0:
                    q0 = (t0 - 1) * 128
                    S.dma("sp", QT[h, :, q0:q0 + n], qkst[sq][:, 0:n], reads=[b_qkst[sq]], writes=[b_QT])
                else:
                    S.dma("sp", KT[h, :, t0 * 128:t0 * 128 + n], qkst[sq][:, 0:n], reads=[b_qkst[sq]], writes=[b_KT])
    cx.phase_end()
    cx.phase_end()
    return b_hout, b_QT, b_KT, b_Vo


def l2_inputs(I, b, half, merged_full_b, consts):
    d = dict(consts)
    cs = slice(half * 128, (half + 1) * 128)
    ls = slice(half * 4096, (half + 1) * 4096)
    d["xin"] = np.ascontiguousarray(np.concatenate([I["ctx"][b][cs], I["x"][b][ls]], 0))
    if merged_full_b is not None:
        d["mergedIn"] = np.ascontiguousarray(np.concatenate([merged_full_b[0:256][cs], merged_full_b[256:][ls]], 0))
    d["cT"] = np.ascontiguousarray(I["c"][b].reshape(8, 128).T)
    d["cctxT"] = np.ascontiguousarray(I["c_ctx"].reshape(8, 128).T)
    d["wmod"] = np.ascontiguousarray(np.concatenate([I["w_mod"][0][:, 2048:6144], I["w_mod"][1][:, 0:2048]], 1))
    d["bmod"] = np.ascontiguousarray(np.concatenate([I["b_mod"][0][2048:6144], I["b_mod"][1][0:2048]])[None, :])
    d["g2_0"] = np.ascontiguousarray(I["norm2_g"][0][None, :])
    d["g1_1"] = np.ascontiguousarray(I["norm1_g"][1][None, :])
    d["w_out"] = I["w_out_even"][0]
    d["Wr"] = np.ascontiguousarray(np.concatenate([I["router_g_w"][0], I["router_e_w"][0]], 1))
    d["br"] = np.ascontiguousarray(np.concatenate([I["router_g_b"][0], I["router_e_b"][0]])[None, :])
    d["w1"] = I["w1"][0]; d["w3"] = I["w3"][0]; d["w2"] = I["w2"][0]
    W = I["w_in_odd"][0]
    perm = np.arange(64)
    perm = np.concatenate([perm[16:32], perm[0:16], perm[48:64], perm[32:48]])
    def swp(M):
        return M.reshape(D, 16, 64)[:, :, perm].reshape(D, 1024)
    d["Wq"] = np.ascontiguousarray(np.concatenate([W[:, 0:1024], swp(W[:, 0:1024])], 1))
    d["Wk"] = np.ascontiguousarray(np.concatenate([W[:, 1024:2048], swp(W[:, 1024:2048])], 1))
    d["Wv"] = np.ascontiguousarray(W[:, 2048:3072])
    return d


NT3 = 32
NK = 66
LAM_INIT = 0.8 - 0.6 * math.exp(-0.3 * 1)


def host_consts_l3():
    c = common_consts()
    c["ones_bf"] = np.ones((128, 128), np.float32).astype(ml_dtypes.bfloat16)
    return c


VCH = [(0, 1024), (1024, 1024), (2048, 1024), (3072, 1024), (4096, 128)]


def build_l3(cx, io, debug=None):
    nc = cx.nc
    S = cx.S
    cx.phase_begin()
    QT = io["QTs"]; b_QTs = io["b_QTs"]
    KTg = io["KTg"]; b_KTg = io["b_KTg"]
    VG = io["VG"]; b_VG = io["b_VG"]
    H2 = io["H2"]; b_H2 = io["b_H2"]
    cT = io["cT"]
    wmod = io["wmod"]; bmod = io["bmod"]
    g2 = io["g2"]; gfin = io["gfin"]
    dalam = io["dalam"]; dagT = io["dagT"]
    w_out = io["w_out"]
    Wr = io["Wr"]; br = io["br"]
    w1 = io["w1"]; w3 = io["w3"]; w2 = io["w2"]
    out = io["out"]
    H3 = cx.dscr("H3", [NT3, 128, D]); b_H3 = [Buf() for _ in range(NT3)]
    Vs = cx.dscr("Vs3", [NT3, 128, D], BF16); b_Vs = [Buf() for _ in range(NT3)]

    def lc(name, shape, dt=F32):
        t = cx.sb(name + "_3s", shape, dt); b = Buf()
        S.dma("sp", t[:], io[name], writes=[b])
        return t, b
    ident_bf = lc("ident_bf", [128, 128], BF16)
    ident_f = lc("ident_f", [128, 128])
    ones_f = lc("ones_f", [128, 128])
    ones_bf = lc("ones_bf", [128, 128], BF16)
    slt = lc("slt", [128, 128])
    blkstart = lc("blkstart", [128, 1])
    pcol = lc("pcol", [128, 1])
    thr = lc("thr", [128, 34])
    consts = {"thr": thr, "ident_bf": ident_bf, "ident_f": ident_f, "ones_f": ones_f, "slt": slt, "blkstart": blkstart, "pcol": pcol}
    moe = MoE(cx, NT3, Wr, br, w1, w3, w2, consts, tag="B")
    moe.alloc_persistent()
    moe.precast()

    mods = cx.sb("mods", [128, 4, D]); b_mods = Buf()
    gfin_s, b_gfin = cx.load_bcast("gfin3", D, gfin)
    cx.phase_begin()
    scb, b_scb = emit_silu_bcast(cx, [cT], ones_f[0], ones_f[1])
    bmod_s, b_bmod = cx.load_bcast("bmod3", 4096, bmod)
    g2_s, b_g2 = cx.load_bcast("g2_3", D, g2)
    wm_s = [cx.sb("wm_s%d" % i, [128, 8, 512]) for i in range(2)]; b_wm = [Buf(), Buf()]
    emit_mod(cx, scb, b_scb, 1, wmod, bmod_s, b_bmod, 4096,
             lambda w, cc: mods[:, cc // 2, (cc % 2) * 512:(cc % 2 + 1) * 512], b_mods, wm_s, b_wm)
    S.op("dve", lambda e: e.scalar_tensor_tensor(out=mods[:, 2, :], in0=mods[:, 2, :], scalar=1.0, in1=g2_s[:], op0=ALU.add, op1=ALU.mult),
         reads=[b_mods, b_g2], writes=[b_mods])
    cx.phase_end()

    cx.phase_begin()
    oT_all = cx.sb("oT_all", [128, 8, NT3 * 128], BF16); b_oT = [Buf() for _ in range(8)]
    lamw = cx.sb("lamw", [128, 256]); b_lamw = Buf()
    S.dma("sp", lamw[:], dalam.partition_broadcast(128), writes=[b_lamw])
    lamt = cx.sb("lamt", [128, 8]); b_lamt = Buf()
    prod = cx.sb("lprod", [128, 128]); b_prod = Buf()
    S.op("dve", lambda e: e.tensor_tensor(out=prod[:, 0:64], in0=lamw[:, 0:64], in1=lamw[:, 64:128], op=ALU.mult), reads=[b_lamw], writes=[b_prod])
    S.op("dve", lambda e: e.tensor_tensor(out=prod[:, 64:128], in0=lamw[:, 128:192], in1=lamw[:, 192:256], op=ALU.mult), reads=[b_lamw, b_prod], writes=[b_prod])
    S.op("dve", lambda e: e.tensor_reduce(out=lamt[:, 0:1], in_=prod[:, 0:64], axis=AX.X, op=ALU.add), reads=[b_prod], writes=[b_lamt])
    S.op("dve", lambda e: e.tensor_reduce(out=lamt[:, 1:2], in_=prod[:, 64:128], axis=AX.X, op=ALU.add), reads=[b_prod, b_lamt], writes=[b_lamt])
    S.op("act", lambda e: e.activation(out=lamt[:, 2:4], in_=lamt[:, 0:2], func=AF.Exp), reads=[b_lamt], writes=[b_lamt])
    S.op("dve", lambda e: e.tensor_tensor(out=lamt[:, 4:5], in0=lamt[:, 3:4], in1=lamt[:, 2:3], op=ALU.subtract), reads=[b_lamt], writes=[b_lamt])
    S.op("dve", lambda e: e.tensor_scalar(out=lamt[:, 4:5], in0=lamt[:, 4:5], scalar1=-LAM_INIT, scalar2=None, op0=ALU.add), reads=[b_lamt], writes=[b_lamt])
    gs = cx.sb("gs", [128, 8]); b_gs = Buf()
    S.dma("sp", gs[:], dagT, writes=[b_gs])
    S.op("dve", lambda e: e.tensor_scalar(out=gs[:], in0=gs[:], scalar1=1.0 - LAM_INIT, scalar2=None, op0=ALU.mult), reads=[b_gs], writes=[b_gs])

    cx.phase_begin()
    KTh = [cx.sb("KTh%d" % i, [128, NK * 128], BF16) for i in range(2)]; b_KTh = [Buf(), Buf()]
    Vh = [cx.sb("Vh%d" % i, [128, NK, 128], BF16) for i in range(2)]; b_Vh = [Buf(), Buf()]
    QTh = [cx.sb("QTh%d" % i, [128, NT3 * 128], BF16) for i in range(2)]; b_QTh = [Buf(), Buf()]
    Pt = [cx.sb("Pt%d" % i, [128, 512], BF16) for i in range(4)]; b_Pt = [Buf() for _ in range(4)]
    rec = cx.sb("rec", [128, 512]); b_rec = Buf()
    on1 = cx.sb("on1", [128, 512]); b_on1 = Buf()
    ot = cx.sb("ot", [128, 512]); b_ot = Buf()
    sq = cx.sb("sqt", [128, 512]); b_sq = Buf()
    rstd = cx.sb("rstdA", [128, 512]); b_rstdA = Buf()
    pi_ = 0
    si_ = 0
    nheads = 8 if debug != "attn1" else 1
    nqt = 8 if debug != "attn1" else 1
    for h in range(nheads):
        p = h % 2
        for r in range(2):
            S.dma("sp", KTh[p][:, r * 4224:(r + 1) * 4224], KTg[h, r * 128:(r + 1) * 128, :], reads=[b_KTg], writes=[b_KTh[p]])
            for (r0, n) in VCH:
                vr = 2 * r0 + r * n
                S.dma("sp", Vh[p][:, r * 33 + r0 // 128: r * 33 + (r0 + n) // 128, :],
                      VG[vr:vr + n, h * 128:(h + 1) * 128].rearrange("(k p) d -> p k d", p=128), reads=[b_VG], writes=[b_Vh[p]])
        S.dma("sp", QTh[p][:], QT[h], reads=[b_QTs], writes=[b_QTh[p]])
        for qt in range(nqt):
            qs = slice(qt * 512, (qt + 1) * 512)
            for m in range(2):
                ms = slice(m * 64, (m + 1) * 64)
                pO, bO = cx.PB[3], cx.PBb[3]
                pS, bS = cx.PB[4], cx.PBb[4]
                for kt in range(NK):
                    pst = si_ % 3; si_ += 1
                    S.op("pe", lambda e, pst=pst, p=p, ms=ms, kt=kt, qs=qs: e.matmul(cx.PB[pst][:], lhsT=KTh[p][ms, kt * 128:(kt + 1) * 128],
                                                                                     rhs=QTh[p][ms, qs], start=True, stop=True),
                         reads=[b_KTh[p], b_QTh[p]], writes=[cx.PBb[pst]])
                    pq = pi_ % 4; pi_ += 1
                    S.op("act", lambda e, pq=pq, pst=pst: e.activation(out=Pt[pq][:], in_=cx.PB[pst][:], func=AF.Exp),
                         reads=[cx.PBb[pst]], writes=[b_Pt[pq]])
                    S.op("pe", lambda e, pq=pq, p=p, kt=kt: e.matmul(pO[:], lhsT=Vh[p][:, kt, :], rhs=Pt[pq][:], start=(kt == 0), stop=(kt == NK - 1)),
                         reads=[b_Vh[p], b_Pt[pq]], writes=[bO])
                    S.op("pe", lambda e, pq=pq, kt=kt: e.matmul(pS[:], lhsT=ones_bf[0][:], rhs=Pt[pq][:], start=(kt == 0), stop=(kt == NK - 1)),
                         reads=[ones_bf[1], b_Pt[pq]], writes=[bS])
                S.op("dve", lambda e: e.reciprocal(out=rec[:], in_=pS[:]), reads=[bS], writes=[b_rec])
                if m == 0:
                    S.op("dve", lambda e: e.tensor_tensor(out=on1[:], in0=pO[:], in1=rec[:], op=ALU.mult), reads=[bO, b_rec], writes=[b_on1])
                else:
                    S.op("dve", lambda e: e.tensor_tensor(out=ot[:], in0=pO[:], in1=rec[:], op=ALU.mult), reads=[bO, b_rec], writes=[b_ot])
                    S.op("dve", lambda e: e.scalar_tensor_tensor(out=ot[:], in0=ot[:], scalar=lamt[:, 4:5], in1=on1[:], op0=ALU.mult, op1=ALU.add),
                         reads=[b_ot, b_on1, b_lamt], writes=[b_ot])
            S.op("act", lambda e: e.activation(out=sq[:], in_=ot[:], func=AF.Square), reads=[b_ot], writes=[b_sq])
            pN, bN = cx.PB[5], cx.PBb[5]
            S.op("pe", lambda e: e.matmul(pN[:], lhsT=ones_f[0][:], rhs=sq[:], start=True, stop=True), reads=[ones_f[1], b_sq], writes=[bN])
            S.op("dve", lambda e: e.tensor_scalar(out=rstd[:], in0=pN[:], scalar1=1.0 / 128, scalar2=EPS, op0=ALU.mult, op1=ALU.add),
                 reads=[bN], writes=[b_rstdA])
            S.op("act", lambda e: e.activation(out=rstd[:], in_=rstd[:], func=AF.Sqrt), reads=[b_rstdA], writes=[b_rstdA])
            S.op("dve", lambda e: e.reciprocal(out=rstd[:], in_=rstd[:]), reads=[b_rstdA], writes=[b_rstdA])
            S.op("dve", lambda e, h=h, qs=qs: e.scalar_tensor_tensor(out=oT_all[:, h, qs], in0=ot[:], scalar=gs[:, h:h + 1], in1=rstd[:],
                                                                    op0=ALU.mult, op1=ALU.mult),
                 reads=[b_ot, b_gs, b_rstdA], writes=[b_oT[h]])

    if debug == "attn1":
        dbg = cx.dout("dbg_oT", [128, 512], BF16); b_dbg = Buf()
        S.dma("sp", dbg, oT_all[:, 0, 0:512], reads=b_oT, writes=[b_dbg])
        cx.phase_end()
        S.finish([b_dbg], "sp")
        return nc

    cx.phase_end()
    cx.phase_begin()
    wo_s = cx.sb("wo_s", [128, 8, D], BF16); b_wo = Buf()
    for j in range(8):
        S.dma("pool", wo_s[:, j, :], w_out[j * 128:(j + 1) * 128, :], writes=[b_wo])
    nb = NormBufs(cx)
    ht = [cx.sb("ht%d" % i, [128, D]) for i in range(2)]; b_ht = [Buf(), Buf()]
    vt = [cx.sb("vt%d" % i, [128, D], BF16) for i in range(2)]; b_vt = [Buf(), Buf()]
    vT = [cx.sb("vT%d" % i, [128, 8, 128], BF16) for i in range(2)]; b_vT = [Buf(), Buf()]
    ytmp = cx.sb("ytmp", [128, D]); b_ytmp = Buf()
    for ti in range(NT3):
        p = ti % 2
        S.dma("sp", ht[p][:], H2[(ti + 1) * 128:(ti + 2) * 128, :], reads=[b_H2], writes=[b_ht[p]])
        for half in range(2):
            pc, bc = cx.PB[half], cx.PBb[half]
            for j in range(8):
                S.op("pe", lambda e, j=j, pc=pc, half=half, ti=ti: e.matmul(pc[:], lhsT=oT_all[:, j, ti * 128:(ti + 1) * 128],
                                                                            rhs=wo_s[:, j, half * 512:(half + 1) * 512], start=(j == 0), stop=(j == 7)),
                     reads=b_oT + [b_wo], writes=[bc])
            sl = slice(half * 512, (half + 1) * 512)
            S.op("dve", lambda e, pc=pc, sl=sl: e.tensor_tensor(out=ytmp[:, sl], in0=pc[:], in1=mods[:, 0, sl], op=ALU.mult),
                 reads=[bc, b_mods], writes=[b_ytmp])
            S.op("dve", lambda e, p=p, sl=sl: e.tensor_tensor(out=ht[p][:, sl], in0=ht[p][:, sl], in1=ytmp[:, sl], op=ALU.add),
                 reads=[b_ytmp, b_ht[p]], writes=[b_ht[p]])
        S.dma("sp", H3[ti], ht[p][:], reads=[b_ht[p]], writes=[b_H3[ti]])
        emit_adaln(cx, nb, ht[p][:], b_ht[p], mods[:, 2, :], mods[:, 1, :], b_mods, vt[p][:], b_vt[p])
        S.dma("sp", Vs[ti], vt[p][:], reads=[b_vt[p]], writes=[b_Vs[ti]])
        emit_transpose(cx, lambda j, p=p: vt[p][:, j * 128:(j + 1) * 128], b_vt[p], 8, ident_bf[0], ident_bf[1], vT[p][:], b_vT[p])
        moe.route_tile(ti, lambda j, p=p: vT[p][:, j, :], b_vT[p])
    cx.phase_end()
    cx.phase_end()

    cx.phase_begin()
    moe.plan()
    vl = [cx.sb("vl%d" % i, [128, D], BF16) for i in range(2)]; b_vl = [Buf(), Buf()]
    for ti in range(NT3):
        p = ti % 2
        S.dma("sp", vl[p][:], Vs[ti], reads=[b_Vs[ti]], writes=[b_vl[p]])
        moe.dispatch_tile(ti, vl[p][:], b_vl[p])
    moe.experts()
    cx.phase_end()

    cx.phase_begin()
    nb = NormBufs(cx, "d")
    h1t = [cx.sb("h1t%d" % i, [128, D]) for i in range(2)]; b_h1t = [Buf(), Buf()]
    y0 = [cx.sb("y0_%d" % i, [128, D]) for i in range(2)]; b_y0 = [Buf(), Buf()]
    y1 = [cx.sb("y1_%d" % i, [128, D]) for i in range(2)]; b_y1 = [Buf(), Buf()]
    ot_ = [cx.sb("ofin%d" % i, [128, D]) for i in range(2)]; b_ofin = [Buf(), Buf()]
    b_out = Buf()
    for ti in range(NT3):
        p = ti % 2
        S.dma("sp", h1t[p][:], H3[ti], reads=[b_H3[ti]], writes=[b_h1t[p]])
        moe.gather_tile(ti, y0[p][:], y1[p][:], b_y0[p], b_y1[p])
        S.op("dve", lambda e, p=p, ti=ti: e.tensor_scalar(out=y0[p][:], in0=y0[p][:], scalar1=moe.gate[:, ti, 0:1], scalar2=None, op0=ALU.mult),
             reads=[b_y0[p], moe.b_gate[ti]], writes=[b_y0[p]])
        S.op("dve", lambda e, p=p, ti=ti: e.scalar_tensor_tensor(out=y0[p][:], in0=y1[p][:], scalar=moe.gate[:, ti, 1:2], in1=y0[p][:],
                                                                op0=ALU.mult, op1=ALU.add),
             reads=[b_y0[p], b_y1[p], moe.b_gate[ti]], writes=[b_y0[p]])
        S.op("dve", lambda e, p=p: e.tensor_tensor(out=y0[p][:], in0=y0[p][:], in1=mods[:, 3, :], op=ALU.mult),
             reads=[b_y0[p], b_mods], writes=[b_y0[p]])
        S.op("dve", lambda e, p=p: e.tensor_tensor(out=h1t[p][:], in0=h1t[p][:], in1=y0[p][:], op=ALU.add),
             reads=[b_y0[p], b_h1t[p]], writes=[b_h1t[p]])
        emit_adaln(cx, nb, h1t[p][:], b_h1t[p], gfin_s[:], None, b_gfin, ot_[p][:], b_ofin[p])
        S.dma("sp", out[ti * 128:(ti + 1) * 128, :], ot_[p][:], reads=[b_ofin[p]], writes=[b_out])
    cx.phase_end()
    cx.phase_end()
    return b_out


def l3_inputs(I, b, half, l2res_pair, consts):
    d = dict(consts)
    if l2res_pair is not None:
        d["QT"] = l2res_pair[half]["QT"]
        d["KT"] = np.ascontiguousarray(np.concatenate([l2res_pair[0]["KT"], l2res_pair[1]["KT"]], 2))
        d["Vf"] = np.ascontiguousarray(np.concatenate([l2res_pair[0]["Vo"], l2res_pair[1]["Vo"]], 0))
        d["hin"] = np.ascontiguousarray(l2res_pair[half]["hout"][128:])
    d["cT"] = np.ascontiguousarray(I["c"][b].reshape(8, 128).T)
    d["wmod"] = np.ascontiguousarray(I["w_mod"][1][:, 2048:6144])
    d["bmod"] = np.ascontiguousarray(I["b_mod"][1][None, 2048:6144])
    d["g2"] = np.ascontiguousarray(I["norm2_g"][1][None, :])
    d["gfin"] = np.ascontiguousarray(I["final_norm_g"][None, :])
    d["dalam"] = np.ascontiguousarray(I["da_lambda"][0].reshape(1, 256))
    d["dagT"] = np.ascontiguousarray(I["da_norm_g"][0].T)
    d["w_out"] = I["w_out_odd"][0]
    d["Wr"] = np.ascontiguousarray(np.concatenate([I["router_g_w"][1], I["router_e_w"][1]], 1))
    d["br"] = np.ascontiguousarray(np.concatenate([I["router_g_b"][1], I["router_e_b"][1]])[None, :])
    d["w1"] = I["w1"][1]; d["w3"] = I["w3"][1]; d["w2"] = I["w2"][1]
    return d


PAIRS = [[0, 1], [2, 3], [4, 5], [6, 7]]
MCH = [(0, 2048), (2048, 2048), (4096, 2048), (6144, 2048), (8192, 256)]

L1_SPECS = [("xin", [NT * 128, D], F32), ("cT", [128, 8], F32), ("cctxT", [128, 8], F32), ("wmod", [D, 2048], F32), ("bmod", [1, 2048], F32),
            ("g1", [1, D], F32), ("Wfm", [D, 1536], F32), ("Wtm", [D, 1032], F32), ("gbias", [1, 8], F32), ("rld", [1, 4], F32),
            ("mlg", [1, 256], F32), ("retg", [1, 256], F32), ("ident_bf", [128, 128], BF16), ("ident_f", [128, 128], F32),
            ("mask_f32", [128, 2, 128], F32), ("mask_bf", [128, 2, 128], BF16), ("ones_f", [128, 128], F32), ("poscol", [128, 4], F32),
            ("ropecos", [128, NT * 128], F32), ("ropesin", [128, NT * 128], F32)]
L2_SPECS = [("xin", [NT2 * 128, D], F32), ("idxm", [128, NT2, 2], I32), ("cT", [128, 8], F32), ("cctxT", [128, 8], F32),
            ("wmod", [D, 6144], F32), ("bmod", [1, 6144], F32), ("g2_0", [1, D], F32), ("g1_1", [1, D], F32), ("w_out", [D, D], F32),
            ("Wr", [D, 36], F32), ("br", [1, 36], F32), ("w1", [32, D, 512], F32), ("w3", [32, D, 512], F32), ("w2", [32, 512, D], F32),
            ("Wq", [D, 2048], F32), ("Wk", [D, 2048], F32), ("Wv", [D, D], F32), ("rc2", [128, NT2 * 128], F32), ("rs2", [128, NT2 * 128], F32),
            ("ident_bf", [128, 128], BF16), ("ident_f", [128, 128], F32), ("ones_f", [128, 128], F32), ("slt", [128, 128], F32),
            ("blkstart", [128, 1], F32), ("pcol", [128, 1], F32), ("thr", [128, 34], F32)]
L3_SPECS = [("cT", [128, 8], F32), ("wmod", [D, 4096], F32), ("bmod", [1, 4096], F32), ("g2", [1, D], F32), ("gfin", [1, D], F32),
            ("dalam", [1, 256], F32), ("dagT", [128, 8], F32), ("w_out", [D, D], F32), ("Wr", [D, 36], F32), ("br", [1, 36], F32),
            ("w1", [32, D, 512], F32), ("w3", [32, D, 512], F32), ("w2", [32, 512, D], F32),
            ("ident_bf", [128, 128], BF16), ("ident_f", [128, 128], F32), ("ones_f", [128, 128], F32), ("ones_bf", [128, 128], BF16),
            ("slt", [128, 128], F32), ("blkstart", [128, 1], F32), ("pcol", [128, 1], F32), ("thr", [128, 34], F32)]


def build_fused(nc):
    cx = Ctx(nc)
    S = cx.S
    io1 = {n: cx.din("l1_" + n, sh, dt) for (n, sh, dt) in L1_SPECS}
    io2 = {n: cx.din("l2_" + n, sh, dt) for (n, sh, dt) in L2_SPECS}
    io3 = {n: cx.din("l3_" + n, sh, dt) for (n, sh, dt) in L3_SPECS}
    out = cx.dout("out", [NT3 * 128, D])

    mergedX = cx.dscr("mergedX", [NT * 128, 512], BF16)
    io1["merged"] = mergedX
    b_merged = build_l1(cx, io1)

    MG = cx.dscr("MG", [2 * NT * 128, 512], BF16); b_MG = Buf()
    for (r0, n) in MCH:
        S.collective("AllGather", [mergedX[r0:r0 + n, :]], [MG[2 * r0:2 * r0 + 2 * n, :]], PAIRS, reads=[b_merged], writes=[b_MG])

    H2 = cx.dscr("H2", [NT2 * 128, D])
    QTs = cx.dscr("QTs", [8, 128, 4096], BF16)
    KTo = cx.dscr("KTo", [8, 128, NT2 * 128], BF16)
    Vown = cx.dscr("Vown", [NT2 * 128, D], BF16)
    io2.update({"MG": MG, "b_MG": b_MG, "H2": H2, "QTs": QTs, "KTo": KTo, "Vown": Vown})
    b_H2, b_QTs, b_KTo, b_Vown = build_l2(cx, io2)

    KTg = cx.dscr("KTg", [8, 256, NT2 * 128], BF16); b_KTg = Buf()
    VG = cx.dscr("VG", [2 * NT2 * 128, D], BF16); b_VG = Buf()
    for h in range(8):
        S.collective("AllGather", [KTo[h]], [KTg[h]], PAIRS, reads=[b_KTo], writes=[b_KTg])
    for (r0, n) in VCH:
        S.collective("AllGather", [Vown[r0:r0 + n, :]], [VG[2 * r0:2 * r0 + 2 * n, :]], PAIRS, reads=[b_Vown], writes=[b_VG])

    io3.update({"QTs": QTs, "b_QTs": b_QTs, "KTg": KTg, "b_KTg": b_KTg, "VG": VG, "b_VG": b_VG, "H2": H2, "b_H2": b_H2, "out": out})
    b_out = build_l3(cx, io3)
    S.finish([b_out], "sp")
    print("fused instructions:", S.n_inst)
    return nc


def fused_inputs(I, core, c1, c2, c3):
    b, g = core // 2, core % 2
    d = {}
    for k, v in l1_inputs(I, b, g, c1).items():
        d["l1_" + k] = v
    d2 = l2_inputs(I, b, g, None, c2[g])
    for k, v in d2.items():
        d["l2_" + k] = v
    trow = np.concatenate([np.arange(g * 128, (g + 1) * 128), 256 + np.arange(g * 4096, (g + 1) * 4096)])
    r0 = np.minimum((trow // 2048) * 2048, 8192)
    n = np.where(r0 < 8192, 2048, 256)
    idx = np.stack([2 * r0 + r * n + (trow - r0) for r in range(2)], 1).astype(np.int32)
    d["l2_idxm"] = np.ascontiguousarray(idx.reshape(NT2, 128, 2).transpose(1, 0, 2))
    d3 = l3_inputs(I, b, g, None, c3)
    for k, v in d3.items():
        d["l3_" + k] = v
    return d


from concourse.bass_utils import run_bass_kernel_spmd

_NC_CACHE = {}


def kernel(**inputs):
    I = {k: np.asarray(v) for k, v in inputs.items()}
    cores = list(range(8))
    if "fused" not in _NC_CACHE:
        nc = bass.Bass("TRN2", target_bir_lowering=False)
        build_fused(nc)
        _NC_CACHE["fused"] = nc
    nc = _NC_CACHE["fused"]
    c1 = host_consts_l1(); c2 = [host_consts_l2(0), host_consts_l2(1)]; c3 = host_consts_l3()
    names = set(["l1_" + n for n, _, _ in L1_SPECS] + ["l2_" + n for n, _, _ in L2_SPECS] + ["l3_" + n for n, _, _ in L3_SPECS])
    in_maps = []
    for core in cores:
        m = fused_inputs(I, core, c1, c2, c3)
        in_maps.append({k: v for k, v in m.items() if k in names})
    res = run_bass_kernel_spmd(nc, in_maps, core_ids=cores).results
    out = np.empty((4, 8192, 1024), np.float32)
    for core in cores:
        b, half = core // 2, core % 2
        out[b, half * 4096:(half + 1) * 4096] = res[core]["out"]
    return out
```

```python
import math
import contextlib
import numpy as np
import ml_dtypes
import concourse.bass as bass
import concourse.mybir as mybir


F32 = mybir.dt.float32
BF16 = mybir.dt.bfloat16
I32 = mybir.dt.int32
U32 = mybir.dt.uint32
AF = mybir.ActivationFunctionType
ALU = mybir.AluOpType
AX = mybir.AxisListType


class Buf:
    __slots__ = ("name", "w", "r")

    def __init__(self, name=""):
        self.name = name
        self.w = None
        self.r = {}


class Sync:
    def __init__(self, nc, n_dma_sems=48, same_engine_wait=True):
        self.nc = nc
        self.eng = {"pe": nc.tensor, "act": nc.scalar, "dve": nc.vector, "pool": nc.gpsimd, "sp": nc.sync}
        self.sem = {}
        self.cnt = {}
        for k in ["pe", "act", "dve", "pool"]:
            self.sem[k] = nc.alloc_semaphore(name="s_" + k)
            self.cnt[k] = 0
        self.dsem = [nc.alloc_semaphore(name="s_dma%d" % i) for i in range(n_dma_sems)]
        self.dcnt = [0] * n_dma_sems
        self.dnext = 0
        self.waited = {k: {} for k in self.eng}
        self.same_engine_wait = same_engine_wait
        self.n_inst = 0

    def _semh(self, key):
        return self.sem[key] if isinstance(key, str) else self.dsem[key]

    def _wait(self, e, key, val):
        if val <= 0:
            return
        w = self.waited[e]
        if w.get(key, 0) >= val:
            return
        if key == e and not self.same_engine_wait:
            return
        if key == e == "pe":
            return
        self.eng[e].wait_ge(self._semh(key), val)
        w[key] = val

    def _deps(self, e, reads, writes):
        for b in reads:
            if b.w is not None:
                self._wait(e, *b.w)
        for b in writes:
            if b.w is not None:
                self._wait(e, *b.w)
            for k, v in b.r.items():
                self._wait(e, k, v)

    def _record(self, ev, reads, writes):
        k, v = ev
        for b in reads:
            if b.r.get(k, 0) < v:
                b.r[k] = v
        for b in writes:
            b.w = ev
            b.r = {}

    def op(self, e, fn, reads=(), writes=(), inc=True):
        self._deps(e, reads, writes)
        ins = fn(self.eng[e])
        if inc:
            self.cnt[e] += 1
            ins.then_inc(self.sem[e], 1)
            self._record((e, self.cnt[e]), reads, writes)
        else:
            self._record((e, self.cnt[e] + 1), reads, writes)
        self.n_inst += 1
        return ins

    def dma(self, q, out, in_, reads=(), writes=(), indirect=None, **kw):
        self._deps(q, reads, writes)
        k = self.dnext
        self.dnext = (self.dnext + 1) % len(self.dsem)
        self._wait(q, k, 16 * self.dcnt[k])
        if indirect is not None:
            ins = self.eng[q].indirect_dma_start(out, indirect.get("out_offset"), in_, indirect.get("in_offset"), **kw)
        else:
            ins = self.eng[q].dma_start(out=out, in_=in_, **kw)
        self.dcnt[k] += 1
        ins.then_inc(self.dsem[k], 16)
        self._record((k, 16 * self.dcnt[k]), reads, writes)
        self.n_inst += 1
        return ins

    def collective(self, kind, ins, outs, groups, reads=(), writes=()):
        self._deps("pool", reads, writes)
        if "cc" not in self.sem:
            self.sem["cc"] = self.nc.alloc_semaphore(name="s_cc")
            self.cnt["cc"] = 0
        ins_ = self.nc.gpsimd.collective_compute(kind, ALU.bypass, replica_groups=groups, ins=ins, outs=outs)
        self.cnt["cc"] += 1
        ins_.then_inc(self.sem["cc"], 1)
        self._record(("cc", self.cnt["cc"]), reads, writes)
        self.n_inst += 1
        return ins_

    def barrier(self):
        for e in self.eng:
            for k in self.sem:
                self._wait(e, k, self.cnt[k])
            for k in range(len(self.dsem)):
                self._wait(e, k, 16 * self.dcnt[k])

    def touch(self, e, reads=(), writes=()):
        self._deps(e, reads, writes)

    def finish(self, bufs, e="sp"):
        for b in bufs:
            if b.w is not None:
                self._wait(e, *b.w)


D = 1024
EPS = 1e-6
XOFF = 524288


class Ctx:
    def __init__(self, nc):
        self.nc = nc
        self.S = Sync(nc)
        self.stk = [contextlib.ExitStack()]
        self.PB = [nc.alloc_psum_tensor("pb%d" % i, [128, 512], F32) for i in range(7)]
        self.PBb = [Buf() for _ in range(7)]
        self.PT = nc.alloc_psum_tensor("pt_bf", [128, 1024], BF16)
        self.b_PT = Buf()
        self.nreg = 0

    def din(self, name, shape, dt=F32):
        return self.nc.dram_tensor(name, list(shape), dt, kind="ExternalInput").ap()

    def dout(self, name, shape, dt=F32):
        return self.nc.dram_tensor(name, list(shape), dt, kind="ExternalOutput").ap()

    def dscr(self, name, shape, dt=F32):
        return self.nc.dram_tensor(name, list(shape), dt).ap()

    def sb(self, name, shape, dt=F32):
        self.nreg += 1
        return self.stk[-1].enter_context(self.nc.sbuf_tensor("%s_u%d" % (name, self.nreg), list(shape), dt))

    def phase_begin(self):
        self.stk.append(contextlib.ExitStack())

    def phase_end(self):
        self.S.barrier()
        self.stk.pop().close()

    def load_const(self, name, shape, dt=F32):
        d = self.din(name, shape, dt)
        t = self.sb(name + "_s", shape, dt)
        b = Buf()
        self.S.dma("sp", t[:], d, writes=[b])
        return t, b

    def load_bcast(self, name, n, dram_ap=None):
        d = dram_ap if dram_ap is not None else self.din(name, [1, n])
        t = self.sb(name + "_s", [128, n])
        b = Buf()
        self.S.dma("sp", t[:], d.partition_broadcast(128), writes=[b])
        return t, b


def common_consts():
    c = {}
    c["ident_bf"] = np.eye(128, dtype=np.float32).astype(ml_dtypes.bfloat16)
    c["ident_f"] = np.eye(128, dtype=np.float32)
    c["ones_f"] = np.ones((128, 128), np.float32)
    s = np.arange(128)
    c["slt"] = (s[:, None] < s[None, :]).astype(np.float32)
    c["blkstart"] = (256.0 * s).astype(np.float32)[:, None]
    c["pcol"] = (1.0 * s).astype(np.float32)[:, None]
    c["thr"] = np.tile((256.0 * np.arange(34)).astype(np.float32)[None, :], (128, 1))
    return c


def emit_silu_bcast(cx, cT_aps, ones_f, b_ones):
    S = cx.S
    n = len(cT_aps)
    cT_s = cx.sb("cT_s", [128, n, 8]); b_cT = Buf()
    for i, a in enumerate(cT_aps):
        S.dma("sp", cT_s[:, i, :], a, writes=[b_cT])
    sc = cx.sb("sc", [128, n, 8]); b_sc = Buf()
    S.op("act", lambda e: e.activation(out=sc[:], in_=cT_s[:], func=AF.Silu), reads=[b_cT], writes=[b_sc])
    scb = cx.sb("scb", [128, n, 8, 128]); b_scb = Buf()
    for w in range(n):
        for j in range(8):
            S.op("dve", lambda e, w=w, j=j: e.tensor_scalar(out=scb[:, w, j, :], in0=ones_f[:], scalar1=sc[:, w, j:j + 1],
                                                           scalar2=None, op0=ALU.mult),
                 reads=[b_ones, b_sc], writes=[b_scb])
    return scb, b_scb


def emit_mod(cx, scb, b_scb, nvec, wmod_ap, bmod_s, b_bmod, ncols, dest_fn, b_dest, wm_s, b_wm):
    S = cx.S
    wmod_v = wmod_ap.rearrange("(j p) n -> p j n", p=128)
    k = 0
    for cc in range(ncols // 512):
        wb = cc % 2
        S.dma("sp", wm_s[wb][:], wmod_v[:, :, cc * 512:(cc + 1) * 512], writes=[b_wm[wb]])
        for w in range(nvec):
            pbi = k % 7; k += 1
            for j in range(8):
                S.op("pe", lambda e, w=w, j=j, wb=wb, pbi=pbi: e.matmul(cx.PB[pbi][:], lhsT=scb[:, w, j, :], rhs=wm_s[wb][:, j, :],
                                                                        start=(j == 0), stop=(j == 7)),
                     reads=[b_scb, b_wm[wb]], writes=[cx.PBb[pbi]])
            S.op("dve", lambda e, w=w, pbi=pbi, cc=cc: e.tensor_tensor(out=dest_fn(w, cc), in0=cx.PB[pbi][:],
                                                                      in1=bmod_s[:, cc * 512:(cc + 1) * 512], op=ALU.add),
                 reads=[cx.PBb[pbi], b_bmod], writes=[b_dest])


class NormBufs:
    def __init__(self, cx, tag=""):
        self.junk = cx.sb("junk" + tag, [128, D], BF16); self.b_junk = Buf()
        self.ss = cx.sb("ss" + tag, [128, 2]); self.b_ss = [Buf(), Buf()]
        self.rstd = cx.sb("rstd" + tag, [128, 2]); self.b_rstd = [Buf(), Buf()]
        self.tmp32 = cx.sb("tmp32" + tag, [128, D]); self.b_tmp32 = Buf()
        self.k = 0


def emit_adaln(cx, nb, x_ap, b_x, A_ap, Bsh_ap, b_mod, out_ap, b_out):
    S = cx.S
    xp = nb.k % 2; nb.k += 1
    ss = nb.ss[:, xp:xp + 1]; rstd = nb.rstd[:, xp:xp + 1]
    S.op("dve", lambda e: e.memset(ss, 0.0), writes=[nb.b_ss[xp]])
    S.op("act", lambda e: e.activation(out=nb.junk[:], in_=x_ap, func=AF.Square, accum_out=ss),
         reads=[b_x], writes=[nb.b_junk, nb.b_ss[xp]])
    S.op("dve", lambda e: e.tensor_scalar(out=rstd, in0=ss, scalar1=1.0 / D, scalar2=EPS, op0=ALU.mult, op1=ALU.add),
         reads=[nb.b_ss[xp]], writes=[nb.b_rstd[xp]])
    S.op("act", lambda e: e.activation(out=rstd, in_=rstd, func=AF.Sqrt), reads=[nb.b_rstd[xp]], writes=[nb.b_rstd[xp]])
    S.op("dve", lambda e: e.reciprocal(out=rstd, in_=rstd), reads=[nb.b_rstd[xp]], writes=[nb.b_rstd[xp]])
    if Bsh_ap is None:
        S.op("dve", lambda e: e.scalar_tensor_tensor(out=out_ap, in0=x_ap, scalar=rstd, in1=A_ap, op0=ALU.mult, op1=ALU.mult),
             reads=[b_x, nb.b_rstd[xp], b_mod], writes=[b_out])
    else:
        S.op("dve", lambda e: e.scalar_tensor_tensor(out=nb.tmp32[:], in0=x_ap, scalar=rstd, in1=A_ap, op0=ALU.mult, op1=ALU.mult),
             reads=[b_x, nb.b_rstd[xp], b_mod], writes=[nb.b_tmp32])
        S.op("dve", lambda e: e.tensor_tensor(out=out_ap, in0=nb.tmp32[:], in1=Bsh_ap, op=ALU.add),
             reads=[nb.b_tmp32, b_mod], writes=[b_out])


def emit_transpose(cx, src_fn, b_src, nblk, ident_bf, b_ident, dst_ap, b_dst):
    S = cx.S
    for j in range(nblk):
        S.op("pe", lambda e, j=j: e.transpose(cx.PT[:, j * 128:(j + 1) * 128], src_fn(j), ident_bf[:]),
             reads=[b_src, b_ident], writes=[cx.b_PT])
    S.op("act", lambda e: e.activation(out=dst_ap, in_=cx.PT[:, 0:nblk * 128].rearrange("p (j t) -> p j t", j=nblk), func=AF.Copy),
         reads=[cx.b_PT], writes=[b_dst])


class MoE:
    def __init__(self, cx, ntile, Wr_d, br_d, w1_d, w3_d, w2_d, consts, tag=""):
        self.cx = cx
        self.nt = ntile
        self.A = 2 * ntile * 128
        self.nblk = (self.A + 255) // 256 + 32
        self.c = consts
        self.tag = tag
        self.w1_d, self.w3_d, self.w2_d = w1_d, w3_d, w2_d
        self.Wr_d, self.br_d = Wr_d, br_d
        nr = self.nblk * 256
        self.xbuf = cx.dscr("xbuf" + tag, [nr, D], BF16); self.b_xbuf = Buf()
        self.ybuf = cx.dscr("ybuf" + tag, [nr, D], F32); self.b_ybuf = Buf()
        self.W1b = cx.dscr("W1b" + tag, [32 * 128, 4096], BF16); self.b_W1b = Buf()
        self.W3b = cx.dscr("W3b" + tag, [32 * 128, 4096], BF16); self.b_W3b = Buf()
        self.W2b = cx.dscr("W2b" + tag, [32 * 128, 4096], BF16); self.b_W2b = Buf()

    def precast(self):
        S = self.cx.S
        for (src, dst, b, j) in [(self.w1_d, self.W1b, self.b_W1b, 8), (self.w3_d, self.W3b, self.b_W3b, 8), (self.w2_d, self.W2b, self.b_W2b, 4)]:
            sv = src.rearrange("e (p j) n -> e p (j n)", j=j)
            for e_ in range(32):
                S.dma("pool", dst[e_ * 128:(e_ + 1) * 128, :].rearrange("p (a b) -> p a b", a=2), sv[e_].rearrange("p (a b) -> p a b", a=2), writes=[b])

    def alloc_persistent(self):
        cx = self.cx
        t = self.tag
        self.OH = cx.sb("OH" + t, [128, self.nt, 2, 32]); self.b_OH = [Buf() for _ in range(self.nt)]
        self.gate = cx.sb("gate" + t, [128, self.nt, 2]); self.b_gate = [Buf() for _ in range(self.nt)]
        self.dest = cx.sb("dest" + t, [128, self.nt, 2], I32); self.b_dest = [Buf() for _ in range(self.nt)]
        self.idxw = cx.sb("idxw" + t, [128, 128], I32); self.b_idxw = Buf()
        self.Wr_s = cx.sb("Wr_s" + t, [128, 8, 36], BF16); self.b_Wr = Buf()
        for j in range(8):
            cx.S.dma("pool", self.Wr_s[:, j, :], self.Wr_d[j * 128:(j + 1) * 128, :], writes=[self.b_Wr])
        self.br_s, self.b_br = cx.load_bcast("br" + t, 36, self.br_d)
        self.lg = cx.sb("lg" + t, [128, 36]); self.b_lg = Buf()
        self.sm = cx.sb("rsm" + t, [128, 64]); self.b_sm = Buf()

    def route_tile(self, ti, vT_fn, b_vT):
        cx, S = self.cx, self.cx.S
        pbi = 6
        PBt, bPB = cx.PB[pbi], cx.PBb[pbi]
        for j in range(8):
            S.op("pe", lambda e, j=j: e.matmul(PBt[:, 0:36], lhsT=vT_fn(j), rhs=self.Wr_s[:, j, :], start=(j == 0), stop=(j == 7)),
                 reads=[b_vT, self.b_Wr], writes=[bPB])
        lg, sm = self.lg, self.sm
        b_lg, b_sm = self.b_lg, self.b_sm
        S.op("dve", lambda e: e.tensor_tensor(out=lg[:], in0=PBt[:, 0:36], in1=self.br_s[:], op=ALU.add), reads=[bPB, self.b_br], writes=[b_lg])
        S.op("dve", lambda e: e.tensor_reduce(out=sm[:, 0:1], in_=lg[:, 0:4], axis=AX.X, op=ALU.max), reads=[b_lg], writes=[b_sm])
        S.op("dve", lambda e: e.tensor_scalar(out=sm[:, 1:2], in0=sm[:, 0:1], scalar1=-1.0, scalar2=None, op0=ALU.mult), reads=[b_sm], writes=[b_sm])
        S.op("dve", lambda e: e.memset(sm[:, 2:3], 0.0), reads=[b_sm], writes=[b_sm])
        S.op("act", lambda e: e.activation(out=sm[:, 44:48], in_=lg[:, 0:4], func=AF.Exp, bias=sm[:, 1:2], accum_out=sm[:, 2:3]),
             reads=[b_lg, b_sm], writes=[b_sm])
        S.op("dve", lambda e: e.reciprocal(out=sm[:, 3:4], in_=sm[:, 2:3]), reads=[b_sm], writes=[b_sm])
        S.op("dve", lambda e: e.tensor_scalar(out=sm[:, 4:8], in0=lg[:, 0:4], scalar1=sm[:, 0:1], scalar2=None, op0=ALU.is_equal),
             reads=[b_lg, b_sm], writes=[b_sm])
        S.op("dve", lambda e: e.tensor_scalar(out=sm[:, 8:16], in0=lg[:, 4:12], scalar1=sm[:, 4:5], scalar2=None, op0=ALU.mult),
             reads=[b_lg, b_sm], writes=[b_sm])
        for g in range(1, 4):
            S.op("dve", lambda e, g=g: e.scalar_tensor_tensor(out=sm[:, 8:16], in0=lg[:, 4 + 8 * g:12 + 8 * g], scalar=sm[:, 4 + g:5 + g],
                                                             in1=sm[:, 8:16], op0=ALU.mult, op1=ALU.add),
                 reads=[b_lg, b_sm], writes=[b_sm])
        S.op("dve", lambda e: e.max(out=sm[:, 16:24], in_=sm[:, 8:16]), reads=[b_sm], writes=[b_sm])
        S.op("dve", lambda e: e.tensor_scalar(out=sm[:, 24:32], in0=sm[:, 8:16], scalar1=sm[:, 16:17], scalar2=None, op0=ALU.is_equal),
             reads=[b_sm], writes=[b_sm])
        S.op("dve", lambda e: e.tensor_scalar(out=sm[:, 32:40], in0=sm[:, 8:16], scalar1=sm[:, 17:18], scalar2=None, op0=ALU.is_equal),
             reads=[b_sm], writes=[b_sm])
        S.op("dve", lambda e: e.tensor_tensor(out=sm[:, 40:41], in0=sm[:, 17:18], in1=sm[:, 16:17], op=ALU.subtract), reads=[b_sm], writes=[b_sm])
        S.op("act", lambda e: e.activation(out=sm[:, 41:42], in_=sm[:, 40:41], func=AF.Exp), reads=[b_sm], writes=[b_sm])
        S.op("dve", lambda e: e.tensor_scalar(out=sm[:, 42:43], in0=sm[:, 41:42], scalar1=1.0, scalar2=None, op0=ALU.add), reads=[b_sm], writes=[b_sm])
        S.op("dve", lambda e: e.reciprocal(out=sm[:, 42:43], in_=sm[:, 42:43]), reads=[b_sm], writes=[b_sm])
        S.op("dve", lambda e: e.tensor_tensor(out=sm[:, 43:44], in0=sm[:, 41:42], in1=sm[:, 42:43], op=ALU.mult), reads=[b_sm], writes=[b_sm])
        S.op("dve", lambda e: e.tensor_scalar(out=self.gate[:, ti, :], in0=sm[:, 42:44], scalar1=sm[:, 3:4], scalar2=None, op0=ALU.mult),
             reads=[b_sm], writes=[self.b_gate[ti]])
        for k in range(2):
            for g in range(4):
                S.op("dve", lambda e, k=k, g=g: e.tensor_scalar(out=self.OH[:, ti, k, g * 8:(g + 1) * 8], in0=sm[:, 24 + 8 * k:32 + 8 * k],
                                                               scalar1=sm[:, 4 + g:5 + g], scalar2=None, op0=ALU.mult),
                     reads=[b_sm], writes=[self.b_OH[ti]])

    def plan(self):
        cx, S = self.cx, self.cx.S
        t = self.tag
        ones_f, b_ones = self.c["ones_f"]
        ident_f, b_identf = self.c["ident_f"]
        blkstart, b_blk = self.c["blkstart"]
        PBt, bPB = cx.PB[0], cx.PBb[0]
        for ti in range(self.nt):
            S.op("pe", lambda e, ti=ti: e.matmul(PBt[:, 0:64], lhsT=ones_f[:], rhs=self.OH[:, ti, :, :].rearrange("p k e -> p (k e)"),
                                                 start=(ti == 0), stop=(ti == self.nt - 1)),
                 reads=[b_ones, self.b_OH[ti]], writes=[bPB])
        pl = cx.sb("pl" + t, [128, 8, 32]); b_pl = Buf()
        S.op("dve", lambda e: e.tensor_copy(out=pl[:, 7, :], in_=PBt[:, 0:32]), reads=[bPB], writes=[b_pl])
        S.op("dve", lambda e: e.tensor_tensor(out=pl[:, 0, :], in0=pl[:, 7, :], in1=PBt[:, 32:64], op=ALU.add), reads=[bPB, b_pl], writes=[b_pl])
        thr, b_thr = self.c["thr"]
        cmp3 = cx.sb("cmp3" + t, [128, 32, 34]); b_cmp3 = Buf()
        S.op("dve", lambda e: e.tensor_tensor(out=cmp3[:], in0=pl[:, 0, :].unsqueeze(2).broadcast_to([128, 32, 34]),
                                              in1=thr[:].unsqueeze(1).broadcast_to([128, 32, 34]), op=ALU.is_gt),
             reads=[b_pl, b_thr], writes=[b_cmp3])
        S.op("dve", lambda e: e.tensor_reduce(out=pl[:, 1, :], in_=cmp3[:], axis=AX.X, op=ALU.add), reads=[b_cmp3], writes=[b_pl])
        S.op("dve", lambda e: e.tensor_scalar(out=pl[:, 3, :], in0=pl[:, 1, :], scalar1=256.0, scalar2=None, op0=ALU.mult), reads=[b_pl], writes=[b_pl])
        S.op("dve", lambda e: e.memset(pl[:, 5, :], 0.0), reads=[b_pl], writes=[b_pl])
        S.op("dve", lambda e: e.tensor_tensor_scan(out=pl[:, 4, :], data0=pl[:, 3, :], data1=pl[:, 5, :], initial=0.0, op0=ALU.add, op1=ALU.add),
             reads=[b_pl], writes=[b_pl])
        self.run = cx.sb("run" + t, [128, 32]); self.b_run = Buf()
        S.op("dve", lambda e: e.tensor_tensor(out=self.run[:], in0=pl[:, 4, :], in1=pl[:, 3, :], op=ALU.subtract), reads=[b_pl], writes=[self.b_run])
        S.op("dve", lambda e: e.tensor_scalar(out=pl[:, 6, :], in0=pl[:, 4, :], scalar1=blkstart[:, 0:1], scalar2=None, op0=ALU.is_le),
             reads=[b_pl, b_blk], writes=[b_pl])
        be = cx.sb("be" + t, [128, 1]); b_be = Buf()
        S.op("dve", lambda e: e.tensor_reduce(out=be[:], in_=pl[:, 6, :], axis=AX.X, op=ALU.add), reads=[b_pl], writes=[b_be])
        S.op("dve", lambda e: e.tensor_scalar(out=be[:], in0=be[:], scalar1=31.0, scalar2=128.0, op0=ALU.min, op1=ALU.mult), reads=[b_be], writes=[b_be])
        bbc = cx.sb("bbc" + t, [128, 128]); b_bbc = Buf()
        pcol, b_pcol = self.c["pcol"]
        S.op("dve", lambda e: e.tensor_scalar(out=bbc[:], in0=ones_f[:], scalar1=be[:, 0:1], scalar2=None, op0=ALU.mult), reads=[b_be, b_ones], writes=[b_bbc])
        PB1, bPB1 = cx.PB[1], cx.PBb[1]
        S.op("pe", lambda e: e.matmul(PB1[:, 0:128], lhsT=bbc[:], rhs=ident_f[:], start=True, stop=True), reads=[b_bbc, b_identf], writes=[bPB1])
        idxf = cx.sb("idxf" + t, [128, 128]); b_idxf = Buf()
        S.op("dve", lambda e: e.tensor_scalar(out=idxf[:], in0=PB1[:, 0:128], scalar1=pcol[:, 0:1], scalar2=None, op0=ALU.add), reads=[bPB1, b_pcol], writes=[b_idxf])
        S.op("dve", lambda e: e.tensor_copy(out=self.idxw[:], in_=idxf[:]), reads=[b_idxf], writes=[self.b_idxw])
        self.d1 = cx.sb("d1" + t, [128, 32]); self.b_d1 = Buf()
        self.destf = cx.sb("destf" + t, [128, 2]); self.b_destf = Buf()

    def dispatch_tile(self, ti, v_ap, b_v, do_scatter=True):
        cx, S = self.cx, self.cx.S
        ones_f, b_ones = self.c["ones_f"]
        slt, b_slt = self.c["slt"]
        for k in range(2):
            pa, ba = cx.PB[2 + k], cx.PBb[2 + k]
            pt_, bt_ = cx.PB[4 + k], cx.PBb[4 + k]
            S.op("pe", lambda e, k=k, pa=pa: e.matmul(pa[:, 0:32], lhsT=slt[:], rhs=self.OH[:, ti, k, :], start=True, stop=True),
                 reads=[b_slt, self.b_OH[ti]], writes=[ba])
            S.op("pe", lambda e, k=k, pt_=pt_: e.matmul(pt_[:, 0:32], lhsT=ones_f[:], rhs=self.OH[:, ti, k, :], start=True, stop=True),
                 reads=[b_ones, self.b_OH[ti]], writes=[bt_])
            S.op("dve", lambda e, pa=pa: e.tensor_tensor(out=self.d1[:], in0=pa[:, 0:32], in1=self.run[:], op=ALU.add),
                 reads=[ba, self.b_run], writes=[self.b_d1])
            S.op("dve", lambda e, k=k: e.tensor_tensor(out=self.d1[:], in0=self.d1[:], in1=self.OH[:, ti, k, :], op=ALU.mult),
                 reads=[self.b_d1, self.b_OH[ti]], writes=[self.b_d1])
            S.op("dve", lambda e, k=k: e.tensor_reduce(out=self.destf[:, k:k + 1], in_=self.d1[:], axis=AX.X, op=ALU.add),
                 reads=[self.b_d1], writes=[self.b_destf])
            S.op("dve", lambda e, k=k: e.tensor_copy(out=self.dest[:, ti, k:k + 1], in_=self.destf[:, k:k + 1]),
                 reads=[self.b_destf], writes=[self.b_dest[ti]])
            S.op("dve", lambda e, pt_=pt_: e.tensor_tensor(out=self.run[:], in0=self.run[:], in1=pt_[:, 0:32], op=ALU.add),
                 reads=[bt_, self.b_run], writes=[self.b_run])
            if do_scatter:
              S.dma("pool", self.xbuf, v_ap, reads=[b_v, self.b_dest[ti]], writes=[self.b_xbuf],
                  indirect={"out_offset": bass.IndirectOffsetOnAxis(ap=self.dest[:, ti, k:k + 1], axis=0)})

    def experts(self):
        cx, S, nc = self.cx, self.cx.S, self.cx.nc
        t = self.tag
        ident_bf, b_identbf = self.c["ident_bf"]
        w1_s = [cx.sb("w1_s%d%s" % (i, t), [128, 8, 512], BF16) for i in range(2)]
        w3_s = [cx.sb("w3_s%d%s" % (i, t), [128, 8, 512], BF16) for i in range(2)]
        w2_s = [cx.sb("w2_s%d%s" % (i, t), [128, 4, 1024], BF16) for i in range(2)]
        b_w1 = [Buf(), Buf()]; b_w3 = [Buf(), Buf()]; b_w2 = [Buf(), Buf()]
        xb = [cx.sb("xb%d%s" % (i, t), [128, D], BF16) for i in range(2)]; b_xb = [Buf(), Buf()]
        xT = [cx.sb("xT%d%s" % (i, t), [128, 8, 128], BF16) for i in range(2)]; b_xT = [Buf(), Buf()]
        s1 = cx.sb("s1" + t, [128, 512]); b_s1 = Buf()
        hh = [cx.sb("hh%d%s" % (i, t), [128, 512], BF16) for i in range(2)]; b_hh = [Buf(), Buf()]
        hhT = [cx.sb("hhT%d%s" % (i, t), [128, 4, 128], BF16) for i in range(2)]; b_hhT = [Buf(), Buf()]
        yst = [cx.sb("yst%d%s" % (i, t), [128, D]) for i in range(2)]; b_yst = [Buf(), Buf()]
        u = 0
        for i in range(self.nblk):
            p = i % 2
            off = bass.IndirectOffsetOnAxis(ap=self.idxw[:, i:i + 1], axis=0)
            S.dma("pool", w1_s[p][:].rearrange("p j n -> p (j n)"), self.W1b, reads=[self.b_idxw, self.b_W1b], writes=[b_w1[p]], indirect={"in_offset": off})
            S.dma("pool", w3_s[p][:].rearrange("p j n -> p (j n)"), self.W3b, reads=[self.b_idxw, self.b_W3b], writes=[b_w3[p]], indirect={"in_offset": off})
            S.dma("pool", w2_s[p][:].rearrange("p j n -> p (j n)"), self.W2b, reads=[self.b_idxw, self.b_W2b], writes=[b_w2[p]], indirect={"in_offset": off})
            for sub in range(2):
                q = u % 2; u += 1
                row0 = (2 * i + sub) * 128
                S.dma("sp", xb[q][:], self.xbuf[row0:row0 + 128, :], reads=[self.b_xbuf], writes=[b_xb[q]])
                emit_transpose(cx, lambda j, q=q: xb[q][:].rearrange("t (p j) -> t j p", j=8)[:, j, :], b_xb[q], 8, ident_bf, b_identbf, xT[q][:], b_xT[q])
                pa, ba = cx.PB[0 + q], cx.PBb[0 + q]
                pb_, bb = cx.PB[2 + q], cx.PBb[2 + q]
                for j in range(8):
                    S.op("pe", lambda e, j=j, q=q, p=p, pa=pa: e.matmul(pa[:], lhsT=xT[q][:, j, :], rhs=w1_s[p][:, j, :], start=(j == 0), stop=(j == 7)),
                         reads=[b_xT[q], b_w1[p]], writes=[ba])
                for j in range(8):
                    S.op("pe", lambda e, j=j, q=q, p=p, pb_=pb_: e.matmul(pb_[:], lhsT=xT[q][:, j, :], rhs=w3_s[p][:, j, :], start=(j == 0), stop=(j == 7)),
                         reads=[b_xT[q], b_w3[p]], writes=[bb])
                S.op("act", lambda e, pa=pa: e.activation(out=s1[:], in_=pa[:], func=AF.Silu), reads=[ba], writes=[b_s1])
                S.op("dve", lambda e, q=q, pb_=pb_: e.tensor_tensor(out=hh[q][:], in0=s1[:], in1=pb_[:], op=ALU.mult), reads=[b_s1, bb], writes=[b_hh[q]])
                emit_transpose(cx, lambda j, q=q: hh[q][:].rearrange("t (p j) -> t j p", j=4)[:, j, :], b_hh[q], 4, ident_bf, b_identbf, hhT[q][:], b_hhT[q])
                for half in range(2):
                    pc, bc = cx.PB[4 + half], cx.PBb[4 + half]
                    for j in range(4):
                        S.op("pe", lambda e, j=j, q=q, p=p, pc=pc, half=half: e.matmul(pc[:], lhsT=hhT[q][:, j, :],
                                                                                        rhs=w2_s[p][:, j, half * 512:(half + 1) * 512],
                                                                                        start=(j == 0), stop=(j == 3)),
                             reads=[b_hhT[q], b_w2[p]], writes=[bc])
                    if half == 0:
                        S.op("act", lambda e, q=q, pc=pc: e.activation(out=yst[q][:, 0:512], in_=pc[:], func=AF.Copy), reads=[bc], writes=[b_yst[q]])
                    else:
                        S.op("dve", lambda e, q=q, pc=pc: e.tensor_copy(out=yst[q][:, 512:1024], in_=pc[:]), reads=[bc], writes=[b_yst[q]])
                S.dma("sp", self.ybuf[row0:row0 + 128, :], yst[q][:], reads=[b_yst[q]], writes=[self.b_ybuf])

    def gather_tile(self, ti, y0_ap, y1_ap, b_y0, b_y1):
        S = self.cx.S
        S.dma("pool", y0_ap, self.ybuf, reads=[self.b_ybuf, self.b_dest[ti]], writes=[b_y0],
              indirect={"in_offset": bass.IndirectOffsetOnAxis(ap=self.dest[:, ti, 0:1], axis=0)})
        S.dma("pool", y1_ap, self.ybuf, reads=[self.b_ybuf, self.b_dest[ti]], writes=[b_y1],
              indirect={"in_offset": bass.IndirectOffsetOnAxis(ap=self.dest[:, ti, 1:2], axis=0)})


D = 1024
NT = 66
NG = 22
RS = 128 ** -0.5
EPS = 1e-6


def host_consts_l1():
    c = {}
    c["ident_bf"] = np.eye(128, dtype=np.float32).astype(ml_dtypes.bfloat16)
    c["ident_f"] = np.eye(128, dtype=np.float32)
    s = np.arange(128)
    mf = (s[:, None] <= s[None, :]).astype(np.float32)
    mb = (s[:, None] >= s[None, :]).astype(np.float32)
    c["mask_f32"] = np.stack([mf, mb], 1)
    c["mask_bf"] = c["mask_f32"].astype(ml_dtypes.bfloat16)
    c["ones_f"] = np.ones((128, 128), np.float32)
    pos = np.arange(128, dtype=np.float32)
    c["poscol"] = np.stack([127.0 - pos, pos, -(127.0 - pos), -pos], 1).astype(np.float32)
    T = NT * 128
    inv = (10000.0 ** (-np.arange(64, dtype=np.float32) / 64)).astype(np.float32)
    ang = (np.arange(T, dtype=np.float32)[:, None] * inv[None, :]).astype(np.float32)
    cos = np.cos(ang).astype(np.float32).T
    sin = np.sin(ang).astype(np.float32).T
    c["ropecos"] = np.ascontiguousarray(np.concatenate([cos, cos], 0))
    c["ropesin"] = np.ascontiguousarray(np.concatenate([-sin, sin], 0))
    return c


def build_l1(cx, io):
    nc = cx.nc
    S = cx.S
    cx.phase_begin()

    def din(name, shape, dt=F32):
        return io[name]

    xin = din("xin", [NT * 128, D])
    cT = din("cT", [128, 8])
    cctxT = din("cctxT", [128, 8])
    wmod = din("wmod", [D, 2048])
    bmod = din("bmod", [1, 2048])
    g1 = din("g1", [1, D])
    Wfm = din("Wfm", [D, 1536])
    Wtm = din("Wtm", [D, 1032])
    gbias = din("gbias", [1, 8])
    rld = din("rld", [1, 4])
    mlg = din("mlg", [1, 256])
    retg = din("retg", [1, 256])
    ident_bf_d = din("ident_bf", [128, 128], BF16)
    ident_f_d = din("ident_f", [128, 128])
    mask_f32_d = din("mask_f32", [128, 2, 128])
    mask_bf_d = din("mask_bf", [128, 2, 128], BF16)
    ones_d = din("ones_f", [128, 128])
    poscol_d = din("poscol", [128, 4])
    ropecos_d = din("ropecos", [128, NT * 128])
    ropesin_d = din("ropesin", [128, NT * 128])
    merged = io["merged"]

    FM = nc.dram_tensor("FM", [NT, 128, 1024], BF16).ap()
    TM = nc.dram_tensor("TM", [NT, 128, 1024], BF16).ap()
    OG = nc.dram_tensor("OG", [NT, 128, 512], BF16).ap()
    HS = nc.dram_tensor("HS", [2, NT, 128, 512], F32).ap()

    sb = cx.sb
    phase_begin = cx.phase_begin
    phase_end = cx.phase_end

    ident_bf = sb("ident_bf_s", [128, 128], BF16); b_identbf = Buf()
    ident_f = sb("ident_f_s", [128, 128]); b_identf = Buf()
    mask_f32 = sb("mask_f32_s", [128, 2, 128]); b_maskf = Buf()
    mask_bf = sb("mask_bf_s", [128, 2, 128], BF16); b_maskbf = Buf()
    ones_f = sb("ones_s", [128, 128]); b_ones = Buf()
    poscol = sb("poscol_s", [128, 4]); b_poscol = Buf()
    S.dma("sp", ident_bf[:], ident_bf_d, writes=[b_identbf])
    S.dma("sp", ident_f[:], ident_f_d, writes=[b_identf])
    S.dma("sp", mask_f32[:], mask_f32_d, writes=[b_maskf])
    S.dma("sp", mask_bf[:], mask_bf_d, writes=[b_maskbf])
    S.dma("sp", ones_f[:], ones_d, writes=[b_ones])
    S.dma("sp", poscol[:], poscol_d, writes=[b_poscol])

    Gall = sb("Gall", [128, 8, NT]); b_G = Buf()
    phase_begin()
    Wfm_s = sb("Wfm_s", [128, 8, 1536], BF16); b_wfm = Buf()
    Wtm_s = sb("Wtm_s", [128, 8, 1032], BF16); b_wtm = Buf()
    for j in range(8):
        S.dma("pool", Wfm_s[:, j, :], Wfm[j * 128:(j + 1) * 128, :], writes=[b_wfm])
        S.dma("pool", Wtm_s[:, j, :], Wtm[j * 128:(j + 1) * 128, :], writes=[b_wtm])

    PB = cx.PB
    PBb = cx.PBb

    cT_s = sb("cT_s", [128, 2, 8]); b_cT = Buf()
    S.dma("sp", cT_s[:, 0, :], cT, writes=[b_cT])
    S.dma("sp", cT_s[:, 1, :], cctxT, writes=[b_cT])
    sc = sb("sc", [128, 2, 8]); b_sc = Buf()
    S.op("act", lambda e: e.activation(out=sc[:], in_=cT_s[:], func=AF.Silu), reads=[b_cT], writes=[b_sc])
    scb = sb("scb", [128, 2, 8, 128]); b_scb = Buf()
    for w in range(2):
        for j in range(8):
            S.op("dve", lambda e, w=w, j=j: e.tensor_scalar(out=scb[:, w, j, :], in0=ones_f[:], scalar1=sc[:, w, j:j + 1],
                                                           scalar2=None, op0=ALU.mult),
                 reads=[b_ones, b_sc], writes=[b_scb])
    bmod_s = sb("bmod_s", [128, 2048]); b_bmod = Buf()
    S.dma("sp", bmod_s[:], bmod.partition_broadcast(128), writes=[b_bmod])
    g1_s = sb("g1_s", [128, D]); b_g1 = Buf()
    S.dma("sp", g1_s[:], g1.partition_broadcast(128), writes=[b_g1])
    modS = sb("modS", [128, 2, D]); modA = sb("modA", [128, 2, D]); b_mod = Buf()
    wm_s = [sb("wm_s%d" % i, [128, 8, 512]) for i in range(2)]
    b_wm = [Buf(), Buf()]
    wmod_v = wmod.rearrange("(j p) n -> p j n", p=128)
    for cc in range(4):
        wb = cc % 2
        S.dma("sp", wm_s[wb][:], wmod_v[:, :, cc * 512:(cc + 1) * 512], writes=[b_wm[wb]])
        for w in range(2):
            pbi = (cc * 2 + w) % 7
            for j in range(8):
                S.op("pe", lambda e, w=w, j=j, wb=wb, pbi=pbi: e.matmul(PB[pbi][:], lhsT=scb[:, w, j, :], rhs=wm_s[wb][:, j, :],
                                                                        start=(j == 0), stop=(j == 7)),
                     reads=[b_scb, b_wm[wb]], writes=[PBb[pbi]])
            half = cc % 2
            if cc < 2:
                S.op("dve", lambda e, w=w, pbi=pbi, cc=cc, half=half: e.tensor_tensor(
                    out=modS[:, w, half * 512:(half + 1) * 512], in0=PB[pbi][:], in1=bmod_s[:, cc * 512:(cc + 1) * 512], op=ALU.add),
                    reads=[PBb[pbi], b_bmod], writes=[b_mod])
            else:
                S.op("dve", lambda e, w=w, pbi=pbi, cc=cc, half=half: e.tensor_tensor(
                    out=modA[:, w, half * 512:(half + 1) * 512], in0=PB[pbi][:], in1=bmod_s[:, cc * 512:(cc + 1) * 512], op=ALU.add),
                    reads=[PBb[pbi], b_bmod], writes=[b_mod])
                S.op("dve", lambda e, w=w, half=half: e.scalar_tensor_tensor(
                    out=modA[:, w, half * 512:(half + 1) * 512], in0=modA[:, w, half * 512:(half + 1) * 512], scalar=1.0,
                    in1=g1_s[:, half * 512:(half + 1) * 512], op0=ALU.add, op1=ALU.mult),
                    reads=[b_mod, b_g1], writes=[b_mod])

    xt = [sb("xt%d" % i, [128, D]) for i in range(2)]; b_xt = [Buf(), Buf()]
    junk = sb("junk", [128, D], BF16); b_junk = Buf()
    ss = sb("ss", [128, 2]); b_ss = [Buf(), Buf()]
    rstd = sb("rstd", [128, 2]); b_rstd = [Buf(), Buf()]
    tmp32 = sb("tmp32", [128, D]); b_tmp32 = Buf()
    u_bf = [sb("u_bf%d" % i, [128, D], BF16) for i in range(2)]; b_u = [Buf(), Buf()]
    uT = [sb("uT%d" % i, [128, 8, 384], BF16) for i in range(2)]
    b_uT = [[Buf() for _ in range(3)] for _ in range(2)]
    FMst = [sb("FMst%d" % i, [128, 3, 8, 128], BF16) for i in range(2)]
    b_FMst = [[Buf() for _ in range(8)] for _ in range(2)]
    TMst = [sb("TMst%d" % i, [128, 3, 1024], BF16) for i in range(2)]
    b_TMst = [[Buf() for _ in range(3)] for _ in range(2)]
    OGst = [sb("OGst%d" % i, [128, 3, 512], BF16) for i in range(2)]
    b_OGst = [[Buf() for _ in range(3)] for _ in range(2)]
    rc = [sb("rc%d" % i, [128, 384]) for i in range(2)]; rsn = [sb("rsn%d" % i, [128, 384]) for i in range(2)]
    b_rope = [Buf(), Buf()]
    t1 = sb("t1", [128, 384]); t2 = sb("t2", [128, 384]); b_t1 = Buf(); b_t2 = Buf()
    PT = cx.PT
    b_PT = cx.b_PT
    b_FM_d = [Buf() for _ in range(NT)]
    b_TM_d = [Buf() for _ in range(NT)]
    b_OG_d = [Buf() for _ in range(NT)]
    tmi = 0
    fmi = 0
    for g in range(NG):
        p = g % 2
        S.dma("sp", rc[p][:], ropecos_d[:, g * 384:(g + 1) * 384], writes=[b_rope[p]])
        S.dma("sp", rsn[p][:], ropesin_d[:, g * 384:(g + 1) * 384], writes=[b_rope[p]])
        for ti in range(3):
            c = g * 3 + ti
            w = 1 if c < 2 else 0
            xp = c % 2
            S.dma("sp", xt[xp][:], xin[c * 128:(c + 1) * 128, :], writes=[b_xt[xp]])
            S.op("dve", lambda e, xp=xp: e.memset(ss[:, xp:xp + 1], 0.0), writes=[b_ss[xp]])
            S.op("act", lambda e, xp=xp: e.activation(out=junk[:], in_=xt[xp][:], func=AF.Square, accum_out=ss[:, xp:xp + 1]),
                 reads=[b_xt[xp]], writes=[b_junk, b_ss[xp]])
            S.op("dve", lambda e, xp=xp: e.tensor_scalar(out=rstd[:, xp:xp + 1], in0=ss[:, xp:xp + 1], scalar1=1.0 / D, scalar2=EPS,
                                                        op0=ALU.mult, op1=ALU.add), reads=[b_ss[xp]], writes=[b_rstd[xp]])
            S.op("act", lambda e, xp=xp: e.activation(out=rstd[:, xp:xp + 1], in_=rstd[:, xp:xp + 1], func=AF.Sqrt),
                 reads=[b_rstd[xp]], writes=[b_rstd[xp]])
            S.op("dve", lambda e, xp=xp: e.reciprocal(out=rstd[:, xp:xp + 1], in_=rstd[:, xp:xp + 1]), reads=[b_rstd[xp]], writes=[b_rstd[xp]])
            S.op("dve", lambda e, xp=xp, w=w: e.scalar_tensor_tensor(out=tmp32[:], in0=xt[xp][:], scalar=rstd[:, xp:xp + 1],
                                                                    in1=modA[:, w, :], op0=ALU.mult, op1=ALU.mult),
                 reads=[b_xt[xp], b_rstd[xp], b_mod], writes=[b_tmp32])
            S.op("dve", lambda e, xp=xp, w=w: e.tensor_tensor(out=u_bf[xp][:], in0=tmp32[:], in1=modS[:, w, :], op=ALU.add),
                 reads=[b_tmp32, b_mod], writes=[b_u[xp]])
            for j in range(8):
                S.op("pe", lambda e, xp=xp, j=j: e.transpose(PT[:, j * 128:(j + 1) * 128], u_bf[xp][:, j * 128:(j + 1) * 128], ident_bf[:]),
                     reads=[b_u[xp], b_identbf], writes=[b_PT])
            S.op("act", lambda e, p=p, ti=ti: e.activation(out=uT[p][:, :, ti * 128:(ti + 1) * 128],
                                                          in_=PT[:].rearrange("p (j t) -> p j t", j=8), func=AF.Copy),
                 reads=[b_PT], writes=[b_uT[p][ti]])
            for (c0, c1) in [(0, 512), (512, 1024), (1024, 1032)]:
                pbi = tmi % 4; tmi += 1
                for j in range(8):
                    S.op("pe", lambda e, p=p, ti=ti, j=j, c0=c0, c1=c1, pbi=pbi: e.matmul(
                        PB[pbi][:, 0:c1 - c0], lhsT=uT[p][:, j, ti * 128:(ti + 1) * 128], rhs=Wtm_s[:, j, c0:c1],
                        start=(j == 0), stop=(j == 7)), reads=[b_uT[p][ti], b_wtm], writes=[PBb[pbi]])
                if c0 == 0:
                    S.op("act", lambda e, p=p, ti=ti, pbi=pbi: e.activation(out=TMst[p][:, ti, 512:1024], in_=PB[pbi][:], func=AF.Copy),
                         reads=[PBb[pbi]], writes=[b_TMst[p][ti]])
                elif c0 == 512:
                    S.op("act", lambda e, p=p, ti=ti, pbi=pbi: e.activation(out=OGst[p][:, ti, :], in_=PB[pbi][:], func=AF.Copy),
                         reads=[PBb[pbi]], writes=[b_OGst[p][ti]])
                else:
                    S.op("dve", lambda e, c=c, pbi=pbi: e.tensor_copy(out=Gall[:, :, c], in_=PB[pbi][:, 0:8]),
                         reads=[PBb[pbi]], writes=[b_G])
        def fm_mm(cb, pbi):
            for j in range(8):
                S.op("pe", lambda e, j=j: e.matmul(PB[pbi][:, 0:384], lhsT=Wfm_s[:, j, cb * 128:(cb + 1) * 128], rhs=uT[p][:, j, :],
                                                   start=(j == 0), stop=(j == 7)),
                     reads=[b_wfm] + b_uT[p], writes=[PBb[pbi]])
        for cb in range(4):
            pbi = 4 + fmi % 3; fmi += 1
            fm_mm(cb, pbi)
            sc_ = 1.0 if cb < 2 else RS
            S.op("act", lambda e, cb=cb, pbi=pbi, sc_=sc_: e.activation(
                out=FMst[p][:, :, cb, :], in_=PB[pbi][:, 0:384].rearrange("p (c t) -> p c t", c=3), func=AF.Copy, scale=sc_),
                reads=[PBb[pbi]], writes=[b_FMst[p][cb]])
        for qk in range(2):
            for h in range(2):
                cb_raw = 4 + qk * 4 + h
                cb_sw = 4 + qk * 4 + 2 + h
                pa = 4 + fmi % 3; fmi += 1
                fm_mm(cb_raw, pa)
                pb_ = 4 + fmi % 3; fmi += 1
                fm_mm(cb_sw, pb_)
                sc_ = 1.0 if qk == 0 else RS
                S.op("dve", lambda e, pa=pa, sc_=sc_: e.scalar_tensor_tensor(out=t1[:], in0=PB[pa][:, 0:384], scalar=sc_, in1=rc[p][:],
                                                                            op0=ALU.mult, op1=ALU.mult),
                     reads=[PBb[pa], b_rope[p]], writes=[b_t1])
                S.op("dve", lambda e, pb_=pb_, sc_=sc_: e.scalar_tensor_tensor(out=t2[:], in0=PB[pb_][:, 0:384], scalar=sc_, in1=rsn[p][:],
                                                                              op0=ALU.mult, op1=ALU.mult),
                     reads=[PBb[pb_], b_rope[p]], writes=[b_t2])
                a = 4 + qk * 2 + h
                S.op("dve", lambda e, a=a: e.tensor_tensor(out=FMst[p][:, :, a, :], in0=t1[:].rearrange("p (c t) -> p c t", c=3),
                                                           in1=t2[:].rearrange("p (c t) -> p c t", c=3), op=ALU.add),
                     reads=[b_t1, b_t2], writes=[b_FMst[p][a]])
        for ti in range(3):
            for di, a in enumerate([2, 3, 6, 7]):
                S.op("pe", lambda e, ti=ti, a=a, di=di: e.transpose(PT[:, di * 128:(di + 1) * 128], FMst[p][:, ti, a, :], ident_bf[:]),
                     reads=[b_FMst[p][a], b_identbf], writes=[b_PT])
            S.op("act", lambda e, ti=ti: e.activation(out=TMst[p][:, ti, 0:512], in_=PT[:, 0:512], func=AF.Copy),
                 reads=[b_PT], writes=[b_TMst[p][ti]])
        c0 = g * 3
        S.dma("sp", FM[c0:c0 + 3].rearrange("c p n -> p c n"), FMst[p][:].rearrange("p c a t -> p c (a t)"),
              reads=b_FMst[p], writes=b_FM_d[c0:c0 + 3])
        S.dma("sp", TM[c0:c0 + 3].rearrange("c p n -> p c n"), TMst[p][:], reads=b_TMst[p], writes=b_TM_d[c0:c0 + 3])
        S.dma("sp", OG[c0:c0 + 3].rearrange("c p n -> p c n"), OGst[p][:], reads=b_OGst[p], writes=b_OG_d[c0:c0 + 3])

    phase_end()
    wml = sb("wml", [128, 4, NT]); b_wml = Buf()
    flo = sb("flo", [128, 4, NT]); b_flo = Buf()
    decb = sb("decb", [128, 4, NT]); b_decb = Buf()
    wret = sb("wret", [128, 4]); rho = sb("rho", [128, 4]); dret = sb("dret", [128, 4]); b_retc = Buf()
    phase_begin()
    gb_s = sb("gb_s", [128, 8]); b_gb = Buf()
    S.dma("sp", gb_s[:], gbias.partition_broadcast(128), writes=[b_gb])
    Gi = sb("Gi", [128, 4, NT]); b_Gi = Buf()
    nlf = sb("nlf", [128, 4, NT]); b_nlf = Buf()
    for k in range(4):
        S.op("dve", lambda e, k=k: e.tensor_scalar(out=Gi[:, k, :], in0=Gall[:, k, :], scalar1=gb_s[:, k:k + 1], scalar2=None, op0=ALU.add),
             reads=[b_G, b_gb], writes=[b_Gi])
        S.op("dve", lambda e, k=k: e.tensor_scalar(out=nlf[:, k, :], in0=Gall[:, 4 + k, :], scalar1=gb_s[:, 4 + k:5 + k], scalar2=None, op0=ALU.add),
             reads=[b_G, b_gb], writes=[b_nlf])
    S.op("act", lambda e: e.activation(out=nlf[:], in_=nlf[:], func=AF.Exp, scale=-1.0), reads=[b_nlf], writes=[b_nlf])
    S.op("act", lambda e: e.activation(out=nlf[:], in_=nlf[:], func=AF.Ln, bias=1.0), reads=[b_nlf], writes=[b_nlf])
    nb = sb("nb", [128, 4, NT]); b_nb = Buf()
    nbL = sb("nbL", [128, 4, NT]); b_nbL = Buf()
    for d in range(2):
        S.op("pe", lambda e, d=d: e.matmul(PB[d][:, 0:2 * NT], lhsT=mask_f32[:, d, :], rhs=nlf[:, 2 * d:2 * d + 2, :].rearrange("p a c -> p (a c)"),
                                           start=True, stop=True), reads=[b_maskf, b_nlf], writes=[PBb[d]])
        S.op("dve", lambda e, d=d: e.tensor_copy(out=nb[:, 2 * d:2 * d + 2, :].rearrange("p a c -> p (a c)"), in_=PB[d][:, 0:2 * NT]),
             reads=[PBb[d]], writes=[b_nb])
    S.op("pe", lambda e: e.matmul(PB[2][:, 0:4 * NT], lhsT=ones_f[:], rhs=nlf[:].rearrange("p a c -> p (a c)"), start=True, stop=True),
         reads=[b_ones, b_nlf], writes=[PBb[2]])
    S.op("dve", lambda e: e.tensor_copy(out=nbL[:].rearrange("p a c -> p (a c)"), in_=PB[2][:, 0:4 * NT]), reads=[PBb[2]], writes=[b_nbL])
    av = sb("av", [128, 4, NT]); b_av = Buf()
    S.op("dve", lambda e: e.tensor_tensor(out=av[:], in0=Gi[:], in1=nb[:], op=ALU.add), reads=[b_Gi, b_nb], writes=[b_av])
    avf = av[:].rearrange("p a c -> p (a c)")
    Acol = sb("Acol", [128, 3]); b_Acol = Buf()
    S.op("dve", lambda e: e.memset(Acol[:], 0.0), writes=[b_Acol])
    pieces = [(0, 128), (128, 256), (256, 264)]
    for pi, (a0, a1) in enumerate(pieces):
        m = a1 - a0
        S.op("pe", lambda e, a0=a0, a1=a1, m=m, pi=pi: e.matmul(PB[3 + pi][0:m, 0:128], lhsT=avf[:, a0:a1], rhs=ident_f[:], start=True, stop=True),
             reads=[b_av, b_identf], writes=[PBb[3 + pi]])
        S.op("dve", lambda e, m=m, pi=pi: e.tensor_reduce(out=Acol[0:m, pi:pi + 1], in_=PB[3 + pi][0:m, 0:128], axis=AX.X, op=ALU.max),
             reads=[PBb[3 + pi]], writes=[b_Acol])
    Arow = sb("Arow", [1, 4, NT]); b_Arow = Buf()
    for pi, (a0, a1) in enumerate(pieces):
        m = a1 - a0
        S.op("pe", lambda e, a0=a0, a1=a1, m=m, pi=pi: e.matmul(PB[6][0:1, a0:a1], lhsT=Acol[0:128, pi:pi + 1], rhs=ident_f[:, 0:m],
                                                                start=True, stop=True), reads=[b_Acol, b_identf], writes=[PBb[6]])
    S.op("dve", lambda e: e.tensor_copy(out=Arow[:].rearrange("p a c -> p (a c)"), in_=PB[6][0:1, 0:4 * NT]), reads=[PBb[6]], writes=[b_Arow])
    MLrow = sb("MLrow", [1, 4, NT]); b_MLrow = Buf()
    dargrow = sb("dargrow", [1, 4, NT]); b_darg = Buf()
    mstate = sb("mstate", [1, 4]); b_ms = [Buf(), Buf()]
    b_MLd = [Buf(), Buf()]; b_dargd = [Buf(), Buf()]
    S.op("dve", lambda e: e.memset(mstate[:], 0.0), writes=b_ms)
    order = [list(range(NT)), [1, 0] + list(range(NT - 1, 1, -1))]
    engs = ["dve", "dve"]
    for i in range(NT):
        for d in range(2):
            c = order[d][i]
            en = engs[d]
            sl = slice(2 * d, 2 * d + 2)
            S.op(en, lambda e, c=c, sl=sl: e.tensor_tensor(out=MLrow[:, sl, c], in0=mstate[:, sl], in1=Arow[:, sl, c], op=ALU.max),
                 reads=[b_ms[d], b_Arow], writes=[b_MLd[d]])
            S.op(en, lambda e, c=c, sl=sl: e.tensor_tensor(out=dargrow[:, sl, c], in0=mstate[:, sl], in1=MLrow[:, sl, c], op=ALU.subtract),
                 reads=[b_ms[d], b_MLd[d]], writes=[b_dargd[d]])
            S.op(en, lambda e, c=c, sl=sl: e.tensor_tensor(out=mstate[:, sl], in0=MLrow[:, sl, c], in1=nbL[0:1, sl, c], op=ALU.subtract),
                 reads=[b_nbL, b_MLd[d]], writes=[b_ms[d]])
    MLb = sb("MLb", [128, 4, NT]); b_MLb = Buf()
    S.op("pe", lambda e: e.matmul(PB[0][:, 0:4 * NT], lhsT=ones_f[0:1, :], rhs=MLrow[:].rearrange("p a c -> p (a c)"), start=True, stop=True),
         reads=[b_ones] + b_MLd + b_dargd, writes=[PBb[0]])
    S.op("dve", lambda e: e.tensor_copy(out=MLb[:].rearrange("p a c -> p (a c)"), in_=PB[0][:, 0:4 * NT]), reads=[PBb[0]], writes=[b_MLb])
    S.op("pe", lambda e: e.matmul(PB[1][:, 0:4 * NT], lhsT=ones_f[0:1, :], rhs=dargrow[:].rearrange("p a c -> p (a c)"), start=True, stop=True),
         reads=[b_ones] + b_MLd + b_dargd, writes=[PBb[1]])
    S.op("act", lambda e: e.activation(out=decb[:].rearrange("p a c -> p (a c)"), in_=PB[1][:, 0:4 * NT], func=AF.Exp), reads=[PBb[1]], writes=[b_decb])
    S.op("dve", lambda e: e.tensor_tensor(out=wml[:], in0=av[:], in1=MLb[:], op=ALU.subtract), reads=[b_av, b_MLb], writes=[b_wml])
    S.op("act", lambda e: e.activation(out=wml[:], in_=wml[:], func=AF.Exp), reads=[b_wml], writes=[b_wml])
    S.op("dve", lambda e: e.tensor_tensor(out=flo[:], in0=nb[:], in1=MLb[:], op=ALU.subtract), reads=[b_nb, b_MLb], writes=[b_flo])
    S.op("act", lambda e: e.activation(out=flo[:], in_=flo[:], func=AF.Exp), reads=[b_flo], writes=[b_flo])
    lg = sb("lg", [128, 4]); b_lg = Buf()
    S.dma("sp", lg[:], rld.partition_broadcast(128), writes=[b_lg])
    for d in range(2):
        for h in range(2):
            x = 2 * d + h
            S.op("act", lambda e, x=x, d=d: e.activation(out=wret[:, x:x + 1], in_=poscol[:, d:d + 1], func=AF.Exp, scale=lg[:, x:x + 1]),
                 reads=[b_poscol, b_lg], writes=[b_retc])
            S.op("act", lambda e, x=x, d=d: e.activation(out=rho[:, x:x + 1], in_=poscol[:, 2 + d:3 + d], func=AF.Exp, scale=lg[:, x:x + 1]),
                 reads=[b_poscol, b_lg], writes=[b_retc])
    S.op("act", lambda e: e.activation(out=dret[:], in_=lg[:], func=AF.Exp, scale=128.0), reads=[b_lg], writes=[b_retc])

    phase_end()
    phase_begin()
    FMl = [[sb("FMl%d%d" % (d, i), [128, 8, 128], BF16) for i in range(2)] for d in range(2)]
    TMl = [[sb("TMl%d%d" % (d, i), [128, 8, 128], BF16) for i in range(2)] for d in range(2)]
    b_FMl = [[Buf() for _ in range(2)] for _ in range(2)]
    b_TMl = [[Buf() for _ in range(2)] for _ in range(2)]
    va = [sb("va%d" % i, [128, 132], BF16) for i in range(4)]; b_va = [Buf() for _ in range(4)]
    sm = [sb("sm%d" % i, [128, 128], BF16) for i in range(4)]; b_sm = [Buf() for _ in range(4)]
    CT32 = sb("CT32", [128, 8, 132]); CTd32 = sb("CTd32", [128, 8, 132]); CTbf = sb("CTbf", [128, 8, 132], BF16)
    b_CT32 = [Buf() for _ in range(8)]; b_CTd = [Buf() for _ in range(8)]; b_CTbf = [Buf() for _ in range(8)]
    S.op("dve", lambda e: e.memset(CTd32[:], 0.0), writes=b_CTd)
    S.op("dve", lambda e: e.memset(CTbf[:], 0.0), writes=b_CTbf)
    HSst = [[sb("HSst%d%d" % (d, i), [128, 512]) for i in range(2)] for d in range(2)]
    b_HSst = [[Buf() for _ in range(2)] for _ in range(2)]
    den = sb("den", [128, 4]); b_den = [Buf() for _ in range(4)]
    b_HS_d = [[Buf() for _ in range(NT)] for _ in range(2)]
    u = 0
    for i in range(NT):
        for d in range(2):
            c = order[d][i]
            cn = order[d][i + 1] if i + 1 < NT else c
            lp = i % 2
            S.dma("sp", FMl[d][lp][:].rearrange("p a t -> p (a t)"), FM[c], reads=[b_FM_d[c]], writes=[b_FMl[d][lp]])
            S.dma("sp", TMl[d][lp][:].rearrange("p a t -> p (a t)"), TM[c], reads=[b_TM_d[c]], writes=[b_TMl[d][lp]])
            hp = i % 2
            for typ in range(2):
                for h in range(2):
                    ch = typ * 4 + h * 2 + d
                    x = 2 * d + h
                    qT = FMl[d][lp][:, typ * 4 + h, :]
                    kT = FMl[d][lp][:, typ * 4 + 2 + h, :]
                    kk = TMl[d][lp][:, typ * 2 + h, :]
                    vv = TMl[d][lp][:, 4 + typ * 2 + h, :]
                    vi = u % 4
                    pS = PB[u % 2]; bS = PBb[u % 2]
                    pO = PB[2 + u % 2]; bO = PBb[2 + u % 2]
                    pC = PB[4 + u % 2]; bC = PBb[4 + u % 2]
                    u += 1
                    if typ == 0:
                        wcol = wml[:, x, c:c + 1]; wb_ = b_wml
                        dn = decb[:, x, cn:cn + 1]; db_ = b_decb
                    else:
                        wcol = wret[:, x:x + 1]; wb_ = b_retc
                        dn = dret[:, x:x + 1]; db_ = b_retc
                    S.op("dve", lambda e, vi=vi, vv=vv, wcol=wcol: e.tensor_scalar(out=va[vi][:, 0:128], in0=vv, scalar1=wcol, scalar2=None, op0=ALU.mult),
                         reads=[b_TMl[d][lp], wb_], writes=[b_va[vi]])
                    S.op("dve", lambda e, vi=vi, wcol=wcol: e.tensor_copy(out=va[vi][:, 128:129], in_=wcol), reads=[wb_], writes=[b_va[vi]])
                    S.op("pe", lambda e, pS=pS, kT=kT, qT=qT: e.matmul(pS[:, 0:128], lhsT=kT, rhs=qT, start=True, stop=True),
                         reads=[b_FMl[d][lp]], writes=[bS])
                    S.op("dve", lambda e, vi=vi, pS=pS, d=d: e.tensor_tensor(out=sm[vi][:], in0=pS[:, 0:128], in1=mask_bf[:, d, :], op=ALU.mult),
                         reads=[bS, b_maskbf], writes=[b_sm[vi]])
                    S.op("pe", lambda e, pO=pO, vi=vi: e.matmul(pO[:, 0:129], lhsT=sm[vi][:], rhs=va[vi][:, 0:129], start=True, stop=False),
                         reads=[b_sm[vi], b_va[vi]], writes=[bO])
                    S.op("pe", lambda e, pO=pO, qT=qT, ch=ch: e.matmul(pO[:, 0:129], lhsT=qT, rhs=CTbf[:, ch, 0:129], start=False, stop=True),
                         reads=[b_FMl[d][lp], b_CTbf[ch]], writes=[bO])
                    S.op("pe", lambda e, pC=pC, kk=kk, vi=vi: e.matmul(pC[:, 0:129], lhsT=kk, rhs=va[vi][:, 0:129], start=True, stop=True),
                         reads=[b_TMl[d][lp], b_va[vi]], writes=[bC])
                    S.op("dve", lambda e, pC=pC, ch=ch: e.tensor_tensor(out=CT32[:, ch, 0:129], in0=pC[:, 0:129], in1=CTd32[:, ch, 0:129], op=ALU.add),
                         reads=[bC, b_CTd[ch]], writes=[b_CT32[ch]])
                    S.op("act", lambda e, ch=ch, dn=dn: e.activation(out=CTd32[:, ch, 0:129], in_=CT32[:, ch, 0:129], func=AF.Copy, scale=dn),
                         reads=[b_CT32[ch], db_], writes=[b_CTd[ch]])
                    S.op("act", lambda e, ch=ch, dn=dn: e.activation(out=CTbf[:, ch, 0:129], in_=CT32[:, ch, 0:129], func=AF.Copy, scale=dn),
                         reads=[b_CT32[ch], db_], writes=[b_CTbf[ch]])
                    oc = (typ * 2 + h) * 128
                    if typ == 0:
                        S.op("act", lambda e, pO=pO, vi=vi: e.activation(out=den[:, vi:vi + 1], in_=pO[:, 128:129], func=AF.Abs),
                             reads=[bO], writes=[b_den[vi]])
                        S.op("dve", lambda e, vi=vi, x=x, c=c: e.tensor_scalar(out=den[:, vi:vi + 1], in0=den[:, vi:vi + 1], scalar1=flo[:, x, c:c + 1],
                                                                               scalar2=None, op0=ALU.max),
                             reads=[b_den[vi], b_flo], writes=[b_den[vi]])
                        S.op("dve", lambda e, vi=vi: e.reciprocal(out=den[:, vi:vi + 1], in_=den[:, vi:vi + 1]), reads=[b_den[vi]], writes=[b_den[vi]])
                        S.op("act", lambda e, pO=pO, vi=vi, oc=oc: e.activation(out=HSst[d][hp][:, oc:oc + 128], in_=pO[:, 0:128], func=AF.Copy,
                                                                               scale=den[:, vi:vi + 1]),
                             reads=[bO, b_den[vi]], writes=[b_HSst[d][hp]])
                    else:
                        S.op("act", lambda e, pO=pO, x=x, oc=oc: e.activation(out=HSst[d][hp][:, oc:oc + 128], in_=pO[:, 0:128], func=AF.Copy,
                                                                             scale=rho[:, x:x + 1]),
                             reads=[bO, b_retc], writes=[b_HSst[d][hp]])
            S.dma("sp", HS[d, c], HSst[d][hp][:], reads=[b_HSst[d][hp]], writes=[b_HS_d[d][c]])

    phase_end()
    phase_begin()
    gml = sb("gml", [128, 256]); gret = sb("gret", [128, 256]); b_gm = Buf()
    S.dma("sp", gml[:], mlg.partition_broadcast(128), writes=[b_gm])
    S.dma("sp", gret[:], retg.partition_broadcast(128), writes=[b_gm])
    h0 = [sb("h0_%d" % i, [128, 512]) for i in range(2)]; h1 = [sb("h1_%d" % i, [128, 512]) for i in range(2)]
    og = [sb("og%d" % i, [128, 512], BF16) for i in range(2)]
    b_h0 = [Buf(), Buf()]; b_h1 = [Buf(), Buf()]; b_og = [Buf(), Buf()]
    hz = sb("hz", [128, 512]); b_hz = Buf()
    sg = sb("sg", [128, 512]); b_sg = Buf()
    st6 = sb("st6", [128, 4, 6]); mv2 = sb("mv2", [128, 4, 2]); b_st = Buf(); b_mv = Buf()
    rs4 = sb("rs4", [128, 4]); b_rs4 = Buf()
    mo_st = [sb("mo_st%d" % i, [128, 512], BF16) for i in range(2)]; b_most = [Buf(), Buf()]
    b_out = Buf()
    for c in range(NT):
        p = c % 2
        S.dma("sp", h0[p][:], HS[0, c], reads=[b_HS_d[0][c]], writes=[b_h0[p]])
        S.dma("sp", h1[p][:], HS[1, c], reads=[b_HS_d[1][c]], writes=[b_h1[p]])
        S.dma("sp", og[p][:], OG[c], reads=[b_OG_d[c]], writes=[b_og[p]])
        S.op("dve", lambda e, p=p: e.tensor_tensor(out=hz[:], in0=h0[p][:], in1=h1[p][:], op=ALU.add), reads=[b_h0[p], b_h1[p]], writes=[b_hz])
        S.op("act", lambda e, p=p: e.activation(out=sg[:, 0:256], in_=og[p][:, 0:256], func=AF.Sigmoid), reads=[b_og[p]], writes=[b_sg])
        S.op("act", lambda e, p=p: e.activation(out=sg[:, 256:512], in_=og[p][:, 256:512], func=AF.Silu), reads=[b_og[p]], writes=[b_sg])
        S.op("dve", lambda e: e.tensor_tensor(out=hz[:, 0:256], in0=hz[:, 0:256], in1=sg[:, 0:256], op=ALU.mult), reads=[b_hz, b_sg], writes=[b_hz])
        for k in range(4):
            S.op("dve", lambda e, k=k: e.bn_stats(out=st6[:, k, :], in_=hz[:, k * 128:(k + 1) * 128]), reads=[b_hz], writes=[b_st])
            S.op("dve", lambda e, k=k: e.bn_aggr(out=mv2[:, k, :], in_=st6[:, k, :]), reads=[b_st], writes=[b_mv])
        S.op("dve", lambda e: e.tensor_scalar(out=rs4[:], in0=mv2[:, :, 1], scalar1=EPS, scalar2=None, op0=ALU.add),
             reads=[b_mv], writes=[b_rs4])
        S.op("act", lambda e: e.activation(out=rs4[:], in_=rs4[:], func=AF.Sqrt), reads=[b_rs4], writes=[b_rs4])
        S.op("dve", lambda e: e.reciprocal(out=rs4[:], in_=rs4[:]), reads=[b_rs4], writes=[b_rs4])
        for k in range(4):
            S.op("dve", lambda e, k=k: e.tensor_scalar(out=hz[:, k * 128:(k + 1) * 128], in0=hz[:, k * 128:(k + 1) * 128],
                                                      scalar1=mv2[:, k, 0:1], scalar2=rs4[:, k:k + 1], op0=ALU.subtract, op1=ALU.mult),
                 reads=[b_hz, b_mv, b_rs4], writes=[b_hz])
        S.op("dve", lambda e, p=p: e.tensor_tensor(out=mo_st[p][:, 0:256], in0=hz[:, 0:256], in1=gml[:], op=ALU.mult),
             reads=[b_hz, b_gm], writes=[b_most[p]])
        S.op("dve", lambda e: e.tensor_tensor(out=hz[:, 256:512], in0=hz[:, 256:512], in1=gret[:], op=ALU.mult), reads=[b_hz, b_gm], writes=[b_hz])
        S.op("dve", lambda e, p=p: e.tensor_tensor(out=mo_st[p][:, 256:512], in0=hz[:, 256:512], in1=sg[:, 256:512], op=ALU.mult),
             reads=[b_hz, b_sg], writes=[b_most[p]])
        S.dma("sp", merged[c * 128:(c + 1) * 128, :], mo_st[p][:], reads=[b_most[p]], writes=[b_out])
    phase_end()
    phase_end()
    return b_out


def l1_inputs(I, b, g, consts):
    d = dict(consts)
    d["xin"] = np.ascontiguousarray(np.concatenate([I["ctx"][b], I["x"][b]], 0))
    d["cT"] = np.ascontiguousarray(I["c"][b].reshape(8, 128).T)
    d["cctxT"] = np.ascontiguousarray(I["c_ctx"].reshape(8, 128).T)
    d["wmod"] = np.ascontiguousarray(I["w_mod"][0][:, 0:2048])
    d["bmod"] = np.ascontiguousarray(I["b_mod"][0][None, 0:2048])
    d["g1"] = np.ascontiguousarray(I["norm1_g"][0][None, :])
    W = I["w_in_even"][0]
    sp = np.cumsum([0, 512, 512, 512, 512, 8, 8, 512, 512, 512, 512])
    mq, mk, mv, mo, mi, mf, rq, rk, rv, rg = [W[:, sp[i]:sp[i + 1]] for i in range(10)]
    hs = [2 * g, 2 * g + 1]
    def hcols(M, h): return M[:, h * 128:(h + 1) * 128]
    def swp(M): return np.concatenate([M[:, 64:128], M[:, 0:64]], 1)
    fm = [hcols(mq, h) for h in hs] + [hcols(mk, h) for h in hs] + [hcols(rq, h) for h in hs] + [swp(hcols(rq, h)) for h in hs] \
        + [hcols(rk, h) for h in hs] + [swp(hcols(rk, h)) for h in hs]
    d["Wfm"] = np.ascontiguousarray(np.concatenate(fm, 1))
    gi = [mi[:, dd * 4 + h:dd * 4 + h + 1] for dd in range(2) for h in hs]
    gf = [mf[:, dd * 4 + h:dd * 4 + h + 1] for dd in range(2) for h in hs]
    tm = [hcols(mv, h) for h in hs] + [hcols(rv, h) for h in hs] + [hcols(mo, h) for h in hs] + [hcols(rg, h) for h in hs] + gi + gf
    d["Wtm"] = np.ascontiguousarray(np.concatenate(tm, 1))
    gb = I["ml_gate_b"][0]
    d["gbias"] = np.array([[gb[dd, 0, h] for dd in range(2) for h in hs] + [gb[dd, 1, h] for dd in range(2) for h in hs]], np.float32)
    d["rld"] = np.array([[I["ret_log_decay"][0][dd, h] for dd in range(2) for h in hs]], np.float32)
    d["mlg"] = np.ascontiguousarray(I["ml_norm_g"][0][hs].reshape(1, 256))
    d["retg"] = np.ascontiguousarray(I["ret_norm_g"][0][hs].reshape(1, 256))
    return d


NT2 = 33
GROUPS2 = [[0]] + [[1 + 4 * g + i for i in range(4)] for g in range(8)]


def host_consts_l2(half):
    c = common_consts()
    T = NT2 * 128
    inv = (10000.0 ** (-np.arange(16, dtype=np.float32) / 16)).astype(np.float32)
    t = np.arange(half * 4096, (half + 1) * 4096)
    rows = (t // 64).astype(np.float32); cols = (t % 64).astype(np.float32)
    ar = (rows[:, None] * inv[None, :]).astype(np.float32)
    ac = (cols[:, None] * inv[None, :]).astype(np.float32)
    cos64 = np.concatenate([np.cos(ar), np.cos(ar), np.cos(ac), np.cos(ac)], 1).astype(np.float32)
    sin64 = np.concatenate([-np.sin(ar), np.sin(ar), -np.sin(ac), np.sin(ac)], 1).astype(np.float32)
    cosT = np.ones((128, T), np.float32); sinT = np.zeros((128, T), np.float32)
    cosT[:, 128:] = np.concatenate([cos64, cos64], 1).T
    sinT[:, 128:] = np.concatenate([sin64, sin64], 1).T
    c["rc2"] = np.ascontiguousarray(cosT); c["rs2"] = np.ascontiguousarray(sinT)
    return c


def build_l2(cx, io, debug=None):
    nc = cx.nc
    S = cx.S
    cx.phase_begin()
    xin = io["xin"]
    MG = io["MG"]; b_MG = io["b_MG"]
    idxm_d = io["idxm"]
    cT = io["cT"]; cctxT = io["cctxT"]
    wmod = io["wmod"]; bmod = io["bmod"]
    g2_0 = io["g2_0"]; g1_1 = io["g1_1"]
    w_out = io["w_out"]
    Wr = io["Wr"]; br = io["br"]
    w1 = io["w1"]; w3 = io["w3"]; w2 = io["w2"]
    Wq = io["Wq"]; Wk = io["Wk"]; Wv = io["Wv"]
    rc2_d = io["rc2"]; rs2_d = io["rs2"]
    hout = io["H2"]
    QT = io["QTs"]
    KT = io["KTo"]
    Vo = io["Vown"]
    H1 = cx.dscr("H1", [NT2, 128, D]); b_H1 = [Buf() for _ in range(NT2)]
    Vs = cx.dscr("Vs", [NT2, 128, D], BF16); b_Vs = [Buf() for _ in range(NT2)]
    UT = cx.dscr("UT", [NT2, 128, 8, 128], BF16); b_UT = [Buf() for _ in range(NT2)]

    def lc(name, shape, dt=F32):
        t = cx.sb(name + "_2s", shape, dt); b = Buf()
        S.dma("sp", t[:], io[name], writes=[b])
        return t, b
    idxm, b_idxm = lc("idxm", [128, NT2, 2], I32)
    ident_bf = lc("ident_bf", [128, 128], BF16)
    ident_f = lc("ident_f", [128, 128])
    ones_f = lc("ones_f", [128, 128])
    slt = lc("slt", [128, 128])
    blkstart = lc("blkstart", [128, 1])
    pcol = lc("pcol", [128, 1])
    thr = lc("thr", [128, 34])
    consts = {"thr": thr, "ident_bf": ident_bf, "ident_f": ident_f, "ones_f": ones_f, "slt": slt, "blkstart": blkstart, "pcol": pcol}
    moe = MoE(cx, NT2, Wr, br, w1, w3, w2, consts, tag="A")
    moe.alloc_persistent()
    moe.precast()

    mods = cx.sb("mods", [128, 2, 6, D]); b_mods = Buf()
    cx.phase_begin()
    scb, b_scb = emit_silu_bcast(cx, [cT, cctxT], ones_f[0], ones_f[1])
    bmod_s, b_bmod = cx.load_bcast("bmod2", 6144, bmod)
    g2_s, b_g2 = cx.load_bcast("g2_0", D, g2_0)
    g1n_s, b_g1n = cx.load_bcast("g1_1", D, g1_1)
    wm_s = [cx.sb("wm_s%d" % i, [128, 8, 512]) for i in range(2)]; b_wm = [Buf(), Buf()]
    emit_mod(cx, scb, b_scb, 2, wmod, bmod_s, b_bmod, 6144,
             lambda w, cc: mods[:, w, cc // 2, (cc % 2) * 512:(cc % 2 + 1) * 512], b_mods, wm_s, b_wm)
    for w in range(2):
        for slot, gt, bg in [(2, g2_s, b_g2), (5, g1n_s, b_g1n)]:
            S.op("dve", lambda e, w=w, slot=slot, gt=gt: e.scalar_tensor_tensor(out=mods[:, w, slot, :], in0=mods[:, w, slot, :], scalar=1.0,
                                                                               in1=gt[:], op0=ALU.add, op1=ALU.mult),
                 reads=[b_mods, bg], writes=[b_mods])
    cx.phase_end()

    cx.phase_begin()
    wo_s = cx.sb("wo_s", [128, 8, D], BF16); b_wo = Buf()
    for j in range(8):
        S.dma("pool", wo_s[:, j, :], w_out[j * 128:(j + 1) * 128, :], writes=[b_wo])
    nb = NormBufs(cx)
    ht = [cx.sb("ht%d" % i, [128, D]) for i in range(2)]; b_ht = [Buf(), Buf()]
    mt = [cx.sb("mt%d" % i, [128, D], BF16) for i in range(2)]; b_mt = [Buf(), Buf()]
    mT = [cx.sb("mT%d" % i, [128, 8, 128], BF16) for i in range(2)]; b_mT = [Buf(), Buf()]
    vt = [cx.sb("vt%d" % i, [128, D], BF16) for i in range(2)]; b_vt = [Buf(), Buf()]
    vT = [cx.sb("vT%d" % i, [128, 8, 128], BF16) for i in range(2)]; b_vT = [Buf(), Buf()]
    ytmp = cx.sb("ytmp", [128, D]); b_ytmp = Buf()
    for ti in range(NT2):
        p = ti % 2
        w = 1 if ti == 0 else 0
        S.dma("sp", ht[p][:], xin[ti * 128:(ti + 1) * 128, :], writes=[b_ht[p]])
        for r in range(2):
            S.dma("pool", mt[p][:, r * 512:(r + 1) * 512], MG, reads=[b_MG, b_idxm], writes=[b_mt[p]],
                  indirect={"in_offset": bass.IndirectOffsetOnAxis(ap=idxm[:, ti, r:r + 1], axis=0)})
        cmap = [(0, 0), (0, 1), (1, 0), (1, 1), (0, 2), (0, 3), (1, 2), (1, 3)]
        emit_transpose(cx, lambda j, p=p: mt[p][:, cmap[j][0] * 512 + cmap[j][1] * 128: cmap[j][0] * 512 + (cmap[j][1] + 1) * 128],
                       b_mt[p], 8, ident_bf[0], ident_bf[1], mT[p][:], b_mT[p])
        for half in range(2):
            pc, bc = cx.PB[half], cx.PBb[half]
            for j in range(8):
                S.op("pe", lambda e, j=j, p=p, pc=pc, half=half: e.matmul(pc[:], lhsT=mT[p][:, j, :], rhs=wo_s[:, j, half * 512:(half + 1) * 512],
                                                                          start=(j == 0), stop=(j == 7)),
                     reads=[b_mT[p], b_wo], writes=[bc])
            sl = slice(half * 512, (half + 1) * 512)
            S.op("dve", lambda e, pc=pc, w=w, sl=sl: e.tensor_tensor(out=ytmp[:, sl], in0=pc[:], in1=mods[:, w, 0, sl], op=ALU.mult),
                 reads=[bc, b_mods], writes=[b_ytmp])
            S.op("dve", lambda e, p=p, sl=sl: e.tensor_tensor(out=ht[p][:, sl], in0=ht[p][:, sl], in1=ytmp[:, sl], op=ALU.add),
                 reads=[b_ytmp, b_ht[p]], writes=[b_ht[p]])
        S.dma("sp", H1[ti], ht[p][:], reads=[b_ht[p]], writes=[b_H1[ti]])
        emit_adaln(cx, nb, ht[p][:], b_ht[p], mods[:, w, 2, :], mods[:, w, 1, :], b_mods, vt[p][:], b_vt[p])
        S.dma("sp", Vs[ti], vt[p][:], reads=[b_vt[p]], writes=[b_Vs[ti]])
        emit_transpose(cx, lambda j, p=p: vt[p][:, j * 128:(j + 1) * 128], b_vt[p], 8, ident_bf[0], ident_bf[1], vT[p][:], b_vT[p])
        moe.route_tile(ti, lambda j, p=p: vT[p][:, j, :], b_vT[p])
    cx.phase_end()

    cx.phase_begin()
    moe.plan()
    vl = [cx.sb("vl%d" % i, [128, D], BF16) for i in range(2)]; b_vl = [Buf(), Buf()]
    for ti in range(NT2):
        p = ti % 2
        S.dma("sp", vl[p][:], Vs[ti], reads=[b_Vs[ti]], writes=[b_vl[p]])
        moe.dispatch_tile(ti, vl[p][:], b_vl[p], do_scatter=(debug != "plan"))
    if debug == "plan":
        dbg_dest = cx.dout("dbg_dest", [128, NT2 * 2], I32)
        dbg_idxw = cx.dout("dbg_idxw", [128, 128], I32)
        dbg_gate = cx.dout("dbg_gate", [128, NT2 * 2])
        dbg_oh = cx.dout("dbg_oh", [128, NT2 * 64])
        b_dbg = Buf()
        S.dma("sp", dbg_dest, moe.dest[:].rearrange("p t k -> p (t k)"), reads=moe.b_dest, writes=[b_dbg])
        S.dma("sp", dbg_idxw, moe.idxw[:], reads=[moe.b_idxw], writes=[b_dbg])
        S.dma("sp", dbg_gate, moe.gate[:].rearrange("p t k -> p (t k)"), reads=moe.b_gate, writes=[b_dbg])
        S.dma("sp", dbg_oh, moe.OH[:].rearrange("p t k e -> p (t k e)"), reads=moe.b_OH, writes=[b_dbg])
        cx.phase_end()
        return None
    moe.experts()
    cx.phase_end()

    cx.phase_begin()
    nb = NormBufs(cx, "d")
    h1t = [cx.sb("h1t%d" % i, [128, D]) for i in range(2)]; b_h1t = [Buf(), Buf()]
    y0 = [cx.sb("y0_%d" % i, [128, D]) for i in range(2)]; b_y0 = [Buf(), Buf()]
    y1 = [cx.sb("y1_%d" % i, [128, D]) for i in range(2)]; b_y1 = [Buf(), Buf()]
    ut = [cx.sb("ut%d" % i, [128, D], BF16) for i in range(2)]; b_ut = [Buf(), Buf()]
    uTt = [cx.sb("uTt%d" % i, [128, 8, 128], BF16) for i in range(2)]; b_uTt = [Buf(), Buf()]
    b_hout = Buf()
    for ti in range(NT2):
        p = ti % 2
        w = 1 if ti == 0 else 0
        S.dma("sp", h1t[p][:], H1[ti], reads=[b_H1[ti]], writes=[b_h1t[p]])
        moe.gather_tile(ti, y0[p][:], y1[p][:], b_y0[p], b_y1[p])
        S.op("dve", lambda e, p=p, ti=ti: e.tensor_scalar(out=y0[p][:], in0=y0[p][:], scalar1=moe.gate[:, ti, 0:1], scalar2=None, op0=ALU.mult),
             reads=[b_y0[p], moe.b_gate[ti]], writes=[b_y0[p]])
        S.op("dve", lambda e, p=p, ti=ti: e.scalar_tensor_tensor(out=y0[p][:], in0=y1[p][:], scalar=moe.gate[:, ti, 1:2], in1=y0[p][:],
                                                                op0=ALU.mult, op1=ALU.add),
             reads=[b_y0[p], b_y1[p], moe.b_gate[ti]], writes=[b_y0[p]])
        S.op("dve", lambda e, p=p, w=w: e.tensor_tensor(out=y0[p][:], in0=y0[p][:], in1=mods[:, w, 3, :], op=ALU.mult),
             reads=[b_y0[p], b_mods], writes=[b_y0[p]])
        S.op("dve", lambda e, p=p: e.tensor_tensor(out=h1t[p][:], in0=h1t[p][:], in1=y0[p][:], op=ALU.add),
             reads=[b_y0[p], b_h1t[p]], writes=[b_h1t[p]])
        S.dma("sp", hout[ti * 128:(ti + 1) * 128, :], h1t[p][:], reads=[b_h1t[p]], writes=[b_hout])
        emit_adaln(cx, nb, h1t[p][:], b_h1t[p], mods[:, w, 5, :], mods[:, w, 4, :], b_mods, ut[p][:], b_ut[p])
        emit_transpose(cx, lambda j, p=p: ut[p][:, j * 128:(j + 1) * 128], b_ut[p], 8, ident_bf[0], ident_bf[1], uTt[p][:], b_uTt[p])
        S.dma("sp", UT[ti], uTt[p][:], reads=[b_uTt[p]], writes=[b_UT[ti]])
    cx.phase_end()

    cx.phase_begin()
    Wq_s = cx.sb("Wq_s", [128, 8, 2048], BF16); Wk_s = cx.sb("Wk_s", [128, 8, 2048], BF16); Wv_s = cx.sb("Wv_s", [128, 8, D], BF16)
    b_Wq = Buf(); b_Wk = Buf(); b_Wv = Buf()
    for j in range(8):
        S.dma("pool", Wq_s[:, j, :], Wq[j * 128:(j + 1) * 128, :], writes=[b_Wq])
        S.dma("pool", Wk_s[:, j, :], Wk[j * 128:(j + 1) * 128, :], writes=[b_Wk])
        S.dma("pool", Wv_s[:, j, :], Wv[j * 128:(j + 1) * 128, :], writes=[b_Wv])
    uTg = [cx.sb("uTg%d" % i, [128, 4, 8, 128], BF16) for i in range(2)]; b_uTg = [Buf(), Buf()]
    rc = [cx.sb("rc%d" % i, [128, 512]) for i in range(2)]; rsn = [cx.sb("rsn%d" % i, [128, 512]) for i in range(2)]; b_rope = [Buf(), Buf()]
    t1 = cx.sb("t1", [128, 512]); t2 = cx.sb("t2", [128, 512]); b_t1 = Buf(); b_t2 = Buf()
    qkst = [cx.sb("qkst%d" % i, [128, 512], BF16) for i in range(4)]; b_qkst = [Buf() for _ in range(4)]
    vst = [cx.sb("vst%d" % i, [128, D], BF16) for i in range(2)]; b_vst = [Buf(), Buf()]
    b_QT = Buf(); b_KT = Buf(); b_Vo = Buf()
    si = 0; vi = 0; fi = 0
    for gi, tiles in enumerate(GROUPS2):
        p = gi % 2
        n = len(tiles) * 128
        t0 = tiles[0]
        S.dma("sp", uTg[p][:, 0:len(tiles)], UT[t0:t0 + len(tiles)].rearrange("c p j t -> p c j t"),
              reads=b_UT[t0:t0 + len(tiles)], writes=[b_uTg[p]])
        S.dma("sp", rc[p][:, 0:n], rc2_d[:, t0 * 128:t0 * 128 + n], writes=[b_rope[p]])
        S.dma("sp", rsn[p][:, 0:n], rs2_d[:, t0 * 128:t0 * 128 + n], writes=[b_rope[p]])
        for ci, ti in enumerate(tiles):
            vq = vi % 2; vi += 1
            for half in range(2):
                pc, bc = cx.PB[half], cx.PBb[half]
                for j in range(8):
                    S.op("pe", lambda e, j=j, ci=ci, pc=pc, half=half: e.matmul(pc[:], lhsT=uTg[p][:, ci, j, :], rhs=Wv_s[:, j, half * 512:(half + 1) * 512],
                                                                                start=(j == 0), stop=(j == 7)),
                         reads=[b_uTg[p], b_Wv], writes=[bc])
                S.op("act", lambda e, vq=vq, pc=pc, half=half: e.activation(out=vst[vq][:, half * 512:(half + 1) * 512], in_=pc[:], func=AF.Copy),
                     reads=[bc], writes=[b_vst[vq]])
            S.dma("sp", Vo[ti * 128:(ti + 1) * 128, :], vst[vq][:], reads=[b_vst[vq]], writes=[b_Vo])
        for qk in range(2):
            if qk == 0 and gi == 0:
                continue
            Ws, bW = (Wq_s, b_Wq) if qk == 0 else (Wk_s, b_Wk)
            sc_ = 0.125 if qk == 0 else 1.0
            for h in range(8):
                pa = 2 + fi % 4; fi += 1
                pb_ = 2 + fi % 4; fi += 1
                for (pp, cb) in [(pa, h), (pb_, 8 + h)]:
                    for j in range(8):
                        S.op("pe", lambda e, j=j, pp=pp, cb=cb: e.matmul(cx.PB[pp][:, 0:n], lhsT=Ws[:, j, cb * 128:(cb + 1) * 128],
                                                                         rhs=uTg[p][:, 0:len(tiles), j, :],
                                                                         start=(j == 0), stop=(j == 7)),
                             reads=[bW, b_uTg[p]], writes=[cx.PBb[pp]])
                S.op("dve", lambda e, pa=pa: e.scalar_tensor_tensor(out=t1[:, 0:n], in0=cx.PB[pa][:, 0:n], scalar=sc_, in1=rc[p][:, 0:n],
                                                                    op0=ALU.mult, op1=ALU.mult), reads=[cx.PBb[pa], b_rope[p]], writes=[b_t1])
                S.op("dve", lambda e, pb_=pb_: e.scalar_tensor_tensor(out=t2[:, 0:n], in0=cx.PB[pb_][:, 0:n], scalar=sc_, in1=rsn[p][:, 0:n],
                                                                      op0=ALU.mult, op1=ALU.mult), reads=[cx.PBb[pb_], b_rope[p]], writes=[b_t2])
                sq = si % 4; si += 1
                S.op("dve", lambda e, sq=sq: e.tensor_tensor(out=qkst[sq][:, 0:n], in0=t1[:, 0:n], in1=t2[:, 0:n], op=ALU.add),
                     reads=[b_t1, b_t2], writes=[b_qkst[sq]])
                if qk == 0:
                    q0 = (t0 - 1) * 128
                    S.dma("sp", QT[h, :, q0:q0 + n], qkst[sq][:, 0:n], reads=[b_qkst[sq]], writes=[b_QT])
                else:
                    S.dma("sp", KT[h, :, t0 * 128:t0 * 128 + n], qkst[sq][:, 0:n], reads=[b_qkst[sq]], writes=[b_KT])
    cx.phase_end()
    cx.phase_end()
    return b_hout, b_QT, b_KT, b_Vo


def l2_inputs(I, b, half, merged_full_b, consts):
    d = dict(consts)
    cs = slice(half * 128, (half + 1) * 128)
    ls = slice(half * 4096, (half + 1) * 4096)
    d["xin"] = np.ascontiguousarray(np.concatenate([I["ctx"][b][cs], I["x"][b][ls]], 0))
    if merged_full_b is not None:
        d["mergedIn"] = np.ascontiguousarray(np.concatenate([merged_full_b[0:256][cs], merged_full_b[256:][ls]], 0))
    d["cT"] = np.ascontiguousarray(I["c"][b].reshape(8, 128).T)
    d["cctxT"] = np.ascontiguousarray(I["c_ctx"].reshape(8, 128).T)
    d["wmod"] = np.ascontiguousarray(np.concatenate([I["w_mod"][0][:, 2048:6144], I["w_mod"][1][:, 0:2048]], 1))
    d["bmod"] = np.ascontiguousarray(np.concatenate([I["b_mod"][0][2048:6144], I["b_mod"][1][0:2048]])[None, :])
    d["g2_0"] = np.ascontiguousarray(I["norm2_g"][0][None, :])
    d["g1_1"] = np.ascontiguousarray(I["norm1_g"][1][None, :])
    d["w_out"] = I["w_out_even"][0]
    d["Wr"] = np.ascontiguousarray(np.concatenate([I["router_g_w"][0], I["router_e_w"][0]], 1))
    d["br"] = np.ascontiguousarray(np.concatenate([I["router_g_b"][0], I["router_e_b"][0]])[None, :])
    d["w1"] = I["w1"][0]; d["w3"] = I["w3"][0]; d["w2"] = I["w2"][0]
    W = I["w_in_odd"][0]
    perm = np.arange(64)
    perm = np.concatenate([perm[16:32], perm[0:16], perm[48:64], perm[32:48]])
    def swp(M):
        return M.reshape(D, 16, 64)[:, :, perm].reshape(D, 1024)
    d["Wq"] = np.ascontiguousarray(np.concatenate([W[:, 0:1024], swp(W[:, 0:1024])], 1))
    d["Wk"] = np.ascontiguousarray(np.concatenate([W[:, 1024:2048], swp(W[:, 1024:2048])], 1))
    d["Wv"] = np.ascontiguousarray(W[:, 2048:3072])
    return d


NT3 = 32
NK = 66
LAM_INIT = 0.8 - 0.6 * math.exp(-0.3 * 1)


def host_consts_l3():
    c = common_consts()
    c["ones_bf"] = np.ones((128, 128), np.float32).astype(ml_dtypes.bfloat16)
    return c


VCH = [(0, 1024), (1024, 1024), (2048, 1024), (3072, 1024), (4096, 128)]


def build_l3(cx, io, debug=None):
    nc = cx.nc
    S = cx.S
    cx.phase_begin()
    QT = io["QTs"]; b_QTs = io["b_QTs"]
    KTg = io["KTg"]; b_KTg = io["b_KTg"]
    VG = io["VG"]; b_VG = io["b_VG"]
    H2 = io["H2"]; b_H2 = io["b_H2"]
    cT = io["cT"]
    wmod = io["wmod"]; bmod = io["bmod"]
    g2 = io["g2"]; gfin = io["gfin"]
    dalam = io["dalam"]; dagT = io["dagT"]
    w_out = io["w_out"]
    Wr = io["Wr"]; br = io["br"]
    w1 = io["w1"]; w3 = io["w3"]; w2 = io["w2"]
    out = io["out"]
    H3 = cx.dscr("H3", [NT3, 128, D]); b_H3 = [Buf() for _ in range(NT3)]
    Vs = cx.dscr("Vs3", [NT3, 128, D], BF16); b_Vs = [Buf() for _ in range(NT3)]

    def lc(name, shape, dt=F32):
        t = cx.sb(name + "_3s", shape, dt); b = Buf()
        S.dma("sp", t[:], io[name], writes=[b])
        return t, b
    ident_bf = lc("ident_bf", [128, 128], BF16)
    ident_f = lc("ident_f", [128, 128])
    ones_f = lc("ones_f", [128, 128])
    ones_bf = lc("ones_bf", [128, 128], BF16)
    slt = lc("slt", [128, 128])
    blkstart = lc("blkstart", [128, 1])
    pcol = lc("pcol", [128, 1])
    thr = lc("thr", [128, 34])
    consts = {"thr": thr, "ident_bf": ident_bf, "ident_f": ident_f, "ones_f": ones_f, "slt": slt, "blkstart": blkstart, "pcol": pcol}
    moe = MoE(cx, NT3, Wr, br, w1, w3, w2, consts, tag="B")
    moe.alloc_persistent()
    moe.precast()

    mods = cx.sb("mods", [128, 4, D]); b_mods = Buf()
    gfin_s, b_gfin = cx.load_bcast("gfin3", D, gfin)
    cx.phase_begin()
    scb, b_scb = emit_silu_bcast(cx, [cT], ones_f[0], ones_f[1])
    bmod_s, b_bmod = cx.load_bcast("bmod3", 4096, bmod)
    g2_s, b_g2 = cx.load_bcast("g2_3", D, g2)
    wm_s = [cx.sb("wm_s%d" % i, [128, 8, 512]) for i in range(2)]; b_wm = [Buf(), Buf()]
    emit_mod(cx, scb, b_scb, 1, wmod, bmod_s, b_bmod, 4096,
             lambda w, cc: mods[:, cc // 2, (cc % 2) * 512:(cc % 2 + 1) * 512], b_mods, wm_s, b_wm)
    S.op("dve", lambda e: e.scalar_tensor_tensor(out=mods[:, 2, :], in0=mods[:, 2, :], scalar=1.0, in1=g2_s[:], op0=ALU.add, op1=ALU.mult),
         reads=[b_mods, b_g2], writes=[b_mods])
    cx.phase_end()

    cx.phase_begin()
    oT_all = cx.sb("oT_all", [128, 8, NT3 * 128], BF16); b_oT = [Buf() for _ in range(8)]
    lamw = cx.sb("lamw", [128, 256]); b_lamw = Buf()
    S.dma("sp", lamw[:], dalam.partition_broadcast(128), writes=[b_lamw])
    lamt = cx.sb("lamt", [128, 8]); b_lamt = Buf()
    prod = cx.sb("lprod", [128, 128]); b_prod = Buf()
    S.op("dve", lambda e: e.tensor_tensor(out=prod[:, 0:64], in0=lamw[:, 0:64], in1=lamw[:, 64:128], op=ALU.mult), reads=[b_lamw], writes=[b_prod])
    S.op("dve", lambda e: e.tensor_tensor(out=prod[:, 64:128], in0=lamw[:, 128:192], in1=lamw[:, 192:256], op=ALU.mult), reads=[b_lamw, b_prod], writes=[b_prod])
    S.op("dve", lambda e: e.tensor_reduce(out=lamt[:, 0:1], in_=prod[:, 0:64], axis=AX.X, op=ALU.add), reads=[b_prod], writes=[b_lamt])
    S.op("dve", lambda e: e.tensor_reduce(out=lamt[:, 1:2], in_=prod[:, 64:128], axis=AX.X, op=ALU.add), reads=[b_prod, b_lamt], writes=[b_lamt])
    S.op("act", lambda e: e.activation(out=lamt[:, 2:4], in_=lamt[:, 0:2], func=AF.Exp), reads=[b_lamt], writes=[b_lamt])
    S.op("dve", lambda e: e.tensor_tensor(out=lamt[:, 4:5], in0=lamt[:, 3:4], in1=lamt[:, 2:3], op=ALU.subtract), reads=[b_lamt], writes=[b_lamt])
    S.op("dve", lambda e: e.tensor_scalar(out=lamt[:, 4:5], in0=lamt[:, 4:5], scalar1=-LAM_INIT, scalar2=None, op0=ALU.add), reads=[b_lamt], writes=[b_lamt])
    gs = cx.sb("gs", [128, 8]); b_gs = Buf()
    S.dma("sp", gs[:], dagT, writes=[b_gs])
    S.op("dve", lambda e: e.tensor_scalar(out=gs[:], in0=gs[:], scalar1=1.0 - LAM_INIT, scalar2=None, op0=ALU.mult), reads=[b_gs], writes=[b_gs])

    cx.phase_begin()
    KTh = [cx.sb("KTh%d" % i, [128, NK * 128], BF16) for i in range(2)]; b_KTh = [Buf(), Buf()]
    Vh = [cx.sb("Vh%d" % i, [128, NK, 128], BF16) for i in range(2)]; b_Vh = [Buf(), Buf()]
    QTh1 = cx.sb("QTh", [128, 2, NT3 * 128], BF16); b_QTh1 = Buf()
    QTh = [QTh1, QTh1]; b_QTh = [b_QTh1, b_QTh1]
    S.op("pool", lambda e: e.memset(QTh1[:], 0.0), writes=[b_QTh1])
    Pt = [cx.sb("Pt%d" % i, [128, 512], BF16) for i in range(4)]; b_Pt = [Buf() for _ in range(4)]
    rec = cx.sb("rec", [128, 512]); b_rec = Buf()
    on1 = cx.sb("on1", [128, 512]); b_on1 = Buf()
    ot = cx.sb("ot", [128, 512]); b_ot = Buf()
    sq = cx.sb("sqt", [128, 512]); b_sq = Buf()
    rstd = cx.sb("rstdA", [128, 512]); b_rstdA = Buf()
    pi_ = 0
    si_ = 0
    nheads = 8 if debug != "attn1" else 1
    nqt = 8 if debug != "attn1" else 1
    for h in range(nheads):
        p = h % 2
        for r in range(2):
            S.dma("sp", KTh[p][:, r * 4224:(r + 1) * 4224], KTg[h, r * 128:(r + 1) * 128, :], reads=[b_KTg], writes=[b_KTh[p]])
            for (r0, n) in VCH:
                vr = 2 * r0 + r * n
                S.dma("sp", Vh[p][:, r * 33 + r0 // 128: r * 33 + (r0 + n) // 128, :],
                      VG[vr:vr + n, h * 128:(h + 1) * 128].rearrange("(k p) d -> p k d", p=128), reads=[b_VG], writes=[b_Vh[p]])
        for m_ in range(2):
            S.dma("sp", QTh[p][m_ * 64:(m_ + 1) * 64, m_, :], QT[h, m_ * 64:(m_ + 1) * 64, :], reads=[b_QTs], writes=[b_QTh[p]])
        steps = [(qt, m, kt) for qt in range(nqt) for m in range(2) for kt in range(NK)]
        LOOK = 2
        sbank = {}

        def issue_S(idx):
            nonlocal si_
            qt_, m_, kt_ = steps[idx]
            pst = si_ % 3; si_ += 1
            sbank[idx] = pst
            ms_ = slice(m_ * 64, (m_ + 1) * 64)
            qs_ = slice(qt_ * 512, (qt_ + 1) * 512)
            S.op("pe", lambda e: e.matmul(cx.PB[pst][:], lhsT=KTh[p][:, kt_ * 128:(kt_ + 1) * 128], rhs=QTh[p][:, m_, qs_], start=True, stop=True),
                 reads=[b_KTh[p], b_QTh[p]], writes=[cx.PBb[pst]])

        for i0 in range(min(LOOK, len(steps))):
            issue_S(i0)
        for idx, (qt, m, kt) in enumerate(steps):
            qs = slice(qt * 512, (qt + 1) * 512)
            pO, bO = cx.PB[3], cx.PBb[3]
            pS, bS = cx.PB[4], cx.PBb[4]
            pst = sbank.pop(idx)
            pq = pi_ % 4; pi_ += 1
            S.op("act", lambda e, pq=pq, pst=pst: e.activation(out=Pt[pq][:], in_=cx.PB[pst][:], func=AF.Exp),
                 reads=[cx.PBb[pst]], writes=[b_Pt[pq]])
            if idx + LOOK < len(steps):
                issue_S(idx + LOOK)
            S.op("pe", lambda e, pq=pq, kt=kt: e.matmul(pO[:], lhsT=Vh[p][:, kt, :], rhs=Pt[pq][:], start=(kt == 0), stop=(kt == NK - 1)),
                 reads=[b_Vh[p], b_Pt[pq]], writes=[bO])
            S.op("pe", lambda e, pq=pq, kt=kt: e.matmul(pS[:], lhsT=ones_bf[0][:], rhs=Pt[pq][:], start=(kt == 0), stop=(kt == NK - 1)),
                 reads=[ones_bf[1], b_Pt[pq]], writes=[bS])
            if kt != NK - 1:
                continue
            S.op("dve", lambda e: e.reciprocal(out=rec[:], in_=pS[:]), reads=[bS], writes=[b_rec])
            if m == 0:
                S.op("dve", lambda e: e.tensor_tensor(out=on1[:], in0=pO[:], in1=rec[:], op=ALU.mult), reads=[bO, b_rec], writes=[b_on1])
                continue
            S.op("dve", lambda e: e.tensor_tensor(out=ot[:], in0=pO[:], in1=rec[:], op=ALU.mult), reads=[bO, b_rec], writes=[b_ot])
            S.op("dve", lambda e: e.scalar_tensor_tensor(out=ot[:], in0=ot[:], scalar=lamt[:, 4:5], in1=on1[:], op0=ALU.mult, op1=ALU.add),
                 reads=[b_ot, b_on1, b_lamt], writes=[b_ot])
            S.op("act", lambda e: e.activation(out=sq[:], in_=ot[:], func=AF.Square), reads=[b_ot], writes=[b_sq])
            pN, bN = cx.PB[5], cx.PBb[5]
            S.op("pe", lambda e: e.matmul(pN[:], lhsT=ones_f[0][:], rhs=sq[:], start=True, stop=True), reads=[ones_f[1], b_sq], writes=[bN])
            S.op("dve", lambda e: e.tensor_scalar(out=rstd[:], in0=pN[:], scalar1=1.0 / 128, scalar2=EPS, op0=ALU.mult, op1=ALU.add),
                 reads=[bN], writes=[b_rstdA])
            S.op("act", lambda e: e.activation(out=rstd[:], in_=rstd[:], func=AF.Sqrt), reads=[b_rstdA], writes=[b_rstdA])
            S.op("dve", lambda e: e.reciprocal(out=rstd[:], in_=rstd[:]), reads=[b_rstdA], writes=[b_rstdA])
            S.op("dve", lambda e, h=h, qs=qs: e.scalar_tensor_tensor(out=oT_all[:, h, qs], in0=ot[:], scalar=gs[:, h:h + 1], in1=rstd[:],
                                                                    op0=ALU.mult, op1=ALU.mult),
                 reads=[b_ot, b_gs, b_rstdA], writes=[b_oT[h]])

    if debug == "attn1":
        dbg = cx.dout("dbg_oT", [128, 512], BF16); b_dbg = Buf()
        S.dma("sp", dbg, oT_all[:, 0, 0:512], reads=b_oT, writes=[b_dbg])
        cx.phase_end()
        S.finish([b_dbg], "sp")
        return nc

    cx.phase_end()
    cx.phase_begin()
    wo_s = cx.sb("wo_s", [128, 8, D], BF16); b_wo = Buf()
    for j in range(8):
        S.dma("pool", wo_s[:, j, :], w_out[j * 128:(j + 1) * 128, :], writes=[b_wo])
    nb = NormBufs(cx)
    ht = [cx.sb("ht%d" % i, [128, D]) for i in range(2)]; b_ht = [Buf(), Buf()]
    vt = [cx.sb("vt%d" % i, [128, D], BF16) for i in range(2)]; b_vt = [Buf(), Buf()]
    vT = [cx.sb("vT%d" % i, [128, 8, 128], BF16) for i in range(2)]; b_vT = [Buf(), Buf()]
    ytmp = cx.sb("ytmp", [128, D]); b_ytmp = Buf()
    for ti in range(NT3):
        p = ti % 2
        S.dma("sp", ht[p][:], H2[(ti + 1) * 128:(ti + 2) * 128, :], reads=[b_H2], writes=[b_ht[p]])
        for half in range(2):
            pc, bc = cx.PB[half], cx.PBb[half]
            for j in range(8):
                S.op("pe", lambda e, j=j, pc=pc, half=half, ti=ti: e.matmul(pc[:], lhsT=oT_all[:, j, ti * 128:(ti + 1) * 128],
                                                                            rhs=wo_s[:, j, half * 512:(half + 1) * 512], start=(j == 0), stop=(j == 7)),
                     reads=b_oT + [b_wo], writes=[bc])
            sl = slice(half * 512, (half + 1) * 512)
            S.op("dve", lambda e, pc=pc, sl=sl: e.tensor_tensor(out=ytmp[:, sl], in0=pc[:], in1=mods[:, 0, sl], op=ALU.mult),
                 reads=[bc, b_mods], writes=[b_ytmp])
            S.op("dve", lambda e, p=p, sl=sl: e.tensor_tensor(out=ht[p][:, sl], in0=ht[p][:, sl], in1=ytmp[:, sl], op=ALU.add),
                 reads=[b_ytmp, b_ht[p]], writes=[b_ht[p]])
        S.dma("sp", H3[ti], ht[p][:], reads=[b_ht[p]], writes=[b_H3[ti]])
        emit_adaln(cx, nb, ht[p][:], b_ht[p], mods[:, 2, :], mods[:, 1, :], b_mods, vt[p][:], b_vt[p])
        S.dma("sp", Vs[ti], vt[p][:], reads=[b_vt[p]], writes=[b_Vs[ti]])
        emit_transpose(cx, lambda j, p=p: vt[p][:, j * 128:(j + 1) * 128], b_vt[p], 8, ident_bf[0], ident_bf[1], vT[p][:], b_vT[p])
        moe.route_tile(ti, lambda j, p=p: vT[p][:, j, :], b_vT[p])
    cx.phase_end()
    cx.phase_end()

    cx.phase_begin()
    moe.plan()
    vl = [cx.sb("vl%d" % i, [128, D], BF16) for i in range(2)]; b_vl = [Buf(), Buf()]
    for ti in range(NT3):
        p = ti % 2
        S.dma("sp", vl[p][:], Vs[ti], reads=[b_Vs[ti]], writes=[b_vl[p]])
        moe.dispatch_tile(ti, vl[p][:], b_vl[p])
    moe.experts()
    cx.phase_end()

    cx.phase_begin()
    nb = NormBufs(cx, "d")
    h1t = [cx.sb("h1t%d" % i, [128, D]) for i in range(2)]; b_h1t = [Buf(), Buf()]
    y0 = [cx.sb("y0_%d" % i, [128, D]) for i in range(2)]; b_y0 = [Buf(), Buf()]
    y1 = [cx.sb("y1_%d" % i, [128, D]) for i in range(2)]; b_y1 = [Buf(), Buf()]
    ot_ = [cx.sb("ofin%d" % i, [128, D]) for i in range(2)]; b_ofin = [Buf(), Buf()]
    b_out = Buf()
    for ti in range(NT3):
        p = ti % 2
        S.dma("sp", h1t[p][:], H3[ti], reads=[b_H3[ti]], writes=[b_h1t[p]])
        moe.gather_tile(ti, y0[p][:], y1[p][:], b_y0[p], b_y1[p])
        S.op("dve", lambda e, p=p, ti=ti: e.tensor_scalar(out=y0[p][:], in0=y0[p][:], scalar1=moe.gate[:, ti, 0:1], scalar2=None, op0=ALU.mult),
             reads=[b_y0[p], moe.b_gate[ti]], writes=[b_y0[p]])
        S.op("dve", lambda e, p=p, ti=ti: e.scalar_tensor_tensor(out=y0[p][:], in0=y1[p][:], scalar=moe.gate[:, ti, 1:2], in1=y0[p][:],
                                                                op0=ALU.mult, op1=ALU.add),
             reads=[b_y0[p], b_y1[p], moe.b_gate[ti]], writes=[b_y0[p]])
        S.op("dve", lambda e, p=p: e.tensor_tensor(out=y0[p][:], in0=y0[p][:], in1=mods[:, 3, :], op=ALU.mult),
             reads=[b_y0[p], b_mods], writes=[b_y0[p]])
        S.op("dve", lambda e, p=p: e.tensor_tensor(out=h1t[p][:], in0=h1t[p][:], in1=y0[p][:], op=ALU.add),
             reads=[b_y0[p], b_h1t[p]], writes=[b_h1t[p]])
        emit_adaln(cx, nb, h1t[p][:], b_h1t[p], gfin_s[:], None, b_gfin, ot_[p][:], b_ofin[p])
        S.dma("sp", out[ti * 128:(ti + 1) * 128, :], ot_[p][:], reads=[b_ofin[p]], writes=[b_out])
    cx.phase_end()
    cx.phase_end()
    return b_out


def l3_inputs(I, b, half, l2res_pair, consts):
    d = dict(consts)
    if l2res_pair is not None:
        d["QT"] = l2res_pair[half]["QT"]
        d["KT"] = np.ascontiguousarray(np.concatenate([l2res_pair[0]["KT"], l2res_pair[1]["KT"]], 2))
        d["Vf"] = np.ascontiguousarray(np.concatenate([l2res_pair[0]["Vo"], l2res_pair[1]["Vo"]], 0))
        d["hin"] = np.ascontiguousarray(l2res_pair[half]["hout"][128:])
    d["cT"] = np.ascontiguousarray(I["c"][b].reshape(8, 128).T)
    d["wmod"] = np.ascontiguousarray(I["w_mod"][1][:, 2048:6144])
    d["bmod"] = np.ascontiguousarray(I["b_mod"][1][None, 2048:6144])
    d["g2"] = np.ascontiguousarray(I["norm2_g"][1][None, :])
    d["gfin"] = np.ascontiguousarray(I["final_norm_g"][None, :])
    d["dalam"] = np.ascontiguousarray(I["da_lambda"][0].reshape(1, 256))
    d["dagT"] = np.ascontiguousarray(I["da_norm_g"][0].T)
    d["w_out"] = I["w_out_odd"][0]
    d["Wr"] = np.ascontiguousarray(np.concatenate([I["router_g_w"][1], I["router_e_w"][1]], 1))
    d["br"] = np.ascontiguousarray(np.concatenate([I["router_g_b"][1], I["router_e_b"][1]])[None, :])
    d["w1"] = I["w1"][1]; d["w3"] = I["w3"][1]; d["w2"] = I["w2"][1]
    return d


PAIRS = [[0, 1], [2, 3], [4, 5], [6, 7]]
MCH = [(0, 2048), (2048, 2048), (4096, 2048), (6144, 2048), (8192, 256)]

L1_SPECS = [("xin", [NT * 128, D], F32), ("cT", [128, 8], F32), ("cctxT", [128, 8], F32), ("wmod", [D, 2048], F32), ("bmod", [1, 2048], F32),
            ("g1", [1, D], F32), ("Wfm", [D, 1536], F32), ("Wtm", [D, 1032], F32), ("gbias", [1, 8], F32), ("rld", [1, 4], F32),
            ("mlg", [1, 256], F32), ("retg", [1, 256], F32), ("ident_bf", [128, 128], BF16), ("ident_f", [128, 128], F32),
            ("mask_f32", [128, 2, 128], F32), ("mask_bf", [128, 2, 128], BF16), ("ones_f", [128, 128], F32), ("poscol", [128, 4], F32),
            ("ropecos", [128, NT * 128], F32), ("ropesin", [128, NT * 128], F32)]
L2_SPECS = [("xin", [NT2 * 128, D], F32), ("idxm", [128, NT2, 2], I32), ("cT", [128, 8], F32), ("cctxT", [128, 8], F32),
            ("wmod", [D, 6144], F32), ("bmod", [1, 6144], F32), ("g2_0", [1, D], F32), ("g1_1", [1, D], F32), ("w_out", [D, D], F32),
            ("Wr", [D, 36], F32), ("br", [1, 36], F32), ("w1", [32, D, 512], F32), ("w3", [32, D, 512], F32), ("w2", [32, 512, D], F32),
            ("Wq", [D, 2048], F32), ("Wk", [D, 2048], F32), ("Wv", [D, D], F32), ("rc2", [128, NT2 * 128], F32), ("rs2", [128, NT2 * 128], F32),
            ("ident_bf", [128, 128], BF16), ("ident_f", [128, 128], F32), ("ones_f", [128, 128], F32), ("slt", [128, 128], F32),
            ("blkstart", [128, 1], F32), ("pcol", [128, 1], F32), ("thr", [128, 34], F32)]
L3_SPECS = [("cT", [128, 8], F32), ("wmod", [D, 4096], F32), ("bmod", [1, 4096], F32), ("g2", [1, D], F32), ("gfin", [1, D], F32),
            ("dalam", [1, 256], F32), ("dagT", [128, 8], F32), ("w_out", [D, D], F32), ("Wr", [D, 36], F32), ("br", [1, 36], F32),
            ("w1", [32, D, 512], F32), ("w3", [32, D, 512], F32), ("w2", [32, 512, D], F32),
            ("ident_bf", [128, 128], BF16), ("ident_f", [128, 128], F32), ("ones_f", [128, 128], F32), ("ones_bf", [128, 128], BF16),
            ("slt", [128, 128], F32), ("blkstart", [128, 1], F32), ("pcol", [128, 1], F32), ("thr", [128, 34], F32)]


def build_fused(nc):
    cx = Ctx(nc)
    S = cx.S
    io1 = {n: cx.din("l1_" + n, sh, dt) for (n, sh, dt) in L1_SPECS}
    io2 = {n: cx.din("l2_" + n, sh, dt) for (n, sh, dt) in L2_SPECS}
    io3 = {n: cx.din("l3_" + n, sh, dt) for (n, sh, dt) in L3_SPECS}
    out = cx.dout("out", [NT3 * 128, D])

    mergedX = cx.dscr("mergedX", [NT * 128, 512], BF16)
    io1["merged"] = mergedX
    b_merged = build_l1(cx, io1)

    MG = cx.dscr("MG", [2 * NT * 128, 512], BF16); b_MG = Buf()
    for (r0, n) in MCH:
        S.collective("AllGather", [mergedX[r0:r0 + n, :]], [MG[2 * r0:2 * r0 + 2 * n, :]], PAIRS, reads=[b_merged], writes=[b_MG])

    H2 = cx.dscr("H2", [NT2 * 128, D])
    QTs = cx.dscr("QTs", [8, 128, 4096], BF16)
    KTo = cx.dscr("KTo", [8, 128, NT2 * 128], BF16)
    Vown = cx.dscr("Vown", [NT2 * 128, D], BF16)
    io2.update({"MG": MG, "b_MG": b_MG, "H2": H2, "QTs": QTs, "KTo": KTo, "Vown": Vown})
    b_H2, b_QTs, b_KTo, b_Vown = build_l2(cx, io2)

    KTg = cx.dscr("KTg", [8, 256, NT2 * 128], BF16); b_KTg = Buf()
    VG = cx.dscr("VG", [2 * NT2 * 128, D], BF16); b_VG = Buf()
    for h in range(8):
        S.collective("AllGather", [KTo[h]], [KTg[h]], PAIRS, reads=[b_KTo], writes=[b_KTg])
    for (r0, n) in VCH:
        S.collective("AllGather", [Vown[r0:r0 + n, :]], [VG[2 * r0:2 * r0 + 2 * n, :]], PAIRS, reads=[b_Vown], writes=[b_VG])

    io3.update({"QTs": QTs, "b_QTs": b_QTs, "KTg": KTg, "b_KTg": b_KTg, "VG": VG, "b_VG": b_VG, "H2": H2, "b_H2": b_H2, "out": out})
    b_out = build_l3(cx, io3)
    S.finish([b_out], "sp")
    print("fused instructions:", S.n_inst)
    return nc


def fused_inputs(I, core, c1, c2, c3):
    b, g = core // 2, core % 2
    d = {}
    for k, v in l1_inputs(I, b, g, c1).items():
        d["l1_" + k] = v
    d2 = l2_inputs(I, b, g, None, c2[g])
    for k, v in d2.items():
        d["l2_" + k] = v
    trow = np.concatenate([np.arange(g * 128, (g + 1) * 128), 256 + np.arange(g * 4096, (g + 1) * 4096)])
    r0 = np.minimum((trow // 2048) * 2048, 8192)
    n = np.where(r0 < 8192, 2048, 256)
    idx = np.stack([2 * r0 + r * n + (trow - r0) for r in range(2)], 1).astype(np.int32)
    d["l2_idxm"] = np.ascontiguousarray(idx.reshape(NT2, 128, 2).transpose(1, 0, 2))
    d3 = l3_inputs(I, b, g, None, c3)
    for k, v in d3.items():
        d["l3_" + k] = v
    return d


from concourse.bass_utils import run_bass_kernel_spmd

_NC_CACHE = {}


def kernel(**inputs):
    I = {k: np.asarray(v) for k, v in inputs.items()}
    cores = list(range(8))
    if "fused" not in _NC_CACHE:
        nc = bass.Bass("TRN2", target_bir_lowering=False)
        build_fused(nc)
        _NC_CACHE["fused"] = nc
    nc = _NC_CACHE["fused"]
    c1 = host_consts_l1(); c2 = [host_consts_l2(0), host_consts_l2(1)]; c3 = host_consts_l3()
    names = set(["l1_" + n for n, _, _ in L1_SPECS] + ["l2_" + n for n, _, _ in L2_SPECS] + ["l3_" + n for n, _, _ in L3_SPECS])
    in_maps = []
    for core in cores:
        m = fused_inputs(I, core, c1, c2, c3)
        in_maps.append({k: v for k, v in m.items() if k in names})
    res = run_bass_kernel_spmd(nc, in_maps, core_ids=cores).results
    out = np.empty((4, 8192, 1024), np.float32)
    for core in cores:
        b, half = core // 2, core % 2
        out[b, half * 4096:(half + 1) * 4096] = res[core]["out"]
    return out
```

```python
import math
import contextlib
import numpy as np
import ml_dtypes
import concourse.bass as bass
import concourse.mybir as mybir


F32 = mybir.dt.float32
BF16 = mybir.dt.bfloat16
I32 = mybir.dt.int32
U32 = mybir.dt.uint32
AF = mybir.ActivationFunctionType
ALU = mybir.AluOpType
AX = mybir.AxisListType


class Buf:
    __slots__ = ("name", "w", "r")

    def __init__(self, name=""):
        self.name = name
        self.w = None
        self.r = {}


class Sync:
    def __init__(self, nc, n_dma_sems=48, same_engine_wait=True):
        self.nc = nc
        self.eng = {"pe": nc.tensor, "act": nc.scalar, "dve": nc.vector, "pool": nc.gpsimd, "sp": nc.sync}
        self.sem = {}
        self.cnt = {}
        for k in ["pe", "act", "dve", "pool"]:
            self.sem[k] = nc.alloc_semaphore(name="s_" + k)
            self.cnt[k] = 0
        self.dsem = [nc.alloc_semaphore(name="s_dma%d" % i) for i in range(n_dma_sems)]
        self.dcnt = [0] * n_dma_sems
        self.dnext = 0
        self.waited = {k: {} for k in self.eng}
        self.same_engine_wait = same_engine_wait
        self.n_inst = 0

    def _semh(self, key):
        return self.sem[key] if isinstance(key, str) else self.dsem[key]

    def _wait(self, e, key, val):
        if val <= 0:
            return
        w = self.waited[e]
        if w.get(key, 0) >= val:
            return
        if key == e and not self.same_engine_wait:
            return
        if key == e == "pe":
            return
        self.eng[e].wait_ge(self._semh(key), val)
        w[key] = val

    def _deps(self, e, reads, writes):
        for b in reads:
            if b.w is not None:
                self._wait(e, *b.w)
        for b in writes:
            if b.w is not None:
                self._wait(e, *b.w)
            for k, v in b.r.items():
                self._wait(e, k, v)

    def _record(self, ev, reads, writes):
        k, v = ev
        for b in reads:
            if b.r.get(k, 0) < v:
                b.r[k] = v
        for b in writes:
            b.w = ev
            b.r = {}

    def op(self, e, fn, reads=(), writes=(), inc=True):
        self._deps(e, reads, writes)
        ins = fn(self.eng[e])
        if inc:
            self.cnt[e] += 1
            ins.then_inc(self.sem[e], 1)
            self._record((e, self.cnt[e]), reads, writes)
        else:
            self._record((e, self.cnt[e] + 1), reads, writes)
        self.n_inst += 1
        return ins

    def dma(self, q, out, in_, reads=(), writes=(), indirect=None, **kw):
        self._deps(q, reads, writes)
        k = self.dnext
        self.dnext = (self.dnext + 1) % len(self.dsem)
        self._wait(q, k, 16 * self.dcnt[k])
        if indirect is not None:
            ins = self.eng[q].indirect_dma_start(out, indirect.get("out_offset"), in_, indirect.get("in_offset"), **kw)
        else:
            ins = self.eng[q].dma_start(out=out, in_=in_, **kw)
        self.dcnt[k] += 1
        ins.then_inc(self.dsem[k], 16)
        self._record((k, 16 * self.dcnt[k]), reads, writes)
        self.n_inst += 1
        return ins

    def collective(self, kind, ins, outs, groups, reads=(), writes=()):
        self._deps("pool", reads, writes)
        if "cc" not in self.sem:
            self.sem["cc"] = self.nc.alloc_semaphore(name="s_cc")
            self.cnt["cc"] = 0
        ins_ = self.nc.gpsimd.collective_compute(kind, ALU.bypass, replica_groups=groups, ins=ins, outs=outs)
        self.cnt["cc"] += 1
        ins_.then_inc(self.sem["cc"], 1)
        self._record(("cc", self.cnt["cc"]), reads, writes)
        self.n_inst += 1
        return ins_

    def barrier(self):
        for e in self.eng:
            for k in self.sem:
                self._wait(e, k, self.cnt[k])
            for k in range(len(self.dsem)):
                self._wait(e, k, 16 * self.dcnt[k])

    def touch(self, e, reads=(), writes=()):
        self._deps(e, reads, writes)

    def finish(self, bufs, e="sp"):
        for b in bufs:
            if b.w is not None:
                self._wait(e, *b.w)


D = 1024
EPS = 1e-6
XOFF = 524288
PROFILE_SCOPES = False


class Ctx:
    def __init__(self, nc):
        self.nc = nc
        self.S = Sync(nc)
        self.stk = [contextlib.ExitStack()]
        self.PB = [nc.alloc_psum_tensor("pb%d" % i, [128, 512], F32) for i in range(7)]
        self.PBb = [Buf() for _ in range(7)]
        self.PT = nc.alloc_psum_tensor("pt_bf", [128, 1024], BF16)
        self.b_PT = Buf()
        self.nreg = 0
        self.scopes = []

    def din(self, name, shape, dt=F32):
        return self.nc.dram_tensor(name, list(shape), dt, kind="ExternalInput").ap()

    def dout(self, name, shape, dt=F32):
        return self.nc.dram_tensor(name, list(shape), dt, kind="ExternalOutput").ap()

    def dscr(self, name, shape, dt=F32):
        return self.nc.dram_tensor(name, list(shape), dt).ap()

    def sb(self, name, shape, dt=F32):
        self.nreg += 1
        return self.stk[-1].enter_context(self.nc.sbuf_tensor("%s_u%d" % (name, self.nreg), list(shape), dt))

    def phase_begin(self, name=None):
        self.stk.append(contextlib.ExitStack())
        sid = None
        if name is not None and PROFILE_SCOPES:
            sid, _ = self.nc.enter_named_scope(name, False)
        self.scopes.append((name, sid))

    def phase_end(self):
        self.S.barrier()
        name, sid = self.scopes.pop()
        if sid is not None:
            self.nc.leave_named_scope(name, sid, False)
        self.stk.pop().close()

    def load_const(self, name, shape, dt=F32):
        d = self.din(name, shape, dt)
        t = self.sb(name + "_s", shape, dt)
        b = Buf()
        self.S.dma("sp", t[:], d, writes=[b])
        return t, b

    def load_bcast(self, name, n, dram_ap=None):
        d = dram_ap if dram_ap is not None else self.din(name, [1, n])
        t = self.sb(name + "_s", [128, n])
        b = Buf()
        self.S.dma("sp", t[:], d.partition_broadcast(128), writes=[b])
        return t, b


def common_consts():
    c = {}
    c["ident_bf"] = np.eye(128, dtype=np.float32).astype(ml_dtypes.bfloat16)
    c["ident_f"] = np.eye(128, dtype=np.float32)
    c["ones_f"] = np.ones((128, 128), np.float32)
    s = np.arange(128)
    c["slt"] = (s[:, None] < s[None, :]).astype(np.float32)
    c["blkstart"] = (256.0 * s).astype(np.float32)[:, None]
    c["pcol"] = (1.0 * s).astype(np.float32)[:, None]
    c["thr"] = np.tile((256.0 * np.arange(34)).astype(np.float32)[None, :], (128, 1))
    return c


def emit_silu_bcast(cx, cT_aps, ones_f, b_ones):
    S = cx.S
    n = len(cT_aps)
    cT_s = cx.sb("cT_s", [128, n, 8]); b_cT = Buf()
    for i, a in enumerate(cT_aps):
        S.dma("sp", cT_s[:, i, :], a, writes=[b_cT])
    sc = cx.sb("sc", [128, n, 8]); b_sc = Buf()
    S.op("act", lambda e: e.activation(out=sc[:], in_=cT_s[:], func=AF.Silu), reads=[b_cT], writes=[b_sc])
    scb = cx.sb("scb", [128, n, 8, 128]); b_scb = Buf()
    for w in range(n):
        for j in range(8):
            S.op("dve", lambda e, w=w, j=j: e.tensor_scalar(out=scb[:, w, j, :], in0=ones_f[:], scalar1=sc[:, w, j:j + 1],
                                                           scalar2=None, op0=ALU.mult),
                 reads=[b_ones, b_sc], writes=[b_scb])
    return scb, b_scb


def emit_mod(cx, scb, b_scb, nvec, wmod_ap, bmod_s, b_bmod, ncols, dest_fn, b_dest, wm_s, b_wm):
    S = cx.S
    wmod_v = wmod_ap.rearrange("(j p) n -> p j n", p=128)
    k = 0
    for cc in range(ncols // 512):
        wb = cc % 2
        S.dma("sp", wm_s[wb][:], wmod_v[:, :, cc * 512:(cc + 1) * 512], writes=[b_wm[wb]])
        for w in range(nvec):
            pbi = k % 7; k += 1
            for j in range(8):
                S.op("pe", lambda e, w=w, j=j, wb=wb, pbi=pbi: e.matmul(cx.PB[pbi][:], lhsT=scb[:, w, j, :], rhs=wm_s[wb][:, j, :],
                                                                        start=(j == 0), stop=(j == 7)),
                     reads=[b_scb, b_wm[wb]], writes=[cx.PBb[pbi]])
            S.op("dve", lambda e, w=w, pbi=pbi, cc=cc: e.tensor_tensor(out=dest_fn(w, cc), in0=cx.PB[pbi][:],
                                                                      in1=bmod_s[:, cc * 512:(cc + 1) * 512], op=ALU.add),
                 reads=[cx.PBb[pbi], b_bmod], writes=[b_dest])


class NormBufs:
    def __init__(self, cx, tag=""):
        self.junk = cx.sb("junk" + tag, [128, D], BF16); self.b_junk = Buf()
        self.ss = cx.sb("ss" + tag, [128, 2]); self.b_ss = [Buf(), Buf()]
        self.rstd = cx.sb("rstd" + tag, [128, 2]); self.b_rstd = [Buf(), Buf()]
        self.tmp32 = cx.sb("tmp32" + tag, [128, D]); self.b_tmp32 = Buf()
        self.k = 0


def emit_adaln(cx, nb, x_ap, b_x, A_ap, Bsh_ap, b_mod, out_ap, b_out):
    S = cx.S
    xp = nb.k % 2; nb.k += 1
    ss = nb.ss[:, xp:xp + 1]; rstd = nb.rstd[:, xp:xp + 1]
    S.op("dve", lambda e: e.memset(ss, 0.0), writes=[nb.b_ss[xp]])
    S.op("act", lambda e: e.activation(out=nb.junk[:], in_=x_ap, func=AF.Square, accum_out=ss),
         reads=[b_x], writes=[nb.b_junk, nb.b_ss[xp]])
    S.op("dve", lambda e: e.tensor_scalar(out=rstd, in0=ss, scalar1=1.0 / D, scalar2=EPS, op0=ALU.mult, op1=ALU.add),
         reads=[nb.b_ss[xp]], writes=[nb.b_rstd[xp]])
    S.op("act", lambda e: e.activation(out=rstd, in_=rstd, func=AF.Sqrt), reads=[nb.b_rstd[xp]], writes=[nb.b_rstd[xp]])
    S.op("dve", lambda e: e.reciprocal(out=rstd, in_=rstd), reads=[nb.b_rstd[xp]], writes=[nb.b_rstd[xp]])
    if Bsh_ap is None:
        S.op("dve", lambda e: e.scalar_tensor_tensor(out=out_ap, in0=x_ap, scalar=rstd, in1=A_ap, op0=ALU.mult, op1=ALU.mult),
             reads=[b_x, nb.b_rstd[xp], b_mod], writes=[b_out])
    else:
        S.op("dve", lambda e: e.scalar_tensor_tensor(out=nb.tmp32[:], in0=x_ap, scalar=rstd, in1=A_ap, op0=ALU.mult, op1=ALU.mult),
             reads=[b_x, nb.b_rstd[xp], b_mod], writes=[nb.b_tmp32])
        S.op("dve", lambda e: e.tensor_tensor(out=out_ap, in0=nb.tmp32[:], in1=Bsh_ap, op=ALU.add),
             reads=[nb.b_tmp32, b_mod], writes=[b_out])


def emit_transpose(cx, src_fn, b_src, nblk, ident_bf, b_ident, dst_ap, b_dst):
    S = cx.S
    for j in range(nblk):
        S.op("pe", lambda e, j=j: e.transpose(cx.PT[:, j * 128:(j + 1) * 128], src_fn(j), ident_bf[:]),
             reads=[b_src, b_ident], writes=[cx.b_PT])
    S.op("act", lambda e: e.activation(out=dst_ap, in_=cx.PT[:, 0:nblk * 128].rearrange("p (j t) -> p j t", j=nblk), func=AF.Copy),
         reads=[cx.b_PT], writes=[b_dst])


class MoE:
    def __init__(self, cx, ntile, Wr_d, br_d, w1_d, w3_d, w2_d, consts=None, tag=""):
        self.cx = cx
        self.nt = ntile
        self.A = 2 * ntile * 128
        self.nblk = (self.A + 255) // 256 + 32
        self.c = consts
        self.tag = tag
        self.w1_d, self.w3_d, self.w2_d = w1_d, w3_d, w2_d
        self.Wr_d, self.br_d = Wr_d, br_d
        nr = self.nblk * 256
        self.xbuf = cx.dscr("xbuf" + tag, [nr, D], BF16); self.b_xbuf = Buf()
        self.ybuf = cx.dscr("ybuf" + tag, [nr, D], F32); self.b_ybuf = Buf()
        self.W1b = cx.dscr("W1b" + tag, [32 * 128, 4096], BF16); self.b_W1b = Buf()
        self.W3b = cx.dscr("W3b" + tag, [32 * 128, 4096], BF16); self.b_W3b = Buf()
        self.W2b = cx.dscr("W2b" + tag, [32 * 128, 4096], BF16); self.b_W2b = Buf()

    def precast(self):
        S = self.cx.S
        for (src, dst, b, j) in [(self.w1_d, self.W1b, self.b_W1b, 8), (self.w3_d, self.W3b, self.b_W3b, 8), (self.w2_d, self.W2b, self.b_W2b, 4)]:
            sv = src.rearrange("e (p j) n -> e p (j n)", j=j)
            for e_ in range(32):
                S.dma("pool", dst[e_ * 128:(e_ + 1) * 128, :].rearrange("p (a b) -> p a b", a=2), sv[e_].rearrange("p (a b) -> p a b", a=2), writes=[b])

    def alloc_persistent(self):
        cx = self.cx
        t = self.tag
        self.OH = cx.sb("OH" + t, [128, self.nt, 2, 32]); self.b_OH = [Buf() for _ in range(self.nt)]
        self.gate = cx.sb("gate" + t, [128, self.nt, 2]); self.b_gate = [Buf() for _ in range(self.nt)]
        self.dest = cx.sb("dest" + t, [128, self.nt, 2], I32); self.b_dest = [Buf() for _ in range(self.nt)]
        self.idxw = cx.sb("idxw" + t, [128, 128], I32); self.b_idxw = Buf()
        self.Wr_s = cx.sb("Wr_s" + t, [128, 8, 36], BF16); self.b_Wr = Buf()
        for j in range(8):
            cx.S.dma("pool", self.Wr_s[:, j, :], self.Wr_d[j * 128:(j + 1) * 128, :], writes=[self.b_Wr])
        self.br_s, self.b_br = cx.load_bcast("br" + t, 36, self.br_d)
        self.lg = cx.sb("lg" + t, [128, 36]); self.b_lg = Buf()
        self.sm = cx.sb("rsm" + t, [128, 64]); self.b_sm = Buf()

    def route_tile(self, ti, vT_fn, b_vT):
        cx, S = self.cx, self.cx.S
        pbi = 6
        PBt, bPB = cx.PB[pbi], cx.PBb[pbi]
        for j in range(8):
            S.op("pe", lambda e, j=j: e.matmul(PBt[:, 0:36], lhsT=vT_fn(j), rhs=self.Wr_s[:, j, :], start=(j == 0), stop=(j == 7)),
                 reads=[b_vT, self.b_Wr], writes=[bPB])
        lg, sm = self.lg, self.sm
        b_lg, b_sm = self.b_lg, self.b_sm
        S.op("dve", lambda e: e.tensor_tensor(out=lg[:], in0=PBt[:, 0:36], in1=self.br_s[:], op=ALU.add), reads=[bPB, self.b_br], writes=[b_lg])
        S.op("dve", lambda e: e.tensor_reduce(out=sm[:, 0:1], in_=lg[:, 0:4], axis=AX.X, op=ALU.max), reads=[b_lg], writes=[b_sm])
        S.op("dve", lambda e: e.tensor_scalar(out=sm[:, 1:2], in0=sm[:, 0:1], scalar1=-1.0, scalar2=None, op0=ALU.mult), reads=[b_sm], writes=[b_sm])
        S.op("dve", lambda e: e.memset(sm[:, 2:3], 0.0), reads=[b_sm], writes=[b_sm])
        S.op("act", lambda e: e.activation(out=sm[:, 44:48], in_=lg[:, 0:4], func=AF.Exp, bias=sm[:, 1:2], accum_out=sm[:, 2:3]),
             reads=[b_lg, b_sm], writes=[b_sm])
        S.op("dve", lambda e: e.reciprocal(out=sm[:, 3:4], in_=sm[:, 2:3]), reads=[b_sm], writes=[b_sm])
        S.op("dve", lambda e: e.tensor_scalar(out=sm[:, 4:8], in0=lg[:, 0:4], scalar1=sm[:, 0:1], scalar2=None, op0=ALU.is_equal),
             reads=[b_lg, b_sm], writes=[b_sm])
        S.op("dve", lambda e: e.tensor_scalar(out=sm[:, 8:16], in0=lg[:, 4:12], scalar1=sm[:, 4:5], scalar2=None, op0=ALU.mult),
             reads=[b_lg, b_sm], writes=[b_sm])
        for g in range(1, 4):
            S.op("dve", lambda e, g=g: e.scalar_tensor_tensor(out=sm[:, 8:16], in0=lg[:, 4 + 8 * g:12 + 8 * g], scalar=sm[:, 4 + g:5 + g],
                                                             in1=sm[:, 8:16], op0=ALU.mult, op1=ALU.add),
                 reads=[b_lg, b_sm], writes=[b_sm])
        S.op("dve", lambda e: e.max(out=sm[:, 16:24], in_=sm[:, 8:16]), reads=[b_sm], writes=[b_sm])
        S.op("dve", lambda e: e.tensor_scalar(out=sm[:, 24:32], in0=sm[:, 8:16], scalar1=sm[:, 16:17], scalar2=None, op0=ALU.is_equal),
             reads=[b_sm], writes=[b_sm])
        S.op("dve", lambda e: e.tensor_scalar(out=sm[:, 32:40], in0=sm[:, 8:16], scalar1=sm[:, 17:18], scalar2=None, op0=ALU.is_equal),
             reads=[b_sm], writes=[b_sm])
        S.op("dve", lambda e: e.tensor_tensor(out=sm[:, 40:41], in0=sm[:, 17:18], in1=sm[:, 16:17], op=ALU.subtract), reads=[b_sm], writes=[b_sm])
        S.op("act", lambda e: e.activation(out=sm[:, 41:42], in_=sm[:, 40:41], func=AF.Exp), reads=[b_sm], writes=[b_sm])
        S.op("dve", lambda e: e.tensor_scalar(out=sm[:, 42:43], in0=sm[:, 41:42], scalar1=1.0, scalar2=None, op0=ALU.add), reads=[b_sm], writes=[b_sm])
        S.op("dve", lambda e: e.reciprocal(out=sm[:, 42:43], in_=sm[:, 42:43]), reads=[b_sm], writes=[b_sm])
        S.op("dve", lambda e: e.tensor_tensor(out=sm[:, 43:44], in0=sm[:, 41:42], in1=sm[:, 42:43], op=ALU.mult), reads=[b_sm], writes=[b_sm])
        S.op("dve", lambda e: e.tensor_scalar(out=self.gate[:, ti, :], in0=sm[:, 42:44], scalar1=sm[:, 3:4], scalar2=None, op0=ALU.mult),
             reads=[b_sm], writes=[self.b_gate[ti]])
        for k in range(2):
            for g in range(4):
                S.op("dve", lambda e, k=k, g=g: e.tensor_scalar(out=self.OH[:, ti, k, g * 8:(g + 1) * 8], in0=sm[:, 24 + 8 * k:32 + 8 * k],
                                                               scalar1=sm[:, 4 + g:5 + g], scalar2=None, op0=ALU.mult),
                     reads=[b_sm], writes=[self.b_OH[ti]])

    def plan(self):
        cx, S = self.cx, self.cx.S
        t = self.tag
        ones_f, b_ones = self.c["ones_f"]
        ident_f, b_identf = self.c["ident_f"]
        blkstart, b_blk = self.c["blkstart"]
        PBt, bPB = cx.PB[0], cx.PBb[0]
        for ti in range(self.nt):
            S.op("pe", lambda e, ti=ti: e.matmul(PBt[:, 0:64], lhsT=ones_f[:], rhs=self.OH[:, ti, :, :].rearrange("p k e -> p (k e)"),
                                                 start=(ti == 0), stop=(ti == self.nt - 1)),
                 reads=[b_ones, self.b_OH[ti]], writes=[bPB])
        pl = cx.sb("pl" + t, [128, 8, 32]); b_pl = Buf()
        S.op("dve", lambda e: e.tensor_copy(out=pl[:, 7, :], in_=PBt[:, 0:32]), reads=[bPB], writes=[b_pl])
        S.op("dve", lambda e: e.tensor_tensor(out=pl[:, 0, :], in0=pl[:, 7, :], in1=PBt[:, 32:64], op=ALU.add), reads=[bPB, b_pl], writes=[b_pl])
        thr, b_thr = self.c["thr"]
        cmp3 = cx.sb("cmp3" + t, [128, 32, 34]); b_cmp3 = Buf()
        S.op("dve", lambda e: e.tensor_tensor(out=cmp3[:], in0=pl[:, 0, :].unsqueeze(2).broadcast_to([128, 32, 34]),
                                              in1=thr[:].unsqueeze(1).broadcast_to([128, 32, 34]), op=ALU.is_gt),
             reads=[b_pl, b_thr], writes=[b_cmp3])
        S.op("dve", lambda e: e.tensor_reduce(out=pl[:, 1, :], in_=cmp3[:], axis=AX.X, op=ALU.add), reads=[b_cmp3], writes=[b_pl])
        S.op("dve", lambda e: e.tensor_scalar(out=pl[:, 3, :], in0=pl[:, 1, :], scalar1=256.0, scalar2=None, op0=ALU.mult), reads=[b_pl], writes=[b_pl])
        S.op("dve", lambda e: e.memset(pl[:, 5, :], 0.0), reads=[b_pl], writes=[b_pl])
        S.op("dve", lambda e: e.tensor_tensor_scan(out=pl[:, 4, :], data0=pl[:, 3, :], data1=pl[:, 5, :], initial=0.0, op0=ALU.add, op1=ALU.add),
             reads=[b_pl], writes=[b_pl])
        self.run = cx.sb("run" + t, [128, 32]); self.b_run = Buf()
        S.op("dve", lambda e: e.tensor_tensor(out=self.run[:], in0=pl[:, 4, :], in1=pl[:, 3, :], op=ALU.subtract), reads=[b_pl], writes=[self.b_run])
        S.op("dve", lambda e: e.tensor_scalar(out=pl[:, 6, :], in0=pl[:, 4, :], scalar1=blkstart[:, 0:1], scalar2=None, op0=ALU.is_le),
             reads=[b_pl, b_blk], writes=[b_pl])
        be = cx.sb("be" + t, [128, 1]); b_be = Buf()
        S.op("dve", lambda e: e.tensor_reduce(out=be[:], in_=pl[:, 6, :], axis=AX.X, op=ALU.add), reads=[b_pl], writes=[b_be])
        S.op("dve", lambda e: e.tensor_scalar(out=be[:], in0=be[:], scalar1=31.0, scalar2=128.0, op0=ALU.min, op1=ALU.mult), reads=[b_be], writes=[b_be])
        bbc = cx.sb("bbc" + t, [128, 128]); b_bbc = Buf()
        pcol, b_pcol = self.c["pcol"]
        S.op("dve", lambda e: e.tensor_scalar(out=bbc[:], in0=ones_f[:], scalar1=be[:, 0:1], scalar2=None, op0=ALU.mult), reads=[b_be, b_ones], writes=[b_bbc])
        PB1, bPB1 = cx.PB[1], cx.PBb[1]
        S.op("pe", lambda e: e.matmul(PB1[:, 0:128], lhsT=bbc[:], rhs=ident_f[:], start=True, stop=True), reads=[b_bbc, b_identf], writes=[bPB1])
        idxf = cx.sb("idxf" + t, [128, 128]); b_idxf = Buf()
        S.op("dve", lambda e: e.tensor_scalar(out=idxf[:], in0=PB1[:, 0:128], scalar1=pcol[:, 0:1], scalar2=None, op0=ALU.add), reads=[bPB1, b_pcol], writes=[b_idxf])
        S.op("dve", lambda e: e.tensor_copy(out=self.idxw[:], in_=idxf[:]), reads=[b_idxf], writes=[self.b_idxw])
        self.d1 = cx.sb("d1" + t, [128, 32]); self.b_d1 = Buf()
        self.destf = cx.sb("destf" + t, [128, 2]); self.b_destf = Buf()

    def dispatch_tile(self, ti, v_ap, b_v, do_scatter=True):
        cx, S = self.cx, self.cx.S
        ones_f, b_ones = self.c["ones_f"]
        slt, b_slt = self.c["slt"]
        for k in range(2):
            pa, ba = cx.PB[2 + k], cx.PBb[2 + k]
            pt_, bt_ = cx.PB[4 + k], cx.PBb[4 + k]
            S.op("pe", lambda e, k=k, pa=pa: e.matmul(pa[:, 0:32], lhsT=slt[:], rhs=self.OH[:, ti, k, :], start=True, stop=True),
                 reads=[b_slt, self.b_OH[ti]], writes=[ba])
            S.op("pe", lambda e, k=k, pt_=pt_: e.matmul(pt_[:, 0:32], lhsT=ones_f[:], rhs=self.OH[:, ti, k, :], start=True, stop=True),
                 reads=[b_ones, self.b_OH[ti]], writes=[bt_])
            S.op("dve", lambda e, pa=pa: e.tensor_tensor(out=self.d1[:], in0=pa[:, 0:32], in1=self.run[:], op=ALU.add),
                 reads=[ba, self.b_run], writes=[self.b_d1])
            S.op("dve", lambda e, k=k: e.tensor_tensor(out=self.d1[:], in0=self.d1[:], in1=self.OH[:, ti, k, :], op=ALU.mult),
                 reads=[self.b_d1, self.b_OH[ti]], writes=[self.b_d1])
            S.op("dve", lambda e, k=k: e.tensor_reduce(out=self.destf[:, k:k + 1], in_=self.d1[:], axis=AX.X, op=ALU.add),
                 reads=[self.b_d1], writes=[self.b_destf])
            S.op("dve", lambda e, k=k: e.tensor_copy(out=self.dest[:, ti, k:k + 1], in_=self.destf[:, k:k + 1]),
                 reads=[self.b_destf], writes=[self.b_dest[ti]])
            S.op("dve", lambda e, pt_=pt_: e.tensor_tensor(out=self.run[:], in0=self.run[:], in1=pt_[:, 0:32], op=ALU.add),
                 reads=[bt_, self.b_run], writes=[self.b_run])
            if do_scatter:
              S.dma("pool", self.xbuf, v_ap, reads=[b_v, self.b_dest[ti]], writes=[self.b_xbuf],
                  indirect={"out_offset": bass.IndirectOffsetOnAxis(ap=self.dest[:, ti, k:k + 1], axis=0)})

    def experts(self):
        cx, S, nc = self.cx, self.cx.S, self.cx.nc
        t = self.tag
        ident_bf, b_identbf = self.c["ident_bf"]
        NW = 3
        w1_s = [cx.sb("w1_s%d%s" % (i, t), [128, 8, 512], BF16) for i in range(NW)]
        w3_s = [cx.sb("w3_s%d%s" % (i, t), [128, 8, 512], BF16) for i in range(NW)]
        w2_s = [cx.sb("w2_s%d%s" % (i, t), [128, 4, 1024], BF16) for i in range(NW)]
        b_w1 = [Buf() for _ in range(NW)]; b_w3 = [Buf() for _ in range(NW)]; b_w2 = [Buf() for _ in range(NW)]
        xb = [cx.sb("xb%d%s" % (i, t), [128, D], BF16) for i in range(3)]; b_xb = [Buf() for _ in range(3)]
        xT = [cx.sb("xT%d%s" % (i, t), [128, 8, 128], BF16) for i in range(2)]; b_xT = [Buf(), Buf()]
        s1 = cx.sb("s1" + t, [128, 512]); b_s1 = Buf()
        hh = [cx.sb("hh%d%s" % (i, t), [128, 512], BF16) for i in range(2)]; b_hh = [Buf(), Buf()]
        hhT = [cx.sb("hhT%d%s" % (i, t), [128, 4, 128], BF16) for i in range(2)]; b_hhT = [Buf(), Buf()]
        yst = [cx.sb("yst%d%s" % (i, t), [128, D]) for i in range(2)]; b_yst = [Buf(), Buf()]
        PTa, b_PTa = cx.PT, cx.b_PT
        PTb, b_PTb = cx.PB[6][:].bitcast(BF16), cx.PBb[6]
        pa, ba = cx.PB[0], cx.PBb[0]
        pb_, bb = cx.PB[1], cx.PBb[1]
        pc = [cx.PB[2], cx.PB[3]]; bc = [cx.PBb[2], cx.PBb[3]]
        nsub = 2 * self.nblk

        def load_w(blk):
            p = blk % NW
            off = bass.IndirectOffsetOnAxis(ap=self.idxw[:, blk:blk + 1], axis=0)
            S.dma("pool", w1_s[p][:].rearrange("p j n -> p (j n)"), self.W1b, reads=[self.b_idxw, self.b_W1b], writes=[b_w1[p]], indirect={"in_offset": off})
            S.dma("pool", w3_s[p][:].rearrange("p j n -> p (j n)"), self.W3b, reads=[self.b_idxw, self.b_W3b], writes=[b_w3[p]], indirect={"in_offset": off})
            S.dma("pool", w2_s[p][:].rearrange("p j n -> p (j n)"), self.W2b, reads=[self.b_idxw, self.b_W2b], writes=[b_w2[p]], indirect={"in_offset": off})

        def load_x(sidx):
            S.dma("sp", xb[sidx % 3][:], self.xbuf[sidx * 128:(sidx + 1) * 128, :], reads=[self.b_xbuf], writes=[b_xb[sidx % 3]])

        def stA(sidx):
            q = sidx % 2
            for j in range(8):
                S.op("pe", lambda e, j=j: e.transpose(PTa[:, j * 128:(j + 1) * 128], xb[sidx % 3][:].rearrange("t (p j) -> t j p", j=8)[:, j, :], ident_bf[:]),
                     reads=[b_xb[sidx % 3], b_identbf], writes=[b_PTa], inc=(j == 7))
            S.op("act", lambda e: e.activation(out=xT[q][:], in_=PTa[:].rearrange("p (j t) -> p j t", j=8), func=AF.Copy), reads=[b_PTa], writes=[b_xT[q]])

        def stB(sidx):
            q = sidx % 2; p = (sidx // 2) % NW
            for j in range(8):
                S.op("pe", lambda e, j=j: e.matmul(pa[:], lhsT=xT[q][:, j, :], rhs=w1_s[p][:, j, :], start=(j == 0), stop=(j == 7)),
                     reads=[b_xT[q], b_w1[p]], writes=[ba], inc=(j == 7))
            for j in range(8):
                S.op("pe", lambda e, j=j: e.matmul(pb_[:], lhsT=xT[q][:, j, :], rhs=w3_s[p][:, j, :], start=(j == 0), stop=(j == 7)),
                     reads=[b_xT[q], b_w3[p]], writes=[bb], inc=(j == 7))
            S.op("act", lambda e: e.activation(out=s1[:], in_=pa[:], func=AF.Silu), reads=[ba], writes=[b_s1])
            S.op("dve", lambda e: e.tensor_tensor(out=hh[q][:], in0=s1[:], in1=pb_[:], op=ALU.mult), reads=[b_s1, bb], writes=[b_hh[q]])

        def stC(sidx):
            q = sidx % 2
            for j in range(4):
                S.op("pe", lambda e, j=j: e.transpose(PTb[:, j * 128:(j + 1) * 128], hh[q][:].rearrange("t (p j) -> t j p", j=4)[:, j, :], ident_bf[:]),
                     reads=[b_hh[q], b_identbf], writes=[b_PTb], inc=(j == 3))
            S.op("act", lambda e: e.activation(out=hhT[q][:], in_=PTb[:, 0:512].rearrange("p (j t) -> p j t", j=4), func=AF.Copy), reads=[b_PTb], writes=[b_hhT[q]])

        def stD(sidx):
            q = sidx % 2; p = (sidx // 2) % NW
            for half in range(2):
                for j in range(4):
                    S.op("pe", lambda e, j=j, half=half: e.matmul(pc[half][:], lhsT=hhT[q][:, j, :], rhs=w2_s[p][:, j, half * 512:(half + 1) * 512],
                                                                  start=(j == 0), stop=(j == 3)),
                         reads=[b_hhT[q], b_w2[p]], writes=[bc[half]], inc=(j == 3))
            S.op("act", lambda e: e.activation(out=yst[q][:, 0:512], in_=pc[0][:], func=AF.Copy), reads=[bc[0]], writes=[b_yst[q]])
            S.op("dve", lambda e: e.tensor_copy(out=yst[q][:, 512:1024], in_=pc[1][:]), reads=[bc[1]], writes=[b_yst[q]])
            S.dma("sp", self.ybuf[sidx * 128:(sidx + 1) * 128, :], yst[q][:], reads=[b_yst[q]], writes=[self.b_ybuf])

        load_w(0)
        load_x(0)
        for i in range(nsub + 2):
            if i % 2 == 0 and i // 2 + 1 < self.nblk:
                load_w(i // 2 + 1)
            if i + 1 < nsub:
                load_x(i + 1)
            if 0 <= i - 2 < nsub:
                stC(i - 2)
            if i < nsub:
                stA(i)
            if 0 <= i - 1 < nsub:
                stB(i - 1)
            if 0 <= i - 2 < nsub:
                stD(i - 2)

    def gather_tile(self, ti, y0_ap, y1_ap, b_y0, b_y1):
        S = self.cx.S
        S.dma("pool", y0_ap, self.ybuf, reads=[self.b_ybuf, self.b_dest[ti]], writes=[b_y0],
              indirect={"in_offset": bass.IndirectOffsetOnAxis(ap=self.dest[:, ti, 0:1], axis=0)})
        S.dma("pool", y1_ap, self.ybuf, reads=[self.b_ybuf, self.b_dest[ti]], writes=[b_y1],
              indirect={"in_offset": bass.IndirectOffsetOnAxis(ap=self.dest[:, ti, 1:2], axis=0)})


D = 1024
NT = 66
NG = 22
RS = 128 ** -0.5
EPS = 1e-6


def host_consts_l1():
    c = {}
    c["ident_bf"] = np.eye(128, dtype=np.float32).astype(ml_dtypes.bfloat16)
    c["ident_f"] = np.eye(128, dtype=np.float32)
    s = np.arange(128)
    mf = (s[:, None] <= s[None, :]).astype(np.float32)
    mb = (s[:, None] >= s[None, :]).astype(np.float32)
    c["mask_f32"] = np.stack([mf, mb], 1)
    c["mask_bf"] = c["mask_f32"].astype(ml_dtypes.bfloat16)
    c["ones_f"] = np.ones((128, 128), np.float32)
    pos = np.arange(128, dtype=np.float32)
    c["poscol"] = np.stack([127.0 - pos, pos, -(127.0 - pos), -pos], 1).astype(np.float32)
    T = NT * 128
    inv = (10000.0 ** (-np.arange(64, dtype=np.float32) / 64)).astype(np.float32)
    ang = (np.arange(T, dtype=np.float32)[:, None] * inv[None, :]).astype(np.float32)
    cos = np.cos(ang).astype(np.float32).T
    sin = np.sin(ang).astype(np.float32).T
    c["ropecos"] = np.ascontiguousarray(np.concatenate([cos, cos], 0))
    c["ropesin"] = np.ascontiguousarray(np.concatenate([-sin, sin], 0))
    return c


def build_l1(cx, io):
    nc = cx.nc
    S = cx.S
    cx.phase_begin("L1")

    def din(name, shape, dt=F32):
        return io[name]

    xin = din("xin", [NT * 128, D])
    cT = din("cT", [128, 8])
    cctxT = din("cctxT", [128, 8])
    wmod = din("wmod", [D, 2048])
    bmod = din("bmod", [1, 2048])
    g1 = din("g1", [1, D])
    Wfm = din("Wfm", [D, 1536])
    Wtm = din("Wtm", [D, 1032])
    gbias = din("gbias", [1, 8])
    rld = din("rld", [1, 4])
    mlg = din("mlg", [1, 256])
    retg = din("retg", [1, 256])
    ident_bf_d = din("ident_bf", [128, 128], BF16)
    ident_f_d = din("ident_f", [128, 128])
    mask_f32_d = din("mask_f32", [128, 2, 128])
    mask_bf_d = din("mask_bf", [128, 2, 128], BF16)
    ones_d = din("ones_f", [128, 128])
    poscol_d = din("poscol", [128, 4])
    ropecos_d = din("ropecos", [128, NT * 128])
    ropesin_d = din("ropesin", [128, NT * 128])
    merged = io["merged"]

    FM = nc.dram_tensor("FM", [NT, 128, 1024], BF16).ap()
    TM = nc.dram_tensor("TM", [NT, 128, 1024], BF16).ap()
    OG = nc.dram_tensor("OG", [NT, 128, 512], BF16).ap()
    HS = nc.dram_tensor("HS", [2, NT, 128, 512], F32).ap()

    sb = cx.sb
    phase_begin = cx.phase_begin
    phase_end = cx.phase_end

    ident_bf = sb("ident_bf_s", [128, 128], BF16); b_identbf = Buf()
    ident_f = sb("ident_f_s", [128, 128]); b_identf = Buf()
    mask_f32 = sb("mask_f32_s", [128, 2, 128]); b_maskf = Buf()
    mask_bf = sb("mask_bf_s", [128, 2, 128], BF16); b_maskbf = Buf()
    ones_f = sb("ones_s", [128, 128]); b_ones = Buf()
    poscol = sb("poscol_s", [128, 4]); b_poscol = Buf()
    S.dma("sp", ident_bf[:], ident_bf_d, writes=[b_identbf])
    S.dma("sp", ident_f[:], ident_f_d, writes=[b_identf])
    S.dma("sp", mask_f32[:], mask_f32_d, writes=[b_maskf])
    S.dma("sp", mask_bf[:], mask_bf_d, writes=[b_maskbf])
    S.dma("sp", ones_f[:], ones_d, writes=[b_ones])
    S.dma("sp", poscol[:], poscol_d, writes=[b_poscol])

    Gall = sb("Gall", [128, 8, NT]); b_G = Buf()
    phase_begin("L1_A")
    Wfm_s = sb("Wfm_s", [128, 8, 1536], BF16); b_wfm = Buf()
    Wtm_s = sb("Wtm_s", [128, 8, 1032], BF16); b_wtm = Buf()
    for j in range(8):
        S.dma("pool", Wfm_s[:, j, :], Wfm[j * 128:(j + 1) * 128, :], writes=[b_wfm])
        S.dma("pool", Wtm_s[:, j, :], Wtm[j * 128:(j + 1) * 128, :], writes=[b_wtm])
    if "after_weights" in io:
        io["after_weights"]()

    PB = cx.PB
    PBb = cx.PBb

    cT_s = sb("cT_s", [128, 2, 8]); b_cT = Buf()
    S.dma("sp", cT_s[:, 0, :], cT, writes=[b_cT])
    S.dma("sp", cT_s[:, 1, :], cctxT, writes=[b_cT])
    sc = sb("sc", [128, 2, 8]); b_sc = Buf()
    S.op("act", lambda e: e.activation(out=sc[:], in_=cT_s[:], func=AF.Silu), reads=[b_cT], writes=[b_sc])
    scb = sb("scb", [128, 2, 8, 128]); b_scb = Buf()
    for w in range(2):
        for j in range(8):
            S.op("dve", lambda e, w=w, j=j: e.tensor_scalar(out=scb[:, w, j, :], in0=ones_f[:], scalar1=sc[:, w, j:j + 1],
                                                           scalar2=None, op0=ALU.mult),
                 reads=[b_ones, b_sc], writes=[b_scb])
    bmod_s = sb("bmod_s", [128, 2048]); b_bmod = Buf()
    S.dma("sp", bmod_s[:], bmod.partition_broadcast(128), writes=[b_bmod])
    g1_s = sb("g1_s", [128, D]); b_g1 = Buf()
    S.dma("sp", g1_s[:], g1.partition_broadcast(128), writes=[b_g1])
    modS = sb("modS", [128, 2, D]); modA = sb("modA", [128, 2, D]); b_mod = Buf()
    wm_s = [sb("wm_s%d" % i, [128, 8, 512]) for i in range(2)]
    b_wm = [Buf(), Buf()]
    wmod_v = wmod.rearrange("(j p) n -> p j n", p=128)
    for cc in range(4):
        wb = cc % 2
        S.dma("sp", wm_s[wb][:], wmod_v[:, :, cc * 512:(cc + 1) * 512], writes=[b_wm[wb]])
        for w in range(2):
            pbi = (cc * 2 + w) % 7
            for j in range(8):
                S.op("pe", lambda e, w=w, j=j, wb=wb, pbi=pbi: e.matmul(PB[pbi][:], lhsT=scb[:, w, j, :], rhs=wm_s[wb][:, j, :],
                                                                        start=(j == 0), stop=(j == 7)),
                     reads=[b_scb, b_wm[wb]], writes=[PBb[pbi]])
            half = cc % 2
            if cc < 2:
                S.op("dve", lambda e, w=w, pbi=pbi, cc=cc, half=half: e.tensor_tensor(
                    out=modS[:, w, half * 512:(half + 1) * 512], in0=PB[pbi][:], in1=bmod_s[:, cc * 512:(cc + 1) * 512], op=ALU.add),
                    reads=[PBb[pbi], b_bmod], writes=[b_mod])
            else:
                S.op("dve", lambda e, w=w, pbi=pbi, cc=cc, half=half: e.tensor_tensor(
                    out=modA[:, w, half * 512:(half + 1) * 512], in0=PB[pbi][:], in1=bmod_s[:, cc * 512:(cc + 1) * 512], op=ALU.add),
                    reads=[PBb[pbi], b_bmod], writes=[b_mod])
                S.op("dve", lambda e, w=w, half=half: e.scalar_tensor_tensor(
                    out=modA[:, w, half * 512:(half + 1) * 512], in0=modA[:, w, half * 512:(half + 1) * 512], scalar=1.0,
                    in1=g1_s[:, half * 512:(half + 1) * 512], op0=ALU.add, op1=ALU.mult),
                    reads=[b_mod, b_g1], writes=[b_mod])

    xt = [sb("xt%d" % i, [128, D]) for i in range(2)]; b_xt = [Buf(), Buf()]
    junk = sb("junk", [128, D], BF16); b_junk = Buf()
    ss = sb("ss", [128, 2]); b_ss = [Buf(), Buf()]
    rstd = sb("rstd", [128, 2]); b_rstd = [Buf(), Buf()]
    tmp32 = sb("tmp32", [128, D]); b_tmp32 = Buf()
    u_bf = [sb("u_bf%d" % i, [128, D], BF16) for i in range(2)]; b_u = [Buf(), Buf()]
    uT = [sb("uT%d" % i, [128, 8, 384], BF16) for i in range(2)]
    b_uT = [[Buf() for _ in range(3)] for _ in range(2)]
    FMst = [sb("FMst%d" % i, [128, 3, 8, 128], BF16) for i in range(2)]
    b_FMst = [[Buf() for _ in range(8)] for _ in range(2)]
    TMst = [sb("TMst%d" % i, [128, 3, 1024], BF16) for i in range(2)]
    b_TMst = [[Buf() for _ in range(3)] for _ in range(2)]
    OGst = [sb("OGst%d" % i, [128, 3, 512], BF16) for i in range(2)]
    b_OGst = [[Buf() for _ in range(3)] for _ in range(2)]
    rc = [sb("rc%d" % i, [128, 384]) for i in range(2)]; rsn = [sb("rsn%d" % i, [128, 384]) for i in range(2)]
    b_rope = [Buf(), Buf()]
    t1 = sb("t1", [128, 384]); t2 = sb("t2", [128, 384]); b_t1 = Buf(); b_t2 = Buf()
    PT = cx.PT
    b_PT = cx.b_PT
    b_FM_d = [Buf() for _ in range(NT)]
    b_TM_d = [Buf() for _ in range(NT)]
    b_OG_d = [Buf() for _ in range(NT)]
    tmi = 0
    fmi = 0
    for g in range(NG):
        p = g % 2
        S.dma("sp", rc[p][:], ropecos_d[:, g * 384:(g + 1) * 384], writes=[b_rope[p]])
        S.dma("sp", rsn[p][:], ropesin_d[:, g * 384:(g + 1) * 384], writes=[b_rope[p]])
        for ti in range(3):
            c = g * 3 + ti
            w = 1 if c < 2 else 0
            xp = c % 2
            S.dma("sp", xt[xp][:], xin[c * 128:(c + 1) * 128, :], writes=[b_xt[xp]])
            S.op("dve", lambda e, xp=xp: e.memset(ss[:, xp:xp + 1], 0.0), writes=[b_ss[xp]])
            S.op("act", lambda e, xp=xp: e.activation(out=junk[:], in_=xt[xp][:], func=AF.Square, accum_out=ss[:, xp:xp + 1]),
                 reads=[b_xt[xp]], writes=[b_junk, b_ss[xp]])
            S.op("dve", lambda e, xp=xp: e.tensor_scalar(out=rstd[:, xp:xp + 1], in0=ss[:, xp:xp + 1], scalar1=1.0 / D, scalar2=EPS,
                                                        op0=ALU.mult, op1=ALU.add), reads=[b_ss[xp]], writes=[b_rstd[xp]])
            S.op("act", lambda e, xp=xp: e.activation(out=rstd[:, xp:xp + 1], in_=rstd[:, xp:xp + 1], func=AF.Sqrt),
                 reads=[b_rstd[xp]], writes=[b_rstd[xp]])
            S.op("dve", lambda e, xp=xp: e.reciprocal(out=rstd[:, xp:xp + 1], in_=rstd[:, xp:xp + 1]), reads=[b_rstd[xp]], writes=[b_rstd[xp]])
            S.op("dve", lambda e, xp=xp, w=w: e.scalar_tensor_tensor(out=tmp32[:], in0=xt[xp][:], scalar=rstd[:, xp:xp + 1],
                                                                    in1=modA[:, w, :], op0=ALU.mult, op1=ALU.mult),
                 reads=[b_xt[xp], b_rstd[xp], b_mod], writes=[b_tmp32])
            S.op("dve", lambda e, xp=xp, w=w: e.tensor_tensor(out=u_bf[xp][:], in0=tmp32[:], in1=modS[:, w, :], op=ALU.add),
                 reads=[b_tmp32, b_mod], writes=[b_u[xp]])
            for j in range(8):
                S.op("pe", lambda e, xp=xp, j=j: e.transpose(PT[:, j * 128:(j + 1) * 128], u_bf[xp][:, j * 128:(j + 1) * 128], ident_bf[:]),
                     reads=[b_u[xp], b_identbf], writes=[b_PT])
            S.op("act", lambda e, p=p, ti=ti: e.activation(out=uT[p][:, :, ti * 128:(ti + 1) * 128],
                                                          in_=PT[:].rearrange("p (j t) -> p j t", j=8), func=AF.Copy),
                 reads=[b_PT], writes=[b_uT[p][ti]])
            for (c0, c1) in [(0, 512), (512, 1024), (1024, 1032)]:
                pbi = tmi % 4; tmi += 1
                for j in range(8):
                    S.op("pe", lambda e, p=p, ti=ti, j=j, c0=c0, c1=c1, pbi=pbi: e.matmul(
                        PB[pbi][:, 0:c1 - c0], lhsT=uT[p][:, j, ti * 128:(ti + 1) * 128], rhs=Wtm_s[:, j, c0:c1],
                        start=(j == 0), stop=(j == 7)), reads=[b_uT[p][ti], b_wtm], writes=[PBb[pbi]])
                if c0 == 0:
                    S.op("act", lambda e, p=p, ti=ti, pbi=pbi: e.activation(out=TMst[p][:, ti, 512:1024], in_=PB[pbi][:], func=AF.Copy),
                         reads=[PBb[pbi]], writes=[b_TMst[p][ti]])
                elif c0 == 512:
                    S.op("act", lambda e, p=p, ti=ti, pbi=pbi: e.activation(out=OGst[p][:, ti, :], in_=PB[pbi][:], func=AF.Copy),
                         reads=[PBb[pbi]], writes=[b_OGst[p][ti]])
                else:
                    S.op("dve", lambda e, c=c, pbi=pbi: e.tensor_copy(out=Gall[:, :, c], in_=PB[pbi][:, 0:8]),
                         reads=[PBb[pbi]], writes=[b_G])
        def fm_mm(cb, pbi):
            for j in range(8):
                S.op("pe", lambda e, j=j: e.matmul(PB[pbi][:, 0:384], lhsT=Wfm_s[:, j, cb * 128:(cb + 1) * 128], rhs=uT[p][:, j, :],
                                                   start=(j == 0), stop=(j == 7)),
                     reads=[b_wfm] + b_uT[p], writes=[PBb[pbi]])
        for cb in range(4):
            pbi = 4 + fmi % 3; fmi += 1
            fm_mm(cb, pbi)
            sc_ = 1.0 if cb < 2 else RS
            S.op("act", lambda e, cb=cb, pbi=pbi, sc_=sc_: e.activation(
                out=FMst[p][:, :, cb, :], in_=PB[pbi][:, 0:384].rearrange("p (c t) -> p c t", c=3), func=AF.Copy, scale=sc_),
                reads=[PBb[pbi]], writes=[b_FMst[p][cb]])
        for qk in range(2):
            for h in range(2):
                cb_raw = 4 + qk * 4 + h
                cb_sw = 4 + qk * 4 + 2 + h
                pa = 4 + fmi % 3; fmi += 1
                fm_mm(cb_raw, pa)
                pb_ = 4 + fmi % 3; fmi += 1
                fm_mm(cb_sw, pb_)
                sc_ = 1.0 if qk == 0 else RS
                S.op("dve", lambda e, pa=pa, sc_=sc_: e.scalar_tensor_tensor(out=t1[:], in0=PB[pa][:, 0:384], scalar=sc_, in1=rc[p][:],
                                                                            op0=ALU.mult, op1=ALU.mult),
                     reads=[PBb[pa], b_rope[p]], writes=[b_t1])
                S.op("dve", lambda e, pb_=pb_, sc_=sc_: e.scalar_tensor_tensor(out=t2[:], in0=PB[pb_][:, 0:384], scalar=sc_, in1=rsn[p][:],
                                                                              op0=ALU.mult, op1=ALU.mult),
                     reads=[PBb[pb_], b_rope[p]], writes=[b_t2])
                a = 4 + qk * 2 + h
                S.op("dve", lambda e, a=a: e.tensor_tensor(out=FMst[p][:, :, a, :], in0=t1[:].rearrange("p (c t) -> p c t", c=3),
                                                           in1=t2[:].rearrange("p (c t) -> p c t", c=3), op=ALU.add),
                     reads=[b_t1, b_t2], writes=[b_FMst[p][a]])
        for ti in range(3):
            for di, a in enumerate([2, 3, 6, 7]):
                S.op("pe", lambda e, ti=ti, a=a, di=di: e.transpose(PT[:, di * 128:(di + 1) * 128], FMst[p][:, ti, a, :], ident_bf[:]),
                     reads=[b_FMst[p][a], b_identbf], writes=[b_PT])
            S.op("act", lambda e, ti=ti: e.activation(out=TMst[p][:, ti, 0:512], in_=PT[:, 0:512], func=AF.Copy),
                 reads=[b_PT], writes=[b_TMst[p][ti]])
        c0 = g * 3
        S.dma("sp", FM[c0:c0 + 3].rearrange("c p n -> p c n"), FMst[p][:].rearrange("p c a t -> p c (a t)"),
              reads=b_FMst[p], writes=b_FM_d[c0:c0 + 3])
        S.dma("sp", TM[c0:c0 + 3].rearrange("c p n -> p c n"), TMst[p][:], reads=b_TMst[p], writes=b_TM_d[c0:c0 + 3])
        S.dma("sp", OG[c0:c0 + 3].rearrange("c p n -> p c n"), OGst[p][:], reads=b_OGst[p], writes=b_OG_d[c0:c0 + 3])

    phase_end()
    wml = sb("wml", [128, 4, NT]); b_wml = Buf()
    flo = sb("flo", [128, 4, NT]); b_flo = Buf()
    decb = sb("decb", [128, 4, NT]); b_decb = Buf()
    wret = sb("wret", [128, 4]); rho = sb("rho", [128, 4]); dret = sb("dret", [128, 4]); b_retc = Buf()
    phase_begin("L1_G")
    gb_s = sb("gb_s", [128, 8]); b_gb = Buf()
    S.dma("sp", gb_s[:], gbias.partition_broadcast(128), writes=[b_gb])
    Gi = sb("Gi", [128, 4, NT]); b_Gi = Buf()
    nlf = sb("nlf", [128, 4, NT]); b_nlf = Buf()
    for k in range(4):
        S.op("dve", lambda e, k=k: e.tensor_scalar(out=Gi[:, k, :], in0=Gall[:, k, :], scalar1=gb_s[:, k:k + 1], scalar2=None, op0=ALU.add),
             reads=[b_G, b_gb], writes=[b_Gi])
        S.op("dve", lambda e, k=k: e.tensor_scalar(out=nlf[:, k, :], in0=Gall[:, 4 + k, :], scalar1=gb_s[:, 4 + k:5 + k], scalar2=None, op0=ALU.add),
             reads=[b_G, b_gb], writes=[b_nlf])
    S.op("act", lambda e: e.activation(out=nlf[:], in_=nlf[:], func=AF.Exp, scale=-1.0), reads=[b_nlf], writes=[b_nlf])
    S.op("act", lambda e: e.activation(out=nlf[:], in_=nlf[:], func=AF.Ln, bias=1.0), reads=[b_nlf], writes=[b_nlf])
    nb = sb("nb", [128, 4, NT]); b_nb = Buf()
    nbL = sb("nbL", [128, 4, NT]); b_nbL = Buf()
    for d in range(2):
        S.op("pe", lambda e, d=d: e.matmul(PB[d][:, 0:2 * NT], lhsT=mask_f32[:, d, :], rhs=nlf[:, 2 * d:2 * d + 2, :].rearrange("p a c -> p (a c)"),
                                           start=True, stop=True), reads=[b_maskf, b_nlf], writes=[PBb[d]])
        S.op("dve", lambda e, d=d: e.tensor_copy(out=nb[:, 2 * d:2 * d + 2, :].rearrange("p a c -> p (a c)"), in_=PB[d][:, 0:2 * NT]),
             reads=[PBb[d]], writes=[b_nb])
    S.op("pe", lambda e: e.matmul(PB[2][:, 0:4 * NT], lhsT=ones_f[:], rhs=nlf[:].rearrange("p a c -> p (a c)"), start=True, stop=True),
         reads=[b_ones, b_nlf], writes=[PBb[2]])
    S.op("dve", lambda e: e.tensor_copy(out=nbL[:].rearrange("p a c -> p (a c)"), in_=PB[2][:, 0:4 * NT]), reads=[PBb[2]], writes=[b_nbL])
    av = sb("av", [128, 4, NT]); b_av = Buf()
    S.op("dve", lambda e: e.tensor_tensor(out=av[:], in0=Gi[:], in1=nb[:], op=ALU.add), reads=[b_Gi, b_nb], writes=[b_av])
    avf = av[:].rearrange("p a c -> p (a c)")
    Acol = sb("Acol", [128, 3]); b_Acol = Buf()
    S.op("dve", lambda e: e.memset(Acol[:], 0.0), writes=[b_Acol])
    pieces = [(0, 128), (128, 256), (256, 264)]
    for pi, (a0, a1) in enumerate(pieces):
        m = a1 - a0
        S.op("pe", lambda e, a0=a0, a1=a1, m=m, pi=pi: e.matmul(PB[3 + pi][0:m, 0:128], lhsT=avf[:, a0:a1], rhs=ident_f[:], start=True, stop=True),
             reads=[b_av, b_identf], writes=[PBb[3 + pi]])
        S.op("dve", lambda e, m=m, pi=pi: e.tensor_reduce(out=Acol[0:m, pi:pi + 1], in_=PB[3 + pi][0:m, 0:128], axis=AX.X, op=ALU.max),
             reads=[PBb[3 + pi]], writes=[b_Acol])
    Arow = sb("Arow", [1, 4, NT]); b_Arow = Buf()
    for pi, (a0, a1) in enumerate(pieces):
        m = a1 - a0
        S.op("pe", lambda e, a0=a0, a1=a1, m=m, pi=pi: e.matmul(PB[6][0:1, a0:a1], lhsT=Acol[0:128, pi:pi + 1], rhs=ident_f[:, 0:m],
                                                                start=True, stop=True), reads=[b_Acol, b_identf], writes=[PBb[6]])
    S.op("dve", lambda e: e.tensor_copy(out=Arow[:].rearrange("p a c -> p (a c)"), in_=PB[6][0:1, 0:4 * NT]), reads=[PBb[6]], writes=[b_Arow])
    MLrow = sb("MLrow", [1, 4, NT]); b_MLrow = Buf()
    dargrow = sb("dargrow", [1, 4, NT]); b_darg = Buf()
    mstate = sb("mstate", [1, 4]); b_ms = [Buf(), Buf()]
    b_MLd = [Buf(), Buf()]; b_dargd = [Buf(), Buf()]
    S.op("dve", lambda e: e.memset(mstate[:], 0.0), writes=b_ms)
    order = [list(range(NT)), [1, 0] + list(range(NT - 1, 1, -1))]
    engs = ["dve", "dve"]
    for i in range(NT):
        for d in range(2):
            c = order[d][i]
            en = engs[d]
            sl = slice(2 * d, 2 * d + 2)
            S.op(en, lambda e, c=c, sl=sl: e.tensor_tensor(out=MLrow[:, sl, c], in0=mstate[:, sl], in1=Arow[:, sl, c], op=ALU.max),
                 reads=[b_ms[d], b_Arow], writes=[b_MLd[d]])
            S.op(en, lambda e, c=c, sl=sl: e.tensor_tensor(out=dargrow[:, sl, c], in0=mstate[:, sl], in1=MLrow[:, sl, c], op=ALU.subtract),
                 reads=[b_ms[d], b_MLd[d]], writes=[b_dargd[d]])
            S.op(en, lambda e, c=c, sl=sl: e.tensor_tensor(out=mstate[:, sl], in0=MLrow[:, sl, c], in1=nbL[0:1, sl, c], op=ALU.subtract),
                 reads=[b_nbL, b_MLd[d]], writes=[b_ms[d]])
    MLb = sb("MLb", [128, 4, NT]); b_MLb = Buf()
    S.op("pe", lambda e: e.matmul(PB[0][:, 0:4 * NT], lhsT=ones_f[0:1, :], rhs=MLrow[:].rearrange("p a c -> p (a c)"), start=True, stop=True),
         reads=[b_ones] + b_MLd + b_dargd, writes=[PBb[0]])
    S.op("dve", lambda e: e.tensor_copy(out=MLb[:].rearrange("p a c -> p (a c)"), in_=PB[0][:, 0:4 * NT]), reads=[PBb[0]], writes=[b_MLb])
    S.op("pe", lambda e: e.matmul(PB[1][:, 0:4 * NT], lhsT=ones_f[0:1, :], rhs=dargrow[:].rearrange("p a c -> p (a c)"), start=True, stop=True),
         reads=[b_ones] + b_MLd + b_dargd, writes=[PBb[1]])
    S.op("act", lambda e: e.activation(out=decb[:].rearrange("p a c -> p (a c)"), in_=PB[1][:, 0:4 * NT], func=AF.Exp), reads=[PBb[1]], writes=[b_decb])
    S.op("dve", lambda e: e.tensor_tensor(out=wml[:], in0=av[:], in1=MLb[:], op=ALU.subtract), reads=[b_av, b_MLb], writes=[b_wml])
    S.op("act", lambda e: e.activation(out=wml[:], in_=wml[:], func=AF.Exp), reads=[b_wml], writes=[b_wml])
    S.op("dve", lambda e: e.tensor_tensor(out=flo[:], in0=nb[:], in1=MLb[:], op=ALU.subtract), reads=[b_nb, b_MLb], writes=[b_flo])
    S.op("act", lambda e: e.activation(out=flo[:], in_=flo[:], func=AF.Exp), reads=[b_flo], writes=[b_flo])
    lg = sb("lg", [128, 4]); b_lg = Buf()
    S.dma("sp", lg[:], rld.partition_broadcast(128), writes=[b_lg])
    for d in range(2):
        for h in range(2):
            x = 2 * d + h
            S.op("act", lambda e, x=x, d=d: e.activation(out=wret[:, x:x + 1], in_=poscol[:, d:d + 1], func=AF.Exp, scale=lg[:, x:x + 1]),
                 reads=[b_poscol, b_lg], writes=[b_retc])
            S.op("act", lambda e, x=x, d=d: e.activation(out=rho[:, x:x + 1], in_=poscol[:, 2 + d:3 + d], func=AF.Exp, scale=lg[:, x:x + 1]),
                 reads=[b_poscol, b_lg], writes=[b_retc])
    S.op("act", lambda e: e.activation(out=dret[:], in_=lg[:], func=AF.Exp, scale=128.0), reads=[b_lg], writes=[b_retc])

    phase_end()
    phase_begin("L1_S")
    FMl = [[sb("FMl%d%d" % (d, i), [128, 8, 128], BF16) for i in range(2)] for d in range(2)]
    TMl = [[sb("TMl%d%d" % (d, i), [128, 8, 128], BF16) for i in range(2)] for d in range(2)]
    b_FMl = [[Buf() for _ in range(2)] for _ in range(2)]
    b_TMl = [[Buf() for _ in range(2)] for _ in range(2)]
    va = [sb("va%d" % i, [128, 132], BF16) for i in range(4)]; b_va = [Buf() for _ in range(4)]
    sm = [sb("sm%d" % i, [128, 128], BF16) for i in range(4)]; b_sm = [Buf() for _ in range(4)]
    CT32 = sb("CT32", [128, 8, 132]); CTd32 = sb("CTd32", [128, 8, 132]); CTbf = sb("CTbf", [128, 8, 132], BF16)
    b_CT32 = [Buf() for _ in range(8)]; b_CTd = [Buf() for _ in range(8)]; b_CTbf = [Buf() for _ in range(8)]
    S.op("dve", lambda e: e.memset(CTd32[:], 0.0), writes=b_CTd)
    S.op("dve", lambda e: e.memset(CTbf[:], 0.0), writes=b_CTbf)
    HSst = [[sb("HSst%d%d" % (d, i), [128, 512]) for i in range(2)] for d in range(2)]
    b_HSst = [[Buf() for _ in range(2)] for _ in range(2)]
    den = sb("den", [128, 4]); b_den = [Buf() for _ in range(4)]
    b_HS_d = [[Buf() for _ in range(NT)] for _ in range(2)]
    u = 0
    for i in range(NT):
        for d in range(2):
            c = order[d][i]
            cn = order[d][i + 1] if i + 1 < NT else c
            lp = i % 2
            S.dma("sp", FMl[d][lp][:].rearrange("p a t -> p (a t)"), FM[c], reads=[b_FM_d[c]], writes=[b_FMl[d][lp]])
            S.dma("sp", TMl[d][lp][:].rearrange("p a t -> p (a t)"), TM[c], reads=[b_TM_d[c]], writes=[b_TMl[d][lp]])
            hp = i % 2
            for typ in range(2):
                for h in range(2):
                    ch = typ * 4 + h * 2 + d
                    x = 2 * d + h
                    qT = FMl[d][lp][:, typ * 4 + h, :]
                    kT = FMl[d][lp][:, typ * 4 + 2 + h, :]
                    kk = TMl[d][lp][:, typ * 2 + h, :]
                    vv = TMl[d][lp][:, 4 + typ * 2 + h, :]
                    vi = u % 4
                    pS = PB[u % 2]; bS = PBb[u % 2]
                    pO = PB[2 + u % 2]; bO = PBb[2 + u % 2]
                    pC = PB[4 + u % 2]; bC = PBb[4 + u % 2]
                    u += 1
                    if typ == 0:
                        wcol = wml[:, x, c:c + 1]; wb_ = b_wml
                        dn = decb[:, x, cn:cn + 1]; db_ = b_decb
                    else:
                        wcol = wret[:, x:x + 1]; wb_ = b_retc
                        dn = dret[:, x:x + 1]; db_ = b_retc
                    S.op("dve", lambda e, vi=vi, vv=vv, wcol=wcol: e.tensor_scalar(out=va[vi][:, 0:128], in0=vv, scalar1=wcol, scalar2=None, op0=ALU.mult),
                         reads=[b_TMl[d][lp], wb_], writes=[b_va[vi]])
                    S.op("dve", lambda e, vi=vi, wcol=wcol: e.tensor_copy(out=va[vi][:, 128:129], in_=wcol), reads=[wb_], writes=[b_va[vi]])
                    S.op("pe", lambda e, pS=pS, kT=kT, qT=qT: e.matmul(pS[:, 0:128], lhsT=kT, rhs=qT, start=True, stop=True),
                         reads=[b_FMl[d][lp]], writes=[bS])
                    S.op("dve", lambda e, vi=vi, pS=pS, d=d: e.tensor_tensor(out=sm[vi][:], in0=pS[:, 0:128], in1=mask_bf[:, d, :], op=ALU.mult),
                         reads=[bS, b_maskbf], writes=[b_sm[vi]])
                    S.op("pe", lambda e, pO=pO, vi=vi: e.matmul(pO[:, 0:129], lhsT=sm[vi][:], rhs=va[vi][:, 0:129], start=True, stop=False),
                         reads=[b_sm[vi], b_va[vi]], writes=[bO])
                    S.op("pe", lambda e, pO=pO, qT=qT, ch=ch: e.matmul(pO[:, 0:129], lhsT=qT, rhs=CTbf[:, ch, 0:129], start=False, stop=True),
                         reads=[b_FMl[d][lp], b_CTbf[ch]], writes=[bO])
                    S.op("pe", lambda e, pC=pC, kk=kk, vi=vi: e.matmul(pC[:, 0:129], lhsT=kk, rhs=va[vi][:, 0:129], start=True, stop=True),
                         reads=[b_TMl[d][lp], b_va[vi]], writes=[bC])
                    S.op("dve", lambda e, pC=pC, ch=ch: e.tensor_tensor(out=CT32[:, ch, 0:129], in0=pC[:, 0:129], in1=CTd32[:, ch, 0:129], op=ALU.add),
                         reads=[bC, b_CTd[ch]], writes=[b_CT32[ch]])
                    S.op("act", lambda e, ch=ch, dn=dn: e.activation(out=CTd32[:, ch, 0:129], in_=CT32[:, ch, 0:129], func=AF.Copy, scale=dn),
                         reads=[b_CT32[ch], db_], writes=[b_CTd[ch]])
                    S.op("act", lambda e, ch=ch, dn=dn: e.activation(out=CTbf[:, ch, 0:129], in_=CT32[:, ch, 0:129], func=AF.Copy, scale=dn),
                         reads=[b_CT32[ch], db_], writes=[b_CTbf[ch]])
                    oc = (typ * 2 + h) * 128
                    if typ == 0:
                        S.op("act", lambda e, pO=pO, vi=vi: e.activation(out=den[:, vi:vi + 1], in_=pO[:, 128:129], func=AF.Abs),
                             reads=[bO], writes=[b_den[vi]])
                        S.op("dve", lambda e, vi=vi, x=x, c=c: e.tensor_scalar(out=den[:, vi:vi + 1], in0=den[:, vi:vi + 1], scalar1=flo[:, x, c:c + 1],
                                                                               scalar2=None, op0=ALU.max),
                             reads=[b_den[vi], b_flo], writes=[b_den[vi]])
                        S.op("dve", lambda e, vi=vi: e.reciprocal(out=den[:, vi:vi + 1], in_=den[:, vi:vi + 1]), reads=[b_den[vi]], writes=[b_den[vi]])
                        S.op("act", lambda e, pO=pO, vi=vi, oc=oc: e.activation(out=HSst[d][hp][:, oc:oc + 128], in_=pO[:, 0:128], func=AF.Copy,
                                                                               scale=den[:, vi:vi + 1]),
                             reads=[bO, b_den[vi]], writes=[b_HSst[d][hp]])
                    else:
                        S.op("act", lambda e, pO=pO, x=x, oc=oc: e.activation(out=HSst[d][hp][:, oc:oc + 128], in_=pO[:, 0:128], func=AF.Copy,
                                                                             scale=rho[:, x:x + 1]),
                             reads=[bO, b_retc], writes=[b_HSst[d][hp]])
            S.dma("sp", HS[d, c], HSst[d][hp][:], reads=[b_HSst[d][hp]], writes=[b_HS_d[d][c]])

    phase_end()
    phase_begin("L1_M")
    gml = sb("gml", [128, 256]); gret = sb("gret", [128, 256]); b_gm = Buf()
    S.dma("sp", gml[:], mlg.partition_broadcast(128), writes=[b_gm])
    S.dma("sp", gret[:], retg.partition_broadcast(128), writes=[b_gm])
    h0 = [sb("h0_%d" % i, [128, 512]) for i in range(2)]; h1 = [sb("h1_%d" % i, [128, 512]) for i in range(2)]
    og = [sb("og%d" % i, [128, 512], BF16) for i in range(2)]
    b_h0 = [Buf(), Buf()]; b_h1 = [Buf(), Buf()]; b_og = [Buf(), Buf()]
    hz = sb("hz", [128, 512]); b_hz = Buf()
    sg = sb("sg", [128, 512]); b_sg = Buf()
    st6 = sb("st6", [128, 4, 6]); mv2 = sb("mv2", [128, 4, 2]); b_st = Buf(); b_mv = Buf()
    rs4 = sb("rs4", [128, 4]); b_rs4 = Buf()
    mo_st = [sb("mo_st%d" % i, [128, 512], BF16) for i in range(2)]; b_most = [Buf(), Buf()]
    b_out = Buf()
    for c in range(NT):
        p = c % 2
        S.dma("sp", h0[p][:], HS[0, c], reads=[b_HS_d[0][c]], writes=[b_h0[p]])
        S.dma("sp", h1[p][:], HS[1, c], reads=[b_HS_d[1][c]], writes=[b_h1[p]])
        S.dma("sp", og[p][:], OG[c], reads=[b_OG_d[c]], writes=[b_og[p]])
        S.op("dve", lambda e, p=p: e.tensor_tensor(out=hz[:], in0=h0[p][:], in1=h1[p][:], op=ALU.add), reads=[b_h0[p], b_h1[p]], writes=[b_hz])
        S.op("act", lambda e, p=p: e.activation(out=sg[:, 0:256], in_=og[p][:, 0:256], func=AF.Sigmoid), reads=[b_og[p]], writes=[b_sg])
        S.op("act", lambda e, p=p: e.activation(out=sg[:, 256:512], in_=og[p][:, 256:512], func=AF.Silu), reads=[b_og[p]], writes=[b_sg])
        S.op("dve", lambda e: e.tensor_tensor(out=hz[:, 0:256], in0=hz[:, 0:256], in1=sg[:, 0:256], op=ALU.mult), reads=[b_hz, b_sg], writes=[b_hz])
        for k in range(4):
            S.op("dve", lambda e, k=k: e.bn_stats(out=st6[:, k, :], in_=hz[:, k * 128:(k + 1) * 128]), reads=[b_hz], writes=[b_st])
            S.op("dve", lambda e, k=k: e.bn_aggr(out=mv2[:, k, :], in_=st6[:, k, :]), reads=[b_st], writes=[b_mv])
        S.op("dve", lambda e: e.tensor_scalar(out=rs4[:], in0=mv2[:, :, 1], scalar1=EPS, scalar2=None, op0=ALU.add),
             reads=[b_mv], writes=[b_rs4])
        S.op("act", lambda e: e.activation(out=rs4[:], in_=rs4[:], func=AF.Sqrt), reads=[b_rs4], writes=[b_rs4])
        S.op("dve", lambda e: e.reciprocal(out=rs4[:], in_=rs4[:]), reads=[b_rs4], writes=[b_rs4])
        for k in range(4):
            S.op("dve", lambda e, k=k: e.tensor_scalar(out=hz[:, k * 128:(k + 1) * 128], in0=hz[:, k * 128:(k + 1) * 128],
                                                      scalar1=mv2[:, k, 0:1], scalar2=rs4[:, k:k + 1], op0=ALU.subtract, op1=ALU.mult),
                 reads=[b_hz, b_mv, b_rs4], writes=[b_hz])
        S.op("dve", lambda e, p=p: e.tensor_tensor(out=mo_st[p][:, 0:256], in0=hz[:, 0:256], in1=gml[:], op=ALU.mult),
             reads=[b_hz, b_gm], writes=[b_most[p]])
        S.op("dve", lambda e: e.tensor_tensor(out=hz[:, 256:512], in0=hz[:, 256:512], in1=gret[:], op=ALU.mult), reads=[b_hz, b_gm], writes=[b_hz])
        S.op("dve", lambda e, p=p: e.tensor_tensor(out=mo_st[p][:, 256:512], in0=hz[:, 256:512], in1=sg[:, 256:512], op=ALU.mult),
             reads=[b_hz, b_sg], writes=[b_most[p]])
        S.dma("sp", merged[c * 128:(c + 1) * 128, :], mo_st[p][:], reads=[b_most[p]], writes=[b_out])
    phase_end()
    phase_end()
    return b_out


def l1_inputs(I, b, g, consts):
    d = dict(consts)
    d["xin"] = np.ascontiguousarray(np.concatenate([I["ctx"][b], I["x"][b]], 0))
    d["cT"] = np.ascontiguousarray(I["c"][b].reshape(8, 128).T)
    d["cctxT"] = np.ascontiguousarray(I["c_ctx"].reshape(8, 128).T)
    d["wmod"] = np.ascontiguousarray(I["w_mod"][0][:, 0:2048])
    d["bmod"] = np.ascontiguousarray(I["b_mod"][0][None, 0:2048])
    d["g1"] = np.ascontiguousarray(I["norm1_g"][0][None, :])
    W = I["w_in_even"][0]
    sp = np.cumsum([0, 512, 512, 512, 512, 8, 8, 512, 512, 512, 512])
    mq, mk, mv, mo, mi, mf, rq, rk, rv, rg = [W[:, sp[i]:sp[i + 1]] for i in range(10)]
    hs = [2 * g, 2 * g + 1]
    def hcols(M, h): return M[:, h * 128:(h + 1) * 128]
    def swp(M): return np.concatenate([M[:, 64:128], M[:, 0:64]], 1)
    fm = [hcols(mq, h) for h in hs] + [hcols(mk, h) for h in hs] + [hcols(rq, h) for h in hs] + [swp(hcols(rq, h)) for h in hs] \
        + [hcols(rk, h) for h in hs] + [swp(hcols(rk, h)) for h in hs]
    d["Wfm"] = np.ascontiguousarray(np.concatenate(fm, 1))
    gi = [mi[:, dd * 4 + h:dd * 4 + h + 1] for dd in range(2) for h in hs]
    gf = [mf[:, dd * 4 + h:dd * 4 + h + 1] for dd in range(2) for h in hs]
    tm = [hcols(mv, h) for h in hs] + [hcols(rv, h) for h in hs] + [hcols(mo, h) for h in hs] + [hcols(rg, h) for h in hs] + gi + gf
    d["Wtm"] = np.ascontiguousarray(np.concatenate(tm, 1))
    gb = I["ml_gate_b"][0]
    d["gbias"] = np.array([[gb[dd, 0, h] for dd in range(2) for h in hs] + [gb[dd, 1, h] for dd in range(2) for h in hs]], np.float32)
    d["rld"] = np.array([[I["ret_log_decay"][0][dd, h] for dd in range(2) for h in hs]], np.float32)
    d["mlg"] = np.ascontiguousarray(I["ml_norm_g"][0][hs].reshape(1, 256))
    d["retg"] = np.ascontiguousarray(I["ret_norm_g"][0][hs].reshape(1, 256))
    return d


NT2 = 33
GROUPS2 = [[0]] + [[1 + 4 * g + i for i in range(4)] for g in range(8)]


def host_consts_l2(half):
    c = common_consts()
    T = NT2 * 128
    inv = (10000.0 ** (-np.arange(16, dtype=np.float32) / 16)).astype(np.float32)
    t = np.arange(half * 4096, (half + 1) * 4096)
    rows = (t // 64).astype(np.float32); cols = (t % 64).astype(np.float32)
    ar = (rows[:, None] * inv[None, :]).astype(np.float32)
    ac = (cols[:, None] * inv[None, :]).astype(np.float32)
    cos64 = np.concatenate([np.cos(ar), np.cos(ar), np.cos(ac), np.cos(ac)], 1).astype(np.float32)
    sin64 = np.concatenate([-np.sin(ar), np.sin(ar), -np.sin(ac), np.sin(ac)], 1).astype(np.float32)
    cosT = np.ones((128, T), np.float32); sinT = np.zeros((128, T), np.float32)
    cosT[:, 128:] = np.concatenate([cos64, cos64], 1).T
    sinT[:, 128:] = np.concatenate([sin64, sin64], 1).T
    c["rc2"] = np.ascontiguousarray(cosT); c["rs2"] = np.ascontiguousarray(sinT)
    return c


def build_l2(cx, io, debug=None):
    nc = cx.nc
    S = cx.S
    cx.phase_begin("L2")
    xin = io["xin"]
    MG = io["MG"]; b_MG = io["b_MG"]
    idxm_d = io["idxm"]
    cT = io["cT"]; cctxT = io["cctxT"]
    wmod = io["wmod"]; bmod = io["bmod"]
    g2_0 = io["g2_0"]; g1_1 = io["g1_1"]
    w_out = io["w_out"]
    Wr = io["Wr"]; br = io["br"]
    w1 = io["w1"]; w3 = io["w3"]; w2 = io["w2"]
    Wq = io["Wq"]; Wk = io["Wk"]; Wv = io["Wv"]
    rc2_d = io["rc2"]; rs2_d = io["rs2"]
    hout = io["H2"]
    QT = io["QTs"]
    KT = io["KTo"]
    Vo = io["Vown"]
    H1 = cx.dscr("H1", [NT2, 128, D]); b_H1 = [Buf() for _ in range(NT2)]
    Vs = cx.dscr("Vs", [NT2, 128, D], BF16); b_Vs = [Buf() for _ in range(NT2)]
    UT = cx.dscr("UT", [NT2, 128, 8, 128], BF16); b_UT = [Buf() for _ in range(NT2)]

    def lc(name, shape, dt=F32):
        t = cx.sb(name + "_2s", shape, dt); b = Buf()
        S.dma("sp", t[:], io[name], writes=[b])
        return t, b
    idxm, b_idxm = lc("idxm", [128, NT2, 2], I32)
    ident_bf = lc("ident_bf", [128, 128], BF16)
    ident_f = lc("ident_f", [128, 128])
    ones_f = lc("ones_f", [128, 128])
    slt = lc("slt", [128, 128])
    blkstart = lc("blkstart", [128, 1])
    pcol = lc("pcol", [128, 1])
    thr = lc("thr", [128, 34])
    consts = {"thr": thr, "ident_bf": ident_bf, "ident_f": ident_f, "ones_f": ones_f, "slt": slt, "blkstart": blkstart, "pcol": pcol}
    moe = io["moe"]
    moe.c = consts
    moe.alloc_persistent()

    mods = cx.sb("mods", [128, 2, 6, D]); b_mods = Buf()
    cx.phase_begin("L2_mod")
    scb, b_scb = emit_silu_bcast(cx, [cT, cctxT], ones_f[0], ones_f[1])
    bmod_s, b_bmod = cx.load_bcast("bmod2", 6144, bmod)
    g2_s, b_g2 = cx.load_bcast("g2_0", D, g2_0)
    g1n_s, b_g1n = cx.load_bcast("g1_1", D, g1_1)
    wm_s = [cx.sb("wm_s%d" % i, [128, 8, 512]) for i in range(2)]; b_wm = [Buf(), Buf()]
    emit_mod(cx, scb, b_scb, 2, wmod, bmod_s, b_bmod, 6144,
             lambda w, cc: mods[:, w, cc // 2, (cc % 2) * 512:(cc % 2 + 1) * 512], b_mods, wm_s, b_wm)
    for w in range(2):
        for slot, gt, bg in [(2, g2_s, b_g2), (5, g1n_s, b_g1n)]:
            S.op("dve", lambda e, w=w, slot=slot, gt=gt: e.scalar_tensor_tensor(out=mods[:, w, slot, :], in0=mods[:, w, slot, :], scalar=1.0,
                                                                               in1=gt[:], op0=ALU.add, op1=ALU.mult),
                 reads=[b_mods, bg], writes=[b_mods])
    cx.phase_end()

    cx.phase_begin("L2_B")
    wo_s = cx.sb("wo_s", [128, 8, D], BF16); b_wo = Buf()
    for j in range(8):
        S.dma("pool", wo_s[:, j, :], w_out[j * 128:(j + 1) * 128, :], writes=[b_wo])
    nb = NormBufs(cx)
    ht = [cx.sb("ht%d" % i, [128, D]) for i in range(2)]; b_ht = [Buf(), Buf()]
    mt = [cx.sb("mt%d" % i, [128, D], BF16) for i in range(2)]; b_mt = [Buf(), Buf()]
    mT = [cx.sb("mT%d" % i, [128, 8, 128], BF16) for i in range(2)]; b_mT = [Buf(), Buf()]
    vt = [cx.sb("vt%d" % i, [128, D], BF16) for i in range(2)]; b_vt = [Buf(), Buf()]
    vT = [cx.sb("vT%d" % i, [128, 8, 128], BF16) for i in range(2)]; b_vT = [Buf(), Buf()]
    ytmp = cx.sb("ytmp", [128, D]); b_ytmp = Buf()
    for ti in range(NT2):
        p = ti % 2
        w = 1 if ti == 0 else 0
        S.dma("sp", ht[p][:], xin[ti * 128:(ti + 1) * 128, :], writes=[b_ht[p]])
        for r in range(2):
            S.dma("pool", mt[p][:, r * 512:(r + 1) * 512], MG, reads=[b_MG, b_idxm], writes=[b_mt[p]],
                  indirect={"in_offset": bass.IndirectOffsetOnAxis(ap=idxm[:, ti, r:r + 1], axis=0)})
        cmap = [(0, 0), (0, 1), (1, 0), (1, 1), (0, 2), (0, 3), (1, 2), (1, 3)]
        emit_transpose(cx, lambda j, p=p: mt[p][:, cmap[j][0] * 512 + cmap[j][1] * 128: cmap[j][0] * 512 + (cmap[j][1] + 1) * 128],
                       b_mt[p], 8, ident_bf[0], ident_bf[1], mT[p][:], b_mT[p])
        for half in range(2):
            pc, bc = cx.PB[half], cx.PBb[half]
            for j in range(8):
                S.op("pe", lambda e, j=j, p=p, pc=pc, half=half: e.matmul(pc[:], lhsT=mT[p][:, j, :], rhs=wo_s[:, j, half * 512:(half + 1) * 512],
                                                                          start=(j == 0), stop=(j == 7)),
                     reads=[b_mT[p], b_wo], writes=[bc])
            sl = slice(half * 512, (half + 1) * 512)
            S.op("dve", lambda e, pc=pc, w=w, sl=sl: e.tensor_tensor(out=ytmp[:, sl], in0=pc[:], in1=mods[:, w, 0, sl], op=ALU.mult),
                 reads=[bc, b_mods], writes=[b_ytmp])
            S.op("dve", lambda e, p=p, sl=sl: e.tensor_tensor(out=ht[p][:, sl], in0=ht[p][:, sl], in1=ytmp[:, sl], op=ALU.add),
                 reads=[b_ytmp, b_ht[p]], writes=[b_ht[p]])
        S.dma("sp", H1[ti], ht[p][:], reads=[b_ht[p]], writes=[b_H1[ti]])
        emit_adaln(cx, nb, ht[p][:], b_ht[p], mods[:, w, 2, :], mods[:, w, 1, :], b_mods, vt[p][:], b_vt[p])
        S.dma("sp", Vs[ti], vt[p][:], reads=[b_vt[p]], writes=[b_Vs[ti]])
        emit_transpose(cx, lambda j, p=p: vt[p][:, j * 128:(j + 1) * 128], b_vt[p], 8, ident_bf[0], ident_bf[1], vT[p][:], b_vT[p])
        moe.route_tile(ti, lambda j, p=p: vT[p][:, j, :], b_vT[p])
    cx.phase_end()

    cx.phase_begin("L2_C")
    moe.plan()
    vl = [cx.sb("vl%d" % i, [128, D], BF16) for i in range(2)]; b_vl = [Buf(), Buf()]
    for ti in range(NT2):
        p = ti % 2
        S.dma("sp", vl[p][:], Vs[ti], reads=[b_Vs[ti]], writes=[b_vl[p]])
        moe.dispatch_tile(ti, vl[p][:], b_vl[p], do_scatter=(debug != "plan"))
    if debug == "plan":
        dbg_dest = cx.dout("dbg_dest", [128, NT2 * 2], I32)
        dbg_idxw = cx.dout("dbg_idxw", [128, 128], I32)
        dbg_gate = cx.dout("dbg_gate", [128, NT2 * 2])
        dbg_oh = cx.dout("dbg_oh", [128, NT2 * 64])
        b_dbg = Buf()
        S.dma("sp", dbg_dest, moe.dest[:].rearrange("p t k -> p (t k)"), reads=moe.b_dest, writes=[b_dbg])
        S.dma("sp", dbg_idxw, moe.idxw[:], reads=[moe.b_idxw], writes=[b_dbg])
        S.dma("sp", dbg_gate, moe.gate[:].rearrange("p t k -> p (t k)"), reads=moe.b_gate, writes=[b_dbg])
        S.dma("sp", dbg_oh, moe.OH[:].rearrange("p t k e -> p (t k e)"), reads=moe.b_OH, writes=[b_dbg])
        cx.phase_end()
        return None
    moe.experts()
    cx.phase_end()

    cx.phase_begin("L2_D1")
    nb = NormBufs(cx, "d")
    h1t = [cx.sb("h1t%d" % i, [128, D]) for i in range(2)]; b_h1t = [Buf(), Buf()]
    y0 = [cx.sb("y0_%d" % i, [128, D]) for i in range(2)]; b_y0 = [Buf(), Buf()]
    y1 = [cx.sb("y1_%d" % i, [128, D]) for i in range(2)]; b_y1 = [Buf(), Buf()]
    ut = [cx.sb("ut%d" % i, [128, D], BF16) for i in range(2)]; b_ut = [Buf(), Buf()]
    uTt = [cx.sb("uTt%d" % i, [128, 8, 128], BF16) for i in range(2)]; b_uTt = [Buf(), Buf()]
    b_hout = Buf()
    for ti in range(NT2):
        p = ti % 2
        w = 1 if ti == 0 else 0
        S.dma("sp", h1t[p][:], H1[ti], reads=[b_H1[ti]], writes=[b_h1t[p]])
        moe.gather_tile(ti, y0[p][:], y1[p][:], b_y0[p], b_y1[p])
        S.op("dve", lambda e, p=p, ti=ti: e.tensor_scalar(out=y0[p][:], in0=y0[p][:], scalar1=moe.gate[:, ti, 0:1], scalar2=None, op0=ALU.mult),
             reads=[b_y0[p], moe.b_gate[ti]], writes=[b_y0[p]])
        S.op("dve", lambda e, p=p, ti=ti: e.scalar_tensor_tensor(out=y0[p][:], in0=y1[p][:], scalar=moe.gate[:, ti, 1:2], in1=y0[p][:],
                                                                op0=ALU.mult, op1=ALU.add),
             reads=[b_y0[p], b_y1[p], moe.b_gate[ti]], writes=[b_y0[p]])
        S.op("dve", lambda e, p=p, w=w: e.tensor_tensor(out=y0[p][:], in0=y0[p][:], in1=mods[:, w, 3, :], op=ALU.mult),
             reads=[b_y0[p], b_mods], writes=[b_y0[p]])
        S.op("dve", lambda e, p=p: e.tensor_tensor(out=h1t[p][:], in0=h1t[p][:], in1=y0[p][:], op=ALU.add),
             reads=[b_y0[p], b_h1t[p]], writes=[b_h1t[p]])
        S.dma("sp", hout[ti * 128:(ti + 1) * 128, :], h1t[p][:], reads=[b_h1t[p]], writes=[b_hout])
        emit_adaln(cx, nb, h1t[p][:], b_h1t[p], mods[:, w, 5, :], mods[:, w, 4, :], b_mods, ut[p][:], b_ut[p])
        emit_transpose(cx, lambda j, p=p: ut[p][:, j * 128:(j + 1) * 128], b_ut[p], 8, ident_bf[0], ident_bf[1], uTt[p][:], b_uTt[p])
        S.dma("sp", UT[ti], uTt[p][:], reads=[b_uTt[p]], writes=[b_UT[ti]])
    cx.phase_end()

    cx.phase_begin("L2_D2")
    Wq_s = cx.sb("Wq_s", [128, 8, 2048], BF16); Wk_s = cx.sb("Wk_s", [128, 8, 2048], BF16); Wv_s = cx.sb("Wv_s", [128, 8, D], BF16)
    b_Wq = Buf(); b_Wk = Buf(); b_Wv = Buf()
    for j in range(8):
        S.dma("pool", Wq_s[:, j, :], Wq[j * 128:(j + 1) * 128, :], writes=[b_Wq])
        S.dma("pool", Wk_s[:, j, :], Wk[j * 128:(j + 1) * 128, :], writes=[b_Wk])
        S.dma("pool", Wv_s[:, j, :], Wv[j * 128:(j + 1) * 128, :], writes=[b_Wv])
    uTg = [cx.sb("uTg%d" % i, [128, 4, 8, 128], BF16) for i in range(2)]; b_uTg = [Buf(), Buf()]
    rc = [cx.sb("rc%d" % i, [128, 512]) for i in range(2)]; rsn = [cx.sb("rsn%d" % i, [128, 512]) for i in range(2)]; b_rope = [Buf(), Buf()]
    t1 = cx.sb("t1", [128, 512]); t2 = cx.sb("t2", [128, 512]); b_t1 = Buf(); b_t2 = Buf()
    qkst = [cx.sb("qkst%d" % i, [128, 512], BF16) for i in range(4)]; b_qkst = [Buf() for _ in range(4)]
    vst = [cx.sb("vst%d" % i, [128, D], BF16) for i in range(2)]; b_vst = [Buf(), Buf()]
    b_QT = Buf(); b_KT = Buf(); b_Vo = Buf()
    si = 0; vi = 0; fi = 0
    for gi, tiles in enumerate(GROUPS2):
        p = gi % 2
        n = len(tiles) * 128
        t0 = tiles[0]
        S.dma("sp", uTg[p][:, 0:len(tiles)], UT[t0:t0 + len(tiles)].rearrange("c p j t -> p c j t"),
              reads=b_UT[t0:t0 + len(tiles)], writes=[b_uTg[p]])
        S.dma("sp", rc[p][:, 0:n], rc2_d[:, t0 * 128:t0 * 128 + n], writes=[b_rope[p]])
        S.dma("sp", rsn[p][:, 0:n], rs2_d[:, t0 * 128:t0 * 128 + n], writes=[b_rope[p]])
        for ci, ti in enumerate(tiles):
            vq = vi % 2; vi += 1
            for half in range(2):
                pc, bc = cx.PB[half], cx.PBb[half]
                for j in range(8):
                    S.op("pe", lambda e, j=j, ci=ci, pc=pc, half=half: e.matmul(pc[:], lhsT=uTg[p][:, ci, j, :], rhs=Wv_s[:, j, half * 512:(half + 1) * 512],
                                                                                start=(j == 0), stop=(j == 7)),
                         reads=[b_uTg[p], b_Wv], writes=[bc])
                S.op("act", lambda e, vq=vq, pc=pc, half=half: e.activation(out=vst[vq][:, half * 512:(half + 1) * 512], in_=pc[:], func=AF.Copy),
                     reads=[bc], writes=[b_vst[vq]])
            S.dma("sp", Vo[ti * 128:(ti + 1) * 128, :], vst[vq][:], reads=[b_vst[vq]], writes=[b_Vo])
        for qk in range(2):
            if qk == 0 and gi == 0:
                continue
            Ws, bW = (Wq_s, b_Wq) if qk == 0 else (Wk_s, b_Wk)
            sc_ = 0.125 if qk == 0 else 1.0
            for h in range(8):
                pa = 2 + fi % 4; fi += 1
                pb_ = 2 + fi % 4; fi += 1
                for (pp, cb) in [(pa, h), (pb_, 8 + h)]:
                    for j in range(8):
                        S.op("pe", lambda e, j=j, pp=pp, cb=cb: e.matmul(cx.PB[pp][:, 0:n], lhsT=Ws[:, j, cb * 128:(cb + 1) * 128],
                                                                         rhs=uTg[p][:, 0:len(tiles), j, :],
                                                                         start=(j == 0), stop=(j == 7)),
                             reads=[bW, b_uTg[p]], writes=[cx.PBb[pp]])
                S.op("dve", lambda e, pa=pa: e.scalar_tensor_tensor(out=t1[:, 0:n], in0=cx.PB[pa][:, 0:n], scalar=sc_, in1=rc[p][:, 0:n],
                                                                    op0=ALU.mult, op1=ALU.mult), reads=[cx.PBb[pa], b_rope[p]], writes=[b_t1])
                S.op("dve", lambda e, pb_=pb_: e.scalar_tensor_tensor(out=t2[:, 0:n], in0=cx.PB[pb_][:, 0:n], scalar=sc_, in1=rsn[p][:, 0:n],
                                                                      op0=ALU.mult, op1=ALU.mult), reads=[cx.PBb[pb_], b_rope[p]], writes=[b_t2])
                sq = si % 4; si += 1
                S.op("dve", lambda e, sq=sq: e.tensor_tensor(out=qkst[sq][:, 0:n], in0=t1[:, 0:n], in1=t2[:, 0:n], op=ALU.add),
                     reads=[b_t1, b_t2], writes=[b_qkst[sq]])
                if qk == 0:
                    q0 = (t0 - 1) * 128
                    S.dma("sp", QT[h, :, q0:q0 + n], qkst[sq][:, 0:n], reads=[b_qkst[sq]], writes=[b_QT])
                else:
                    S.dma("sp", KT[h, :, t0 * 128:t0 * 128 + n], qkst[sq][:, 0:n], reads=[b_qkst[sq]], writes=[b_KT])
    cx.phase_end()
    cx.phase_end()
    return b_hout, b_QT, b_KT, b_Vo


def l2_inputs(I, b, half, merged_full_b, consts):
    d = dict(consts)
    cs = slice(half * 128, (half + 1) * 128)
    ls = slice(half * 4096, (half + 1) * 4096)
    d["xin"] = np.ascontiguousarray(np.concatenate([I["ctx"][b][cs], I["x"][b][ls]], 0))
    if merged_full_b is not None:
        d["mergedIn"] = np.ascontiguousarray(np.concatenate([merged_full_b[0:256][cs], merged_full_b[256:][ls]], 0))
    d["cT"] = np.ascontiguousarray(I["c"][b].reshape(8, 128).T)
    d["cctxT"] = np.ascontiguousarray(I["c_ctx"].reshape(8, 128).T)
    d["wmod"] = np.ascontiguousarray(np.concatenate([I["w_mod"][0][:, 2048:6144], I["w_mod"][1][:, 0:2048]], 1))
    d["bmod"] = np.ascontiguousarray(np.concatenate([I["b_mod"][0][2048:6144], I["b_mod"][1][0:2048]])[None, :])
    d["g2_0"] = np.ascontiguousarray(I["norm2_g"][0][None, :])
    d["g1_1"] = np.ascontiguousarray(I["norm1_g"][1][None, :])
    d["w_out"] = I["w_out_even"][0]
    d["Wr"] = np.ascontiguousarray(np.concatenate([I["router_g_w"][0], I["router_e_w"][0]], 1))
    d["br"] = np.ascontiguousarray(np.concatenate([I["router_g_b"][0], I["router_e_b"][0]])[None, :])
    d["w1"] = I["w1"][0]; d["w3"] = I["w3"][0]; d["w2"] = I["w2"][0]
    W = I["w_in_odd"][0]
    perm = np.arange(64)
    perm = np.concatenate([perm[16:32], perm[0:16], perm[48:64], perm[32:48]])
    def swp(M):
        return M.reshape(D, 16, 64)[:, :, perm].reshape(D, 1024)
    d["Wq"] = np.ascontiguousarray(np.concatenate([W[:, 0:1024], swp(W[:, 0:1024])], 1))
    d["Wk"] = np.ascontiguousarray(np.concatenate([W[:, 1024:2048], swp(W[:, 1024:2048])], 1))
    d["Wv"] = np.ascontiguousarray(W[:, 2048:3072])
    return d


NT3 = 32
NK = 66
LAM_INIT = 0.8 - 0.6 * math.exp(-0.3 * 1)


def host_consts_l3():
    c = common_consts()
    c["ones_bf"] = np.ones((128, 128), np.float32).astype(ml_dtypes.bfloat16)
    return c


VCH = [(0, 1024), (1024, 1024), (2048, 1024), (3072, 1024), (4096, 128)]


def build_l3(cx, io, debug=None):
    nc = cx.nc
    S = cx.S
    cx.phase_begin("L3")
    QT = io["QTs"]; b_QTs = io["b_QTs"]
    KTg = io["KTg"]; b_KTg = io["b_KTg"]
    VG = io["VG"]; b_VG = io["b_VG"]
    H2 = io["H2"]; b_H2 = io["b_H2"]
    cT = io["cT"]
    wmod = io["wmod"]; bmod = io["bmod"]
    g2 = io["g2"]; gfin = io["gfin"]
    dalam = io["dalam"]; dagT = io["dagT"]
    w_out = io["w_out"]
    Wr = io["Wr"]; br = io["br"]
    w1 = io["w1"]; w3 = io["w3"]; w2 = io["w2"]
    out = io["out"]
    H3 = cx.dscr("H3", [NT3, 128, D]); b_H3 = [Buf() for _ in range(NT3)]
    Vs = cx.dscr("Vs3", [NT3, 128, D], BF16); b_Vs = [Buf() for _ in range(NT3)]

    def lc(name, shape, dt=F32):
        t = cx.sb(name + "_3s", shape, dt); b = Buf()
        S.dma("sp", t[:], io[name], writes=[b])
        return t, b
    ident_bf = lc("ident_bf", [128, 128], BF16)
    ident_f = lc("ident_f", [128, 128])
    ones_f = lc("ones_f", [128, 128])
    ones_bf = lc("ones_bf", [128, 128], BF16)
    slt = lc("slt", [128, 128])
    blkstart = lc("blkstart", [128, 1])
    pcol = lc("pcol", [128, 1])
    thr = lc("thr", [128, 34])
    consts = {"thr": thr, "ident_bf": ident_bf, "ident_f": ident_f, "ones_f": ones_f, "slt": slt, "blkstart": blkstart, "pcol": pcol}
    moe = io["moe"]
    moe.c = consts
    moe.alloc_persistent()

    mods = cx.sb("mods", [128, 4, D]); b_mods = Buf()
    gfin_s, b_gfin = cx.load_bcast("gfin3", D, gfin)
    cx.phase_begin("L3_mod")
    scb, b_scb = emit_silu_bcast(cx, [cT], ones_f[0], ones_f[1])
    bmod_s, b_bmod = cx.load_bcast("bmod3", 4096, bmod)
    g2_s, b_g2 = cx.load_bcast("g2_3", D, g2)
    wm_s = [cx.sb("wm_s%d" % i, [128, 8, 512]) for i in range(2)]; b_wm = [Buf(), Buf()]
    emit_mod(cx, scb, b_scb, 1, wmod, bmod_s, b_bmod, 4096,
             lambda w, cc: mods[:, cc // 2, (cc % 2) * 512:(cc % 2 + 1) * 512], b_mods, wm_s, b_wm)
    S.op("dve", lambda e: e.scalar_tensor_tensor(out=mods[:, 2, :], in0=mods[:, 2, :], scalar=1.0, in1=g2_s[:], op0=ALU.add, op1=ALU.mult),
         reads=[b_mods, b_g2], writes=[b_mods])
    cx.phase_end()

    cx.phase_begin("L3_attnouter")
    oT_all = cx.sb("oT_all", [128, 8, NT3 * 128], BF16); b_oT = [Buf() for _ in range(8)]
    lamw = cx.sb("lamw", [128, 256]); b_lamw = Buf()
    S.dma("sp", lamw[:], dalam.partition_broadcast(128), writes=[b_lamw])
    lamt = cx.sb("lamt", [128, 8]); b_lamt = Buf()
    prod = cx.sb("lprod", [128, 128]); b_prod = Buf()
    S.op("dve", lambda e: e.tensor_tensor(out=prod[:, 0:64], in0=lamw[:, 0:64], in1=lamw[:, 64:128], op=ALU.mult), reads=[b_lamw], writes=[b_prod])
    S.op("dve", lambda e: e.tensor_tensor(out=prod[:, 64:128], in0=lamw[:, 128:192], in1=lamw[:, 192:256], op=ALU.mult), reads=[b_lamw, b_prod], writes=[b_prod])
    S.op("dve", lambda e: e.tensor_reduce(out=lamt[:, 0:1], in_=prod[:, 0:64], axis=AX.X, op=ALU.add), reads=[b_prod], writes=[b_lamt])
    S.op("dve", lambda e: e.tensor_reduce(out=lamt[:, 1:2], in_=prod[:, 64:128], axis=AX.X, op=ALU.add), reads=[b_prod, b_lamt], writes=[b_lamt])
    S.op("act", lambda e: e.activation(out=lamt[:, 2:4], in_=lamt[:, 0:2], func=AF.Exp), reads=[b_lamt], writes=[b_lamt])
    S.op("dve", lambda e: e.tensor_tensor(out=lamt[:, 4:5], in0=lamt[:, 3:4], in1=lamt[:, 2:3], op=ALU.subtract), reads=[b_lamt], writes=[b_lamt])
    S.op("dve", lambda e: e.tensor_scalar(out=lamt[:, 4:5], in0=lamt[:, 4:5], scalar1=-LAM_INIT, scalar2=None, op0=ALU.add), reads=[b_lamt], writes=[b_lamt])
    gs = cx.sb("gs", [128, 8]); b_gs = Buf()
    S.dma("sp", gs[:], dagT, writes=[b_gs])
    S.op("dve", lambda e: e.tensor_scalar(out=gs[:], in0=gs[:], scalar1=1.0 - LAM_INIT, scalar2=None, op0=ALU.mult), reads=[b_gs], writes=[b_gs])

    cx.phase_begin("L3_attn")
    KTh = [cx.sb("KTh%d" % i, [128, NK * 128], BF16) for i in range(2)]; b_KTh = [Buf(), Buf()]
    Vh = [cx.sb("Vh%d" % i, [128, NK, 128], BF16) for i in range(2)]; b_Vh = [Buf(), Buf()]
    QTh1 = cx.sb("QTh", [128, 2, NT3 * 128], BF16); b_QTh1 = Buf()
    QTh = [QTh1, QTh1]; b_QTh = [b_QTh1, b_QTh1]
    S.op("dve", lambda e: e.memset(QTh1[:], 0.0), writes=[b_QTh1])
    Pt = [cx.sb("Pt%d" % i, [128, 512], BF16) for i in range(4)]; b_Pt = [Buf() for _ in range(4)]
    rec = cx.sb("rec", [128, 512]); b_rec = Buf()
    on1 = cx.sb("on1", [128, 512]); b_on1 = Buf()
    ot = cx.sb("ot", [128, 512]); b_ot = Buf()
    sq = cx.sb("sqt", [128, 512]); b_sq = Buf()
    rstd = cx.sb("rstdA", [128, 512]); b_rstdA = Buf()
    pi_ = 0
    si_ = 0
    nheads = 8 if debug != "attn1" else 1
    nqt = 8 if debug != "attn1" else 1
    for h in range(nheads):
        p = h % 2
        for r in range(2):
            S.dma("sp", KTh[p][:, r * 4224:(r + 1) * 4224], KTg[h, r * 128:(r + 1) * 128, :], reads=[b_KTg], writes=[b_KTh[p]])
            for (r0, n) in VCH:
                vr = 2 * r0 + r * n
                S.dma("sp", Vh[p][:, r * 33 + r0 // 128: r * 33 + (r0 + n) // 128, :],
                      VG[vr:vr + n, h * 128:(h + 1) * 128].rearrange("(k p) d -> p k d", p=128), reads=[b_VG], writes=[b_Vh[p]])
        for m_ in range(2):
            S.dma("sp", QTh[p][m_ * 64:(m_ + 1) * 64, m_, :], QT[h, m_ * 64:(m_ + 1) * 64, :], reads=[b_QTs], writes=[b_QTh[p]])
        steps = [(qt, m, kt) for qt in range(nqt) for m in range(2) for kt in range(NK)]
        LOOK = 2
        sbank = {}

        def issue_S(idx):
            nonlocal si_
            qt_, m_, kt_ = steps[idx]
            pst = si_ % 3; si_ += 1
            sbank[idx] = pst
            ms_ = slice(m_ * 64, (m_ + 1) * 64)
            qs_ = slice(qt_ * 512, (qt_ + 1) * 512)
            S.op("pe", lambda e: e.matmul(cx.PB[pst][:], lhsT=KTh[p][:, kt_ * 128:(kt_ + 1) * 128], rhs=QTh[p][:, m_, qs_], start=True, stop=True),
                 reads=[b_KTh[p], b_QTh[p]], writes=[cx.PBb[pst]])

        for i0 in range(min(LOOK, len(steps))):
            issue_S(i0)
        for idx, (qt, m, kt) in enumerate(steps):
            qs = slice(qt * 512, (qt + 1) * 512)
            pO, bO = cx.PB[3], cx.PBb[3]
            pS, bS = cx.PB[4], cx.PBb[4]
            pst = sbank.pop(idx)
            pq = pi_ % 4; pi_ += 1
            S.op("act", lambda e, pq=pq, pst=pst: e.activation(out=Pt[pq][:], in_=cx.PB[pst][:], func=AF.Exp),
                 reads=[cx.PBb[pst]], writes=[b_Pt[pq]])
            if idx + LOOK < len(steps):
                issue_S(idx + LOOK)
            S.op("pe", lambda e, pq=pq, kt=kt: e.matmul(pO[:], lhsT=Vh[p][:, kt, :], rhs=Pt[pq][:], start=(kt == 0), stop=(kt == NK - 1)),
                 reads=[b_Vh[p], b_Pt[pq]], writes=[bO])
            S.op("pe", lambda e, pq=pq, kt=kt: e.matmul(pS[:], lhsT=ones_bf[0][:], rhs=Pt[pq][:], start=(kt == 0), stop=(kt == NK - 1)),
                 reads=[ones_bf[1], b_Pt[pq]], writes=[bS])
            if kt != NK - 1:
                continue
            S.op("dve", lambda e: e.reciprocal(out=rec[:], in_=pS[:]), reads=[bS], writes=[b_rec])
            if m == 0:
                S.op("dve", lambda e: e.tensor_tensor(out=on1[:], in0=pO[:], in1=rec[:], op=ALU.mult), reads=[bO, b_rec], writes=[b_on1])
                continue
            S.op("dve", lambda e: e.tensor_tensor(out=ot[:], in0=pO[:], in1=rec[:], op=ALU.mult), reads=[bO, b_rec], writes=[b_ot])
            S.op("dve", lambda e: e.scalar_tensor_tensor(out=ot[:], in0=ot[:], scalar=lamt[:, 4:5], in1=on1[:], op0=ALU.mult, op1=ALU.add),
                 reads=[b_ot, b_on1, b_lamt], writes=[b_ot])
            S.op("act", lambda e: e.activation(out=sq[:], in_=ot[:], func=AF.Square), reads=[b_ot], writes=[b_sq])
            pN, bN = cx.PB[5], cx.PBb[5]
            S.op("pe", lambda e: e.matmul(pN[:], lhsT=ones_f[0][:], rhs=sq[:], start=True, stop=True), reads=[ones_f[1], b_sq], writes=[bN])
            S.op("dve", lambda e: e.tensor_scalar(out=rstd[:], in0=pN[:], scalar1=1.0 / 128, scalar2=EPS, op0=ALU.mult, op1=ALU.add),
                 reads=[bN], writes=[b_rstdA])
            S.op("act", lambda e: e.activation(out=rstd[:], in_=rstd[:], func=AF.Sqrt), reads=[b_rstdA], writes=[b_rstdA])
            S.op("dve", lambda e: e.reciprocal(out=rstd[:], in_=rstd[:]), reads=[b_rstdA], writes=[b_rstdA])
            S.op("dve", lambda e, h=h, qs=qs: e.scalar_tensor_tensor(out=oT_all[:, h, qs], in0=ot[:], scalar=gs[:, h:h + 1], in1=rstd[:],
                                                                    op0=ALU.mult, op1=ALU.mult),
                 reads=[b_ot, b_gs, b_rstdA], writes=[b_oT[h]])

    if debug == "attn1":
        dbg = cx.dout("dbg_oT", [128, 512], BF16); b_dbg = Buf()
        S.dma("sp", dbg, oT_all[:, 0, 0:512], reads=b_oT, writes=[b_dbg])
        cx.phase_end()
        S.finish([b_dbg], "sp")
        return nc

    cx.phase_end()
    cx.phase_begin("L3_wout")
    wo_s = cx.sb("wo_s", [128, 8, D], BF16); b_wo = Buf()
    for j in range(8):
        S.dma("pool", wo_s[:, j, :], w_out[j * 128:(j + 1) * 128, :], writes=[b_wo])
    nb = NormBufs(cx)
    ht = [cx.sb("ht%d" % i, [128, D]) for i in range(2)]; b_ht = [Buf(), Buf()]
    vt = [cx.sb("vt%d" % i, [128, D], BF16) for i in range(2)]; b_vt = [Buf(), Buf()]
    vT = [cx.sb("vT%d" % i, [128, 8, 128], BF16) for i in range(2)]; b_vT = [Buf(), Buf()]
    ytmp = cx.sb("ytmp", [128, D]); b_ytmp = Buf()
    for ti in range(NT3):
        p = ti % 2
        S.dma("sp", ht[p][:], H2[(ti + 1) * 128:(ti + 2) * 128, :], reads=[b_H2], writes=[b_ht[p]])
        for half in range(2):
            pc, bc = cx.PB[half], cx.PBb[half]
            for j in range(8):
                S.op("pe", lambda e, j=j, pc=pc, half=half, ti=ti: e.matmul(pc[:], lhsT=oT_all[:, j, ti * 128:(ti + 1) * 128],
                                                                            rhs=wo_s[:, j, half * 512:(half + 1) * 512], start=(j == 0), stop=(j == 7)),
                     reads=b_oT + [b_wo], writes=[bc])
            sl = slice(half * 512, (half + 1) * 512)
            S.op("dve", lambda e, pc=pc, sl=sl: e.tensor_tensor(out=ytmp[:, sl], in0=pc[:], in1=mods[:, 0, sl], op=ALU.mult),
                 reads=[bc, b_mods], writes=[b_ytmp])
            S.op("dve", lambda e, p=p, sl=sl: e.tensor_tensor(out=ht[p][:, sl], in0=ht[p][:, sl], in1=ytmp[:, sl], op=ALU.add),
                 reads=[b_ytmp, b_ht[p]], writes=[b_ht[p]])
        S.dma("sp", H3[ti], ht[p][:], reads=[b_ht[p]], writes=[b_H3[ti]])
        emit_adaln(cx, nb, ht[p][:], b_ht[p], mods[:, 2, :], mods[:, 1, :], b_mods, vt[p][:], b_vt[p])
        S.dma("sp", Vs[ti], vt[p][:], reads=[b_vt[p]], writes=[b_Vs[ti]])
        emit_transpose(cx, lambda j, p=p: vt[p][:, j * 128:(j + 1) * 128], b_vt[p], 8, ident_bf[0], ident_bf[1], vT[p][:], b_vT[p])
        moe.route_tile(ti, lambda j, p=p: vT[p][:, j, :], b_vT[p])
    cx.phase_end()
    cx.phase_end()

    cx.phase_begin("L3_moe")
    moe.plan()
    vl = [cx.sb("vl%d" % i, [128, D], BF16) for i in range(2)]; b_vl = [Buf(), Buf()]
    for ti in range(NT3):
        p = ti % 2
        S.dma("sp", vl[p][:], Vs[ti], reads=[b_Vs[ti]], writes=[b_vl[p]])
        moe.dispatch_tile(ti, vl[p][:], b_vl[p])
    moe.experts()
    cx.phase_end()

    cx.phase_begin("L3_fin")
    nb = NormBufs(cx, "d")
    h1t = [cx.sb("h1t%d" % i, [128, D]) for i in range(2)]; b_h1t = [Buf(), Buf()]
    y0 = [cx.sb("y0_%d" % i, [128, D]) for i in range(2)]; b_y0 = [Buf(), Buf()]
    y1 = [cx.sb("y1_%d" % i, [128, D]) for i in range(2)]; b_y1 = [Buf(), Buf()]
    ot_ = [cx.sb("ofin%d" % i, [128, D]) for i in range(2)]; b_ofin = [Buf(), Buf()]
    b_out = Buf()
    for ti in range(NT3):
        p = ti % 2
        S.dma("sp", h1t[p][:], H3[ti], reads=[b_H3[ti]], writes=[b_h1t[p]])
        moe.gather_tile(ti, y0[p][:], y1[p][:], b_y0[p], b_y1[p])
        S.op("dve", lambda e, p=p, ti=ti: e.tensor_scalar(out=y0[p][:], in0=y0[p][:], scalar1=moe.gate[:, ti, 0:1], scalar2=None, op0=ALU.mult),
             reads=[b_y0[p], moe.b_gate[ti]], writes=[b_y0[p]])
        S.op("dve", lambda e, p=p, ti=ti: e.scalar_tensor_tensor(out=y0[p][:], in0=y1[p][:], scalar=moe.gate[:, ti, 1:2], in1=y0[p][:],
                                                                op0=ALU.mult, op1=ALU.add),
             reads=[b_y0[p], b_y1[p], moe.b_gate[ti]], writes=[b_y0[p]])
        S.op("dve", lambda e, p=p: e.tensor_tensor(out=y0[p][:], in0=y0[p][:], in1=mods[:, 3, :], op=ALU.mult),
             reads=[b_y0[p], b_mods], writes=[b_y0[p]])
        S.op("dve", lambda e, p=p: e.tensor_tensor(out=h1t[p][:], in0=h1t[p][:], in1=y0[p][:], op=ALU.add),
             reads=[b_y0[p], b_h1t[p]], writes=[b_h1t[p]])
        emit_adaln(cx, nb, h1t[p][:], b_h1t[p], gfin_s[:], None, b_gfin, ot_[p][:], b_ofin[p])
        S.dma("sp", out[ti * 128:(ti + 1) * 128, :], ot_[p][:], reads=[b_ofin[p]], writes=[b_out])
    cx.phase_end()
    cx.phase_end()
    return b_out


def l3_inputs(I, b, half, l2res_pair, consts):
    d = dict(consts)
    if l2res_pair is not None:
        d["QT"] = l2res_pair[half]["QT"]
        d["KT"] = np.ascontiguousarray(np.concatenate([l2res_pair[0]["KT"], l2res_pair[1]["KT"]], 2))
        d["Vf"] = np.ascontiguousarray(np.concatenate([l2res_pair[0]["Vo"], l2res_pair[1]["Vo"]], 0))
        d["hin"] = np.ascontiguousarray(l2res_pair[half]["hout"][128:])
    d["cT"] = np.ascontiguousarray(I["c"][b].reshape(8, 128).T)
    d["wmod"] = np.ascontiguousarray(I["w_mod"][1][:, 2048:6144])
    d["bmod"] = np.ascontiguousarray(I["b_mod"][1][None, 2048:6144])
    d["g2"] = np.ascontiguousarray(I["norm2_g"][1][None, :])
    d["gfin"] = np.ascontiguousarray(I["final_norm_g"][None, :])
    d["dalam"] = np.ascontiguousarray(I["da_lambda"][0].reshape(1, 256))
    d["dagT"] = np.ascontiguousarray(I["da_norm_g"][0].T)
    d["w_out"] = I["w_out_odd"][0]
    d["Wr"] = np.ascontiguousarray(np.concatenate([I["router_g_w"][1], I["router_e_w"][1]], 1))
    d["br"] = np.ascontiguousarray(np.concatenate([I["router_g_b"][1], I["router_e_b"][1]])[None, :])
    d["w1"] = I["w1"][1]; d["w3"] = I["w3"][1]; d["w2"] = I["w2"][1]
    return d


PAIRS = [[0, 1], [2, 3], [4, 5], [6, 7]]
MCH = [(0, 2048), (2048, 2048), (4096, 2048), (6144, 2048), (8192, 256)]

L1_SPECS = [("xin", [NT * 128, D], F32), ("cT", [128, 8], F32), ("cctxT", [128, 8], F32), ("wmod", [D, 2048], F32), ("bmod", [1, 2048], F32),
            ("g1", [1, D], F32), ("Wfm", [D, 1536], F32), ("Wtm", [D, 1032], F32), ("gbias", [1, 8], F32), ("rld", [1, 4], F32),
            ("mlg", [1, 256], F32), ("retg", [1, 256], F32), ("ident_bf", [128, 128], BF16), ("ident_f", [128, 128], F32),
            ("mask_f32", [128, 2, 128], F32), ("mask_bf", [128, 2, 128], BF16), ("ones_f", [128, 128], F32), ("poscol", [128, 4], F32),
            ("ropecos", [128, NT * 128], F32), ("ropesin", [128, NT * 128], F32)]
L2_SPECS = [("xin", [NT2 * 128, D], F32), ("idxm", [128, NT2, 2], I32), ("cT", [128, 8], F32), ("cctxT", [128, 8], F32),
            ("wmod", [D, 6144], F32), ("bmod", [1, 6144], F32), ("g2_0", [1, D], F32), ("g1_1", [1, D], F32), ("w_out", [D, D], F32),
            ("Wr", [D, 36], F32), ("br", [1, 36], F32), ("w1", [32, D, 512], F32), ("w3", [32, D, 512], F32), ("w2", [32, 512, D], F32),
            ("Wq", [D, 2048], F32), ("Wk", [D, 2048], F32), ("Wv", [D, D], F32), ("rc2", [128, NT2 * 128], F32), ("rs2", [128, NT2 * 128], F32),
            ("ident_bf", [128, 128], BF16), ("ident_f", [128, 128], F32), ("ones_f", [128, 128], F32), ("slt", [128, 128], F32),
            ("blkstart", [128, 1], F32), ("pcol", [128, 1], F32), ("thr", [128, 34], F32)]
L3_SPECS = [("cT", [128, 8], F32), ("wmod", [D, 4096], F32), ("bmod", [1, 4096], F32), ("g2", [1, D], F32), ("gfin", [1, D], F32),
            ("dalam", [1, 256], F32), ("dagT", [128, 8], F32), ("w_out", [D, D], F32), ("Wr", [D, 36], F32), ("br", [1, 36], F32),
            ("w1", [32, D, 512], F32), ("w3", [32, D, 512], F32), ("w2", [32, 512, D], F32),
            ("ident_bf", [128, 128], BF16), ("ident_f", [128, 128], F32), ("ones_f", [128, 128], F32), ("ones_bf", [128, 128], BF16),
            ("slt", [128, 128], F32), ("blkstart", [128, 1], F32), ("pcol", [128, 1], F32), ("thr", [128, 34], F32)]


def build_fused(nc):
    cx = Ctx(nc)
    S = cx.S
    io1 = {n: cx.din("l1_" + n, sh, dt) for (n, sh, dt) in L1_SPECS}
    io2 = {n: cx.din("l2_" + n, sh, dt) for (n, sh, dt) in L2_SPECS}
    io3 = {n: cx.din("l3_" + n, sh, dt) for (n, sh, dt) in L3_SPECS}
    out = cx.dout("out", [NT3 * 128, D])

    moeA = MoE(cx, NT2, io2["Wr"], io2["br"], io2["w1"], io2["w3"], io2["w2"], tag="A")
    moeB = MoE(cx, NT3, io3["Wr"], io3["br"], io3["w1"], io3["w3"], io3["w2"], tag="B")
    io1["after_weights"] = moeA.precast
    io2["moe"] = moeA
    io3["moe"] = moeB

    mergedX = cx.dscr("mergedX", [NT * 128, 512], BF16)
    io1["merged"] = mergedX
    b_merged = build_l1(cx, io1)

    MG = cx.dscr("MG", [2 * NT * 128, 512], BF16); b_MG = Buf()
    for (r0, n) in MCH:
        S.collective("AllGather", [mergedX[r0:r0 + n, :]], [MG[2 * r0:2 * r0 + 2 * n, :]], PAIRS, reads=[b_merged], writes=[b_MG])

    H2 = cx.dscr("H2", [NT2 * 128, D])
    QTs = cx.dscr("QTs", [8, 128, 4096], BF16)
    KTo = cx.dscr("KTo", [8, 128, NT2 * 128], BF16)
    Vown = cx.dscr("Vown", [NT2 * 128, D], BF16)
    io2.update({"MG": MG, "b_MG": b_MG, "H2": H2, "QTs": QTs, "KTo": KTo, "Vown": Vown})
    b_H2, b_QTs, b_KTo, b_Vown = build_l2(cx, io2)

    KTg = cx.dscr("KTg", [8, 256, NT2 * 128], BF16); b_KTg = Buf()
    VG = cx.dscr("VG", [2 * NT2 * 128, D], BF16); b_VG = Buf()
    for h in range(8):
        S.collective("AllGather", [KTo[h]], [KTg[h]], PAIRS, reads=[b_KTo], writes=[b_KTg])
    for (r0, n) in VCH:
        S.collective("AllGather", [Vown[r0:r0 + n, :]], [VG[2 * r0:2 * r0 + 2 * n, :]], PAIRS, reads=[b_Vown], writes=[b_VG])

    moeB.precast()
    io3.update({"QTs": QTs, "b_QTs": b_QTs, "KTg": KTg, "b_KTg": b_KTg, "VG": VG, "b_VG": b_VG, "H2": H2, "b_H2": b_H2, "out": out})
    b_out = build_l3(cx, io3)
    S.finish([b_out], "sp")
    print("fused instructions:", S.n_inst)
    return nc


def fused_inputs(I, core, c1, c2, c3):
    b, g = core // 2, core % 2
    d = {}
    for k, v in l1_inputs(I, b, g, c1).items():
        d["l1_" + k] = v
    d2 = l2_inputs(I, b, g, None, c2[g])
    for k, v in d2.items():
        d["l2_" + k] = v
    trow = np.concatenate([np.arange(g * 128, (g + 1) * 128), 256 + np.arange(g * 4096, (g + 1) * 4096)])
    r0 = np.minimum((trow // 2048) * 2048, 8192)
    n = np.where(r0 < 8192, 2048, 256)
    idx = np.stack([2 * r0 + r * n + (trow - r0) for r in range(2)], 1).astype(np.int32)
    d["l2_idxm"] = np.ascontiguousarray(idx.reshape(NT2, 128, 2).transpose(1, 0, 2))
    d3 = l3_inputs(I, b, g, None, c3)
    for k, v in d3.items():
        d["l3_" + k] = v
    return d


from concourse.bass_utils import run_bass_kernel_spmd

_NC_CACHE = {}


def kernel(**inputs):
    I = {k: np.asarray(v) for k, v in inputs.items()}
    cores = list(range(8))
    if "fused" not in _NC_CACHE:
        nc = bass.Bass("TRN2", target_bir_lowering=False)
        build_fused(nc)
        _NC_CACHE["fused"] = nc
    nc = _NC_CACHE["fused"]
    c1 = host_consts_l1(); c2 = [host_consts_l2(0), host_consts_l2(1)]; c3 = host_consts_l3()
    names = set(["l1_" + n for n, _, _ in L1_SPECS] + ["l2_" + n for n, _, _ in L2_SPECS] + ["l3_" + n for n, _, _ in L3_SPECS])
    in_maps = []
    for core in cores:
        m = fused_inputs(I, core, c1, c2, c3)
        in_maps.append({k: v for k, v in m.items() if k in names})
    res = run_bass_kernel_spmd(nc, in_maps, core_ids=cores).results
    out = np.empty((4, 8192, 1024), np.float32)
    for core in cores:
        b, half = core // 2, core % 2
        out[b, half * 4096:(half + 1) * 4096] = res[core]["out"]
    return out
```

```python
import math
import contextlib
import numpy as np
import ml_dtypes
import concourse.bass as bass
import concourse.mybir as mybir


F32 = mybir.dt.float32
BF16 = mybir.dt.bfloat16
I32 = mybir.dt.int32
U32 = mybir.dt.uint32
AF = mybir.ActivationFunctionType
ALU = mybir.AluOpType
AX = mybir.AxisListType


class Buf:
    __slots__ = ("name", "w", "r")

    def __init__(self, name=""):
        self.name = name
        self.w = None
        self.r = {}


class Sync:
    def __init__(self, nc, n_dma_sems=48, same_engine_wait=True):
        self.nc = nc
        self.eng = {"pe": nc.tensor, "act": nc.scalar, "dve": nc.vector, "pool": nc.gpsimd, "sp": nc.sync}
        self.sem = {}
        self.cnt = {}
        for k in ["pe", "act", "dve", "pool"]:
            self.sem[k] = nc.alloc_semaphore(name="s_" + k)
            self.cnt[k] = 0
        self.dsem = [nc.alloc_semaphore(name="s_dma%d" % i) for i in range(n_dma_sems)]
        self.dcnt = [0] * n_dma_sems
        self.dnext = 0
        self.waited = {k: {} for k in self.eng}
        self.same_engine_wait = same_engine_wait
        self.n_inst = 0

    def _semh(self, key):
        return self.sem[key] if isinstance(key, str) else self.dsem[key]

    def _wait(self, e, key, val):
        if val <= 0:
            return
        w = self.waited[e]
        if w.get(key, 0) >= val:
            return
        if key == e and not self.same_engine_wait:
            return
        if key == e == "pe":
            return
        self.eng[e].wait_ge(self._semh(key), val)
        w[key] = val

    def _deps(self, e, reads, writes):
        for b in reads:
            if b.w is not None:
                self._wait(e, *b.w)
        for b in writes:
            if b.w is not None:
                self._wait(e, *b.w)
            for k, v in b.r.items():
                self._wait(e, k, v)

    def _record(self, ev, reads, writes):
        k, v = ev
        for b in reads:
            if b.r.get(k, 0) < v:
                b.r[k] = v
        for b in writes:
            b.w = ev
            b.r = {}

    def op(self, e, fn, reads=(), writes=(), inc=True):
        self._deps(e, reads, writes)
        ins = fn(self.eng[e])
        if inc:
            self.cnt[e] += 1
            ins.then_inc(self.sem[e], 1)
            self._record((e, self.cnt[e]), reads, writes)
        else:
            self._record((e, self.cnt[e] + 1), reads, writes)
        self.n_inst += 1
        return ins

    def dma(self, q, out, in_, reads=(), writes=(), indirect=None, **kw):
        self._deps(q, reads, writes)
        k = self.dnext
        self.dnext = (self.dnext + 1) % len(self.dsem)
        self._wait(q, k, 16 * self.dcnt[k])
        if indirect is not None:
            ins = self.eng[q].indirect_dma_start(out, indirect.get("out_offset"), in_, indirect.get("in_offset"), **kw)
        else:
            ins = self.eng[q].dma_start(out=out, in_=in_, **kw)
        self.dcnt[k] += 1
        ins.then_inc(self.dsem[k], 16)
        self._record((k, 16 * self.dcnt[k]), reads, writes)
        self.n_inst += 1
        return ins

    def collective(self, kind, ins, outs, groups, reads=(), writes=()):
        self._deps("pool", reads, writes)
        if "cc" not in self.sem:
            self.sem["cc"] = self.nc.alloc_semaphore(name="s_cc")
            self.cnt["cc"] = 0
        ins_ = self.nc.gpsimd.collective_compute(kind, ALU.bypass, replica_groups=groups, ins=ins, outs=outs)
        self.cnt["cc"] += 1
        ins_.then_inc(self.sem["cc"], 1)
        self._record(("cc", self.cnt["cc"]), reads, writes)
        self.n_inst += 1
        return ins_

    def barrier(self):
        for e in self.eng:
            for k in self.sem:
                self._wait(e, k, self.cnt[k])
            for k in range(len(self.dsem)):
                self._wait(e, k, 16 * self.dcnt[k])

    def touch(self, e, reads=(), writes=()):
        self._deps(e, reads, writes)

    def finish(self, bufs, e="sp"):
        for b in bufs:
            if b.w is not None:
                self._wait(e, *b.w)


D = 1024
EPS = 1e-6
XOFF = 524288
PROFILE_SCOPES = False
SAME_ENGINE_WAIT = True


class Ctx:
    def __init__(self, nc):
        self.nc = nc
        self.S = Sync(nc, same_engine_wait=SAME_ENGINE_WAIT)
        self.stk = [contextlib.ExitStack()]
        self.PB = [nc.alloc_psum_tensor("pb%d" % i, [128, 512], F32) for i in range(7)]
        self.PBb = [Buf() for _ in range(7)]
        self.PT = nc.alloc_psum_tensor("pt_bf", [128, 1024], BF16)
        self.b_PT = Buf()
        self.nreg = 0
        self.scopes = []

    def din(self, name, shape, dt=F32):
        return self.nc.dram_tensor(name, list(shape), dt, kind="ExternalInput").ap()

    def dout(self, name, shape, dt=F32):
        return self.nc.dram_tensor(name, list(shape), dt, kind="ExternalOutput").ap()

    def dscr(self, name, shape, dt=F32):
        return self.nc.dram_tensor(name, list(shape), dt).ap()

    def sb(self, name, shape, dt=F32):
        self.nreg += 1
        return self.stk[-1].enter_context(self.nc.sbuf_tensor("%s_u%d" % (name, self.nreg), list(shape), dt))

    def phase_begin(self, name=None):
        self.stk.append(contextlib.ExitStack())
        sid = None
        if name is not None and PROFILE_SCOPES:
            sid, _ = self.nc.enter_named_scope(name, False)
        self.scopes.append((name, sid))

    def phase_end(self):
        self.S.barrier()
        name, sid = self.scopes.pop()
        if sid is not None:
            self.nc.leave_named_scope(name, sid, False)
        self.stk.pop().close()

    def load_const(self, name, shape, dt=F32):
        d = self.din(name, shape, dt)
        t = self.sb(name + "_s", shape, dt)
        b = Buf()
        self.S.dma("sp", t[:], d, writes=[b])
        return t, b

    def load_bcast(self, name, n, dram_ap=None):
        d = dram_ap if dram_ap is not None else self.din(name, [1, n])
        t = self.sb(name + "_s", [128, n])
        b = Buf()
        self.S.dma("sp", t[:], d.partition_broadcast(128), writes=[b])
        return t, b


def common_consts():
    c = {}
    c["ident_bf"] = np.eye(128, dtype=np.float32).astype(ml_dtypes.bfloat16)
    c["ident_f"] = np.eye(128, dtype=np.float32)
    c["ones_f"] = np.ones((128, 128), np.float32)
    s = np.arange(128)
    c["slt"] = (s[:, None] < s[None, :]).astype(np.float32)
    c["blkstart"] = (256.0 * s).astype(np.float32)[:, None]
    c["pcol"] = (1.0 * s).astype(np.float32)[:, None]
    c["thr"] = np.tile((256.0 * np.arange(34)).astype(np.float32)[None, :], (128, 1))
    return c


def emit_silu_bcast(cx, cT_aps, ones_f, b_ones):
    S = cx.S
    n = len(cT_aps)
    cT_s = cx.sb("cT_s", [128, n, 8]); b_cT = Buf()
    for i, a in enumerate(cT_aps):
        S.dma("sp", cT_s[:, i, :], a, writes=[b_cT])
    sc = cx.sb("sc", [128, n, 8]); b_sc = Buf()
    S.op("act", lambda e: e.activation(out=sc[:], in_=cT_s[:], func=AF.Silu), reads=[b_cT], writes=[b_sc])
    scb = cx.sb("scb", [128, n, 8, 128]); b_scb = Buf()
    for w in range(n):
        for j in range(8):
            S.op("dve", lambda e, w=w, j=j: e.tensor_scalar(out=scb[:, w, j, :], in0=ones_f[:], scalar1=sc[:, w, j:j + 1],
                                                           scalar2=None, op0=ALU.mult),
                 reads=[b_ones, b_sc], writes=[b_scb])
    return scb, b_scb


def emit_mod(cx, scb, b_scb, nvec, wmod_ap, bmod_s, b_bmod, ncols, dest_fn, b_dest, wm_s, b_wm):
    S = cx.S
    wmod_v = wmod_ap.rearrange("(j p) n -> p j n", p=128)
    k = 0
    for cc in range(ncols // 512):
        wb = cc % 2
        S.dma("sp", wm_s[wb][:], wmod_v[:, :, cc * 512:(cc + 1) * 512], writes=[b_wm[wb]])
        for w in range(nvec):
            pbi = k % 7; k += 1
            for j in range(8):
                S.op("pe", lambda e, w=w, j=j, wb=wb, pbi=pbi: e.matmul(cx.PB[pbi][:], lhsT=scb[:, w, j, :], rhs=wm_s[wb][:, j, :],
                                                                        start=(j == 0), stop=(j == 7)),
                     reads=[b_scb, b_wm[wb]], writes=[cx.PBb[pbi]])
            S.op("dve", lambda e, w=w, pbi=pbi, cc=cc: e.tensor_tensor(out=dest_fn(w, cc), in0=cx.PB[pbi][:],
                                                                      in1=bmod_s[:, cc * 512:(cc + 1) * 512], op=ALU.add),
                 reads=[cx.PBb[pbi], b_bmod], writes=[b_dest])


class NormBufs:
    def __init__(self, cx, tag=""):
        self.junk = cx.sb("junk" + tag, [128, D], BF16); self.b_junk = Buf()
        self.ss = cx.sb("ss" + tag, [128, 2]); self.b_ss = [Buf(), Buf()]
        self.rstd = cx.sb("rstd" + tag, [128, 2]); self.b_rstd = [Buf(), Buf()]
        self.tmp32 = cx.sb("tmp32" + tag, [128, D]); self.b_tmp32 = Buf()
        self.k = 0


def emit_adaln(cx, nb, x_ap, b_x, A_ap, Bsh_ap, b_mod, out_ap, b_out):
    S = cx.S
    xp = nb.k % 2; nb.k += 1
    ss = nb.ss[:, xp:xp + 1]; rstd = nb.rstd[:, xp:xp + 1]
    S.op("dve", lambda e: e.memset(ss, 0.0), writes=[nb.b_ss[xp]])
    S.op("act", lambda e: e.activation(out=nb.junk[:], in_=x_ap, func=AF.Square, accum_out=ss),
         reads=[b_x], writes=[nb.b_junk, nb.b_ss[xp]])
    S.op("dve", lambda e: e.tensor_scalar(out=rstd, in0=ss, scalar1=1.0 / D, scalar2=EPS, op0=ALU.mult, op1=ALU.add),
         reads=[nb.b_ss[xp]], writes=[nb.b_rstd[xp]])
    S.op("act", lambda e: e.activation(out=rstd, in_=rstd, func=AF.Sqrt), reads=[nb.b_rstd[xp]], writes=[nb.b_rstd[xp]])
    S.op("dve", lambda e: e.reciprocal(out=rstd, in_=rstd), reads=[nb.b_rstd[xp]], writes=[nb.b_rstd[xp]])
    if Bsh_ap is None:
        S.op("dve", lambda e: e.scalar_tensor_tensor(out=out_ap, in0=x_ap, scalar=rstd, in1=A_ap, op0=ALU.mult, op1=ALU.mult),
             reads=[b_x, nb.b_rstd[xp], b_mod], writes=[b_out])
    else:
        S.op("dve", lambda e: e.scalar_tensor_tensor(out=nb.tmp32[:], in0=x_ap, scalar=rstd, in1=A_ap, op0=ALU.mult, op1=ALU.mult),
             reads=[b_x, nb.b_rstd[xp], b_mod], writes=[nb.b_tmp32])
        S.op("dve", lambda e: e.tensor_tensor(out=out_ap, in0=nb.tmp32[:], in1=Bsh_ap, op=ALU.add),
             reads=[nb.b_tmp32, b_mod], writes=[b_out])


def emit_transpose(cx, src_fn, b_src, nblk, ident_bf, b_ident, dst_ap, b_dst):
    S = cx.S
    for j in range(nblk):
        S.op("pe", lambda e, j=j: e.transpose(cx.PT[:, j * 128:(j + 1) * 128], src_fn(j), ident_bf[:]),
             reads=[b_src, b_ident], writes=[cx.b_PT])
    S.op("act", lambda e: e.activation(out=dst_ap, in_=cx.PT[:, 0:nblk * 128].rearrange("p (j t) -> p j t", j=nblk), func=AF.Copy),
         reads=[cx.b_PT], writes=[b_dst])


class MoE:
    def __init__(self, cx, ntile, Wr_d, br_d, w1_d, w3_d, w2_d, consts=None, tag=""):
        self.cx = cx
        self.nt = ntile
        self.A = 2 * ntile * 128
        self.nblk = (self.A + 255) // 256 + 32
        self.c = consts
        self.tag = tag
        self.w1_d, self.w3_d, self.w2_d = w1_d, w3_d, w2_d
        self.Wr_d, self.br_d = Wr_d, br_d
        nr = self.nblk * 256
        self.xbuf = cx.dscr("xbuf" + tag, [nr, D], BF16); self.b_xbuf = Buf()
        self.ybuf = cx.dscr("ybuf" + tag, [nr, D], F32); self.b_ybuf = Buf()
        self.W1b = cx.dscr("W1b" + tag, [32 * 128, 4096], BF16); self.b_W1b = Buf()
        self.W3b = cx.dscr("W3b" + tag, [32 * 128, 4096], BF16); self.b_W3b = Buf()
        self.W2b = cx.dscr("W2b" + tag, [32 * 128, 4096], BF16); self.b_W2b = Buf()

    def precast(self):
        S = self.cx.S
        for (src, dst, b, j) in [(self.w1_d, self.W1b, self.b_W1b, 8), (self.w3_d, self.W3b, self.b_W3b, 8), (self.w2_d, self.W2b, self.b_W2b, 4)]:
            sv = src.rearrange("e (p j) n -> e p (j n)", j=j)
            for e_ in range(32):
                S.dma("pool", dst[e_ * 128:(e_ + 1) * 128, :].rearrange("p (a b) -> p a b", a=2), sv[e_].rearrange("p (a b) -> p a b", a=2), writes=[b])

    def alloc_persistent(self):
        cx = self.cx
        t = self.tag
        self.OH = cx.sb("OH" + t, [128, self.nt, 2, 32]); self.b_OH = [Buf() for _ in range(self.nt)]
        self.gate = cx.sb("gate" + t, [128, self.nt, 2]); self.b_gate = [Buf() for _ in range(self.nt)]
        self.dest = cx.sb("dest" + t, [128, self.nt, 2], I32); self.b_dest = [Buf() for _ in range(self.nt)]
        self.idxw = cx.sb("idxw" + t, [128, 128], I32); self.b_idxw = Buf()
        self.Wr_s = cx.sb("Wr_s" + t, [128, 8, 36], BF16); self.b_Wr = Buf()
        for j in range(8):
            cx.S.dma("pool", self.Wr_s[:, j, :], self.Wr_d[j * 128:(j + 1) * 128, :], writes=[self.b_Wr])
        self.br_s, self.b_br = cx.load_bcast("br" + t, 36, self.br_d)
        self.lg = cx.sb("lg" + t, [128, 36]); self.b_lg = Buf()
        self.sm = cx.sb("rsm" + t, [128, 64]); self.b_sm = Buf()

    def route_tile(self, ti, vT_fn, b_vT):
        cx, S = self.cx, self.cx.S
        pbi = 6
        PBt, bPB = cx.PB[pbi], cx.PBb[pbi]
        for j in range(8):
            S.op("pe", lambda e, j=j: e.matmul(PBt[:, 0:36], lhsT=vT_fn(j), rhs=self.Wr_s[:, j, :], start=(j == 0), stop=(j == 7)),
                 reads=[b_vT, self.b_Wr], writes=[bPB])
        lg, sm = self.lg, self.sm
        b_lg, b_sm = self.b_lg, self.b_sm
        S.op("dve", lambda e: e.tensor_tensor(out=lg[:], in0=PBt[:, 0:36], in1=self.br_s[:], op=ALU.add), reads=[bPB, self.b_br], writes=[b_lg])
        S.op("dve", lambda e: e.tensor_reduce(out=sm[:, 0:1], in_=lg[:, 0:4], axis=AX.X, op=ALU.max), reads=[b_lg], writes=[b_sm])
        S.op("dve", lambda e: e.tensor_scalar(out=sm[:, 1:2], in0=sm[:, 0:1], scalar1=-1.0, scalar2=None, op0=ALU.mult), reads=[b_sm], writes=[b_sm])
        S.op("dve", lambda e: e.memset(sm[:, 2:3], 0.0), reads=[b_sm], writes=[b_sm])
        S.op("act", lambda e: e.activation(out=sm[:, 44:48], in_=lg[:, 0:4], func=AF.Exp, bias=sm[:, 1:2], accum_out=sm[:, 2:3]),
             reads=[b_lg, b_sm], writes=[b_sm])
        S.op("dve", lambda e: e.reciprocal(out=sm[:, 3:4], in_=sm[:, 2:3]), reads=[b_sm], writes=[b_sm])
        S.op("dve", lambda e: e.tensor_scalar(out=sm[:, 4:8], in0=lg[:, 0:4], scalar1=sm[:, 0:1], scalar2=None, op0=ALU.is_equal),
             reads=[b_lg, b_sm], writes=[b_sm])
        S.op("dve", lambda e: e.tensor_scalar(out=sm[:, 8:16], in0=lg[:, 4:12], scalar1=sm[:, 4:5], scalar2=None, op0=ALU.mult),
             reads=[b_lg, b_sm], writes=[b_sm])
        for g in range(1, 4):
            S.op("dve", lambda e, g=g: e.scalar_tensor_tensor(out=sm[:, 8:16], in0=lg[:, 4 + 8 * g:12 + 8 * g], scalar=sm[:, 4 + g:5 + g],
                                                             in1=sm[:, 8:16], op0=ALU.mult, op1=ALU.add),
                 reads=[b_lg, b_sm], writes=[b_sm])
        S.op("dve", lambda e: e.max(out=sm[:, 16:24], in_=sm[:, 8:16]), reads=[b_sm], writes=[b_sm])
        S.op("dve", lambda e: e.tensor_scalar(out=sm[:, 24:32], in0=sm[:, 8:16], scalar1=sm[:, 16:17], scalar2=None, op0=ALU.is_equal),
             reads=[b_sm], writes=[b_sm])
        S.op("dve", lambda e: e.tensor_scalar(out=sm[:, 32:40], in0=sm[:, 8:16], scalar1=sm[:, 17:18], scalar2=None, op0=ALU.is_equal),
             reads=[b_sm], writes=[b_sm])
        S.op("dve", lambda e: e.tensor_tensor(out=sm[:, 40:41], in0=sm[:, 17:18], in1=sm[:, 16:17], op=ALU.subtract), reads=[b_sm], writes=[b_sm])
        S.op("act", lambda e: e.activation(out=sm[:, 41:42], in_=sm[:, 40:41], func=AF.Exp), reads=[b_sm], writes=[b_sm])
        S.op("dve", lambda e: e.tensor_scalar(out=sm[:, 42:43], in0=sm[:, 41:42], scalar1=1.0, scalar2=None, op0=ALU.add), reads=[b_sm], writes=[b_sm])
        S.op("dve", lambda e: e.reciprocal(out=sm[:, 42:43], in_=sm[:, 42:43]), reads=[b_sm], writes=[b_sm])
        S.op("dve", lambda e: e.tensor_tensor(out=sm[:, 43:44], in0=sm[:, 41:42], in1=sm[:, 42:43], op=ALU.mult), reads=[b_sm], writes=[b_sm])
        S.op("dve", lambda e: e.tensor_scalar(out=self.gate[:, ti, :], in0=sm[:, 42:44], scalar1=sm[:, 3:4], scalar2=None, op0=ALU.mult),
             reads=[b_sm], writes=[self.b_gate[ti]])
        for k in range(2):
            for g in range(4):
                S.op("dve", lambda e, k=k, g=g: e.tensor_scalar(out=self.OH[:, ti, k, g * 8:(g + 1) * 8], in0=sm[:, 24 + 8 * k:32 + 8 * k],
                                                               scalar1=sm[:, 4 + g:5 + g], scalar2=None, op0=ALU.mult),
                     reads=[b_sm], writes=[self.b_OH[ti]])

    def plan(self):
        cx, S = self.cx, self.cx.S
        t = self.tag
        ones_f, b_ones = self.c["ones_f"]
        ident_f, b_identf = self.c["ident_f"]
        blkstart, b_blk = self.c["blkstart"]
        PBt, bPB = cx.PB[0], cx.PBb[0]
        for ti in range(self.nt):
            S.op("pe", lambda e, ti=ti: e.matmul(PBt[:, 0:64], lhsT=ones_f[:], rhs=self.OH[:, ti, :, :].rearrange("p k e -> p (k e)"),
                                                 start=(ti == 0), stop=(ti == self.nt - 1)),
                 reads=[b_ones, self.b_OH[ti]], writes=[bPB])
        pl = cx.sb("pl" + t, [128, 8, 32]); b_pl = Buf()
        S.op("dve", lambda e: e.tensor_copy(out=pl[:, 7, :], in_=PBt[:, 0:32]), reads=[bPB], writes=[b_pl])
        S.op("dve", lambda e: e.tensor_tensor(out=pl[:, 0, :], in0=pl[:, 7, :], in1=PBt[:, 32:64], op=ALU.add), reads=[bPB, b_pl], writes=[b_pl])
        thr, b_thr = self.c["thr"]
        cmp3 = cx.sb("cmp3" + t, [128, 32, 34]); b_cmp3 = Buf()
        S.op("dve", lambda e: e.tensor_tensor(out=cmp3[:], in0=pl[:, 0, :].unsqueeze(2).broadcast_to([128, 32, 34]),
                                              in1=thr[:].unsqueeze(1).broadcast_to([128, 32, 34]), op=ALU.is_gt),
             reads=[b_pl, b_thr], writes=[b_cmp3])
        S.op("dve", lambda e: e.tensor_reduce(out=pl[:, 1, :], in_=cmp3[:], axis=AX.X, op=ALU.add), reads=[b_cmp3], writes=[b_pl])
        S.op("dve", lambda e: e.tensor_scalar(out=pl[:, 3, :], in0=pl[:, 1, :], scalar1=256.0, scalar2=None, op0=ALU.mult), reads=[b_pl], writes=[b_pl])
        S.op("dve", lambda e: e.memset(pl[:, 5, :], 0.0), reads=[b_pl], writes=[b_pl])
        S.op("dve", lambda e: e.tensor_tensor_scan(out=pl[:, 4, :], data0=pl[:, 3, :], data1=pl[:, 5, :], initial=0.0, op0=ALU.add, op1=ALU.add),
             reads=[b_pl], writes=[b_pl])
        self.run = cx.sb("run" + t, [128, 32]); self.b_run = Buf()
        S.op("dve", lambda e: e.tensor_tensor(out=self.run[:], in0=pl[:, 4, :], in1=pl[:, 3, :], op=ALU.subtract), reads=[b_pl], writes=[self.b_run])
        S.op("dve", lambda e: e.tensor_scalar(out=pl[:, 6, :], in0=pl[:, 4, :], scalar1=blkstart[:, 0:1], scalar2=None, op0=ALU.is_le),
             reads=[b_pl, b_blk], writes=[b_pl])
        be = cx.sb("be" + t, [128, 1]); b_be = Buf()
        S.op("dve", lambda e: e.tensor_reduce(out=be[:], in_=pl[:, 6, :], axis=AX.X, op=ALU.add), reads=[b_pl], writes=[b_be])
        S.op("dve", lambda e: e.tensor_scalar(out=be[:], in0=be[:], scalar1=31.0, scalar2=128.0, op0=ALU.min, op1=ALU.mult), reads=[b_be], writes=[b_be])
        bbc = cx.sb("bbc" + t, [128, 128]); b_bbc = Buf()
        pcol, b_pcol = self.c["pcol"]
        S.op("dve", lambda e: e.tensor_scalar(out=bbc[:], in0=ones_f[:], scalar1=be[:, 0:1], scalar2=None, op0=ALU.mult), reads=[b_be, b_ones], writes=[b_bbc])
        PB1, bPB1 = cx.PB[1], cx.PBb[1]
        S.op("pe", lambda e: e.matmul(PB1[:, 0:128], lhsT=bbc[:], rhs=ident_f[:], start=True, stop=True), reads=[b_bbc, b_identf], writes=[bPB1])
        idxf = cx.sb("idxf" + t, [128, 128]); b_idxf = Buf()
        S.op("dve", lambda e: e.tensor_scalar(out=idxf[:], in0=PB1[:, 0:128], scalar1=pcol[:, 0:1], scalar2=None, op0=ALU.add), reads=[bPB1, b_pcol], writes=[b_idxf])
        S.op("dve", lambda e: e.tensor_copy(out=self.idxw[:], in_=idxf[:]), reads=[b_idxf], writes=[self.b_idxw])
        self.d1 = cx.sb("d1" + t, [128, 32]); self.b_d1 = Buf()
        self.destf = cx.sb("destf" + t, [128, 2]); self.b_destf = Buf()

    def dispatch_tile(self, ti, v_ap, b_v, do_scatter=True):
        cx, S = self.cx, self.cx.S
        ones_f, b_ones = self.c["ones_f"]
        slt, b_slt = self.c["slt"]
        for k in range(2):
            pa, ba = cx.PB[2 + k], cx.PBb[2 + k]
            pt_, bt_ = cx.PB[4 + k], cx.PBb[4 + k]
            S.op("pe", lambda e, k=k, pa=pa: e.matmul(pa[:, 0:32], lhsT=slt[:], rhs=self.OH[:, ti, k, :], start=True, stop=True),
                 reads=[b_slt, self.b_OH[ti]], writes=[ba])
            S.op("pe", lambda e, k=k, pt_=pt_: e.matmul(pt_[:, 0:32], lhsT=ones_f[:], rhs=self.OH[:, ti, k, :], start=True, stop=True),
                 reads=[b_ones, self.b_OH[ti]], writes=[bt_])
            S.op("dve", lambda e, pa=pa: e.tensor_tensor(out=self.d1[:], in0=pa[:, 0:32], in1=self.run[:], op=ALU.add),
                 reads=[ba, self.b_run], writes=[self.b_d1])
            S.op("dve", lambda e, k=k: e.tensor_tensor(out=self.d1[:], in0=self.d1[:], in1=self.OH[:, ti, k, :], op=ALU.mult),
                 reads=[self.b_d1, self.b_OH[ti]], writes=[self.b_d1])
            S.op("dve", lambda e, k=k: e.tensor_reduce(out=self.destf[:, k:k + 1], in_=self.d1[:], axis=AX.X, op=ALU.add),
                 reads=[self.b_d1], writes=[self.b_destf])
            S.op("dve", lambda e, k=k: e.tensor_copy(out=self.dest[:, ti, k:k + 1], in_=self.destf[:, k:k + 1]),
                 reads=[self.b_destf], writes=[self.b_dest[ti]])
            S.op("dve", lambda e, pt_=pt_: e.tensor_tensor(out=self.run[:], in0=self.run[:], in1=pt_[:, 0:32], op=ALU.add),
                 reads=[bt_, self.b_run], writes=[self.b_run])
            if do_scatter:
              S.dma("pool", self.xbuf, v_ap, reads=[b_v, self.b_dest[ti]], writes=[self.b_xbuf],
                  indirect={"out_offset": bass.IndirectOffsetOnAxis(ap=self.dest[:, ti, k:k + 1], axis=0)})

    def experts(self):
        cx, S, nc = self.cx, self.cx.S, self.cx.nc
        t = self.tag
        ident_bf, b_identbf = self.c["ident_bf"]
        NW = 3
        w1_s = [cx.sb("w1_s%d%s" % (i, t), [128, 8, 512], BF16) for i in range(NW)]
        w3_s = [cx.sb("w3_s%d%s" % (i, t), [128, 8, 512], BF16) for i in range(NW)]
        w2_s = [cx.sb("w2_s%d%s" % (i, t), [128, 4, 1024], BF16) for i in range(NW)]
        b_w1 = [Buf() for _ in range(NW)]; b_w3 = [Buf() for _ in range(NW)]; b_w2 = [Buf() for _ in range(NW)]
        xb = [cx.sb("xb%d%s" % (i, t), [128, D], BF16) for i in range(3)]; b_xb = [Buf() for _ in range(3)]
        xT = [cx.sb("xT%d%s" % (i, t), [128, 8, 128], BF16) for i in range(2)]; b_xT = [Buf(), Buf()]
        s1 = cx.sb("s1" + t, [128, 512]); b_s1 = Buf()
        hh = [cx.sb("hh%d%s" % (i, t), [128, 512], BF16) for i in range(2)]; b_hh = [Buf(), Buf()]
        hhT = [cx.sb("hhT%d%s" % (i, t), [128, 4, 128], BF16) for i in range(2)]; b_hhT = [Buf(), Buf()]
        yst = [cx.sb("yst%d%s" % (i, t), [128, D]) for i in range(2)]; b_yst = [Buf(), Buf()]
        PTa, b_PTa = cx.PT, cx.b_PT
        PTb, b_PTb = cx.PB[6][:].bitcast(BF16), cx.PBb[6]
        pa, ba = cx.PB[0], cx.PBb[0]
        pb_, bb = cx.PB[1], cx.PBb[1]
        pc = [cx.PB[2], cx.PB[3]]; bc = [cx.PBb[2], cx.PBb[3]]
        nsub = 2 * self.nblk

        def load_w(blk):
            p = blk % NW
            off = bass.IndirectOffsetOnAxis(ap=self.idxw[:, blk:blk + 1], axis=0)
            S.dma("pool", w1_s[p][:].rearrange("p j n -> p (j n)"), self.W1b, reads=[self.b_idxw, self.b_W1b], writes=[b_w1[p]], indirect={"in_offset": off})
            S.dma("pool", w3_s[p][:].rearrange("p j n -> p (j n)"), self.W3b, reads=[self.b_idxw, self.b_W3b], writes=[b_w3[p]], indirect={"in_offset": off})
            S.dma("pool", w2_s[p][:].rearrange("p j n -> p (j n)"), self.W2b, reads=[self.b_idxw, self.b_W2b], writes=[b_w2[p]], indirect={"in_offset": off})

        def load_x(sidx):
            S.dma("sp", xb[sidx % 3][:], self.xbuf[sidx * 128:(sidx + 1) * 128, :], reads=[self.b_xbuf], writes=[b_xb[sidx % 3]])

        def stA(sidx):
            q = sidx % 2
            for j in range(8):
                S.op("pe", lambda e, j=j: e.transpose(PTa[:, j * 128:(j + 1) * 128], xb[sidx % 3][:].rearrange("t (p j) -> t j p", j=8)[:, j, :], ident_bf[:]),
                     reads=[b_xb[sidx % 3], b_identbf], writes=[b_PTa], inc=(j == 7))
            S.op("act", lambda e: e.activation(out=xT[q][:], in_=PTa[:].rearrange("p (j t) -> p j t", j=8), func=AF.Copy), reads=[b_PTa], writes=[b_xT[q]])

        def stB(sidx):
            q = sidx % 2; p = (sidx // 2) % NW
            for j in range(8):
                S.op("pe", lambda e, j=j: e.matmul(pa[:], lhsT=xT[q][:, j, :], rhs=w1_s[p][:, j, :], start=(j == 0), stop=(j == 7)),
                     reads=[b_xT[q], b_w1[p]], writes=[ba], inc=(j == 7))
            for j in range(8):
                S.op("pe", lambda e, j=j: e.matmul(pb_[:], lhsT=xT[q][:, j, :], rhs=w3_s[p][:, j, :], start=(j == 0), stop=(j == 7)),
                     reads=[b_xT[q], b_w3[p]], writes=[bb], inc=(j == 7))
            S.op("act", lambda e: e.activation(out=s1[:], in_=pa[:], func=AF.Silu), reads=[ba], writes=[b_s1])
            S.op("dve", lambda e: e.tensor_tensor(out=hh[q][:], in0=s1[:], in1=pb_[:], op=ALU.mult), reads=[b_s1, bb], writes=[b_hh[q]])

        def stC(sidx):
            q = sidx % 2
            for j in range(4):
                S.op("pe", lambda e, j=j: e.transpose(PTb[:, j * 128:(j + 1) * 128], hh[q][:].rearrange("t (p j) -> t j p", j=4)[:, j, :], ident_bf[:]),
                     reads=[b_hh[q], b_identbf], writes=[b_PTb], inc=(j == 3))
            S.op("act", lambda e: e.activation(out=hhT[q][:], in_=PTb[:, 0:512].rearrange("p (j t) -> p j t", j=4), func=AF.Copy), reads=[b_PTb], writes=[b_hhT[q]])

        def stD(sidx):
            q = sidx % 2; p = (sidx // 2) % NW
            for half in range(2):
                for j in range(4):
                    S.op("pe", lambda e, j=j, half=half: e.matmul(pc[half][:], lhsT=hhT[q][:, j, :], rhs=w2_s[p][:, j, half * 512:(half + 1) * 512],
                                                                  start=(j == 0), stop=(j == 3)),
                         reads=[b_hhT[q], b_w2[p]], writes=[bc[half]], inc=(j == 3))
            S.op("act", lambda e: e.activation(out=yst[q][:, 0:512], in_=pc[0][:], func=AF.Copy), reads=[bc[0]], writes=[b_yst[q]])
            S.op("dve", lambda e: e.tensor_copy(out=yst[q][:, 512:1024], in_=pc[1][:]), reads=[bc[1]], writes=[b_yst[q]])
            S.dma("sp", self.ybuf[sidx * 128:(sidx + 1) * 128, :], yst[q][:], reads=[b_yst[q]], writes=[self.b_ybuf])

        load_w(0)
        load_x(0)
        for i in range(nsub + 2):
            if i % 2 == 0 and i // 2 + 1 < self.nblk:
                load_w(i // 2 + 1)
            if i + 1 < nsub:
                load_x(i + 1)
            if 0 <= i - 2 < nsub:
                stC(i - 2)
            if i < nsub:
                stA(i)
            if 0 <= i - 1 < nsub:
                stB(i - 1)
            if 0 <= i - 2 < nsub:
                stD(i - 2)

    def gather_tile(self, ti, y0_ap, y1_ap, b_y0, b_y1):
        S = self.cx.S
        S.dma("pool", y0_ap, self.ybuf, reads=[self.b_ybuf, self.b_dest[ti]], writes=[b_y0],
              indirect={"in_offset": bass.IndirectOffsetOnAxis(ap=self.dest[:, ti, 0:1], axis=0)})
        S.dma("pool", y1_ap, self.ybuf, reads=[self.b_ybuf, self.b_dest[ti]], writes=[b_y1],
              indirect={"in_offset": bass.IndirectOffsetOnAxis(ap=self.dest[:, ti, 1:2], axis=0)})


D = 1024
NT = 66
NG = 22
RS = 128 ** -0.5
EPS = 1e-6


def host_consts_l1():
    c = {}
    c["ident_bf"] = np.eye(128, dtype=np.float32).astype(ml_dtypes.bfloat16)
    c["ident_f"] = np.eye(128, dtype=np.float32)
    s = np.arange(128)
    mf = (s[:, None] <= s[None, :]).astype(np.float32)
    mb = (s[:, None] >= s[None, :]).astype(np.float32)
    c["mask_f32"] = np.stack([mf, mb], 1)
    c["mask_bf"] = c["mask_f32"].astype(ml_dtypes.bfloat16)
    c["ones_f"] = np.ones((128, 128), np.float32)
    pos = np.arange(128, dtype=np.float32)
    c["poscol"] = np.stack([127.0 - pos, pos, -(127.0 - pos), -pos], 1).astype(np.float32)
    T = NT * 128
    inv = (10000.0 ** (-np.arange(64, dtype=np.float32) / 64)).astype(np.float32)
    ang = (np.arange(T, dtype=np.float32)[:, None] * inv[None, :]).astype(np.float32)
    cos = np.cos(ang).astype(np.float32).T
    sin = np.sin(ang).astype(np.float32).T
    c["ropecos"] = np.ascontiguousarray(np.concatenate([cos, cos], 0))
    c["ropesin"] = np.ascontiguousarray(np.concatenate([-sin, sin], 0))
    return c


def build_l1(cx, io):
    nc = cx.nc
    S = cx.S
    cx.phase_begin("L1")

    def din(name, shape, dt=F32):
        return io[name]

    xin = din("xin", [NT * 128, D])
    cT = din("cT", [128, 8])
    cctxT = din("cctxT", [128, 8])
    wmod = din("wmod", [D, 2048])
    bmod = din("bmod", [1, 2048])
    g1 = din("g1", [1, D])
    Wfm = din("Wfm", [D, 1536])
    Wtm = din("Wtm", [D, 1032])
    gbias = din("gbias", [1, 8])
    rld = din("rld", [1, 4])
    mlg = din("mlg", [1, 256])
    retg = din("retg", [1, 256])
    ident_bf_d = din("ident_bf", [128, 128], BF16)
    ident_f_d = din("ident_f", [128, 128])
    mask_f32_d = din("mask_f32", [128, 2, 128])
    mask_bf_d = din("mask_bf", [128, 2, 128], BF16)
    ones_d = din("ones_f", [128, 128])
    poscol_d = din("poscol", [128, 4])
    ropecos_d = din("ropecos", [128, NT * 128])
    ropesin_d = din("ropesin", [128, NT * 128])
    merged = io["merged"]

    FM = nc.dram_tensor("FM", [NT, 128, 1024], BF16).ap()
    TM = nc.dram_tensor("TM", [NT, 128, 1024], BF16).ap()
    OG = nc.dram_tensor("OG", [NT, 128, 512], BF16).ap()
    HS = nc.dram_tensor("HS", [2, NT, 128, 512], F32).ap()

    sb = cx.sb
    phase_begin = cx.phase_begin
    phase_end = cx.phase_end

    ident_bf = sb("ident_bf_s", [128, 128], BF16); b_identbf = Buf()
    ident_f = sb("ident_f_s", [128, 128]); b_identf = Buf()
    mask_f32 = sb("mask_f32_s", [128, 2, 128]); b_maskf = Buf()
    mask_bf = sb("mask_bf_s", [128, 2, 128], BF16); b_maskbf = Buf()
    ones_f = sb("ones_s", [128, 128]); b_ones = Buf()
    poscol = sb("poscol_s", [128, 4]); b_poscol = Buf()
    S.dma("sp", ident_bf[:], ident_bf_d, writes=[b_identbf])
    S.dma("sp", ident_f[:], ident_f_d, writes=[b_identf])
    S.dma("sp", mask_f32[:], mask_f32_d, writes=[b_maskf])
    S.dma("sp", mask_bf[:], mask_bf_d, writes=[b_maskbf])
    S.dma("sp", ones_f[:], ones_d, writes=[b_ones])
    S.dma("sp", poscol[:], poscol_d, writes=[b_poscol])

    Gall = sb("Gall", [128, 8, NT]); b_G = Buf()
    phase_begin("L1_A")
    Wfm_s = sb("Wfm_s", [128, 8, 1536], BF16); b_wfm = Buf()
    Wtm_s = sb("Wtm_s", [128, 8, 1032], BF16); b_wtm = Buf()
    for j in range(8):
        S.dma("pool", Wfm_s[:, j, :], Wfm[j * 128:(j + 1) * 128, :], writes=[b_wfm])
        S.dma("pool", Wtm_s[:, j, :], Wtm[j * 128:(j + 1) * 128, :], writes=[b_wtm])

    PB = cx.PB
    PBb = cx.PBb

    cT_s = sb("cT_s", [128, 2, 8]); b_cT = Buf()
    S.dma("sp", cT_s[:, 0, :], cT, writes=[b_cT])
    S.dma("sp", cT_s[:, 1, :], cctxT, writes=[b_cT])
    sc = sb("sc", [128, 2, 8]); b_sc = Buf()
    S.op("act", lambda e: e.activation(out=sc[:], in_=cT_s[:], func=AF.Silu), reads=[b_cT], writes=[b_sc])
    scb = sb("scb", [128, 2, 8, 128]); b_scb = Buf()
    for w in range(2):
        for j in range(8):
            S.op("dve", lambda e, w=w, j=j: e.tensor_scalar(out=scb[:, w, j, :], in0=ones_f[:], scalar1=sc[:, w, j:j + 1],
                                                           scalar2=None, op0=ALU.mult),
                 reads=[b_ones, b_sc], writes=[b_scb])
    bmod_s = sb("bmod_s", [128, 2048]); b_bmod = Buf()
    S.dma("sp", bmod_s[:], bmod.partition_broadcast(128), writes=[b_bmod])
    g1_s = sb("g1_s", [128, D]); b_g1 = Buf()
    S.dma("sp", g1_s[:], g1.partition_broadcast(128), writes=[b_g1])
    modS = sb("modS", [128, 2, D]); modA = sb("modA", [128, 2, D]); b_mod = Buf()
    wm_s = [sb("wm_s%d" % i, [128, 8, 512]) for i in range(2)]
    b_wm = [Buf(), Buf()]
    wmod_v = wmod.rearrange("(j p) n -> p j n", p=128)
    for cc in range(4):
        wb = cc % 2
        S.dma("sp", wm_s[wb][:], wmod_v[:, :, cc * 512:(cc + 1) * 512], writes=[b_wm[wb]])
        for w in range(2):
            pbi = (cc * 2 + w) % 7
            for j in range(8):
                S.op("pe", lambda e, w=w, j=j, wb=wb, pbi=pbi: e.matmul(PB[pbi][:], lhsT=scb[:, w, j, :], rhs=wm_s[wb][:, j, :],
                                                                        start=(j == 0), stop=(j == 7)),
                     reads=[b_scb, b_wm[wb]], writes=[PBb[pbi]])
            half = cc % 2
            if cc < 2:
                S.op("dve", lambda e, w=w, pbi=pbi, cc=cc, half=half: e.tensor_tensor(
                    out=modS[:, w, half * 512:(half + 1) * 512], in0=PB[pbi][:], in1=bmod_s[:, cc * 512:(cc + 1) * 512], op=ALU.add),
                    reads=[PBb[pbi], b_bmod], writes=[b_mod])
            else:
                S.op("dve", lambda e, w=w, pbi=pbi, cc=cc, half=half: e.tensor_tensor(
                    out=modA[:, w, half * 512:(half + 1) * 512], in0=PB[pbi][:], in1=bmod_s[:, cc * 512:(cc + 1) * 512], op=ALU.add),
                    reads=[PBb[pbi], b_bmod], writes=[b_mod])
                S.op("dve", lambda e, w=w, half=half: e.scalar_tensor_tensor(
                    out=modA[:, w, half * 512:(half + 1) * 512], in0=modA[:, w, half * 512:(half + 1) * 512], scalar=1.0,
                    in1=g1_s[:, half * 512:(half + 1) * 512], op0=ALU.add, op1=ALU.mult),
                    reads=[b_mod, b_g1], writes=[b_mod])

    xt = [sb("xt%d" % i, [128, D]) for i in range(2)]; b_xt = [Buf(), Buf()]
    junk = sb("junk", [128, D], BF16); b_junk = Buf()
    ss = sb("ss", [128, 2]); b_ss = [Buf(), Buf()]
    rstd = sb("rstd", [128, 2]); b_rstd = [Buf(), Buf()]
    tmp32 = sb("tmp32", [128, D]); b_tmp32 = Buf()
    u_bf = [sb("u_bf%d" % i, [128, D], BF16) for i in range(2)]; b_u = [Buf(), Buf()]
    uT = [sb("uT%d" % i, [128, 8, 384], BF16) for i in range(2)]
    b_uT = [[Buf() for _ in range(3)] for _ in range(2)]
    FMst = [sb("FMst%d" % i, [128, 3, 8, 128], BF16) for i in range(2)]
    b_FMst = [[Buf() for _ in range(8)] for _ in range(2)]
    TMst = [sb("TMst%d" % i, [128, 3, 1024], BF16) for i in range(2)]
    b_TMst = [[Buf() for _ in range(3)] for _ in range(2)]
    OGst = [sb("OGst%d" % i, [128, 3, 512], BF16) for i in range(2)]
    b_OGst = [[Buf() for _ in range(3)] for _ in range(2)]
    rc = [sb("rc%d" % i, [128, 384]) for i in range(2)]; rsn = [sb("rsn%d" % i, [128, 384]) for i in range(2)]
    b_rope = [Buf(), Buf()]
    t1 = sb("t1", [128, 384]); t2 = sb("t2", [128, 384]); b_t1 = Buf(); b_t2 = Buf()
    PT = cx.PT
    b_PT = cx.b_PT
    b_FM_d = [Buf() for _ in range(NT)]
    b_TM_d = [Buf() for _ in range(NT)]
    b_OG_d = [Buf() for _ in range(NT)]
    tmi = 0
    fmi = 0
    def pre_group(g):
        p = g % 2
        S.dma("sp", rc[p][:], ropecos_d[:, g * 384:(g + 1) * 384], writes=[b_rope[p]])
        S.dma("sp", rsn[p][:], ropesin_d[:, g * 384:(g + 1) * 384], writes=[b_rope[p]])
        for ti in range(3):
            c = g * 3 + ti
            w = 1 if c < 2 else 0
            xp = c % 2
            S.dma("sp", xt[xp][:], xin[c * 128:(c + 1) * 128, :], writes=[b_xt[xp]])
            S.op("dve", lambda e, xp=xp: e.memset(ss[:, xp:xp + 1], 0.0), writes=[b_ss[xp]])
            S.op("act", lambda e, xp=xp: e.activation(out=junk[:], in_=xt[xp][:], func=AF.Square, accum_out=ss[:, xp:xp + 1]),
                 reads=[b_xt[xp]], writes=[b_junk, b_ss[xp]])
            S.op("dve", lambda e, xp=xp: e.tensor_scalar(out=rstd[:, xp:xp + 1], in0=ss[:, xp:xp + 1], scalar1=1.0 / D, scalar2=EPS,
                                                        op0=ALU.mult, op1=ALU.add), reads=[b_ss[xp]], writes=[b_rstd[xp]])
            S.op("act", lambda e, xp=xp: e.activation(out=rstd[:, xp:xp + 1], in_=rstd[:, xp:xp + 1], func=AF.Sqrt),
                 reads=[b_rstd[xp]], writes=[b_rstd[xp]])
            S.op("dve", lambda e, xp=xp: e.reciprocal(out=rstd[:, xp:xp + 1], in_=rstd[:, xp:xp + 1]), reads=[b_rstd[xp]], writes=[b_rstd[xp]])
            S.op("dve", lambda e, xp=xp, w=w: e.scalar_tensor_tensor(out=tmp32[:], in0=xt[xp][:], scalar=rstd[:, xp:xp + 1],
                                                                    in1=modA[:, w, :], op0=ALU.mult, op1=ALU.mult),
                 reads=[b_xt[xp], b_rstd[xp], b_mod], writes=[b_tmp32])
            S.op("dve", lambda e, xp=xp, w=w: e.tensor_tensor(out=u_bf[xp][:], in0=tmp32[:], in1=modS[:, w, :], op=ALU.add),
                 reads=[b_tmp32, b_mod], writes=[b_u[xp]])
            for j in range(8):
                S.op("pe", lambda e, xp=xp, j=j: e.transpose(PT[:, j * 128:(j + 1) * 128], u_bf[xp][:, j * 128:(j + 1) * 128], ident_bf[:]),
                     reads=[b_u[xp], b_identbf], writes=[b_PT])
            S.op("act", lambda e, p=p, ti=ti: e.activation(out=uT[p][:, :, ti * 128:(ti + 1) * 128],
                                                          in_=PT[:].rearrange("p (j t) -> p j t", j=8), func=AF.Copy),
                 reads=[b_PT], writes=[b_uT[p][ti]])

    def main_group(g):
        nonlocal tmi, fmi
        p = g % 2
        for ti in range(3):
            c = g * 3 + ti
            for (c0, c1) in [(0, 512), (512, 1024), (1024, 1032)]:
                pbi = tmi % 3; tmi += 1
                for j in range(8):
                    S.op("pe", lambda e, p=p, ti=ti, j=j, c0=c0, c1=c1, pbi=pbi: e.matmul(
                        PB[pbi][:, 0:c1 - c0], lhsT=uT[p][:, j, ti * 128:(ti + 1) * 128], rhs=Wtm_s[:, j, c0:c1],
                        start=(j == 0), stop=(j == 7)), reads=[b_uT[p][ti], b_wtm], writes=[PBb[pbi]])
                if c0 == 0:
                    S.op("act", lambda e, p=p, ti=ti, pbi=pbi: e.activation(out=TMst[p][:, ti, 512:1024], in_=PB[pbi][:], func=AF.Copy),
                         reads=[PBb[pbi]], writes=[b_TMst[p][ti]])
                elif c0 == 512:
                    S.op("act", lambda e, p=p, ti=ti, pbi=pbi: e.activation(out=OGst[p][:, ti, :], in_=PB[pbi][:], func=AF.Copy),
                         reads=[PBb[pbi]], writes=[b_OGst[p][ti]])
                else:
                    S.op("dve", lambda e, c=c, pbi=pbi: e.tensor_copy(out=Gall[:, :, c], in_=PB[pbi][:, 0:8]),
                         reads=[PBb[pbi]], writes=[b_G])
        def fm_mm(cb, pbi):
            for j in range(8):
                S.op("pe", lambda e, j=j: e.matmul(PB[pbi][:, 0:384], lhsT=Wfm_s[:, j, cb * 128:(cb + 1) * 128], rhs=uT[p][:, j, :],
                                                   start=(j == 0), stop=(j == 7)),
                     reads=[b_wfm] + b_uT[p], writes=[PBb[pbi]])
        for cb in range(4):
            pbi = 4 + fmi % 3; fmi += 1
            fm_mm(cb, pbi)
            sc_ = 1.0 if cb < 2 else RS
            S.op("act", lambda e, cb=cb, pbi=pbi, sc_=sc_: e.activation(
                out=FMst[p][:, :, cb, :], in_=PB[pbi][:, 0:384].rearrange("p (c t) -> p c t", c=3), func=AF.Copy, scale=sc_),
                reads=[PBb[pbi]], writes=[b_FMst[p][cb]])
        for qk in range(2):
            for h in range(2):
                cb_raw = 4 + qk * 4 + h
                cb_sw = 4 + qk * 4 + 2 + h
                pa = 4 + fmi % 3; fmi += 1
                fm_mm(cb_raw, pa)
                pb_ = 4 + fmi % 3; fmi += 1
                fm_mm(cb_sw, pb_)
                sc_ = 1.0 if qk == 0 else RS
                S.op("dve", lambda e, pa=pa, sc_=sc_: e.scalar_tensor_tensor(out=t1[:], in0=PB[pa][:, 0:384], scalar=sc_, in1=rc[p][:],
                                                                            op0=ALU.mult, op1=ALU.mult),
                     reads=[PBb[pa], b_rope[p]], writes=[b_t1])
                S.op("dve", lambda e, pb_=pb_, sc_=sc_: e.scalar_tensor_tensor(out=t2[:], in0=PB[pb_][:, 0:384], scalar=sc_, in1=rsn[p][:],
                                                                              op0=ALU.mult, op1=ALU.mult),
                     reads=[PBb[pb_], b_rope[p]], writes=[b_t2])
                a = 4 + qk * 2 + h
                S.op("dve", lambda e, a=a: e.tensor_tensor(out=FMst[p][:, :, a, :], in0=t1[:].rearrange("p (c t) -> p c t", c=3),
                                                           in1=t2[:].rearrange("p (c t) -> p c t", c=3), op=ALU.add),
                     reads=[b_t1, b_t2], writes=[b_FMst[p][a]])
        for ti in range(3):
            for di, a in enumerate([2, 3, 6, 7]):
                S.op("pe", lambda e, ti=ti, a=a, di=di: e.transpose(PTk[:, di * 128:(di + 1) * 128], FMst[p][:, ti, a, :], ident_bf[:]),
                     reads=[b_FMst[p][a], b_identbf], writes=[PBb[3]])
            S.op("act", lambda e, ti=ti: e.activation(out=TMst[p][:, ti, 0:512], in_=PTk[:, 0:512], func=AF.Copy),
                 reads=[PBb[3]], writes=[b_TMst[p][ti]])
        c0 = g * 3
        S.dma("sp", FM[c0:c0 + 3].rearrange("c p n -> p c n"), FMst[p][:].rearrange("p c a t -> p c (a t)"),
              reads=b_FMst[p], writes=b_FM_d[c0:c0 + 3])
        S.dma("sp", TM[c0:c0 + 3].rearrange("c p n -> p c n"), TMst[p][:], reads=b_TMst[p], writes=b_TM_d[c0:c0 + 3])
        S.dma("sp", OG[c0:c0 + 3].rearrange("c p n -> p c n"), OGst[p][:], reads=b_OGst[p], writes=b_OG_d[c0:c0 + 3])

    PTk = PB[3][:].bitcast(BF16)
    pre_group(0)
    for g in range(NG):
        if g + 1 < NG:
            pre_group(g + 1)
        main_group(g)
    phase_end()
    wml = sb("wml", [128, 4, NT]); b_wml = Buf()
    flo = sb("flo", [128, 4, NT]); b_flo = Buf()
    decb = sb("decb", [128, 4, NT]); b_decb = Buf()
    wret = sb("wret", [128, 4]); rho = sb("rho", [128, 4]); dret = sb("dret", [128, 4]); b_retc = Buf()
    phase_begin("L1_G")
    gb_s = sb("gb_s", [128, 8]); b_gb = Buf()
    S.dma("sp", gb_s[:], gbias.partition_broadcast(128), writes=[b_gb])
    Gi = sb("Gi", [128, 4, NT]); b_Gi = Buf()
    nlf = sb("nlf", [128, 4, NT]); b_nlf = Buf()
    for k in range(4):
        S.op("dve", lambda e, k=k: e.tensor_scalar(out=Gi[:, k, :], in0=Gall[:, k, :], scalar1=gb_s[:, k:k + 1], scalar2=None, op0=ALU.add),
             reads=[b_G, b_gb], writes=[b_Gi])
        S.op("dve", lambda e, k=k: e.tensor_scalar(out=nlf[:, k, :], in0=Gall[:, 4 + k, :], scalar1=gb_s[:, 4 + k:5 + k], scalar2=None, op0=ALU.add),
             reads=[b_G, b_gb], writes=[b_nlf])
    S.op("act", lambda e: e.activation(out=nlf[:], in_=nlf[:], func=AF.Exp, scale=-1.0), reads=[b_nlf], writes=[b_nlf])
    S.op("act", lambda e: e.activation(out=nlf[:], in_=nlf[:], func=AF.Ln, bias=1.0), reads=[b_nlf], writes=[b_nlf])
    nb = sb("nb", [128, 4, NT]); b_nb = Buf()
    nbL = sb("nbL", [128, 4, NT]); b_nbL = Buf()
    for d in range(2):
        S.op("pe", lambda e, d=d: e.matmul(PB[d][:, 0:2 * NT], lhsT=mask_f32[:, d, :], rhs=nlf[:, 2 * d:2 * d + 2, :].rearrange("p a c -> p (a c)"),
                                           start=True, stop=True), reads=[b_maskf, b_nlf], writes=[PBb[d]])
        S.op("dve", lambda e, d=d: e.tensor_copy(out=nb[:, 2 * d:2 * d + 2, :].rearrange("p a c -> p (a c)"), in_=PB[d][:, 0:2 * NT]),
             reads=[PBb[d]], writes=[b_nb])
    S.op("pe", lambda e: e.matmul(PB[2][:, 0:4 * NT], lhsT=ones_f[:], rhs=nlf[:].rearrange("p a c -> p (a c)"), start=True, stop=True),
         reads=[b_ones, b_nlf], writes=[PBb[2]])
    S.op("dve", lambda e: e.tensor_copy(out=nbL[:].rearrange("p a c -> p (a c)"), in_=PB[2][:, 0:4 * NT]), reads=[PBb[2]], writes=[b_nbL])
    av = sb("av", [128, 4, NT]); b_av = Buf()
    S.op("dve", lambda e: e.tensor_tensor(out=av[:], in0=Gi[:], in1=nb[:], op=ALU.add), reads=[b_Gi, b_nb], writes=[b_av])
    avf = av[:].rearrange("p a c -> p (a c)")
    Acol = sb("Acol", [128, 3]); b_Acol = Buf()
    S.op("dve", lambda e: e.memset(Acol[:], 0.0), writes=[b_Acol])
    pieces = [(0, 128), (128, 256), (256, 264)]
    for pi, (a0, a1) in enumerate(pieces):
        m = a1 - a0
        S.op("pe", lambda e, a0=a0, a1=a1, m=m, pi=pi: e.matmul(PB[3 + pi][0:m, 0:128], lhsT=avf[:, a0:a1], rhs=ident_f[:], start=True, stop=True),
             reads=[b_av, b_identf], writes=[PBb[3 + pi]])
        S.op("dve", lambda e, m=m, pi=pi: e.tensor_reduce(out=Acol[0:m, pi:pi + 1], in_=PB[3 + pi][0:m, 0:128], axis=AX.X, op=ALU.max),
             reads=[PBb[3 + pi]], writes=[b_Acol])
    Arow = sb("Arow", [1, 4, NT]); b_Arow = Buf()
    for pi, (a0, a1) in enumerate(pieces):
        m = a1 - a0
        S.op("pe", lambda e, a0=a0, a1=a1, m=m, pi=pi: e.matmul(PB[6][0:1, a0:a1], lhsT=Acol[0:128, pi:pi + 1], rhs=ident_f[:, 0:m],
                                                                start=True, stop=True), reads=[b_Acol, b_identf], writes=[PBb[6]])
    S.op("dve", lambda e: e.tensor_copy(out=Arow[:].rearrange("p a c -> p (a c)"), in_=PB[6][0:1, 0:4 * NT]), reads=[PBb[6]], writes=[b_Arow])
    MLrow = sb("MLrow", [1, 4, NT]); b_MLrow = Buf()
    dargrow = sb("dargrow", [1, 4, NT]); b_darg = Buf()
    mstate = sb("mstate", [1, 4]); b_ms = [Buf(), Buf()]
    b_MLd = [Buf(), Buf()]; b_dargd = [Buf(), Buf()]
    S.op("dve", lambda e: e.memset(mstate[:], 0.0), writes=b_ms)
    order = [list(range(NT)), [1, 0] + list(range(NT - 1, 1, -1))]
    engs = ["dve", "dve"]
    for i in range(NT):
        for d in range(2):
            c = order[d][i]
            en = engs[d]
            sl = slice(2 * d, 2 * d + 2)
            S.op(en, lambda e, c=c, sl=sl: e.tensor_tensor(out=MLrow[:, sl, c], in0=mstate[:, sl], in1=Arow[:, sl, c], op=ALU.max),
                 reads=[b_ms[d], b_Arow], writes=[b_MLd[d]])
            S.op(en, lambda e, c=c, sl=sl: e.tensor_tensor(out=dargrow[:, sl, c], in0=mstate[:, sl], in1=MLrow[:, sl, c], op=ALU.subtract),
                 reads=[b_ms[d], b_MLd[d]], writes=[b_dargd[d]])
            S.op(en, lambda e, c=c, sl=sl: e.tensor_tensor(out=mstate[:, sl], in0=MLrow[:, sl, c], in1=nbL[0:1, sl, c], op=ALU.subtract),
                 reads=[b_nbL, b_MLd[d]], writes=[b_ms[d]])
    MLb = sb("MLb", [128, 4, NT]); b_MLb = Buf()
    S.op("pe", lambda e: e.matmul(PB[0][:, 0:4 * NT], lhsT=ones_f[0:1, :], rhs=MLrow[:].rearrange("p a c -> p (a c)"), start=True, stop=True),
         reads=[b_ones] + b_MLd + b_dargd, writes=[PBb[0]])
    S.op("dve", lambda e: e.tensor_copy(out=MLb[:].rearrange("p a c -> p (a c)"), in_=PB[0][:, 0:4 * NT]), reads=[PBb[0]], writes=[b_MLb])
    S.op("pe", lambda e: e.matmul(PB[1][:, 0:4 * NT], lhsT=ones_f[0:1, :], rhs=dargrow[:].rearrange("p a c -> p (a c)"), start=True, stop=True),
         reads=[b_ones] + b_MLd + b_dargd, writes=[PBb[1]])
    S.op("act", lambda e: e.activation(out=decb[:].rearrange("p a c -> p (a c)"), in_=PB[1][:, 0:4 * NT], func=AF.Exp), reads=[PBb[1]], writes=[b_decb])
    S.op("dve", lambda e: e.tensor_tensor(out=wml[:], in0=av[:], in1=MLb[:], op=ALU.subtract), reads=[b_av, b_MLb], writes=[b_wml])
    S.op("act", lambda e: e.activation(out=wml[:], in_=wml[:], func=AF.Exp), reads=[b_wml], writes=[b_wml])
    S.op("dve", lambda e: e.tensor_tensor(out=flo[:], in0=nb[:], in1=MLb[:], op=ALU.subtract), reads=[b_nb, b_MLb], writes=[b_flo])
    S.op("act", lambda e: e.activation(out=flo[:], in_=flo[:], func=AF.Exp), reads=[b_flo], writes=[b_flo])
    lg = sb("lg", [128, 4]); b_lg = Buf()
    S.dma("sp", lg[:], rld.partition_broadcast(128), writes=[b_lg])
    for d in range(2):
        for h in range(2):
            x = 2 * d + h
            S.op("act", lambda e, x=x, d=d: e.activation(out=wret[:, x:x + 1], in_=poscol[:, d:d + 1], func=AF.Exp, scale=lg[:, x:x + 1]),
                 reads=[b_poscol, b_lg], writes=[b_retc])
            S.op("act", lambda e, x=x, d=d: e.activation(out=rho[:, x:x + 1], in_=poscol[:, 2 + d:3 + d], func=AF.Exp, scale=lg[:, x:x + 1]),
                 reads=[b_poscol, b_lg], writes=[b_retc])
    S.op("act", lambda e: e.activation(out=dret[:], in_=lg[:], func=AF.Exp, scale=128.0), reads=[b_lg], writes=[b_retc])

    phase_end()
    phase_begin("L1_S")
    if "after_weights" in io:
        io["after_weights"]()
    FMl = [[sb("FMl%d%d" % (d, i), [128, 8, 128], BF16) for i in range(2)] for d in range(2)]
    TMl = [[sb("TMl%d%d" % (d, i), [128, 8, 128], BF16) for i in range(2)] for d in range(2)]
    b_FMl = [[Buf() for _ in range(2)] for _ in range(2)]
    b_TMl = [[Buf() for _ in range(2)] for _ in range(2)]
    va = [sb("va%d" % i, [128, 132], BF16) for i in range(4)]; b_va = [Buf() for _ in range(4)]
    sm = [sb("sm%d" % i, [128, 128], BF16) for i in range(4)]; b_sm = [Buf() for _ in range(4)]
    CT32 = sb("CT32", [128, 8, 132]); CTd32 = sb("CTd32", [128, 8, 132]); CTbf = sb("CTbf", [128, 8, 132], BF16)
    b_CT32 = [Buf() for _ in range(8)]; b_CTd = [Buf() for _ in range(8)]; b_CTbf = [Buf() for _ in range(8)]
    S.op("dve", lambda e: e.memset(CTd32[:], 0.0), writes=b_CTd)
    S.op("dve", lambda e: e.memset(CTbf[:], 0.0), writes=b_CTbf)
    HSst = [[sb("HSst%d%d" % (d, i), [128, 512]) for i in range(2)] for d in range(2)]
    b_HSst = [[Buf() for _ in range(2)] for _ in range(2)]
    den = sb("den", [128, 4]); b_den = [Buf() for _ in range(4)]
    b_HS_d = [[Buf() for _ in range(NT)] for _ in range(2)]
    units = []
    groups = []
    for i in range(NT):
        for d in range(2):
            groups.append((i, d))
            for typ in range(2):
                for h in range(2):
                    units.append(dict(i=i, d=d, typ=typ, h=h, first=(typ == 0 and h == 0), last=(typ == 1 and h == 1), g=len(groups) - 1))

    def load_group(gi):
        i, d = groups[gi]
        c = order[d][i]; lp = i % 2
        S.dma("sp", FMl[d][lp][:].rearrange("p a t -> p (a t)"), FM[c], reads=[b_FM_d[c]], writes=[b_FMl[d][lp]])
        S.dma("sp", TMl[d][lp][:].rearrange("p a t -> p (a t)"), TM[c], reads=[b_TM_d[c]], writes=[b_TMl[d][lp]])

    def uinfo(u):
        U = units[u]
        i, d, typ, h = U["i"], U["d"], U["typ"], U["h"]
        c = order[d][i]
        cn = order[d][i + 1] if i + 1 < NT else c
        lp = i % 2
        x = 2 * d + h
        r = dict(U)
        r.update(c=c, cn=cn, lp=lp, hp=i % 2, ch=typ * 4 + h * 2 + d, x=x, vi=u % 4,
                 qT=FMl[d][lp][:, typ * 4 + h, :], kT=FMl[d][lp][:, typ * 4 + 2 + h, :],
                 kk=TMl[d][lp][:, typ * 2 + h, :], vv=TMl[d][lp][:, 4 + typ * 2 + h, :],
                 pS=PB[u % 2], bS=PBb[u % 2], pO=PB[2 + u % 2], bO=PBb[2 + u % 2], pC=PB[4 + u % 2], bC=PBb[4 + u % 2])
        if typ == 0:
            r.update(wcol=wml[:, x, c:c + 1], wb_=b_wml, dn=decb[:, x, cn:cn + 1], db_=b_decb)
        else:
            r.update(wcol=wret[:, x:x + 1], wb_=b_retc, dn=dret[:, x:x + 1], db_=b_retc)
        return r

    def st1(u):
        U = uinfo(u)
        d, lp, vi = U["d"], U["lp"], U["vi"]
        if U["first"] and U["g"] + 1 < len(groups):
            load_group(U["g"] + 1)
        S.op("dve", lambda e: e.tensor_scalar(out=va[vi][:, 0:128], in0=U["vv"], scalar1=U["wcol"], scalar2=None, op0=ALU.mult),
             reads=[b_TMl[d][lp], U["wb_"]], writes=[b_va[vi]])
        S.op("dve", lambda e: e.tensor_copy(out=va[vi][:, 128:129], in_=U["wcol"]), reads=[U["wb_"]], writes=[b_va[vi]])
        S.op("pe", lambda e: e.matmul(U["pS"][:, 0:128], lhsT=U["kT"], rhs=U["qT"], start=True, stop=True),
             reads=[b_FMl[d][lp]], writes=[U["bS"]])

    def st2(u):
        U = uinfo(u)
        d, lp, vi, ch = U["d"], U["lp"], U["vi"], U["ch"]
        S.op("dve", lambda e: e.tensor_tensor(out=sm[vi][:], in0=U["pS"][:, 0:128], in1=mask_bf[:, d, :], op=ALU.mult),
             reads=[U["bS"], b_maskbf], writes=[b_sm[vi]])
        S.op("pe", lambda e: e.matmul(U["pO"][:, 0:129], lhsT=sm[vi][:], rhs=va[vi][:, 0:129], start=True, stop=False),
             reads=[b_sm[vi], b_va[vi]], writes=[U["bO"]], inc=False)
        S.op("pe", lambda e: e.matmul(U["pO"][:, 0:129], lhsT=U["qT"], rhs=CTbf[:, ch, 0:129], start=False, stop=True),
             reads=[b_FMl[d][lp], b_CTbf[ch]], writes=[U["bO"]])
        S.op("pe", lambda e: e.matmul(U["pC"][:, 0:129], lhsT=U["kk"], rhs=va[vi][:, 0:129], start=True, stop=True),
             reads=[b_TMl[d][lp], b_va[vi]], writes=[U["bC"]])

    def st3(u):
        U = uinfo(u)
        d, vi, ch, x, c, hp, typ, h = U["d"], U["vi"], U["ch"], U["x"], U["c"], U["hp"], U["typ"], U["h"]
        pO, bO, pC, bC, dn, db_ = U["pO"], U["bO"], U["pC"], U["bC"], U["dn"], U["db_"]
        S.op("dve", lambda e: e.tensor_tensor(out=CT32[:, ch, 0:129], in0=pC[:, 0:129], in1=CTd32[:, ch, 0:129], op=ALU.add),
             reads=[bC, b_CTd[ch]], writes=[b_CT32[ch]])
        S.op("act", lambda e: e.activation(out=CTd32[:, ch, 0:129], in_=CT32[:, ch, 0:129], func=AF.Copy, scale=dn),
             reads=[b_CT32[ch], db_], writes=[b_CTd[ch]])
        S.op("act", lambda e: e.activation(out=CTbf[:, ch, 0:129], in_=CT32[:, ch, 0:129], func=AF.Copy, scale=dn),
             reads=[b_CT32[ch], db_], writes=[b_CTbf[ch]])
        oc = (typ * 2 + h) * 128
        if typ == 0:
            S.op("act", lambda e: e.activation(out=den[:, vi:vi + 1], in_=pO[:, 128:129], func=AF.Abs), reads=[bO], writes=[b_den[vi]])
            S.op("dve", lambda e: e.tensor_scalar(out=den[:, vi:vi + 1], in0=den[:, vi:vi + 1], scalar1=flo[:, x, c:c + 1], scalar2=None, op0=ALU.max),
                 reads=[b_den[vi], b_flo], writes=[b_den[vi]])
            S.op("dve", lambda e: e.reciprocal(out=den[:, vi:vi + 1], in_=den[:, vi:vi + 1]), reads=[b_den[vi]], writes=[b_den[vi]])
            S.op("act", lambda e: e.activation(out=HSst[d][hp][:, oc:oc + 128], in_=pO[:, 0:128], func=AF.Copy, scale=den[:, vi:vi + 1]),
                 reads=[bO, b_den[vi]], writes=[b_HSst[d][hp]])
        else:
            S.op("act", lambda e: e.activation(out=HSst[d][hp][:, oc:oc + 128], in_=pO[:, 0:128], func=AF.Copy, scale=rho[:, x:x + 1]),
                 reads=[bO, b_retc], writes=[b_HSst[d][hp]])
        if U["last"]:
            S.dma("sp", HS[d, c], HSst[d][hp][:], reads=[b_HSst[d][hp]], writes=[b_HS_d[d][c]])

    load_group(0)
    nu = len(units)
    for k in range(nu + 2):
        if k < nu:
            st1(k)
        if 0 <= k - 1 < nu:
            st2(k - 1)
        if 0 <= k - 2 < nu:
            st3(k - 2)

    phase_end()
    phase_begin("L1_M")
    gml = sb("gml", [128, 256]); gret = sb("gret", [128, 256]); b_gm = Buf()
    S.dma("sp", gml[:], mlg.partition_broadcast(128), writes=[b_gm])
    S.dma("sp", gret[:], retg.partition_broadcast(128), writes=[b_gm])
    h0 = [sb("h0_%d" % i, [128, 512]) for i in range(2)]; h1 = [sb("h1_%d" % i, [128, 512]) for i in range(2)]
    og = [sb("og%d" % i, [128, 512], BF16) for i in range(2)]
    b_h0 = [Buf(), Buf()]; b_h1 = [Buf(), Buf()]; b_og = [Buf(), Buf()]
    hz = sb("hz", [128, 512]); b_hz = Buf()
    sg = sb("sg", [128, 512]); b_sg = Buf()
    st6 = sb("st6", [128, 4, 6]); mv2 = sb("mv2", [128, 4, 2]); b_st = Buf(); b_mv = Buf()
    rs4 = sb("rs4", [128, 4]); b_rs4 = Buf()
    mo_st = [sb("mo_st%d" % i, [128, 512], BF16) for i in range(2)]; b_most = [Buf(), Buf()]
    b_out = Buf()
    for c in range(NT):
        p = c % 2
        S.dma("sp", h0[p][:], HS[0, c], reads=[b_HS_d[0][c]], writes=[b_h0[p]])
        S.dma("sp", h1[p][:], HS[1, c], reads=[b_HS_d[1][c]], writes=[b_h1[p]])
        S.dma("sp", og[p][:], OG[c], reads=[b_OG_d[c]], writes=[b_og[p]])
        S.op("dve", lambda e, p=p: e.tensor_tensor(out=hz[:], in0=h0[p][:], in1=h1[p][:], op=ALU.add), reads=[b_h0[p], b_h1[p]], writes=[b_hz])
        S.op("act", lambda e, p=p: e.activation(out=sg[:, 0:256], in_=og[p][:, 0:256], func=AF.Sigmoid), reads=[b_og[p]], writes=[b_sg])
        S.op("act", lambda e, p=p: e.activation(out=sg[:, 256:512], in_=og[p][:, 256:512], func=AF.Silu), reads=[b_og[p]], writes=[b_sg])
        S.op("dve", lambda e: e.tensor_tensor(out=hz[:, 0:256], in0=hz[:, 0:256], in1=sg[:, 0:256], op=ALU.mult), reads=[b_hz, b_sg], writes=[b_hz])
        for k in range(4):
            S.op("dve", lambda e, k=k: e.bn_stats(out=st6[:, k, :], in_=hz[:, k * 128:(k + 1) * 128]), reads=[b_hz], writes=[b_st])
            S.op("dve", lambda e, k=k: e.bn_aggr(out=mv2[:, k, :], in_=st6[:, k, :]), reads=[b_st], writes=[b_mv])
        S.op("dve", lambda e: e.tensor_scalar(out=rs4[:], in0=mv2[:, :, 1], scalar1=EPS, scalar2=None, op0=ALU.add),
             reads=[b_mv], writes=[b_rs4])
        S.op("act", lambda e: e.activation(out=rs4[:], in_=rs4[:], func=AF.Sqrt), reads=[b_rs4], writes=[b_rs4])
        S.op("dve", lambda e: e.reciprocal(out=rs4[:], in_=rs4[:]), reads=[b_rs4], writes=[b_rs4])
        for k in range(4):
            S.op("dve", lambda e, k=k: e.tensor_scalar(out=hz[:, k * 128:(k + 1) * 128], in0=hz[:, k * 128:(k + 1) * 128],
                                                      scalar1=mv2[:, k, 0:1], scalar2=rs4[:, k:k + 1], op0=ALU.subtract, op1=ALU.mult),
                 reads=[b_hz, b_mv, b_rs4], writes=[b_hz])
        S.op("dve", lambda e, p=p: e.tensor_tensor(out=mo_st[p][:, 0:256], in0=hz[:, 0:256], in1=gml[:], op=ALU.mult),
             reads=[b_hz, b_gm], writes=[b_most[p]])
        S.op("dve", lambda e: e.tensor_tensor(out=hz[:, 256:512], in0=hz[:, 256:512], in1=gret[:], op=ALU.mult), reads=[b_hz, b_gm], writes=[b_hz])
        S.op("dve", lambda e, p=p: e.tensor_tensor(out=mo_st[p][:, 256:512], in0=hz[:, 256:512], in1=sg[:, 256:512], op=ALU.mult),
             reads=[b_hz, b_sg], writes=[b_most[p]])
        S.dma("sp", merged[c * 128:(c + 1) * 128, :], mo_st[p][:], reads=[b_most[p]], writes=[b_out])
    phase_end()
    phase_end()
    return b_out


def l1_inputs(I, b, g, consts):
    d = dict(consts)
    d["xin"] = np.ascontiguousarray(np.concatenate([I["ctx"][b], I["x"][b]], 0))
    d["cT"] = np.ascontiguousarray(I["c"][b].reshape(8, 128).T)
    d["cctxT"] = np.ascontiguousarray(I["c_ctx"].reshape(8, 128).T)
    d["wmod"] = np.ascontiguousarray(I["w_mod"][0][:, 0:2048])
    d["bmod"] = np.ascontiguousarray(I["b_mod"][0][None, 0:2048])
    d["g1"] = np.ascontiguousarray(I["norm1_g"][0][None, :])
    W = I["w_in_even"][0]
    sp = np.cumsum([0, 512, 512, 512, 512, 8, 8, 512, 512, 512, 512])
    mq, mk, mv, mo, mi, mf, rq, rk, rv, rg = [W[:, sp[i]:sp[i + 1]] for i in range(10)]
    hs = [2 * g, 2 * g + 1]
    def hcols(M, h): return M[:, h * 128:(h + 1) * 128]
    def swp(M): return np.concatenate([M[:, 64:128], M[:, 0:64]], 1)
    fm = [hcols(mq, h) for h in hs] + [hcols(mk, h) for h in hs] + [hcols(rq, h) for h in hs] + [swp(hcols(rq, h)) for h in hs] \
        + [hcols(rk, h) for h in hs] + [swp(hcols(rk, h)) for h in hs]
    d["Wfm"] = np.ascontiguousarray(np.concatenate(fm, 1))
    gi = [mi[:, dd * 4 + h:dd * 4 + h + 1] for dd in range(2) for h in hs]
    gf = [mf[:, dd * 4 + h:dd * 4 + h + 1] for dd in range(2) for h in hs]
    tm = [hcols(mv, h) for h in hs] + [hcols(rv, h) for h in hs] + [hcols(mo, h) for h in hs] + [hcols(rg, h) for h in hs] + gi + gf
    d["Wtm"] = np.ascontiguousarray(np.concatenate(tm, 1))
    gb = I["ml_gate_b"][0]
    d["gbias"] = np.array([[gb[dd, 0, h] for dd in range(2) for h in hs] + [gb[dd, 1, h] for dd in range(2) for h in hs]], np.float32)
    d["rld"] = np.array([[I["ret_log_decay"][0][dd, h] for dd in range(2) for h in hs]], np.float32)
    d["mlg"] = np.ascontiguousarray(I["ml_norm_g"][0][hs].reshape(1, 256))
    d["retg"] = np.ascontiguousarray(I["ret_norm_g"][0][hs].reshape(1, 256))
    return d


NT2 = 33
GROUPS2 = [[0]] + [[1 + 4 * g + i for i in range(4)] for g in range(8)]


def host_consts_l2(half):
    c = common_consts()
    T = NT2 * 128
    inv = (10000.0 ** (-np.arange(16, dtype=np.float32) / 16)).astype(np.float32)
    t = np.arange(half * 4096, (half + 1) * 4096)
    rows = (t // 64).astype(np.float32); cols = (t % 64).astype(np.float32)
    ar = (rows[:, None] * inv[None, :]).astype(np.float32)
    ac = (cols[:, None] * inv[None, :]).astype(np.float32)
    cos64 = np.concatenate([np.cos(ar), np.cos(ar), np.cos(ac), np.cos(ac)], 1).astype(np.float32)
    sin64 = np.concatenate([-np.sin(ar), np.sin(ar), -np.sin(ac), np.sin(ac)], 1).astype(np.float32)
    cosT = np.ones((128, T), np.float32); sinT = np.zeros((128, T), np.float32)
    cosT[:, 128:] = np.concatenate([cos64, cos64], 1).T
    sinT[:, 128:] = np.concatenate([sin64, sin64], 1).T
    c["rc2"] = np.ascontiguousarray(cosT); c["rs2"] = np.ascontiguousarray(sinT)
    return c


def build_l2(cx, io, debug=None):
    nc = cx.nc
    S = cx.S
    cx.phase_begin("L2")
    xin = io["xin"]
    MG = io["MG"]; b_MG = io["b_MG"]
    idxm_d = io["idxm"]
    cT = io["cT"]; cctxT = io["cctxT"]
    wmod = io["wmod"]; bmod = io["bmod"]
    g2_0 = io["g2_0"]; g1_1 = io["g1_1"]
    w_out = io["w_out"]
    Wr = io["Wr"]; br = io["br"]
    w1 = io["w1"]; w3 = io["w3"]; w2 = io["w2"]
    Wq = io["Wq"]; Wk = io["Wk"]; Wv = io["Wv"]
    rc2_d = io["rc2"]; rs2_d = io["rs2"]
    hout = io["H2"]
    QT = io["QTs"]
    KT = io["KTo"]
    Vo = io["Vown"]
    H1 = cx.dscr("H1", [NT2, 128, D]); b_H1 = [Buf() for _ in range(NT2)]
    Vs = cx.dscr("Vs", [NT2, 128, D], BF16); b_Vs = [Buf() for _ in range(NT2)]
    UT = cx.dscr("UT", [NT2, 128, 8, 128], BF16); b_UT = [Buf() for _ in range(NT2)]

    def lc(name, shape, dt=F32):
        t = cx.sb(name + "_2s", shape, dt); b = Buf()
        S.dma("sp", t[:], io[name], writes=[b])
        return t, b
    idxm, b_idxm = lc("idxm", [128, NT2, 2], I32)
    ident_bf = lc("ident_bf", [128, 128], BF16)
    ident_f = lc("ident_f", [128, 128])
    ones_f = lc("ones_f", [128, 128])
    slt = lc("slt", [128, 128])
    blkstart = lc("blkstart", [128, 1])
    pcol = lc("pcol", [128, 1])
    thr = lc("thr", [128, 34])
    consts = {"thr": thr, "ident_bf": ident_bf, "ident_f": ident_f, "ones_f": ones_f, "slt": slt, "blkstart": blkstart, "pcol": pcol}
    moe = io["moe"]
    moe.c = consts
    moe.alloc_persistent()

    mods = cx.sb("mods", [128, 2, 6, D]); b_mods = Buf()
    cx.phase_begin("L2_mod")
    scb, b_scb = emit_silu_bcast(cx, [cT, cctxT], ones_f[0], ones_f[1])
    bmod_s, b_bmod = cx.load_bcast("bmod2", 6144, bmod)
    g2_s, b_g2 = cx.load_bcast("g2_0", D, g2_0)
    g1n_s, b_g1n = cx.load_bcast("g1_1", D, g1_1)
    wm_s = [cx.sb("wm_s%d" % i, [128, 8, 512]) for i in range(2)]; b_wm = [Buf(), Buf()]
    emit_mod(cx, scb, b_scb, 2, wmod, bmod_s, b_bmod, 6144,
             lambda w, cc: mods[:, w, cc // 2, (cc % 2) * 512:(cc % 2 + 1) * 512], b_mods, wm_s, b_wm)
    for w in range(2):
        for slot, gt, bg in [(2, g2_s, b_g2), (5, g1n_s, b_g1n)]:
            S.op("dve", lambda e, w=w, slot=slot, gt=gt: e.scalar_tensor_tensor(out=mods[:, w, slot, :], in0=mods[:, w, slot, :], scalar=1.0,
                                                                               in1=gt[:], op0=ALU.add, op1=ALU.mult),
                 reads=[b_mods, bg], writes=[b_mods])
    cx.phase_end()

    cx.phase_begin("L2_B")
    wo_s = cx.sb("wo_s", [128, 8, D], BF16); b_wo = Buf()
    for j in range(8):
        S.dma("pool", wo_s[:, j, :], w_out[j * 128:(j + 1) * 128, :], writes=[b_wo])
    nb = NormBufs(cx)
    ht = [cx.sb("ht%d" % i, [128, D]) for i in range(2)]; b_ht = [Buf(), Buf()]
    mt = [cx.sb("mt%d" % i, [128, D], BF16) for i in range(2)]; b_mt = [Buf(), Buf()]
    mT = [cx.sb("mT%d" % i, [128, 8, 128], BF16) for i in range(2)]; b_mT = [Buf(), Buf()]
    vt = [cx.sb("vt%d" % i, [128, D], BF16) for i in range(2)]; b_vt = [Buf(), Buf()]
    vT = [cx.sb("vT%d" % i, [128, 8, 128], BF16) for i in range(2)]; b_vT = [Buf(), Buf()]
    ytmp = cx.sb("ytmp", [128, D]); b_ytmp = Buf()
    for ti in range(NT2):
        p = ti % 2
        w = 1 if ti == 0 else 0
        S.dma("sp", ht[p][:], xin[ti * 128:(ti + 1) * 128, :], writes=[b_ht[p]])
        for r in range(2):
            S.dma("pool", mt[p][:, r * 512:(r + 1) * 512], MG, reads=[b_MG, b_idxm], writes=[b_mt[p]],
                  indirect={"in_offset": bass.IndirectOffsetOnAxis(ap=idxm[:, ti, r:r + 1], axis=0)})
        cmap = [(0, 0), (0, 1), (1, 0), (1, 1), (0, 2), (0, 3), (1, 2), (1, 3)]
        emit_transpose(cx, lambda j, p=p: mt[p][:, cmap[j][0] * 512 + cmap[j][1] * 128: cmap[j][0] * 512 + (cmap[j][1] + 1) * 128],
                       b_mt[p], 8, ident_bf[0], ident_bf[1], mT[p][:], b_mT[p])
        for half in range(2):
            pc, bc = cx.PB[half], cx.PBb[half]
            for j in range(8):
                S.op("pe", lambda e, j=j, p=p, pc=pc, half=half: e.matmul(pc[:], lhsT=mT[p][:, j, :], rhs=wo_s[:, j, half * 512:(half + 1) * 512],
                                                                          start=(j == 0), stop=(j == 7)),
                     reads=[b_mT[p], b_wo], writes=[bc])
            sl = slice(half * 512, (half + 1) * 512)
            S.op("dve", lambda e, pc=pc, w=w, sl=sl: e.tensor_tensor(out=ytmp[:, sl], in0=pc[:], in1=mods[:, w, 0, sl], op=ALU.mult),
                 reads=[bc, b_mods], writes=[b_ytmp])
            S.op("dve", lambda e, p=p, sl=sl: e.tensor_tensor(out=ht[p][:, sl], in0=ht[p][:, sl], in1=ytmp[:, sl], op=ALU.add),
                 reads=[b_ytmp, b_ht[p]], writes=[b_ht[p]])
        S.dma("sp", H1[ti], ht[p][:], reads=[b_ht[p]], writes=[b_H1[ti]])
        emit_adaln(cx, nb, ht[p][:], b_ht[p], mods[:, w, 2, :], mods[:, w, 1, :], b_mods, vt[p][:], b_vt[p])
        S.dma("sp", Vs[ti], vt[p][:], reads=[b_vt[p]], writes=[b_Vs[ti]])
        emit_transpose(cx, lambda j, p=p: vt[p][:, j * 128:(j + 1) * 128], b_vt[p], 8, ident_bf[0], ident_bf[1], vT[p][:], b_vT[p])
        moe.route_tile(ti, lambda j, p=p: vT[p][:, j, :], b_vT[p])
    cx.phase_end()

    cx.phase_begin("L2_C")
    moe.plan()
    vl = [cx.sb("vl%d" % i, [128, D], BF16) for i in range(2)]; b_vl = [Buf(), Buf()]
    for ti in range(NT2):
        p = ti % 2
        S.dma("sp", vl[p][:], Vs[ti], reads=[b_Vs[ti]], writes=[b_vl[p]])
        moe.dispatch_tile(ti, vl[p][:], b_vl[p], do_scatter=(debug != "plan"))
    if debug == "plan":
        dbg_dest = cx.dout("dbg_dest", [128, NT2 * 2], I32)
        dbg_idxw = cx.dout("dbg_idxw", [128, 128], I32)
        dbg_gate = cx.dout("dbg_gate", [128, NT2 * 2])
        dbg_oh = cx.dout("dbg_oh", [128, NT2 * 64])
        b_dbg = Buf()
        S.dma("sp", dbg_dest, moe.dest[:].rearrange("p t k -> p (t k)"), reads=moe.b_dest, writes=[b_dbg])
        S.dma("sp", dbg_idxw, moe.idxw[:], reads=[moe.b_idxw], writes=[b_dbg])
        S.dma("sp", dbg_gate, moe.gate[:].rearrange("p t k -> p (t k)"), reads=moe.b_gate, writes=[b_dbg])
        S.dma("sp", dbg_oh, moe.OH[:].rearrange("p t k e -> p (t k e)"), reads=moe.b_OH, writes=[b_dbg])
        cx.phase_end()
        return None
    moe.experts()
    cx.phase_end()

    cx.phase_begin("L2_D1")
    nb = NormBufs(cx, "d")
    h1t = [cx.sb("h1t%d" % i, [128, D]) for i in range(2)]; b_h1t = [Buf(), Buf()]
    y0 = [cx.sb("y0_%d" % i, [128, D]) for i in range(2)]; b_y0 = [Buf(), Buf()]
    y1 = [cx.sb("y1_%d" % i, [128, D]) for i in range(2)]; b_y1 = [Buf(), Buf()]
    ut = [cx.sb("ut%d" % i, [128, D], BF16) for i in range(2)]; b_ut = [Buf(), Buf()]
    uTt = [cx.sb("uTt%d" % i, [128, 8, 128], BF16) for i in range(2)]; b_uTt = [Buf(), Buf()]
    b_hout = Buf()
    for ti in range(NT2):
        p = ti % 2
        w = 1 if ti == 0 else 0
        S.dma("sp", h1t[p][:], H1[ti], reads=[b_H1[ti]], writes=[b_h1t[p]])
        moe.gather_tile(ti, y0[p][:], y1[p][:], b_y0[p], b_y1[p])
        S.op("dve", lambda e, p=p, ti=ti: e.tensor_scalar(out=y0[p][:], in0=y0[p][:], scalar1=moe.gate[:, ti, 0:1], scalar2=None, op0=ALU.mult),
             reads=[b_y0[p], moe.b_gate[ti]], writes=[b_y0[p]])
        S.op("dve", lambda e, p=p, ti=ti: e.scalar_tensor_tensor(out=y0[p][:], in0=y1[p][:], scalar=moe.gate[:, ti, 1:2], in1=y0[p][:],
                                                                op0=ALU.mult, op1=ALU.add),
             reads=[b_y0[p], b_y1[p], moe.b_gate[ti]], writes=[b_y0[p]])
        S.op("dve", lambda e, p=p, w=w: e.tensor_tensor(out=y0[p][:], in0=y0[p][:], in1=mods[:, w, 3, :], op=ALU.mult),
             reads=[b_y0[p], b_mods], writes=[b_y0[p]])
        S.op("dve", lambda e, p=p: e.tensor_tensor(out=h1t[p][:], in0=h1t[p][:], in1=y0[p][:], op=ALU.add),
             reads=[b_y0[p], b_h1t[p]], writes=[b_h1t[p]])
        S.dma("sp", hout[ti * 128:(ti + 1) * 128, :], h1t[p][:], reads=[b_h1t[p]], writes=[b_hout])
        emit_adaln(cx, nb, h1t[p][:], b_h1t[p], mods[:, w, 5, :], mods[:, w, 4, :], b_mods, ut[p][:], b_ut[p])
        emit_transpose(cx, lambda j, p=p: ut[p][:, j * 128:(j + 1) * 128], b_ut[p], 8, ident_bf[0], ident_bf[1], uTt[p][:], b_uTt[p])
        S.dma("sp", UT[ti], uTt[p][:], reads=[b_uTt[p]], writes=[b_UT[ti]])
    cx.phase_end()

    cx.phase_begin("L2_D2")
    Wq_s = cx.sb("Wq_s", [128, 8, 2048], BF16); Wk_s = cx.sb("Wk_s", [128, 8, 2048], BF16); Wv_s = cx.sb("Wv_s", [128, 8, D], BF16)
    b_Wq = Buf(); b_Wk = Buf(); b_Wv = Buf()
    for j in range(8):
        S.dma("pool", Wq_s[:, j, :], Wq[j * 128:(j + 1) * 128, :], writes=[b_Wq])
        S.dma("pool", Wk_s[:, j, :], Wk[j * 128:(j + 1) * 128, :], writes=[b_Wk])
        S.dma("pool", Wv_s[:, j, :], Wv[j * 128:(j + 1) * 128, :], writes=[b_Wv])
    uTg = [cx.sb("uTg%d" % i, [128, 4, 8, 128], BF16) for i in range(2)]; b_uTg = [Buf(), Buf()]
    rc = [cx.sb("rc%d" % i, [128, 512]) for i in range(2)]; rsn = [cx.sb("rsn%d" % i, [128, 512]) for i in range(2)]; b_rope = [Buf(), Buf()]
    t1 = cx.sb("t1", [128, 512]); t2 = cx.sb("t2", [128, 512]); b_t1 = Buf(); b_t2 = Buf()
    qkst = [cx.sb("qkst%d" % i, [128, 512], BF16) for i in range(4)]; b_qkst = [Buf() for _ in range(4)]
    vst = [cx.sb("vst%d" % i, [128, D], BF16) for i in range(2)]; b_vst = [Buf(), Buf()]
    b_QT = Buf(); b_KT = Buf(); b_Vo = Buf()
    si = 0; vi = 0; fi = 0
    for gi, tiles in enumerate(GROUPS2):
        p = gi % 2
        n = len(tiles) * 128
        t0 = tiles[0]
        S.dma("sp", uTg[p][:, 0:len(tiles)], UT[t0:t0 + len(tiles)].rearrange("c p j t -> p c j t"),
              reads=b_UT[t0:t0 + len(tiles)], writes=[b_uTg[p]])
        S.dma("sp", rc[p][:, 0:n], rc2_d[:, t0 * 128:t0 * 128 + n], writes=[b_rope[p]])
        S.dma("sp", rsn[p][:, 0:n], rs2_d[:, t0 * 128:t0 * 128 + n], writes=[b_rope[p]])
        for ci, ti in enumerate(tiles):
            vq = vi % 2; vi += 1
            for half in range(2):
                pc, bc = cx.PB[half], cx.PBb[half]
                for j in range(8):
                    S.op("pe", lambda e, j=j, ci=ci, pc=pc, half=half: e.matmul(pc[:], lhsT=uTg[p][:, ci, j, :], rhs=Wv_s[:, j, half * 512:(half + 1) * 512],
                                                                                start=(j == 0), stop=(j == 7)),
                         reads=[b_uTg[p], b_Wv], writes=[bc])
                S.op("act", lambda e, vq=vq, pc=pc, half=half: e.activation(out=vst[vq][:, half * 512:(half + 1) * 512], in_=pc[:], func=AF.Copy),
                     reads=[bc], writes=[b_vst[vq]])
            S.dma("sp", Vo[ti * 128:(ti + 1) * 128, :], vst[vq][:], reads=[b_vst[vq]], writes=[b_Vo])
        for qk in range(2):
            if qk == 0 and gi == 0:
                continue
            Ws, bW = (Wq_s, b_Wq) if qk == 0 else (Wk_s, b_Wk)
            sc_ = 0.125 if qk == 0 else 1.0
            for h in range(8):
                pa = 2 + fi % 4; fi += 1
                pb_ = 2 + fi % 4; fi += 1
                for (pp, cb) in [(pa, h), (pb_, 8 + h)]:
                    for j in range(8):
                        S.op("pe", lambda e, j=j, pp=pp, cb=cb: e.matmul(cx.PB[pp][:, 0:n], lhsT=Ws[:, j, cb * 128:(cb + 1) * 128],
                                                                         rhs=uTg[p][:, 0:len(tiles), j, :],
                                                                         start=(j == 0), stop=(j == 7)),
                             reads=[bW, b_uTg[p]], writes=[cx.PBb[pp]])
                S.op("dve", lambda e, pa=pa: e.scalar_tensor_tensor(out=t1[:, 0:n], in0=cx.PB[pa][:, 0:n], scalar=sc_, in1=rc[p][:, 0:n],
                                                                    op0=ALU.mult, op1=ALU.mult), reads=[cx.PBb[pa], b_rope[p]], writes=[b_t1])
                S.op("dve", lambda e, pb_=pb_: e.scalar_tensor_tensor(out=t2[:, 0:n], in0=cx.PB[pb_][:, 0:n], scalar=sc_, in1=rsn[p][:, 0:n],
                                                                      op0=ALU.mult, op1=ALU.mult), reads=[cx.PBb[pb_], b_rope[p]], writes=[b_t2])
                sq = si % 4; si += 1
                S.op("dve", lambda e, sq=sq: e.tensor_tensor(out=qkst[sq][:, 0:n], in0=t1[:, 0:n], in1=t2[:, 0:n], op=ALU.add),
                     reads=[b_t1, b_t2], writes=[b_qkst[sq]])
                if qk == 0:
                    q0 = (t0 - 1) * 128
                    S.dma("sp", QT[h, :, q0:q0 + n], qkst[sq][:, 0:n], reads=[b_qkst[sq]], writes=[b_QT])
                else:
                    S.dma("sp", KT[h, :, t0 * 128:t0 * 128 + n], qkst[sq][:, 0:n], reads=[b_qkst[sq]], writes=[b_KT])
    cx.phase_end()
    cx.phase_end()
    return b_hout, b_QT, b_KT, b_Vo


def l2_inputs(I, b, half, merged_full_b, consts):
    d = dict(consts)
    cs = slice(half * 128, (half + 1) * 128)
    ls = slice(half * 4096, (half + 1) * 4096)
    d["xin"] = np.ascontiguousarray(np.concatenate([I["ctx"][b][cs], I["x"][b][ls]], 0))
    if merged_full_b is not None:
        d["mergedIn"] = np.ascontiguousarray(np.concatenate([merged_full_b[0:256][cs], merged_full_b[256:][ls]], 0))
    d["cT"] = np.ascontiguousarray(I["c"][b].reshape(8, 128).T)
    d["cctxT"] = np.ascontiguousarray(I["c_ctx"].reshape(8, 128).T)
    d["wmod"] = np.ascontiguousarray(np.concatenate([I["w_mod"][0][:, 2048:6144], I["w_mod"][1][:, 0:2048]], 1))
    d["bmod"] = np.ascontiguousarray(np.concatenate([I["b_mod"][0][2048:6144], I["b_mod"][1][0:2048]])[None, :])
    d["g2_0"] = np.ascontiguousarray(I["norm2_g"][0][None, :])
    d["g1_1"] = np.ascontiguousarray(I["norm1_g"][1][None, :])
    d["w_out"] = I["w_out_even"][0]
    d["Wr"] = np.ascontiguousarray(np.concatenate([I["router_g_w"][0], I["router_e_w"][0]], 1))
    d["br"] = np.ascontiguousarray(np.concatenate([I["router_g_b"][0], I["router_e_b"][0]])[None, :])
    d["w1"] = I["w1"][0]; d["w3"] = I["w3"][0]; d["w2"] = I["w2"][0]
    W = I["w_in_odd"][0]
    perm = np.arange(64)
    perm = np.concatenate([perm[16:32], perm[0:16], perm[48:64], perm[32:48]])
    def swp(M):
        return M.reshape(D, 16, 64)[:, :, perm].reshape(D, 1024)
    d["Wq"] = np.ascontiguousarray(np.concatenate([W[:, 0:1024], swp(W[:, 0:1024])], 1))
    d["Wk"] = np.ascontiguousarray(np.concatenate([W[:, 1024:2048], swp(W[:, 1024:2048])], 1))
    d["Wv"] = np.ascontiguousarray(W[:, 2048:3072])
    return d


NT3 = 32
NK = 66
LAM_INIT = 0.8 - 0.6 * math.exp(-0.3 * 1)


def host_consts_l3():
    c = common_consts()
    c["ones_bf"] = np.ones((128, 128), np.float32).astype(ml_dtypes.bfloat16)
    return c


VCH = [(0, 1024), (1024, 1024), (2048, 1024), (3072, 1024), (4096, 128)]


def build_l3(cx, io, debug=None):
    nc = cx.nc
    S = cx.S
    cx.phase_begin("L3")
    QT = io["QTs"]; b_QTs = io["b_QTs"]
    KTg = io["KTg"]; b_KTg = io["b_KTg"]
    VG = io["VG"]; b_VG = io["b_VG"]
    H2 = io["H2"]; b_H2 = io["b_H2"]
    cT = io["cT"]
    wmod = io["wmod"]; bmod = io["bmod"]
    g2 = io["g2"]; gfin = io["gfin"]
    dalam = io["dalam"]; dagT = io["dagT"]
    w_out = io["w_out"]
    Wr = io["Wr"]; br = io["br"]
    w1 = io["w1"]; w3 = io["w3"]; w2 = io["w2"]
    out = io["out"]
    H3 = cx.dscr("H3", [NT3, 128, D]); b_H3 = [Buf() for _ in range(NT3)]
    Vs = cx.dscr("Vs3", [NT3, 128, D], BF16); b_Vs = [Buf() for _ in range(NT3)]

    def lc(name, shape, dt=F32):
        t = cx.sb(name + "_3s", shape, dt); b = Buf()
        S.dma("sp", t[:], io[name], writes=[b])
        return t, b
    ident_bf = lc("ident_bf", [128, 128], BF16)
    ident_f = lc("ident_f", [128, 128])
    ones_f = lc("ones_f", [128, 128])
    ones_bf = lc("ones_bf", [128, 128], BF16)
    slt = lc("slt", [128, 128])
    blkstart = lc("blkstart", [128, 1])
    pcol = lc("pcol", [128, 1])
    thr = lc("thr", [128, 34])
    consts = {"thr": thr, "ident_bf": ident_bf, "ident_f": ident_f, "ones_f": ones_f, "slt": slt, "blkstart": blkstart, "pcol": pcol}
    moe = io["moe"]
    moe.c = consts
    moe.alloc_persistent()

    mods = cx.sb("mods", [128, 4, D]); b_mods = Buf()
    gfin_s, b_gfin = cx.load_bcast("gfin3", D, gfin)
    cx.phase_begin("L3_mod")
    scb, b_scb = emit_silu_bcast(cx, [cT], ones_f[0], ones_f[1])
    bmod_s, b_bmod = cx.load_bcast("bmod3", 4096, bmod)
    g2_s, b_g2 = cx.load_bcast("g2_3", D, g2)
    wm_s = [cx.sb("wm_s%d" % i, [128, 8, 512]) for i in range(2)]; b_wm = [Buf(), Buf()]
    emit_mod(cx, scb, b_scb, 1, wmod, bmod_s, b_bmod, 4096,
             lambda w, cc: mods[:, cc // 2, (cc % 2) * 512:(cc % 2 + 1) * 512], b_mods, wm_s, b_wm)
    S.op("dve", lambda e: e.scalar_tensor_tensor(out=mods[:, 2, :], in0=mods[:, 2, :], scalar=1.0, in1=g2_s[:], op0=ALU.add, op1=ALU.mult),
         reads=[b_mods, b_g2], writes=[b_mods])
    cx.phase_end()

    cx.phase_begin("L3_attnouter")
    oT_all = cx.sb("oT_all", [128, 8, NT3 * 128], BF16); b_oT = [Buf() for _ in range(8)]
    lamw = cx.sb("lamw", [128, 256]); b_lamw = Buf()
    S.dma("sp", lamw[:], dalam.partition_broadcast(128), writes=[b_lamw])
    lamt = cx.sb("lamt", [128, 8]); b_lamt = Buf()
    prod = cx.sb("lprod", [128, 128]); b_prod = Buf()
    S.op("dve", lambda e: e.tensor_tensor(out=prod[:, 0:64], in0=lamw[:, 0:64], in1=lamw[:, 64:128], op=ALU.mult), reads=[b_lamw], writes=[b_prod])
    S.op("dve", lambda e: e.tensor_tensor(out=prod[:, 64:128], in0=lamw[:, 128:192], in1=lamw[:, 192:256], op=ALU.mult), reads=[b_lamw, b_prod], writes=[b_prod])
    S.op("dve", lambda e: e.tensor_reduce(out=lamt[:, 0:1], in_=prod[:, 0:64], axis=AX.X, op=ALU.add), reads=[b_prod], writes=[b_lamt])
    S.op("dve", lambda e: e.tensor_reduce(out=lamt[:, 1:2], in_=prod[:, 64:128], axis=AX.X, op=ALU.add), reads=[b_prod, b_lamt], writes=[b_lamt])
    S.op("act", lambda e: e.activation(out=lamt[:, 2:4], in_=lamt[:, 0:2], func=AF.Exp), reads=[b_lamt], writes=[b_lamt])
    S.op("dve", lambda e: e.tensor_tensor(out=lamt[:, 4:5], in0=lamt[:, 3:4], in1=lamt[:, 2:3], op=ALU.subtract), reads=[b_lamt], writes=[b_lamt])
    S.op("dve", lambda e: e.tensor_scalar(out=lamt[:, 4:5], in0=lamt[:, 4:5], scalar1=-LAM_INIT, scalar2=None, op0=ALU.add), reads=[b_lamt], writes=[b_lamt])
    gs = cx.sb("gs", [128, 8]); b_gs = Buf()
    S.dma("sp", gs[:], dagT, writes=[b_gs])
    S.op("dve", lambda e: e.tensor_scalar(out=gs[:], in0=gs[:], scalar1=1.0 - LAM_INIT, scalar2=None, op0=ALU.mult), reads=[b_gs], writes=[b_gs])

    cx.phase_begin("L3_attn")
    KTh = [cx.sb("KTh%d" % i, [128, NK * 128], BF16) for i in range(2)]; b_KTh = [Buf(), Buf()]
    Vh = [cx.sb("Vh%d" % i, [128, NK, 128], BF16) for i in range(2)]; b_Vh = [Buf(), Buf()]
    QTh1 = cx.sb("QTh", [128, 2, NT3 * 128], BF16); b_QTh1 = Buf()
    QTh = [QTh1, QTh1]; b_QTh = [b_QTh1, b_QTh1]
    S.op("dve", lambda e: e.memset(QTh1[:], 0.0), writes=[b_QTh1])
    Pt = [cx.sb("Pt%d" % i, [128, 512], BF16) for i in range(4)]; b_Pt = [Buf() for _ in range(4)]
    rec = cx.sb("rec", [128, 512]); b_rec = Buf()
    on1 = cx.sb("on1", [128, 512]); b_on1 = Buf()
    ot = cx.sb("ot", [128, 512]); b_ot = Buf()
    sq = cx.sb("sqt", [128, 512]); b_sq = Buf()
    rstd = cx.sb("rstdA", [128, 512]); b_rstdA = Buf()
    pi_ = 0
    si_ = 0
    nheads = 8 if debug != "attn1" else 1
    nqt = 8 if debug != "attn1" else 1
    for h in range(nheads):
        p = h % 2
        for r in range(2):
            S.dma("sp", KTh[p][:, r * 4224:(r + 1) * 4224], KTg[h, r * 128:(r + 1) * 128, :], reads=[b_KTg], writes=[b_KTh[p]])
            for (r0, n) in VCH:
                vr = 2 * r0 + r * n
                S.dma("sp", Vh[p][:, r * 33 + r0 // 128: r * 33 + (r0 + n) // 128, :],
                      VG[vr:vr + n, h * 128:(h + 1) * 128].rearrange("(k p) d -> p k d", p=128), reads=[b_VG], writes=[b_Vh[p]])
        for m_ in range(2):
            S.dma("sp", QTh[p][m_ * 64:(m_ + 1) * 64, m_, :], QT[h, m_ * 64:(m_ + 1) * 64, :], reads=[b_QTs], writes=[b_QTh[p]])
        steps = [(qt, m, kt) for qt in range(nqt) for m in range(2) for kt in range(NK)]
        LOOK = 2
        sbank = {}

        def issue_S(idx):
            nonlocal si_
            qt_, m_, kt_ = steps[idx]
            pst = si_ % 3; si_ += 1
            sbank[idx] = pst
            ms_ = slice(m_ * 64, (m_ + 1) * 64)
            qs_ = slice(qt_ * 512, (qt_ + 1) * 512)
            S.op("pe", lambda e: e.matmul(cx.PB[pst][:], lhsT=KTh[p][:, kt_ * 128:(kt_ + 1) * 128], rhs=QTh[p][:, m_, qs_], start=True, stop=True),
                 reads=[b_KTh[p], b_QTh[p]], writes=[cx.PBb[pst]])

        for i0 in range(min(LOOK, len(steps))):
            issue_S(i0)
        for idx, (qt, m, kt) in enumerate(steps):
            qs = slice(qt * 512, (qt + 1) * 512)
            pO, bO = cx.PB[3], cx.PBb[3]
            pS, bS = cx.PB[4], cx.PBb[4]
            pst = sbank.pop(idx)
            pq = pi_ % 4; pi_ += 1
            S.op("act", lambda e, pq=pq, pst=pst: e.activation(out=Pt[pq][:], in_=cx.PB[pst][:], func=AF.Exp),
                 reads=[cx.PBb[pst]], writes=[b_Pt[pq]])
            if idx + LOOK < len(steps):
                issue_S(idx + LOOK)
            S.op("pe", lambda e, pq=pq, kt=kt: e.matmul(pO[:], lhsT=Vh[p][:, kt, :], rhs=Pt[pq][:], start=(kt == 0), stop=(kt == NK - 1)),
                 reads=[b_Vh[p], b_Pt[pq]], writes=[bO])
            S.op("pe", lambda e, pq=pq, kt=kt: e.matmul(pS[:], lhsT=ones_bf[0][:], rhs=Pt[pq][:], start=(kt == 0), stop=(kt == NK - 1)),
                 reads=[ones_bf[1], b_Pt[pq]], writes=[bS])
            if kt != NK - 1:
                continue
            S.op("dve", lambda e: e.reciprocal(out=rec[:], in_=pS[:]), reads=[bS], writes=[b_rec])
            if m == 0:
                S.op("dve", lambda e: e.tensor_tensor(out=on1[:], in0=pO[:], in1=rec[:], op=ALU.mult), reads=[bO, b_rec], writes=[b_on1])
                continue
            S.op("dve", lambda e: e.tensor_tensor(out=ot[:], in0=pO[:], in1=rec[:], op=ALU.mult), reads=[bO, b_rec], writes=[b_ot])
            S.op("dve", lambda e: e.scalar_tensor_tensor(out=ot[:], in0=ot[:], scalar=lamt[:, 4:5], in1=on1[:], op0=ALU.mult, op1=ALU.add),
                 reads=[b_ot, b_on1, b_lamt], writes=[b_ot])
            S.op("act", lambda e: e.activation(out=sq[:], in_=ot[:], func=AF.Square), reads=[b_ot], writes=[b_sq])
            pN, bN = cx.PB[5], cx.PBb[5]
            S.op("pe", lambda e: e.matmul(pN[:], lhsT=ones_f[0][:], rhs=sq[:], start=True, stop=True), reads=[ones_f[1], b_sq], writes=[bN])
            S.op("dve", lambda e: e.tensor_scalar(out=rstd[:], in0=pN[:], scalar1=1.0 / 128, scalar2=EPS, op0=ALU.mult, op1=ALU.add),
                 reads=[bN], writes=[b_rstdA])
            S.op("act", lambda e: e.activation(out=rstd[:], in_=rstd[:], func=AF.Sqrt), reads=[b_rstdA], writes=[b_rstdA])
            S.op("dve", lambda e: e.reciprocal(out=rstd[:], in_=rstd[:]), reads=[b_rstdA], writes=[b_rstdA])
            S.op("dve", lambda e, h=h, qs=qs: e.scalar_tensor_tensor(out=oT_all[:, h, qs], in0=ot[:], scalar=gs[:, h:h + 1], in1=rstd[:],
                                                                    op0=ALU.mult, op1=ALU.mult),
                 reads=[b_ot, b_gs, b_rstdA], writes=[b_oT[h]])

    if debug == "attn1":
        dbg = cx.dout("dbg_oT", [128, 512], BF16); b_dbg = Buf()
        S.dma("sp", dbg, oT_all[:, 0, 0:512], reads=b_oT, writes=[b_dbg])
        cx.phase_end()
        S.finish([b_dbg], "sp")
        return nc

    cx.phase_end()
    cx.phase_begin("L3_wout")
    wo_s = cx.sb("wo_s", [128, 8, D], BF16); b_wo = Buf()
    for j in range(8):
        S.dma("pool", wo_s[:, j, :], w_out[j * 128:(j + 1) * 128, :], writes=[b_wo])
    nb = NormBufs(cx)
    ht = [cx.sb("ht%d" % i, [128, D]) for i in range(2)]; b_ht = [Buf(), Buf()]
    vt = [cx.sb("vt%d" % i, [128, D], BF16) for i in range(2)]; b_vt = [Buf(), Buf()]
    vT = [cx.sb("vT%d" % i, [128, 8, 128], BF16) for i in range(2)]; b_vT = [Buf(), Buf()]
    ytmp = cx.sb("ytmp", [128, D]); b_ytmp = Buf()
    for ti in range(NT3):
        p = ti % 2
        S.dma("sp", ht[p][:], H2[(ti + 1) * 128:(ti + 2) * 128, :], reads=[b_H2], writes=[b_ht[p]])
        for half in range(2):
            pc, bc = cx.PB[half], cx.PBb[half]
            for j in range(8):
                S.op("pe", lambda e, j=j, pc=pc, half=half, ti=ti: e.matmul(pc[:], lhsT=oT_all[:, j, ti * 128:(ti + 1) * 128],
                                                                            rhs=wo_s[:, j, half * 512:(half + 1) * 512], start=(j == 0), stop=(j == 7)),
                     reads=b_oT + [b_wo], writes=[bc])
            sl = slice(half * 512, (half + 1) * 512)
            S.op("dve", lambda e, pc=pc, sl=sl: e.tensor_tensor(out=ytmp[:, sl], in0=pc[:], in1=mods[:, 0, sl], op=ALU.mult),
                 reads=[bc, b_mods], writes=[b_ytmp])
            S.op("dve", lambda e, p=p, sl=sl: e.tensor_tensor(out=ht[p][:, sl], in0=ht[p][:, sl], in1=ytmp[:, sl], op=ALU.add),
                 reads=[b_ytmp, b_ht[p]], writes=[b_ht[p]])
        S.dma("sp", H3[ti], ht[p][:], reads=[b_ht[p]], writes=[b_H3[ti]])
        emit_adaln(cx, nb, ht[p][:], b_ht[p], mods[:, 2, :], mods[:, 1, :], b_mods, vt[p][:], b_vt[p])
        S.dma("sp", Vs[ti], vt[p][:], reads=[b_vt[p]], writes=[b_Vs[ti]])
        emit_transpose(cx, lambda j, p=p: vt[p][:, j * 128:(j + 1) * 128], b_vt[p], 8, ident_bf[0], ident_bf[1], vT[p][:], b_vT[p])
        moe.route_tile(ti, lambda j, p=p: vT[p][:, j, :], b_vT[p])
    cx.phase_end()
    cx.phase_end()

    cx.phase_begin("L3_moe")
    moe.plan()
    vl = [cx.sb("vl%d" % i, [128, D], BF16) for i in range(2)]; b_vl = [Buf(), Buf()]
    for ti in range(NT3):
        p = ti % 2
        S.dma("sp", vl[p][:], Vs[ti], reads=[b_Vs[ti]], writes=[b_vl[p]])
        moe.dispatch_tile(ti, vl[p][:], b_vl[p])
    moe.experts()
    cx.phase_end()

    cx.phase_begin("L3_fin")
    nb = NormBufs(cx, "d")
    h1t = [cx.sb("h1t%d" % i, [128, D]) for i in range(2)]; b_h1t = [Buf(), Buf()]
    y0 = [cx.sb("y0_%d" % i, [128, D]) for i in range(2)]; b_y0 = [Buf(), Buf()]
    y1 = [cx.sb("y1_%d" % i, [128, D]) for i in range(2)]; b_y1 = [Buf(), Buf()]
    ot_ = [cx.sb("ofin%d" % i, [128, D]) for i in range(2)]; b_ofin = [Buf(), Buf()]
    b_out = Buf()
    for ti in range(NT3):
        p = ti % 2
        S.dma("sp", h1t[p][:], H3[ti], reads=[b_H3[ti]], writes=[b_h1t[p]])
        moe.gather_tile(ti, y0[p][:], y1[p][:], b_y0[p], b_y1[p])
        S.op("dve", lambda e, p=p, ti=ti: e.tensor_scalar(out=y0[p][:], in0=y0[p][:], scalar1=moe.gate[:, ti, 0:1], scalar2=None, op0=ALU.mult),
             reads=[b_y0[p], moe.b_gate[ti]], writes=[b_y0[p]])
        S.op("dve", lambda e, p=p, ti=ti: e.scalar_tensor_tensor(out=y0[p][:], in0=y1[p][:], scalar=moe.gate[:, ti, 1:2], in1=y0[p][:],
                                                                op0=ALU.mult, op1=ALU.add),
             reads=[b_y0[p], b_y1[p], moe.b_gate[ti]], writes=[b_y0[p]])
        S.op("dve", lambda e, p=p: e.tensor_tensor(out=y0[p][:], in0=y0[p][:], in1=mods[:, 3, :], op=ALU.mult),
             reads=[b_y0[p], b_mods], writes=[b_y0[p]])
        S.op("dve", lambda e, p=p: e.tensor_tensor(out=h1t[p][:], in0=h1t[p][:], in1=y0[p][:], op=ALU.add),
             reads=[b_y0[p], b_h1t[p]], writes=[b_h1t[p]])
        emit_adaln(cx, nb, h1t[p][:], b_h1t[p], gfin_s[:], None, b_gfin, ot_[p][:], b_ofin[p])
        S.dma("sp", out[ti * 128:(ti + 1) * 128, :], ot_[p][:], reads=[b_ofin[p]], writes=[b_out])
    cx.phase_end()
    cx.phase_end()
    return b_out


def l3_inputs(I, b, half, l2res_pair, consts):
    d = dict(consts)
    if l2res_pair is not None:
        d["QT"] = l2res_pair[half]["QT"]
        d["KT"] = np.ascontiguousarray(np.concatenate([l2res_pair[0]["KT"], l2res_pair[1]["KT"]], 2))
        d["Vf"] = np.ascontiguousarray(np.concatenate([l2res_pair[0]["Vo"], l2res_pair[1]["Vo"]], 0))
        d["hin"] = np.ascontiguousarray(l2res_pair[half]["hout"][128:])
    d["cT"] = np.ascontiguousarray(I["c"][b].reshape(8, 128).T)
    d["wmod"] = np.ascontiguousarray(I["w_mod"][1][:, 2048:6144])
    d["bmod"] = np.ascontiguousarray(I["b_mod"][1][None, 2048:6144])
    d["g2"] = np.ascontiguousarray(I["norm2_g"][1][None, :])
    d["gfin"] = np.ascontiguousarray(I["final_norm_g"][None, :])
    d["dalam"] = np.ascontiguousarray(I["da_lambda"][0].reshape(1, 256))
    d["dagT"] = np.ascontiguousarray(I["da_norm_g"][0].T)
    d["w_out"] = I["w_out_odd"][0]
    d["Wr"] = np.ascontiguousarray(np.concatenate([I["router_g_w"][1], I["router_e_w"][1]], 1))
    d["br"] = np.ascontiguousarray(np.concatenate([I["router_g_b"][1], I["router_e_b"][1]])[None, :])
    d["w1"] = I["w1"][1]; d["w3"] = I["w3"][1]; d["w2"] = I["w2"][1]
    return d


PAIRS = [[0, 1], [2, 3], [4, 5], [6, 7]]
MCH = [(0, 2048), (2048, 2048), (4096, 2048), (6144, 2048), (8192, 256)]

L1_SPECS = [("xin", [NT * 128, D], F32), ("cT", [128, 8], F32), ("cctxT", [128, 8], F32), ("wmod", [D, 2048], F32), ("bmod", [1, 2048], F32),
            ("g1", [1, D], F32), ("Wfm", [D, 1536], F32), ("Wtm", [D, 1032], F32), ("gbias", [1, 8], F32), ("rld", [1, 4], F32),
            ("mlg", [1, 256], F32), ("retg", [1, 256], F32), ("ident_bf", [128, 128], BF16), ("ident_f", [128, 128], F32),
            ("mask_f32", [128, 2, 128], F32), ("mask_bf", [128, 2, 128], BF16), ("ones_f", [128, 128], F32), ("poscol", [128, 4], F32),
            ("ropecos", [128, NT * 128], F32), ("ropesin", [128, NT * 128], F32)]
L2_SPECS = [("xin", [NT2 * 128, D], F32), ("idxm", [128, NT2, 2], I32), ("cT", [128, 8], F32), ("cctxT", [128, 8], F32),
            ("wmod", [D, 6144], F32), ("bmod", [1, 6144], F32), ("g2_0", [1, D], F32), ("g1_1", [1, D], F32), ("w_out", [D, D], F32),
            ("Wr", [D, 36], F32), ("br", [1, 36], F32), ("w1", [32, D, 512], F32), ("w3", [32, D, 512], F32), ("w2", [32, 512, D], F32),
            ("Wq", [D, 2048], F32), ("Wk", [D, 2048], F32), ("Wv", [D, D], F32), ("rc2", [128, NT2 * 128], F32), ("rs2", [128, NT2 * 128], F32),
            ("ident_bf", [128, 128], BF16), ("ident_f", [128, 128], F32), ("ones_f", [128, 128], F32), ("slt", [128, 128], F32),
            ("blkstart", [128, 1], F32), ("pcol", [128, 1], F32), ("thr", [128, 34], F32)]
L3_SPECS = [("cT", [128, 8], F32), ("wmod", [D, 4096], F32), ("bmod", [1, 4096], F32), ("g2", [1, D], F32), ("gfin", [1, D], F32),
            ("dalam", [1, 256], F32), ("dagT", [128, 8], F32), ("w_out", [D, D], F32), ("Wr", [D, 36], F32), ("br", [1, 36], F32),
            ("w1", [32, D, 512], F32), ("w3", [32, D, 512], F32), ("w2", [32, 512, D], F32),
            ("ident_bf", [128, 128], BF16), ("ident_f", [128, 128], F32), ("ones_f", [128, 128], F32), ("ones_bf", [128, 128], BF16),
            ("slt", [128, 128], F32), ("blkstart", [128, 1], F32), ("pcol", [128, 1], F32), ("thr", [128, 34], F32)]


def build_fused(nc):
    cx = Ctx(nc)
    S = cx.S
    io1 = {n: cx.din("l1_" + n, sh, dt) for (n, sh, dt) in L1_SPECS}
    io2 = {n: cx.din("l2_" + n, sh, dt) for (n, sh, dt) in L2_SPECS}
    io3 = {n: cx.din("l3_" + n, sh, dt) for (n, sh, dt) in L3_SPECS}
    out = cx.dout("out", [NT3 * 128, D])

    moeA = MoE(cx, NT2, io2["Wr"], io2["br"], io2["w1"], io2["w3"], io2["w2"], tag="A")
    moeB = MoE(cx, NT3, io3["Wr"], io3["br"], io3["w1"], io3["w3"], io3["w2"], tag="B")
    io1["after_weights"] = moeA.precast
    io2["moe"] = moeA
    io3["moe"] = moeB

    mergedX = cx.dscr("mergedX", [NT * 128, 512], BF16)
    io1["merged"] = mergedX
    b_merged = build_l1(cx, io1)

    MG = cx.dscr("MG", [2 * NT * 128, 512], BF16); b_MG = Buf()
    for (r0, n) in MCH:
        S.collective("AllGather", [mergedX[r0:r0 + n, :]], [MG[2 * r0:2 * r0 + 2 * n, :]], PAIRS, reads=[b_merged], writes=[b_MG])

    H2 = cx.dscr("H2", [NT2 * 128, D])
    QTs = cx.dscr("QTs", [8, 128, 4096], BF16)
    KTo = cx.dscr("KTo", [8, 128, NT2 * 128], BF16)
    Vown = cx.dscr("Vown", [NT2 * 128, D], BF16)
    io2.update({"MG": MG, "b_MG": b_MG, "H2": H2, "QTs": QTs, "KTo": KTo, "Vown": Vown})
    b_H2, b_QTs, b_KTo, b_Vown = build_l2(cx, io2)

    KTg = cx.dscr("KTg", [8, 256, NT2 * 128], BF16); b_KTg = Buf()
    VG = cx.dscr("VG", [2 * NT2 * 128, D], BF16); b_VG = Buf()
    for h in range(8):
        S.collective("AllGather", [KTo[h]], [KTg[h]], PAIRS, reads=[b_KTo], writes=[b_KTg])
    for (r0, n) in VCH:
        S.collective("AllGather", [Vown[r0:r0 + n, :]], [VG[2 * r0:2 * r0 + 2 * n, :]], PAIRS, reads=[b_Vown], writes=[b_VG])

    moeB.precast()
    io3.update({"QTs": QTs, "b_QTs": b_QTs, "KTg": KTg, "b_KTg": b_KTg, "VG": VG, "b_VG": b_VG, "H2": H2, "b_H2": b_H2, "out": out})
    b_out = build_l3(cx, io3)
    S.finish([b_out], "sp")
    print("fused instructions:", S.n_inst)
    return nc


def fused_inputs(I, core, c1, c2, c3):
    b, g = core // 2, core % 2
    d = {}
    for k, v in l1_inputs(I, b, g, c1).items():
        d["l1_" + k] = v
    d2 = l2_inputs(I, b, g, None, c2[g])
    for k, v in d2.items():
        d["l2_" + k] = v
    trow = np.concatenate([np.arange(g * 128, (g + 1) * 128), 256 + np.arange(g * 4096, (g + 1) * 4096)])
    r0 = np.minimum((trow // 2048) * 2048, 8192)
    n = np.where(r0 < 8192, 2048, 256)
    idx = np.stack([2 * r0 + r * n + (trow - r0) for r in range(2)], 1).astype(np.int32)
    d["l2_idxm"] = np.ascontiguousarray(idx.reshape(NT2, 128, 2).transpose(1, 0, 2))
    d3 = l3_inputs(I, b, g, None, c3)
    for k, v in d3.items():
        d["l3_" + k] = v
    return d


from concourse.bass_utils import run_bass_kernel_spmd

_NC_CACHE = {}


def kernel(**inputs):
    I = {k: np.asarray(v) for k, v in inputs.items()}
    cores = list(range(8))
    if "fused" not in _NC_CACHE:
        nc = bass.Bass("TRN2", target_bir_lowering=False)
        build_fused(nc)
        _NC_CACHE["fused"] = nc
    nc = _NC_CACHE["fused"]
    c1 = host_consts_l1(); c2 = [host_consts_l2(0), host_consts_l2(1)]; c3 = host_consts_l3()
    names = set(["l1_" + n for n, _, _ in L1_SPECS] + ["l2_" + n for n, _, _ in L2_SPECS] + ["l3_" + n for n, _, _ in L3_SPECS])
    in_maps = []
    for core in cores:
        m = fused_inputs(I, core, c1, c2, c3)
        in_maps.append({k: v for k, v in m.items() if k in names})
    res = run_bass_kernel_spmd(nc, in_maps, core_ids=cores).results
    out = np.empty((4, 8192, 1024), np.float32)
    for core in cores:
        b, half = core // 2, core % 2
        out[b, half * 4096:(half + 1) * 4096] = res[core]["out"]
    return out
```

```python
import math
import contextlib
import numpy as np
import ml_dtypes
import concourse.bass as bass
import concourse.mybir as mybir


F32 = mybir.dt.float32
BF16 = mybir.dt.bfloat16
I32 = mybir.dt.int32
U32 = mybir.dt.uint32
AF = mybir.ActivationFunctionType
ALU = mybir.AluOpType
AX = mybir.AxisListType


class Buf:
    __slots__ = ("name", "w", "r")

    def __init__(self, name=""):
        self.name = name
        self.w = None
        self.r = {}


class Sync:
    def __init__(self, nc, n_dma_sems=48, same_engine_wait=True):
        self.nc = nc
        self.eng = {"pe": nc.tensor, "act": nc.scalar, "dve": nc.vector, "pool": nc.gpsimd, "sp": nc.sync}
        self.sem = {}
        self.cnt = {}
        for k in ["pe", "act", "dve", "pool"]:
            self.sem[k] = nc.alloc_semaphore(name="s_" + k)
            self.cnt[k] = 0
        self.dsem = [nc.alloc_semaphore(name="s_dma%d" % i) for i in range(n_dma_sems)]
        self.dcnt = [0] * n_dma_sems
        self.dnext = 0
        self.waited = {k: {} for k in self.eng}
        self.same_engine_wait = same_engine_wait
        self.n_inst = 0
        self.bg = []

    def _semh(self, key):
        return self.sem[key] if isinstance(key, str) else self.dsem[key]

    def _wait(self, e, key, val):
        if val <= 0:
            return
        w = self.waited[e]
        if w.get(key, 0) >= val:
            return
        if key == e and not self.same_engine_wait:
            return
        if key == e == "pe":
            return
        self.eng[e].wait_ge(self._semh(key), val)
        w[key] = val

    def _deps(self, e, reads, writes):
        for b in reads:
            if b.w is not None:
                self._wait(e, *b.w)
        for b in writes:
            if b.w is not None:
                self._wait(e, *b.w)
            for k, v in b.r.items():
                self._wait(e, k, v)

    def _record(self, ev, reads, writes):
        k, v = ev
        for b in reads:
            if b.r.get(k, 0) < v:
                b.r[k] = v
        for b in writes:
            b.w = ev
            b.r = {}

    def op(self, e, fn, reads=(), writes=(), inc=True):
        self._deps(e, reads, writes)
        ins = fn(self.eng[e])
        if inc:
            self.cnt[e] += 1
            ins.then_inc(self.sem[e], 1)
            self._record((e, self.cnt[e]), reads, writes)
        else:
            self._record((e, self.cnt[e] + 1), reads, writes)
        self.n_inst += 1
        return ins

    def dma(self, q, out, in_, reads=(), writes=(), indirect=None, **kw):
        self._deps(q, reads, writes)
        k = self.dnext
        self.dnext = (self.dnext + 1) % len(self.dsem)
        self._wait(q, k, 16 * self.dcnt[k])
        if indirect is not None:
            ins = self.eng[q].indirect_dma_start(out, indirect.get("out_offset"), in_, indirect.get("in_offset"), **kw)
        else:
            ins = self.eng[q].dma_start(out=out, in_=in_, **kw)
        self.dcnt[k] += 1
        ins.then_inc(self.dsem[k], 16)
        self._record((k, 16 * self.dcnt[k]), reads, writes)
        self.n_inst += 1
        return ins

    def collective(self, kind, ins, outs, groups, reads=(), writes=()):
        self._deps("pool", reads, writes)
        if "cc" not in self.sem:
            self.sem["cc"] = self.nc.alloc_semaphore(name="s_cc")
            self.cnt["cc"] = 0
        ins_ = self.nc.gpsimd.collective_compute(kind, ALU.bypass, replica_groups=groups, ins=ins, outs=outs)
        self.cnt["cc"] += 1
        ins_.then_inc(self.sem["cc"], 1)
        self._record(("cc", self.cnt["cc"]), reads, writes)
        self.n_inst += 1
        return ins_

    def dma_bg(self, q, pairs, reads=(), writes=()):
        self._deps(q, reads, writes)
        key = "bg%d" % len(self.bg)
        self.sem[key] = self.nc.alloc_semaphore(name="s_" + key)
        self.cnt[key] = 0
        self.bg.append(key)
        for (out, in_) in pairs:
            ins = self.eng[q].dma_start(out=out, in_=in_)
            ins.then_inc(self.sem[key], 16)
            self.cnt[key] += 16
            self.n_inst += 1
        self._record((key, self.cnt[key]), reads, writes)

    def barrier(self):
        for e in self.eng:
            for k in self.sem:
                self._wait(e, k, self.cnt[k])
            for k in range(len(self.dsem)):
                self._wait(e, k, 16 * self.dcnt[k])

    def touch(self, e, reads=(), writes=()):
        self._deps(e, reads, writes)

    def finish(self, bufs, e="sp"):
        for b in bufs:
            if b.w is not None:
                self._wait(e, *b.w)


D = 1024
EPS = 1e-6
XOFF = 524288
PROFILE_SCOPES = False
SAME_ENGINE_WAIT = True


class Ctx:
    def __init__(self, nc):
        self.nc = nc
        self.S = Sync(nc, same_engine_wait=SAME_ENGINE_WAIT)
        self.stk = [contextlib.ExitStack()]
        self.PB = [nc.alloc_psum_tensor("pb%d" % i, [128, 512], F32) for i in range(7)]
        self.PBb = [Buf() for _ in range(7)]
        self.PT = nc.alloc_psum_tensor("pt_bf", [128, 1024], BF16)
        self.b_PT = Buf()
        self.nreg = 0
        self.scopes = []

    def din(self, name, shape, dt=F32):
        return self.nc.dram_tensor(name, list(shape), dt, kind="ExternalInput").ap()

    def dout(self, name, shape, dt=F32):
        return self.nc.dram_tensor(name, list(shape), dt, kind="ExternalOutput").ap()

    def dscr(self, name, shape, dt=F32):
        return self.nc.dram_tensor(name, list(shape), dt).ap()

    def sb(self, name, shape, dt=F32):
        self.nreg += 1
        return self.stk[-1].enter_context(self.nc.sbuf_tensor("%s_u%d" % (name, self.nreg), list(shape), dt))

    def phase_begin(self, name=None):
        self.stk.append(contextlib.ExitStack())
        sid = None
        if name is not None and PROFILE_SCOPES:
            sid, _ = self.nc.enter_named_scope(name, False)
        self.scopes.append((name, sid))

    def phase_end(self):
        self.S.barrier()
        name, sid = self.scopes.pop()
        if sid is not None:
            self.nc.leave_named_scope(name, sid, False)
        self.stk.pop().close()

    def load_const(self, name, shape, dt=F32):
        d = self.din(name, shape, dt)
        t = self.sb(name + "_s", shape, dt)
        b = Buf()
        self.S.dma("sp", t[:], d, writes=[b])
        return t, b

    def load_bcast(self, name, n, dram_ap=None):
        d = dram_ap if dram_ap is not None else self.din(name, [1, n])
        t = self.sb(name + "_s", [128, n])
        b = Buf()
        self.S.dma("sp", t[:], d.partition_broadcast(128), writes=[b])
        return t, b


def common_consts():
    c = {}
    c["ident_bf"] = np.eye(128, dtype=np.float32).astype(ml_dtypes.bfloat16)
    c["ident_f"] = np.eye(128, dtype=np.float32)
    c["ones_f"] = np.ones((128, 128), np.float32)
    s = np.arange(128)
    c["slt"] = (s[:, None] < s[None, :]).astype(np.float32)
    c["blkstart"] = (256.0 * s).astype(np.float32)[:, None]
    c["pcol"] = (1.0 * s).astype(np.float32)[:, None]
    c["thr"] = np.tile((256.0 * np.arange(34)).astype(np.float32)[None, :], (128, 1))
    return c


def emit_silu_bcast(cx, cT_aps, ones_f, b_ones):
    S = cx.S
    n = len(cT_aps)
    cT_s = cx.sb("cT_s", [128, n, 8]); b_cT = Buf()
    for i, a in enumerate(cT_aps):
        S.dma("sp", cT_s[:, i, :], a, writes=[b_cT])
    sc = cx.sb("sc", [128, n, 8]); b_sc = Buf()
    S.op("act", lambda e: e.activation(out=sc[:], in_=cT_s[:], func=AF.Silu), reads=[b_cT], writes=[b_sc])
    scb = cx.sb("scb", [128, n, 8, 128]); b_scb = Buf()
    for w in range(n):
        for j in range(8):
            S.op("dve", lambda e, w=w, j=j: e.tensor_scalar(out=scb[:, w, j, :], in0=ones_f[:], scalar1=sc[:, w, j:j + 1],
                                                           scalar2=None, op0=ALU.mult),
                 reads=[b_ones, b_sc], writes=[b_scb])
    return scb, b_scb


def emit_mod(cx, scb, b_scb, nvec, wmod_ap, bmod_s, b_bmod, ncols, dest_fn, b_dest, wm_s, b_wm):
    S = cx.S
    wmod_v = wmod_ap.rearrange("(j p) n -> p j n", p=128)
    k = 0
    for cc in range(ncols // 512):
        wb = cc % 2
        S.dma("sp", wm_s[wb][:], wmod_v[:, :, cc * 512:(cc + 1) * 512], writes=[b_wm[wb]])
        for w in range(nvec):
            pbi = k % 7; k += 1
            for j in range(8):
                S.op("pe", lambda e, w=w, j=j, wb=wb, pbi=pbi: e.matmul(cx.PB[pbi][:], lhsT=scb[:, w, j, :], rhs=wm_s[wb][:, j, :],
                                                                        start=(j == 0), stop=(j == 7)),
                     reads=[b_scb, b_wm[wb]], writes=[cx.PBb[pbi]])
            S.op("dve", lambda e, w=w, pbi=pbi, cc=cc: e.tensor_tensor(out=dest_fn(w, cc), in0=cx.PB[pbi][:],
                                                                      in1=bmod_s[:, cc * 512:(cc + 1) * 512], op=ALU.add),
                 reads=[cx.PBb[pbi], b_bmod], writes=[b_dest])


class NormBufs:
    def __init__(self, cx, tag=""):
        self.junk = cx.sb("junk" + tag, [128, D], BF16); self.b_junk = Buf()
        self.ss = cx.sb("ss" + tag, [128, 2]); self.b_ss = [Buf(), Buf()]
        self.rstd = cx.sb("rstd" + tag, [128, 2]); self.b_rstd = [Buf(), Buf()]
        self.tmp32 = cx.sb("tmp32" + tag, [128, D]); self.b_tmp32 = Buf()
        self.k = 0


def emit_adaln(cx, nb, x_ap, b_x, A_ap, Bsh_ap, b_mod, out_ap, b_out):
    S = cx.S
    xp = nb.k % 2; nb.k += 1
    ss = nb.ss[:, xp:xp + 1]; rstd = nb.rstd[:, xp:xp + 1]
    S.op("dve", lambda e: e.memset(ss, 0.0), writes=[nb.b_ss[xp]])
    S.op("act", lambda e: e.activation(out=nb.junk[:], in_=x_ap, func=AF.Square, accum_out=ss),
         reads=[b_x], writes=[nb.b_junk, nb.b_ss[xp]])
    S.op("dve", lambda e: e.tensor_scalar(out=rstd, in0=ss, scalar1=1.0 / D, scalar2=EPS, op0=ALU.mult, op1=ALU.add),
         reads=[nb.b_ss[xp]], writes=[nb.b_rstd[xp]])
    S.op("act", lambda e: e.activation(out=rstd, in_=rstd, func=AF.Sqrt), reads=[nb.b_rstd[xp]], writes=[nb.b_rstd[xp]])
    S.op("dve", lambda e: e.reciprocal(out=rstd, in_=rstd), reads=[nb.b_rstd[xp]], writes=[nb.b_rstd[xp]])
    if Bsh_ap is None:
        S.op("dve", lambda e: e.scalar_tensor_tensor(out=out_ap, in0=x_ap, scalar=rstd, in1=A_ap, op0=ALU.mult, op1=ALU.mult),
             reads=[b_x, nb.b_rstd[xp], b_mod], writes=[b_out])
    else:
        S.op("dve", lambda e: e.scalar_tensor_tensor(out=nb.tmp32[:], in0=x_ap, scalar=rstd, in1=A_ap, op0=ALU.mult, op1=ALU.mult),
             reads=[b_x, nb.b_rstd[xp], b_mod], writes=[nb.b_tmp32])
        S.op("dve", lambda e: e.tensor_tensor(out=out_ap, in0=nb.tmp32[:], in1=Bsh_ap, op=ALU.add),
             reads=[nb.b_tmp32, b_mod], writes=[b_out])


def emit_transpose(cx, src_fn, b_src, nblk, ident_bf, b_ident, dst_ap, b_dst):
    S = cx.S
    for j in range(nblk):
        S.op("pe", lambda e, j=j: e.transpose(cx.PT[:, j * 128:(j + 1) * 128], src_fn(j), ident_bf[:]),
             reads=[b_src, b_ident], writes=[cx.b_PT])
    S.op("act", lambda e: e.activation(out=dst_ap, in_=cx.PT[:, 0:nblk * 128].rearrange("p (j t) -> p j t", j=nblk), func=AF.Copy),
         reads=[cx.b_PT], writes=[b_dst])


class MoE:
    def __init__(self, cx, ntile, Wr_d, br_d, w1_d, w3_d, w2_d, consts=None, tag=""):
        self.cx = cx
        self.nt = ntile
        self.A = 2 * ntile * 128
        self.nblk = (self.A + 255) // 256 + 32
        self.c = consts
        self.tag = tag
        self.w1_d, self.w3_d, self.w2_d = w1_d, w3_d, w2_d
        self.Wr_d, self.br_d = Wr_d, br_d
        nr = self.nblk * 256
        self.xbuf = cx.dscr("xbuf" + tag, [nr, D], BF16); self.b_xbuf = Buf()
        self.ybuf = cx.dscr("ybuf" + tag, [nr, D], F32); self.b_ybuf = Buf()
        self.W1b = cx.dscr("W1b" + tag, [32 * 128, 4096], BF16); self.b_W1b = Buf()
        self.W3b = cx.dscr("W3b" + tag, [32 * 128, 4096], BF16); self.b_W3b = Buf()
        self.W2b = cx.dscr("W2b" + tag, [32 * 128, 4096], BF16); self.b_W2b = Buf()

    def precast(self):
        S = self.cx.S
        for (src, dst, b, j) in [(self.w1_d, self.W1b, self.b_W1b, 8), (self.w3_d, self.W3b, self.b_W3b, 8), (self.w2_d, self.W2b, self.b_W2b, 4)]:
            sv = src.rearrange("e (p j) n -> e p (j n)", j=j)
            for e_ in range(32):
                S.dma("pool", dst[e_ * 128:(e_ + 1) * 128, :].rearrange("p (a b) -> p a b", a=2), sv[e_].rearrange("p (a b) -> p a b", a=2), writes=[b])

    def alloc_persistent(self):
        cx = self.cx
        t = self.tag
        self.OH = cx.sb("OH" + t, [128, self.nt, 2, 32]); self.b_OH = [Buf() for _ in range(self.nt)]
        self.gate = cx.sb("gate" + t, [128, self.nt, 2]); self.b_gate = [Buf() for _ in range(self.nt)]
        self.dest = cx.sb("dest" + t, [128, self.nt, 2], I32); self.b_dest = [Buf() for _ in range(self.nt)]
        self.idxw = cx.sb("idxw" + t, [128, 128], I32); self.b_idxw = Buf()
        self.Wr_s = cx.sb("Wr_s" + t, [128, 8, 36], BF16); self.b_Wr = Buf()
        for j in range(8):
            cx.S.dma("pool", self.Wr_s[:, j, :], self.Wr_d[j * 128:(j + 1) * 128, :], writes=[self.b_Wr])
        self.br_s, self.b_br = cx.load_bcast("br" + t, 36, self.br_d)
        self.lg = cx.sb("lg" + t, [128, 36]); self.b_lg = Buf()
        self.sm = cx.sb("rsm" + t, [128, 64]); self.b_sm = Buf()

    def route_tile(self, ti, vT_fn, b_vT):
        cx, S = self.cx, self.cx.S
        pbi = 6
        PBt, bPB = cx.PB[pbi], cx.PBb[pbi]
        for j in range(8):
            S.op("pe", lambda e, j=j: e.matmul(PBt[:, 0:36], lhsT=vT_fn(j), rhs=self.Wr_s[:, j, :], start=(j == 0), stop=(j == 7)),
                 reads=[b_vT, self.b_Wr], writes=[bPB])
        lg, sm = self.lg, self.sm
        b_lg, b_sm = self.b_lg, self.b_sm
        S.op("dve", lambda e: e.tensor_tensor(out=lg[:], in0=PBt[:, 0:36], in1=self.br_s[:], op=ALU.add), reads=[bPB, self.b_br], writes=[b_lg])
        S.op("dve", lambda e: e.tensor_reduce(out=sm[:, 0:1], in_=lg[:, 0:4], axis=AX.X, op=ALU.max), reads=[b_lg], writes=[b_sm])
        S.op("dve", lambda e: e.tensor_scalar(out=sm[:, 1:2], in0=sm[:, 0:1], scalar1=-1.0, scalar2=None, op0=ALU.mult), reads=[b_sm], writes=[b_sm])
        S.op("dve", lambda e: e.memset(sm[:, 2:3], 0.0), reads=[b_sm], writes=[b_sm])
        S.op("act", lambda e: e.activation(out=sm[:, 44:48], in_=lg[:, 0:4], func=AF.Exp, bias=sm[:, 1:2], accum_out=sm[:, 2:3]),
             reads=[b_lg, b_sm], writes=[b_sm])
        S.op("dve", lambda e: e.reciprocal(out=sm[:, 3:4], in_=sm[:, 2:3]), reads=[b_sm], writes=[b_sm])
        S.op("dve", lambda e: e.tensor_scalar(out=sm[:, 4:8], in0=lg[:, 0:4], scalar1=sm[:, 0:1], scalar2=None, op0=ALU.is_equal),
             reads=[b_lg, b_sm], writes=[b_sm])
        S.op("dve", lambda e: e.tensor_scalar(out=sm[:, 8:16], in0=lg[:, 4:12], scalar1=sm[:, 4:5], scalar2=None, op0=ALU.mult),
             reads=[b_lg, b_sm], writes=[b_sm])
        for g in range(1, 4):
            S.op("dve", lambda e, g=g: e.scalar_tensor_tensor(out=sm[:, 8:16], in0=lg[:, 4 + 8 * g:12 + 8 * g], scalar=sm[:, 4 + g:5 + g],
                                                             in1=sm[:, 8:16], op0=ALU.mult, op1=ALU.add),
                 reads=[b_lg, b_sm], writes=[b_sm])
        S.op("dve", lambda e: e.max(out=sm[:, 16:24], in_=sm[:, 8:16]), reads=[b_sm], writes=[b_sm])
        S.op("dve", lambda e: e.tensor_scalar(out=sm[:, 24:32], in0=sm[:, 8:16], scalar1=sm[:, 16:17], scalar2=None, op0=ALU.is_equal),
             reads=[b_sm], writes=[b_sm])
        S.op("dve", lambda e: e.tensor_scalar(out=sm[:, 32:40], in0=sm[:, 8:16], scalar1=sm[:, 17:18], scalar2=None, op0=ALU.is_equal),
             reads=[b_sm], writes=[b_sm])
        S.op("dve", lambda e: e.tensor_tensor(out=sm[:, 40:41], in0=sm[:, 17:18], in1=sm[:, 16:17], op=ALU.subtract), reads=[b_sm], writes=[b_sm])
        S.op("act", lambda e: e.activation(out=sm[:, 41:42], in_=sm[:, 40:41], func=AF.Exp), reads=[b_sm], writes=[b_sm])
        S.op("dve", lambda e: e.tensor_scalar(out=sm[:, 42:43], in0=sm[:, 41:42], scalar1=1.0, scalar2=None, op0=ALU.add), reads=[b_sm], writes=[b_sm])
        S.op("dve", lambda e: e.reciprocal(out=sm[:, 42:43], in_=sm[:, 42:43]), reads=[b_sm], writes=[b_sm])
        S.op("dve", lambda e: e.tensor_tensor(out=sm[:, 43:44], in0=sm[:, 41:42], in1=sm[:, 42:43], op=ALU.mult), reads=[b_sm], writes=[b_sm])
        S.op("dve", lambda e: e.tensor_scalar(out=self.gate[:, ti, :], in0=sm[:, 42:44], scalar1=sm[:, 3:4], scalar2=None, op0=ALU.mult),
             reads=[b_sm], writes=[self.b_gate[ti]])
        for k in range(2):
            for g in range(4):
                S.op("dve", lambda e, k=k, g=g: e.tensor_scalar(out=self.OH[:, ti, k, g * 8:(g + 1) * 8], in0=sm[:, 24 + 8 * k:32 + 8 * k],
                                                               scalar1=sm[:, 4 + g:5 + g], scalar2=None, op0=ALU.mult),
                     reads=[b_sm], writes=[self.b_OH[ti]])

    def plan(self):
        cx, S = self.cx, self.cx.S
        t = self.tag
        ones_f, b_ones = self.c["ones_f"]
        ident_f, b_identf = self.c["ident_f"]
        blkstart, b_blk = self.c["blkstart"]
        PBt, bPB = cx.PB[0], cx.PBb[0]
        for ti in range(self.nt):
            S.op("pe", lambda e, ti=ti: e.matmul(PBt[:, 0:64], lhsT=ones_f[:], rhs=self.OH[:, ti, :, :].rearrange("p k e -> p (k e)"),
                                                 start=(ti == 0), stop=(ti == self.nt - 1)),
                 reads=[b_ones, self.b_OH[ti]], writes=[bPB])
        pl = cx.sb("pl" + t, [128, 8, 32]); b_pl = Buf()
        S.op("dve", lambda e: e.tensor_copy(out=pl[:, 7, :], in_=PBt[:, 0:32]), reads=[bPB], writes=[b_pl])
        S.op("dve", lambda e: e.tensor_tensor(out=pl[:, 0, :], in0=pl[:, 7, :], in1=PBt[:, 32:64], op=ALU.add), reads=[bPB, b_pl], writes=[b_pl])
        thr, b_thr = self.c["thr"]
        cmp3 = cx.sb("cmp3" + t, [128, 32, 34]); b_cmp3 = Buf()
        S.op("dve", lambda e: e.tensor_tensor(out=cmp3[:], in0=pl[:, 0, :].unsqueeze(2).broadcast_to([128, 32, 34]),
                                              in1=thr[:].unsqueeze(1).broadcast_to([128, 32, 34]), op=ALU.is_gt),
             reads=[b_pl, b_thr], writes=[b_cmp3])
        S.op("dve", lambda e: e.tensor_reduce(out=pl[:, 1, :], in_=cmp3[:], axis=AX.X, op=ALU.add), reads=[b_cmp3], writes=[b_pl])
        S.op("dve", lambda e: e.tensor_scalar(out=pl[:, 3, :], in0=pl[:, 1, :], scalar1=256.0, scalar2=None, op0=ALU.mult), reads=[b_pl], writes=[b_pl])
        S.op("dve", lambda e: e.memset(pl[:, 5, :], 0.0), reads=[b_pl], writes=[b_pl])
        S.op("dve", lambda e: e.tensor_tensor_scan(out=pl[:, 4, :], data0=pl[:, 3, :], data1=pl[:, 5, :], initial=0.0, op0=ALU.add, op1=ALU.add),
             reads=[b_pl], writes=[b_pl])
        self.run = cx.sb("run" + t, [128, 32]); self.b_run = Buf()
        S.op("dve", lambda e: e.tensor_tensor(out=self.run[:], in0=pl[:, 4, :], in1=pl[:, 3, :], op=ALU.subtract), reads=[b_pl], writes=[self.b_run])
        S.op("dve", lambda e: e.tensor_scalar(out=pl[:, 6, :], in0=pl[:, 4, :], scalar1=blkstart[:, 0:1], scalar2=None, op0=ALU.is_le),
             reads=[b_pl, b_blk], writes=[b_pl])
        be = cx.sb("be" + t, [128, 1]); b_be = Buf()
        S.op("dve", lambda e: e.tensor_reduce(out=be[:], in_=pl[:, 6, :], axis=AX.X, op=ALU.add), reads=[b_pl], writes=[b_be])
        S.op("dve", lambda e: e.tensor_scalar(out=be[:], in0=be[:], scalar1=31.0, scalar2=128.0, op0=ALU.min, op1=ALU.mult), reads=[b_be], writes=[b_be])
        bbc = cx.sb("bbc" + t, [128, 128]); b_bbc = Buf()
        pcol, b_pcol = self.c["pcol"]
        S.op("dve", lambda e: e.tensor_scalar(out=bbc[:], in0=ones_f[:], scalar1=be[:, 0:1], scalar2=None, op0=ALU.mult), reads=[b_be, b_ones], writes=[b_bbc])
        PB1, bPB1 = cx.PB[1], cx.PBb[1]
        S.op("pe", lambda e: e.matmul(PB1[:, 0:128], lhsT=bbc[:], rhs=ident_f[:], start=True, stop=True), reads=[b_bbc, b_identf], writes=[bPB1])
        idxf = cx.sb("idxf" + t, [128, 128]); b_idxf = Buf()
        S.op("dve", lambda e: e.tensor_scalar(out=idxf[:], in0=PB1[:, 0:128], scalar1=pcol[:, 0:1], scalar2=None, op0=ALU.add), reads=[bPB1, b_pcol], writes=[b_idxf])
        S.op("dve", lambda e: e.tensor_copy(out=self.idxw[:], in_=idxf[:]), reads=[b_idxf], writes=[self.b_idxw])
        self.d1 = cx.sb("d1" + t, [128, 32]); self.b_d1 = Buf()
        self.destf = cx.sb("destf" + t, [128, 2]); self.b_destf = Buf()

    def dispatch_tile(self, ti, v_ap, b_v, do_scatter=True):
        cx, S = self.cx, self.cx.S
        ones_f, b_ones = self.c["ones_f"]
        slt, b_slt = self.c["slt"]
        for k in range(2):
            pa, ba = cx.PB[2 + k], cx.PBb[2 + k]
            pt_, bt_ = cx.PB[4 + k], cx.PBb[4 + k]
            S.op("pe", lambda e, k=k, pa=pa: e.matmul(pa[:, 0:32], lhsT=slt[:], rhs=self.OH[:, ti, k, :], start=True, stop=True),
                 reads=[b_slt, self.b_OH[ti]], writes=[ba])
            S.op("pe", lambda e, k=k, pt_=pt_: e.matmul(pt_[:, 0:32], lhsT=ones_f[:], rhs=self.OH[:, ti, k, :], start=True, stop=True),
                 reads=[b_ones, self.b_OH[ti]], writes=[bt_])
            S.op("dve", lambda e, pa=pa: e.tensor_tensor(out=self.d1[:], in0=pa[:, 0:32], in1=self.run[:], op=ALU.add),
                 reads=[ba, self.b_run], writes=[self.b_d1])
            S.op("dve", lambda e, k=k: e.tensor_tensor(out=self.d1[:], in0=self.d1[:], in1=self.OH[:, ti, k, :], op=ALU.mult),
                 reads=[self.b_d1, self.b_OH[ti]], writes=[self.b_d1])
            S.op("dve", lambda e, k=k: e.tensor_reduce(out=self.destf[:, k:k + 1], in_=self.d1[:], axis=AX.X, op=ALU.add),
                 reads=[self.b_d1], writes=[self.b_destf])
            S.op("dve", lambda e, k=k: e.tensor_copy(out=self.dest[:, ti, k:k + 1], in_=self.destf[:, k:k + 1]),
                 reads=[self.b_destf], writes=[self.b_dest[ti]])
            S.op("dve", lambda e, pt_=pt_: e.tensor_tensor(out=self.run[:], in0=self.run[:], in1=pt_[:, 0:32], op=ALU.add),
                 reads=[bt_, self.b_run], writes=[self.b_run])
            if do_scatter:
              S.dma("pool", self.xbuf, v_ap, reads=[b_v, self.b_dest[ti]], writes=[self.b_xbuf],
                  indirect={"out_offset": bass.IndirectOffsetOnAxis(ap=self.dest[:, ti, k:k + 1], axis=0)})

    def experts(self):
        cx, S, nc = self.cx, self.cx.S, self.cx.nc
        t = self.tag
        ident_bf, b_identbf = self.c["ident_bf"]
        NW = 3
        w1_s = [cx.sb("w1_s%d%s" % (i, t), [128, 8, 512], BF16) for i in range(NW)]
        w3_s = [cx.sb("w3_s%d%s" % (i, t), [128, 8, 512], BF16) for i in range(NW)]
        w2_s = [cx.sb("w2_s%d%s" % (i, t), [128, 4, 1024], BF16) for i in range(NW)]
        b_w1 = [Buf() for _ in range(NW)]; b_w3 = [Buf() for _ in range(NW)]; b_w2 = [Buf() for _ in range(NW)]
        xb = [cx.sb("xb%d%s" % (i, t), [128, D], BF16) for i in range(3)]; b_xb = [Buf() for _ in range(3)]
        xT = [cx.sb("xT%d%s" % (i, t), [128, 8, 128], BF16) for i in range(2)]; b_xT = [Buf(), Buf()]
        s1 = cx.sb("s1" + t, [128, 512]); b_s1 = Buf()
        hh = [cx.sb("hh%d%s" % (i, t), [128, 512], BF16) for i in range(2)]; b_hh = [Buf(), Buf()]
        hhT = [cx.sb("hhT%d%s" % (i, t), [128, 4, 128], BF16) for i in range(2)]; b_hhT = [Buf(), Buf()]
        yst = [cx.sb("yst%d%s" % (i, t), [128, D]) for i in range(2)]; b_yst = [Buf(), Buf()]
        PTa, b_PTa = cx.PT, cx.b_PT
        PTb, b_PTb = cx.PB[6][:].bitcast(BF16), cx.PBb[6]
        pa, ba = cx.PB[0], cx.PBb[0]
        pb_, bb = cx.PB[1], cx.PBb[1]
        pc = [cx.PB[2], cx.PB[3]]; bc = [cx.PBb[2], cx.PBb[3]]
        nsub = 2 * self.nblk

        def load_w(blk):
            p = blk % NW
            off = bass.IndirectOffsetOnAxis(ap=self.idxw[:, blk:blk + 1], axis=0)
            S.dma("pool", w1_s[p][:].rearrange("p j n -> p (j n)"), self.W1b, reads=[self.b_idxw, self.b_W1b], writes=[b_w1[p]], indirect={"in_offset": off})
            S.dma("pool", w3_s[p][:].rearrange("p j n -> p (j n)"), self.W3b, reads=[self.b_idxw, self.b_W3b], writes=[b_w3[p]], indirect={"in_offset": off})
            S.dma("pool", w2_s[p][:].rearrange("p j n -> p (j n)"), self.W2b, reads=[self.b_idxw, self.b_W2b], writes=[b_w2[p]], indirect={"in_offset": off})

        def load_x(sidx):
            S.dma("sp", xb[sidx % 3][:], self.xbuf[sidx * 128:(sidx + 1) * 128, :], reads=[self.b_xbuf], writes=[b_xb[sidx % 3]])

        def stA(sidx):
            q = sidx % 2
            for j in range(8):
                S.op("pe", lambda e, j=j: e.transpose(PTa[:, j * 128:(j + 1) * 128], xb[sidx % 3][:].rearrange("t (p j) -> t j p", j=8)[:, j, :], ident_bf[:]),
                     reads=[b_xb[sidx % 3], b_identbf], writes=[b_PTa], inc=(j == 7))
            S.op("act", lambda e: e.activation(out=xT[q][:], in_=PTa[:].rearrange("p (j t) -> p j t", j=8), func=AF.Copy), reads=[b_PTa], writes=[b_xT[q]])

        def stB(sidx):
            q = sidx % 2; p = (sidx // 2) % NW
            for j in range(8):
                S.op("pe", lambda e, j=j: e.matmul(pa[:], lhsT=xT[q][:, j, :], rhs=w1_s[p][:, j, :], start=(j == 0), stop=(j == 7)),
                     reads=[b_xT[q], b_w1[p]], writes=[ba], inc=(j == 7))
            for j in range(8):
                S.op("pe", lambda e, j=j: e.matmul(pb_[:], lhsT=xT[q][:, j, :], rhs=w3_s[p][:, j, :], start=(j == 0), stop=(j == 7)),
                     reads=[b_xT[q], b_w3[p]], writes=[bb], inc=(j == 7))
            S.op("act", lambda e: e.activation(out=s1[:], in_=pa[:], func=AF.Silu), reads=[ba], writes=[b_s1])
            S.op("dve", lambda e: e.tensor_tensor(out=hh[q][:], in0=s1[:], in1=pb_[:], op=ALU.mult), reads=[b_s1, bb], writes=[b_hh[q]])

        def stC(sidx):
            q = sidx % 2
            for j in range(4):
                S.op("pe", lambda e, j=j: e.transpose(PTb[:, j * 128:(j + 1) * 128], hh[q][:].rearrange("t (p j) -> t j p", j=4)[:, j, :], ident_bf[:]),
                     reads=[b_hh[q], b_identbf], writes=[b_PTb], inc=(j == 3))
            S.op("act", lambda e: e.activation(out=hhT[q][:], in_=PTb[:, 0:512].rearrange("p (j t) -> p j t", j=4), func=AF.Copy), reads=[b_PTb], writes=[b_hhT[q]])

        def stD(sidx):
            q = sidx % 2; p = (sidx // 2) % NW
            for half in range(2):
                for j in range(4):
                    S.op("pe", lambda e, j=j, half=half: e.matmul(pc[half][:], lhsT=hhT[q][:, j, :], rhs=w2_s[p][:, j, half * 512:(half + 1) * 512],
                                                                  start=(j == 0), stop=(j == 3)),
                         reads=[b_hhT[q], b_w2[p]], writes=[bc[half]], inc=(j == 3))
            S.op("act", lambda e: e.activation(out=yst[q][:, 0:512], in_=pc[0][:], func=AF.Copy), reads=[bc[0]], writes=[b_yst[q]])
            S.op("dve", lambda e: e.tensor_copy(out=yst[q][:, 512:1024], in_=pc[1][:]), reads=[bc[1]], writes=[b_yst[q]])
            S.dma("sp", self.ybuf[sidx * 128:(sidx + 1) * 128, :], yst[q][:], reads=[b_yst[q]], writes=[self.b_ybuf])

        load_w(0)
        load_x(0)
        for i in range(nsub + 2):
            if i % 2 == 0 and i // 2 + 1 < self.nblk:
                load_w(i // 2 + 1)
            if i + 1 < nsub:
                load_x(i + 1)
            if 0 <= i - 2 < nsub:
                stC(i - 2)
            if i < nsub:
                stA(i)
            if 0 <= i - 1 < nsub:
                stB(i - 1)
            if 0 <= i - 2 < nsub:
                stD(i - 2)

    def gather_tile(self, ti, y0_ap, y1_ap, b_y0, b_y1):
        S = self.cx.S
        S.dma("pool", y0_ap, self.ybuf, reads=[self.b_ybuf, self.b_dest[ti]], writes=[b_y0],
              indirect={"in_offset": bass.IndirectOffsetOnAxis(ap=self.dest[:, ti, 0:1], axis=0)})
        S.dma("pool", y1_ap, self.ybuf, reads=[self.b_ybuf, self.b_dest[ti]], writes=[b_y1],
              indirect={"in_offset": bass.IndirectOffsetOnAxis(ap=self.dest[:, ti, 1:2], axis=0)})


D = 1024
NT = 66
NG = 22
RS = 128 ** -0.5
EPS = 1e-6


def host_consts_l1():
    c = {}
    c["ident_bf"] = np.eye(128, dtype=np.float32).astype(ml_dtypes.bfloat16)
    c["ident_f"] = np.eye(128, dtype=np.float32)
    s = np.arange(128)
    mf = (s[:, None] <= s[None, :]).astype(np.float32)
    mb = (s[:, None] >= s[None, :]).astype(np.float32)
    c["mask_f32"] = np.stack([mf, mb], 1)
    c["mask_bf"] = c["mask_f32"].astype(ml_dtypes.bfloat16)
    c["ones_f"] = np.ones((128, 128), np.float32)
    pos = np.arange(128, dtype=np.float32)
    c["poscol"] = np.stack([127.0 - pos, pos, -(127.0 - pos), -pos], 1).astype(np.float32)
    T = NT * 128
    inv = (10000.0 ** (-np.arange(64, dtype=np.float32) / 64)).astype(np.float32)
    ang = (np.arange(T, dtype=np.float32)[:, None] * inv[None, :]).astype(np.float32)
    cos = np.cos(ang).astype(np.float32).T
    sin = np.sin(ang).astype(np.float32).T
    c["ropecos"] = np.ascontiguousarray(np.concatenate([cos, cos], 0))
    c["ropesin"] = np.ascontiguousarray(np.concatenate([-sin, sin], 0))
    return c


def build_l1(cx, io):
    nc = cx.nc
    S = cx.S
    cx.phase_begin("L1")

    def din(name, shape, dt=F32):
        return io[name]

    xin = din("xin", [NT * 128, D])
    cT = din("cT", [128, 8])
    cctxT = din("cctxT", [128, 8])
    wmod = din("wmod", [D, 2048])
    bmod = din("bmod", [1, 2048])
    g1 = din("g1", [1, D])
    Wfm = din("Wfm", [D, 1536])
    Wtm = din("Wtm", [D, 1032])
    gbias = din("gbias", [1, 8])
    rld = din("rld", [1, 4])
    mlg = din("mlg", [1, 256])
    retg = din("retg", [1, 256])
    ident_bf_d = din("ident_bf", [128, 128], BF16)
    ident_f_d = din("ident_f", [128, 128])
    mask_f32_d = din("mask_f32", [128, 2, 128])
    mask_bf_d = din("mask_bf", [128, 2, 128], BF16)
    ones_d = din("ones_f", [128, 128])
    poscol_d = din("poscol", [128, 4])
    ropecos_d = din("ropecos", [128, NT * 128])
    ropesin_d = din("ropesin", [128, NT * 128])
    merged = io["merged"]

    FM = nc.dram_tensor("FM", [NT, 128, 1024], BF16).ap()
    TM = nc.dram_tensor("TM", [NT, 128, 1024], BF16).ap()
    OG = nc.dram_tensor("OG", [NT, 128, 512], BF16).ap()
    HS = nc.dram_tensor("HS", [2, NT, 128, 512], F32).ap()

    sb = cx.sb
    phase_begin = cx.phase_begin
    phase_end = cx.phase_end

    ident_bf = sb("ident_bf_s", [128, 128], BF16); b_identbf = Buf()
    ident_f = sb("ident_f_s", [128, 128]); b_identf = Buf()
    mask_f32 = sb("mask_f32_s", [128, 2, 128]); b_maskf = Buf()
    mask_bf = sb("mask_bf_s", [128, 2, 128], BF16); b_maskbf = Buf()
    ones_f = sb("ones_s", [128, 128]); b_ones = Buf()
    poscol = sb("poscol_s", [128, 4]); b_poscol = Buf()
    S.dma("sp", ident_bf[:], ident_bf_d, writes=[b_identbf])
    S.dma("sp", ident_f[:], ident_f_d, writes=[b_identf])
    S.dma("sp", mask_f32[:], mask_f32_d, writes=[b_maskf])
    S.dma("sp", mask_bf[:], mask_bf_d, writes=[b_maskbf])
    S.dma("sp", ones_f[:], ones_d, writes=[b_ones])
    S.dma("sp", poscol[:], poscol_d, writes=[b_poscol])

    Gall = sb("Gall", [128, 8, NT]); b_G = Buf()
    phase_begin("L1_A")
    Wfm_s = sb("Wfm_s", [128, 8, 1536], BF16); b_wfm = Buf()
    Wtm_s = sb("Wtm_s", [128, 8, 1032], BF16); b_wtm = Buf()
    for j in range(8):
        S.dma("pool", Wfm_s[:, j, :], Wfm[j * 128:(j + 1) * 128, :], writes=[b_wfm])
        S.dma("pool", Wtm_s[:, j, :], Wtm[j * 128:(j + 1) * 128, :], writes=[b_wtm])

    PB = cx.PB
    PBb = cx.PBb

    cT_s = sb("cT_s", [128, 2, 8]); b_cT = Buf()
    S.dma("sp", cT_s[:, 0, :], cT, writes=[b_cT])
    S.dma("sp", cT_s[:, 1, :], cctxT, writes=[b_cT])
    sc = sb("sc", [128, 2, 8]); b_sc = Buf()
    S.op("act", lambda e: e.activation(out=sc[:], in_=cT_s[:], func=AF.Silu), reads=[b_cT], writes=[b_sc])
    scb = sb("scb", [128, 2, 8, 128]); b_scb = Buf()
    for w in range(2):
        for j in range(8):
            S.op("dve", lambda e, w=w, j=j: e.tensor_scalar(out=scb[:, w, j, :], in0=ones_f[:], scalar1=sc[:, w, j:j + 1],
                                                           scalar2=None, op0=ALU.mult),
                 reads=[b_ones, b_sc], writes=[b_scb])
    bmod_s = sb("bmod_s", [128, 2048]); b_bmod = Buf()
    S.dma("sp", bmod_s[:], bmod.partition_broadcast(128), writes=[b_bmod])
    g1_s = sb("g1_s", [128, D]); b_g1 = Buf()
    S.dma("sp", g1_s[:], g1.partition_broadcast(128), writes=[b_g1])
    modS = sb("modS", [128, 2, D]); modA = sb("modA", [128, 2, D]); b_mod = Buf()
    wm_s = [sb("wm_s%d" % i, [128, 8, 512]) for i in range(2)]
    b_wm = [Buf(), Buf()]
    wmod_v = wmod.rearrange("(j p) n -> p j n", p=128)
    for cc in range(4):
        wb = cc % 2
        S.dma("sp", wm_s[wb][:], wmod_v[:, :, cc * 512:(cc + 1) * 512], writes=[b_wm[wb]])
        for w in range(2):
            pbi = (cc * 2 + w) % 7
            for j in range(8):
                S.op("pe", lambda e, w=w, j=j, wb=wb, pbi=pbi: e.matmul(PB[pbi][:], lhsT=scb[:, w, j, :], rhs=wm_s[wb][:, j, :],
                                                                        start=(j == 0), stop=(j == 7)),
                     reads=[b_scb, b_wm[wb]], writes=[PBb[pbi]])
            half = cc % 2
            if cc < 2:
                S.op("dve", lambda e, w=w, pbi=pbi, cc=cc, half=half: e.tensor_tensor(
                    out=modS[:, w, half * 512:(half + 1) * 512], in0=PB[pbi][:], in1=bmod_s[:, cc * 512:(cc + 1) * 512], op=ALU.add),
                    reads=[PBb[pbi], b_bmod], writes=[b_mod])
            else:
                S.op("dve", lambda e, w=w, pbi=pbi, cc=cc, half=half: e.tensor_tensor(
                    out=modA[:, w, half * 512:(half + 1) * 512], in0=PB[pbi][:], in1=bmod_s[:, cc * 512:(cc + 1) * 512], op=ALU.add),
                    reads=[PBb[pbi], b_bmod], writes=[b_mod])
                S.op("dve", lambda e, w=w, half=half: e.scalar_tensor_tensor(
                    out=modA[:, w, half * 512:(half + 1) * 512], in0=modA[:, w, half * 512:(half + 1) * 512], scalar=1.0,
                    in1=g1_s[:, half * 512:(half + 1) * 512], op0=ALU.add, op1=ALU.mult),
                    reads=[b_mod, b_g1], writes=[b_mod])

    xt = [sb("xt%d" % i, [128, D]) for i in range(2)]; b_xt = [Buf(), Buf()]
    junk = sb("junk", [128, D], BF16); b_junk = Buf()
    ss = sb("ss", [128, 2]); b_ss = [Buf(), Buf()]
    rstd = sb("rstd", [128, 2]); b_rstd = [Buf(), Buf()]
    tmp32 = sb("tmp32", [128, D]); b_tmp32 = Buf()
    u_bf = [sb("u_bf%d" % i, [128, D], BF16) for i in range(2)]; b_u = [Buf(), Buf()]
    uT = [sb("uT%d" % i, [128, 8, 384], BF16) for i in range(2)]
    b_uT = [[Buf() for _ in range(3)] for _ in range(2)]
    FMst = [sb("FMst%d" % i, [128, 3, 8, 128], BF16) for i in range(2)]
    b_FMst = [[Buf() for _ in range(8)] for _ in range(2)]
    TMst = [sb("TMst%d" % i, [128, 3, 1024], BF16) for i in range(2)]
    b_TMst = [[Buf() for _ in range(3)] for _ in range(2)]
    OGst = [sb("OGst%d" % i, [128, 3, 512], BF16) for i in range(2)]
    b_OGst = [[Buf() for _ in range(3)] for _ in range(2)]
    rc = [sb("rc%d" % i, [128, 384]) for i in range(2)]; rsn = [sb("rsn%d" % i, [128, 384]) for i in range(2)]
    b_rope = [Buf(), Buf()]
    t1 = sb("t1", [128, 384]); t2 = sb("t2", [128, 384]); b_t1 = Buf(); b_t2 = Buf()
    PT = cx.PT
    b_PT = cx.b_PT
    b_FM_d = [Buf() for _ in range(NT)]
    b_TM_d = [Buf() for _ in range(NT)]
    b_OG_d = [Buf() for _ in range(NT)]
    tmi = 0
    fmi = 0
    def pre_group(g):
        p = g % 2
        S.dma("sp", rc[p][:], ropecos_d[:, g * 384:(g + 1) * 384], writes=[b_rope[p]])
        S.dma("sp", rsn[p][:], ropesin_d[:, g * 384:(g + 1) * 384], writes=[b_rope[p]])
        for ti in range(3):
            c = g * 3 + ti
            w = 1 if c < 2 else 0
            xp = c % 2
            S.dma("sp", xt[xp][:], xin[c * 128:(c + 1) * 128, :], writes=[b_xt[xp]])
            S.op("dve", lambda e, xp=xp: e.memset(ss[:, xp:xp + 1], 0.0), writes=[b_ss[xp]])
            S.op("act", lambda e, xp=xp: e.activation(out=junk[:], in_=xt[xp][:], func=AF.Square, accum_out=ss[:, xp:xp + 1]),
                 reads=[b_xt[xp]], writes=[b_junk, b_ss[xp]])
            S.op("dve", lambda e, xp=xp: e.tensor_scalar(out=rstd[:, xp:xp + 1], in0=ss[:, xp:xp + 1], scalar1=1.0 / D, scalar2=EPS,
                                                        op0=ALU.mult, op1=ALU.add), reads=[b_ss[xp]], writes=[b_rstd[xp]])
            S.op("act", lambda e, xp=xp: e.activation(out=rstd[:, xp:xp + 1], in_=rstd[:, xp:xp + 1], func=AF.Sqrt),
                 reads=[b_rstd[xp]], writes=[b_rstd[xp]])
            S.op("dve", lambda e, xp=xp: e.reciprocal(out=rstd[:, xp:xp + 1], in_=rstd[:, xp:xp + 1]), reads=[b_rstd[xp]], writes=[b_rstd[xp]])
            S.op("dve", lambda e, xp=xp, w=w: e.scalar_tensor_tensor(out=tmp32[:], in0=xt[xp][:], scalar=rstd[:, xp:xp + 1],
                                                                    in1=modA[:, w, :], op0=ALU.mult, op1=ALU.mult),
                 reads=[b_xt[xp], b_rstd[xp], b_mod], writes=[b_tmp32])
            S.op("dve", lambda e, xp=xp, w=w: e.tensor_tensor(out=u_bf[xp][:], in0=tmp32[:], in1=modS[:, w, :], op=ALU.add),
                 reads=[b_tmp32, b_mod], writes=[b_u[xp]])
            for j in range(8):
                S.op("pe", lambda e, xp=xp, j=j: e.transpose(PT[:, j * 128:(j + 1) * 128], u_bf[xp][:, j * 128:(j + 1) * 128], ident_bf[:]),
                     reads=[b_u[xp], b_identbf], writes=[b_PT])
            S.op("act", lambda e, p=p, ti=ti: e.activation(out=uT[p][:, :, ti * 128:(ti + 1) * 128],
                                                          in_=PT[:].rearrange("p (j t) -> p j t", j=8), func=AF.Copy),
                 reads=[b_PT], writes=[b_uT[p][ti]])

    def main_group(g):
        nonlocal tmi, fmi
        p = g % 2
        for ti in range(3):
            c = g * 3 + ti
            for (c0, c1) in [(0, 512), (512, 1024), (1024, 1032)]:
                pbi = tmi % 3; tmi += 1
                for j in range(8):
                    S.op("pe", lambda e, p=p, ti=ti, j=j, c0=c0, c1=c1, pbi=pbi: e.matmul(
                        PB[pbi][:, 0:c1 - c0], lhsT=uT[p][:, j, ti * 128:(ti + 1) * 128], rhs=Wtm_s[:, j, c0:c1],
                        start=(j == 0), stop=(j == 7)), reads=[b_uT[p][ti], b_wtm], writes=[PBb[pbi]])
                if c0 == 0:
                    S.op("act", lambda e, p=p, ti=ti, pbi=pbi: e.activation(out=TMst[p][:, ti, 512:1024], in_=PB[pbi][:], func=AF.Copy),
                         reads=[PBb[pbi]], writes=[b_TMst[p][ti]])
                elif c0 == 512:
                    S.op("act", lambda e, p=p, ti=ti, pbi=pbi: e.activation(out=OGst[p][:, ti, :], in_=PB[pbi][:], func=AF.Copy),
                         reads=[PBb[pbi]], writes=[b_OGst[p][ti]])
                else:
                    S.op("dve", lambda e, c=c, pbi=pbi: e.tensor_copy(out=Gall[:, :, c], in_=PB[pbi][:, 0:8]),
                         reads=[PBb[pbi]], writes=[b_G])
        def fm_mm(cb, pbi):
            for j in range(8):
                S.op("pe", lambda e, j=j: e.matmul(PB[pbi][:, 0:384], lhsT=Wfm_s[:, j, cb * 128:(cb + 1) * 128], rhs=uT[p][:, j, :],
                                                   start=(j == 0), stop=(j == 7)),
                     reads=[b_wfm] + b_uT[p], writes=[PBb[pbi]])
        for cb in range(4):
            pbi = 4 + fmi % 3; fmi += 1
            fm_mm(cb, pbi)
            sc_ = 1.0 if cb < 2 else RS
            S.op("act", lambda e, cb=cb, pbi=pbi, sc_=sc_: e.activation(
                out=FMst[p][:, :, cb, :], in_=PB[pbi][:, 0:384].rearrange("p (c t) -> p c t", c=3), func=AF.Copy, scale=sc_),
                reads=[PBb[pbi]], writes=[b_FMst[p][cb]])
        for qk in range(2):
            for h in range(2):
                cb_raw = 4 + qk * 4 + h
                cb_sw = 4 + qk * 4 + 2 + h
                pa = 4 + fmi % 3; fmi += 1
                fm_mm(cb_raw, pa)
                pb_ = 4 + fmi % 3; fmi += 1
                fm_mm(cb_sw, pb_)
                sc_ = 1.0 if qk == 0 else RS
                S.op("dve", lambda e, pa=pa, sc_=sc_: e.scalar_tensor_tensor(out=t1[:], in0=PB[pa][:, 0:384], scalar=sc_, in1=rc[p][:],
                                                                            op0=ALU.mult, op1=ALU.mult),
                     reads=[PBb[pa], b_rope[p]], writes=[b_t1])
                S.op("dve", lambda e, pb_=pb_, sc_=sc_: e.scalar_tensor_tensor(out=t2[:], in0=PB[pb_][:, 0:384], scalar=sc_, in1=rsn[p][:],
                                                                              op0=ALU.mult, op1=ALU.mult),
                     reads=[PBb[pb_], b_rope[p]], writes=[b_t2])
                a = 4 + qk * 2 + h
                S.op("dve", lambda e, a=a: e.tensor_tensor(out=FMst[p][:, :, a, :], in0=t1[:].rearrange("p (c t) -> p c t", c=3),
                                                           in1=t2[:].rearrange("p (c t) -> p c t", c=3), op=ALU.add),
                     reads=[b_t1, b_t2], writes=[b_FMst[p][a]])
        for ti in range(3):
            for di, a in enumerate([2, 3, 6, 7]):
                S.op("pe", lambda e, ti=ti, a=a, di=di: e.transpose(PTk[:, di * 128:(di + 1) * 128], FMst[p][:, ti, a, :], ident_bf[:]),
                     reads=[b_FMst[p][a], b_identbf], writes=[PBb[3]])
            S.op("act", lambda e, ti=ti: e.activation(out=TMst[p][:, ti, 0:512], in_=PTk[:, 0:512], func=AF.Copy),
                 reads=[PBb[3]], writes=[b_TMst[p][ti]])
        c0 = g * 3
        S.dma("sp", FM[c0:c0 + 3].rearrange("c p n -> p c n"), FMst[p][:].rearrange("p c a t -> p c (a t)"),
              reads=b_FMst[p], writes=b_FM_d[c0:c0 + 3])
        S.dma("sp", TM[c0:c0 + 3].rearrange("c p n -> p c n"), TMst[p][:], reads=b_TMst[p], writes=b_TM_d[c0:c0 + 3])
        S.dma("sp", OG[c0:c0 + 3].rearrange("c p n -> p c n"), OGst[p][:], reads=b_OGst[p], writes=b_OG_d[c0:c0 + 3])

    PTk = PB[3][:].bitcast(BF16)
    pre_group(0)
    for g in range(NG):
        if g + 1 < NG:
            pre_group(g + 1)
        main_group(g)
    phase_end()
    wml = sb("wml", [128, 4, NT]); b_wml = Buf()
    flo = sb("flo", [128, 4, NT]); b_flo = Buf()
    decb = sb("decb", [128, 4, NT]); b_decb = Buf()
    wret = sb("wret", [128, 4]); rho = sb("rho", [128, 4]); dret = sb("dret", [128, 4]); b_retc = Buf()
    phase_begin("L1_G")
    gb_s = sb("gb_s", [128, 8]); b_gb = Buf()
    S.dma("sp", gb_s[:], gbias.partition_broadcast(128), writes=[b_gb])
    Gi = sb("Gi", [128, 4, NT]); b_Gi = Buf()
    nlf = sb("nlf", [128, 4, NT]); b_nlf = Buf()
    for k in range(4):
        S.op("dve", lambda e, k=k: e.tensor_scalar(out=Gi[:, k, :], in0=Gall[:, k, :], scalar1=gb_s[:, k:k + 1], scalar2=None, op0=ALU.add),
             reads=[b_G, b_gb], writes=[b_Gi])
        S.op("dve", lambda e, k=k: e.tensor_scalar(out=nlf[:, k, :], in0=Gall[:, 4 + k, :], scalar1=gb_s[:, 4 + k:5 + k], scalar2=None, op0=ALU.add),
             reads=[b_G, b_gb], writes=[b_nlf])
    S.op("act", lambda e: e.activation(out=nlf[:], in_=nlf[:], func=AF.Exp, scale=-1.0), reads=[b_nlf], writes=[b_nlf])
    S.op("act", lambda e: e.activation(out=nlf[:], in_=nlf[:], func=AF.Ln, bias=1.0), reads=[b_nlf], writes=[b_nlf])
    nb = sb("nb", [128, 4, NT]); b_nb = Buf()
    nbL = sb("nbL", [128, 4, NT]); b_nbL = Buf()
    for d in range(2):
        S.op("pe", lambda e, d=d: e.matmul(PB[d][:, 0:2 * NT], lhsT=mask_f32[:, d, :], rhs=nlf[:, 2 * d:2 * d + 2, :].rearrange("p a c -> p (a c)"),
                                           start=True, stop=True), reads=[b_maskf, b_nlf], writes=[PBb[d]])
        S.op("dve", lambda e, d=d: e.tensor_copy(out=nb[:, 2 * d:2 * d + 2, :].rearrange("p a c -> p (a c)"), in_=PB[d][:, 0:2 * NT]),
             reads=[PBb[d]], writes=[b_nb])
    S.op("pe", lambda e: e.matmul(PB[2][:, 0:4 * NT], lhsT=ones_f[:], rhs=nlf[:].rearrange("p a c -> p (a c)"), start=True, stop=True),
         reads=[b_ones, b_nlf], writes=[PBb[2]])
    S.op("dve", lambda e: e.tensor_copy(out=nbL[:].rearrange("p a c -> p (a c)"), in_=PB[2][:, 0:4 * NT]), reads=[PBb[2]], writes=[b_nbL])
    av = sb("av", [128, 4, NT]); b_av = Buf()
    S.op("dve", lambda e: e.tensor_tensor(out=av[:], in0=Gi[:], in1=nb[:], op=ALU.add), reads=[b_Gi, b_nb], writes=[b_av])
    avf = av[:].rearrange("p a c -> p (a c)")
    Acol = sb("Acol", [128, 3]); b_Acol = Buf()
    S.op("dve", lambda e: e.memset(Acol[:], 0.0), writes=[b_Acol])
    pieces = [(0, 128), (128, 256), (256, 264)]
    for pi, (a0, a1) in enumerate(pieces):
        m = a1 - a0
        S.op("pe", lambda e, a0=a0, a1=a1, m=m, pi=pi: e.matmul(PB[3 + pi][0:m, 0:128], lhsT=avf[:, a0:a1], rhs=ident_f[:], start=True, stop=True),
             reads=[b_av, b_identf], writes=[PBb[3 + pi]])
        S.op("dve", lambda e, m=m, pi=pi: e.tensor_reduce(out=Acol[0:m, pi:pi + 1], in_=PB[3 + pi][0:m, 0:128], axis=AX.X, op=ALU.max),
             reads=[PBb[3 + pi]], writes=[b_Acol])
    Arow = sb("Arow", [1, 4, NT]); b_Arow = Buf()
    for pi, (a0, a1) in enumerate(pieces):
        m = a1 - a0
        S.op("pe", lambda e, a0=a0, a1=a1, m=m, pi=pi: e.matmul(PB[6][0:1, a0:a1], lhsT=Acol[0:128, pi:pi + 1], rhs=ident_f[:, 0:m],
                                                                start=True, stop=True), reads=[b_Acol, b_identf], writes=[PBb[6]])
    S.op("dve", lambda e: e.tensor_copy(out=Arow[:].rearrange("p a c -> p (a c)"), in_=PB[6][0:1, 0:4 * NT]), reads=[PBb[6]], writes=[b_Arow])
    MLrow = sb("MLrow", [1, 4, NT]); b_MLrow = Buf()
    dargrow = sb("dargrow", [1, 4, NT]); b_darg = Buf()
    mstate = sb("mstate", [1, 4]); b_ms = [Buf(), Buf()]
    b_MLd = [Buf(), Buf()]; b_dargd = [Buf(), Buf()]
    S.op("dve", lambda e: e.memset(mstate[:], 0.0), writes=b_ms)
    order = [list(range(NT)), [1, 0] + list(range(NT - 1, 1, -1))]
    engs = ["dve", "dve"]
    for i in range(NT):
        for d in range(2):
            c = order[d][i]
            en = engs[d]
            sl = slice(2 * d, 2 * d + 2)
            S.op(en, lambda e, c=c, sl=sl: e.tensor_tensor(out=MLrow[:, sl, c], in0=mstate[:, sl], in1=Arow[:, sl, c], op=ALU.max),
                 reads=[b_ms[d], b_Arow], writes=[b_MLd[d]])
            S.op(en, lambda e, c=c, sl=sl: e.tensor_tensor(out=dargrow[:, sl, c], in0=mstate[:, sl], in1=MLrow[:, sl, c], op=ALU.subtract),
                 reads=[b_ms[d], b_MLd[d]], writes=[b_dargd[d]])
            S.op(en, lambda e, c=c, sl=sl: e.tensor_tensor(out=mstate[:, sl], in0=MLrow[:, sl, c], in1=nbL[0:1, sl, c], op=ALU.subtract),
                 reads=[b_nbL, b_MLd[d]], writes=[b_ms[d]])
    MLb = sb("MLb", [128, 4, NT]); b_MLb = Buf()
    S.op("pe", lambda e: e.matmul(PB[0][:, 0:4 * NT], lhsT=ones_f[0:1, :], rhs=MLrow[:].rearrange("p a c -> p (a c)"), start=True, stop=True),
         reads=[b_ones] + b_MLd + b_dargd, writes=[PBb[0]])
    S.op("dve", lambda e: e.tensor_copy(out=MLb[:].rearrange("p a c -> p (a c)"), in_=PB[0][:, 0:4 * NT]), reads=[PBb[0]], writes=[b_MLb])
    S.op("pe", lambda e: e.matmul(PB[1][:, 0:4 * NT], lhsT=ones_f[0:1, :], rhs=dargrow[:].rearrange("p a c -> p (a c)"), start=True, stop=True),
         reads=[b_ones] + b_MLd + b_dargd, writes=[PBb[1]])
    S.op("act", lambda e: e.activation(out=decb[:].rearrange("p a c -> p (a c)"), in_=PB[1][:, 0:4 * NT], func=AF.Exp), reads=[PBb[1]], writes=[b_decb])
    S.op("dve", lambda e: e.tensor_tensor(out=wml[:], in0=av[:], in1=MLb[:], op=ALU.subtract), reads=[b_av, b_MLb], writes=[b_wml])
    S.op("act", lambda e: e.activation(out=wml[:], in_=wml[:], func=AF.Exp), reads=[b_wml], writes=[b_wml])
    S.op("dve", lambda e: e.tensor_tensor(out=flo[:], in0=nb[:], in1=MLb[:], op=ALU.subtract), reads=[b_nb, b_MLb], writes=[b_flo])
    S.op("act", lambda e: e.activation(out=flo[:], in_=flo[:], func=AF.Exp), reads=[b_flo], writes=[b_flo])
    lg = sb("lg", [128, 4]); b_lg = Buf()
    S.dma("sp", lg[:], rld.partition_broadcast(128), writes=[b_lg])
    for d in range(2):
        for h in range(2):
            x = 2 * d + h
            S.op("act", lambda e, x=x, d=d: e.activation(out=wret[:, x:x + 1], in_=poscol[:, d:d + 1], func=AF.Exp, scale=lg[:, x:x + 1]),
                 reads=[b_poscol, b_lg], writes=[b_retc])
            S.op("act", lambda e, x=x, d=d: e.activation(out=rho[:, x:x + 1], in_=poscol[:, 2 + d:3 + d], func=AF.Exp, scale=lg[:, x:x + 1]),
                 reads=[b_poscol, b_lg], writes=[b_retc])
    S.op("act", lambda e: e.activation(out=dret[:], in_=lg[:], func=AF.Exp, scale=128.0), reads=[b_lg], writes=[b_retc])

    phase_end()
    phase_begin("L1_S")
    if "after_weights" in io:
        io["after_weights"]()
    FMl = [[sb("FMl%d%d" % (d, i), [128, 8, 128], BF16) for i in range(2)] for d in range(2)]
    TMl = [[sb("TMl%d%d" % (d, i), [128, 8, 128], BF16) for i in range(2)] for d in range(2)]
    b_FMl = [[Buf() for _ in range(2)] for _ in range(2)]
    b_TMl = [[Buf() for _ in range(2)] for _ in range(2)]
    va = [sb("va%d" % i, [128, 132], BF16) for i in range(4)]; b_va = [Buf() for _ in range(4)]
    sm = [sb("sm%d" % i, [128, 128], BF16) for i in range(4)]; b_sm = [Buf() for _ in range(4)]
    CT32 = sb("CT32", [128, 8, 132]); CTd32 = sb("CTd32", [128, 8, 132]); CTbf = sb("CTbf", [128, 8, 132], BF16)
    b_CT32 = [Buf() for _ in range(8)]; b_CTd = [Buf() for _ in range(8)]; b_CTbf = [Buf() for _ in range(8)]
    S.op("dve", lambda e: e.memset(CTd32[:], 0.0), writes=b_CTd)
    S.op("dve", lambda e: e.memset(CTbf[:], 0.0), writes=b_CTbf)
    HSst = [[sb("HSst%d%d" % (d, i), [128, 512]) for i in range(2)] for d in range(2)]
    b_HSst = [[Buf() for _ in range(2)] for _ in range(2)]
    den = sb("den", [128, 4]); b_den = [Buf() for _ in range(4)]
    b_HS_d = [[Buf() for _ in range(NT)] for _ in range(2)]
    units = []
    groups = []
    for i in range(NT):
        for d in range(2):
            groups.append((i, d))
            for typ in range(2):
                for h in range(2):
                    units.append(dict(i=i, d=d, typ=typ, h=h, first=(typ == 0 and h == 0), last=(typ == 1 and h == 1), g=len(groups) - 1))

    def load_group(gi):
        i, d = groups[gi]
        c = order[d][i]; lp = i % 2
        S.dma("sp", FMl[d][lp][:].rearrange("p a t -> p (a t)"), FM[c], reads=[b_FM_d[c]], writes=[b_FMl[d][lp]])
        S.dma("sp", TMl[d][lp][:].rearrange("p a t -> p (a t)"), TM[c], reads=[b_TM_d[c]], writes=[b_TMl[d][lp]])

    def uinfo(u):
        U = units[u]
        i, d, typ, h = U["i"], U["d"], U["typ"], U["h"]
        c = order[d][i]
        cn = order[d][i + 1] if i + 1 < NT else c
        lp = i % 2
        x = 2 * d + h
        r = dict(U)
        r.update(c=c, cn=cn, lp=lp, hp=i % 2, ch=typ * 4 + h * 2 + d, x=x, vi=u % 4,
                 qT=FMl[d][lp][:, typ * 4 + h, :], kT=FMl[d][lp][:, typ * 4 + 2 + h, :],
                 kk=TMl[d][lp][:, typ * 2 + h, :], vv=TMl[d][lp][:, 4 + typ * 2 + h, :],
                 pS=PB[u % 2], bS=PBb[u % 2], pO=PB[2 + u % 2], bO=PBb[2 + u % 2], pC=PB[4 + u % 2], bC=PBb[4 + u % 2])
        if typ == 0:
            r.update(wcol=wml[:, x, c:c + 1], wb_=b_wml, dn=decb[:, x, cn:cn + 1], db_=b_decb)
        else:
            r.update(wcol=wret[:, x:x + 1], wb_=b_retc, dn=dret[:, x:x + 1], db_=b_retc)
        return r

    def st1(u):
        U = uinfo(u)
        d, lp, vi = U["d"], U["lp"], U["vi"]
        if U["first"] and U["g"] + 1 < len(groups):
            load_group(U["g"] + 1)
        S.op("dve", lambda e: e.tensor_scalar(out=va[vi][:, 0:128], in0=U["vv"], scalar1=U["wcol"], scalar2=None, op0=ALU.mult),
             reads=[b_TMl[d][lp], U["wb_"]], writes=[b_va[vi]])
        S.op("dve", lambda e: e.tensor_copy(out=va[vi][:, 128:129], in_=U["wcol"]), reads=[U["wb_"]], writes=[b_va[vi]])
        S.op("pe", lambda e: e.matmul(U["pS"][:, 0:128], lhsT=U["kT"], rhs=U["qT"], start=True, stop=True),
             reads=[b_FMl[d][lp]], writes=[U["bS"]])

    def st2(u):
        U = uinfo(u)
        d, lp, vi, ch = U["d"], U["lp"], U["vi"], U["ch"]
        S.op("dve", lambda e: e.tensor_tensor(out=sm[vi][:], in0=U["pS"][:, 0:128], in1=mask_bf[:, d, :], op=ALU.mult),
             reads=[U["bS"], b_maskbf], writes=[b_sm[vi]])
        S.op("pe", lambda e: e.matmul(U["pO"][:, 0:129], lhsT=sm[vi][:], rhs=va[vi][:, 0:129], start=True, stop=False),
             reads=[b_sm[vi], b_va[vi]], writes=[U["bO"]], inc=False)
        S.op("pe", lambda e: e.matmul(U["pO"][:, 0:129], lhsT=U["qT"], rhs=CTbf[:, ch, 0:129], start=False, stop=True),
             reads=[b_FMl[d][lp], b_CTbf[ch]], writes=[U["bO"]])
        S.op("pe", lambda e: e.matmul(U["pC"][:, 0:129], lhsT=U["kk"], rhs=va[vi][:, 0:129], start=True, stop=True),
             reads=[b_TMl[d][lp], b_va[vi]], writes=[U["bC"]])

    def st3(u):
        U = uinfo(u)
        d, vi, ch, x, c, hp, typ, h = U["d"], U["vi"], U["ch"], U["x"], U["c"], U["hp"], U["typ"], U["h"]
        pO, bO, pC, bC, dn, db_ = U["pO"], U["bO"], U["pC"], U["bC"], U["dn"], U["db_"]
        S.op("dve", lambda e: e.tensor_tensor(out=CT32[:, ch, 0:129], in0=pC[:, 0:129], in1=CTd32[:, ch, 0:129], op=ALU.add),
             reads=[bC, b_CTd[ch]], writes=[b_CT32[ch]])
        S.op("act", lambda e: e.activation(out=CTd32[:, ch, 0:129], in_=CT32[:, ch, 0:129], func=AF.Copy, scale=dn),
             reads=[b_CT32[ch], db_], writes=[b_CTd[ch]])
        S.op("act", lambda e: e.activation(out=CTbf[:, ch, 0:129], in_=CT32[:, ch, 0:129], func=AF.Copy, scale=dn),
             reads=[b_CT32[ch], db_], writes=[b_CTbf[ch]])
        oc = (typ * 2 + h) * 128
        if typ == 0:
            S.op("act", lambda e: e.activation(out=den[:, vi:vi + 1], in_=pO[:, 128:129], func=AF.Abs), reads=[bO], writes=[b_den[vi]])
            S.op("dve", lambda e: e.tensor_scalar(out=den[:, vi:vi + 1], in0=den[:, vi:vi + 1], scalar1=flo[:, x, c:c + 1], scalar2=None, op0=ALU.max),
                 reads=[b_den[vi], b_flo], writes=[b_den[vi]])
            S.op("dve", lambda e: e.reciprocal(out=den[:, vi:vi + 1], in_=den[:, vi:vi + 1]), reads=[b_den[vi]], writes=[b_den[vi]])
            S.op("act", lambda e: e.activation(out=HSst[d][hp][:, oc:oc + 128], in_=pO[:, 0:128], func=AF.Copy, scale=den[:, vi:vi + 1]),
                 reads=[bO, b_den[vi]], writes=[b_HSst[d][hp]])
        else:
            S.op("act", lambda e: e.activation(out=HSst[d][hp][:, oc:oc + 128], in_=pO[:, 0:128], func=AF.Copy, scale=rho[:, x:x + 1]),
                 reads=[bO, b_retc], writes=[b_HSst[d][hp]])
        if U["last"]:
            S.dma("sp", HS[d, c], HSst[d][hp][:], reads=[b_HSst[d][hp]], writes=[b_HS_d[d][c]])

    load_group(0)
    nu = len(units)
    for k in range(nu + 2):
        if k < nu:
            st1(k)
        if 0 <= k - 1 < nu:
            st2(k - 1)
        if 0 <= k - 2 < nu:
            st3(k - 2)

    phase_end()
    phase_begin("L1_M")
    gml = sb("gml", [128, 256]); gret = sb("gret", [128, 256]); b_gm = Buf()
    S.dma("sp", gml[:], mlg.partition_broadcast(128), writes=[b_gm])
    S.dma("sp", gret[:], retg.partition_broadcast(128), writes=[b_gm])
    h0 = [sb("h0_%d" % i, [128, 512]) for i in range(2)]; h1 = [sb("h1_%d" % i, [128, 512]) for i in range(2)]
    og = [sb("og%d" % i, [128, 512], BF16) for i in range(2)]
    b_h0 = [Buf(), Buf()]; b_h1 = [Buf(), Buf()]; b_og = [Buf(), Buf()]
    hz = sb("hz", [128, 512]); b_hz = Buf()
    sg = sb("sg", [128, 512]); b_sg = Buf()
    st6 = sb("st6", [128, 4, 6]); mv2 = sb("mv2", [128, 4, 2]); b_st = Buf(); b_mv = Buf()
    rs4 = sb("rs4", [128, 4]); b_rs4 = Buf()
    mo_st = [sb("mo_st%d" % i, [128, 512], BF16) for i in range(2)]; b_most = [Buf(), Buf()]
    b_out = Buf()
    for c in range(NT):
        p = c % 2
        S.dma("sp", h0[p][:], HS[0, c], reads=[b_HS_d[0][c]], writes=[b_h0[p]])
        S.dma("sp", h1[p][:], HS[1, c], reads=[b_HS_d[1][c]], writes=[b_h1[p]])
        S.dma("sp", og[p][:], OG[c], reads=[b_OG_d[c]], writes=[b_og[p]])
        S.op("dve", lambda e, p=p: e.tensor_tensor(out=hz[:], in0=h0[p][:], in1=h1[p][:], op=ALU.add), reads=[b_h0[p], b_h1[p]], writes=[b_hz])
        S.op("act", lambda e, p=p: e.activation(out=sg[:, 0:256], in_=og[p][:, 0:256], func=AF.Sigmoid), reads=[b_og[p]], writes=[b_sg])
        S.op("act", lambda e, p=p: e.activation(out=sg[:, 256:512], in_=og[p][:, 256:512], func=AF.Silu), reads=[b_og[p]], writes=[b_sg])
        S.op("dve", lambda e: e.tensor_tensor(out=hz[:, 0:256], in0=hz[:, 0:256], in1=sg[:, 0:256], op=ALU.mult), reads=[b_hz, b_sg], writes=[b_hz])
        for k in range(4):
            S.op("dve", lambda e, k=k: e.bn_stats(out=st6[:, k, :], in_=hz[:, k * 128:(k + 1) * 128]), reads=[b_hz], writes=[b_st])
            S.op("dve", lambda e, k=k: e.bn_aggr(out=mv2[:, k, :], in_=st6[:, k, :]), reads=[b_st], writes=[b_mv])
        S.op("dve", lambda e: e.tensor_scalar(out=rs4[:], in0=mv2[:, :, 1], scalar1=EPS, scalar2=None, op0=ALU.add),
             reads=[b_mv], writes=[b_rs4])
        S.op("act", lambda e: e.activation(out=rs4[:], in_=rs4[:], func=AF.Sqrt), reads=[b_rs4], writes=[b_rs4])
        S.op("dve", lambda e: e.reciprocal(out=rs4[:], in_=rs4[:]), reads=[b_rs4], writes=[b_rs4])
        for k in range(4):
            S.op("dve", lambda e, k=k: e.tensor_scalar(out=hz[:, k * 128:(k + 1) * 128], in0=hz[:, k * 128:(k + 1) * 128],
                                                      scalar1=mv2[:, k, 0:1], scalar2=rs4[:, k:k + 1], op0=ALU.subtract, op1=ALU.mult),
                 reads=[b_hz, b_mv, b_rs4], writes=[b_hz])
        S.op("dve", lambda e, p=p: e.tensor_tensor(out=mo_st[p][:, 0:256], in0=hz[:, 0:256], in1=gml[:], op=ALU.mult),
             reads=[b_hz, b_gm], writes=[b_most[p]])
        S.op("dve", lambda e: e.tensor_tensor(out=hz[:, 256:512], in0=hz[:, 256:512], in1=gret[:], op=ALU.mult), reads=[b_hz, b_gm], writes=[b_hz])
        S.op("dve", lambda e, p=p: e.tensor_tensor(out=mo_st[p][:, 256:512], in0=hz[:, 256:512], in1=sg[:, 256:512], op=ALU.mult),
             reads=[b_hz, b_sg], writes=[b_most[p]])
        S.dma("sp", merged[c * 128:(c + 1) * 128, :], mo_st[p][:], reads=[b_most[p]], writes=[b_out])
    phase_end()
    phase_end()
    return b_out


def l1_inputs(I, b, g, consts):
    d = dict(consts)
    d["xin"] = np.ascontiguousarray(np.concatenate([I["ctx"][b], I["x"][b]], 0))
    d["cT"] = np.ascontiguousarray(I["c"][b].reshape(8, 128).T)
    d["cctxT"] = np.ascontiguousarray(I["c_ctx"].reshape(8, 128).T)
    d["wmod"] = np.ascontiguousarray(I["w_mod"][0][:, 0:2048])
    d["bmod"] = np.ascontiguousarray(I["b_mod"][0][None, 0:2048])
    d["g1"] = np.ascontiguousarray(I["norm1_g"][0][None, :])
    W = I["w_in_even"][0]
    sp = np.cumsum([0, 512, 512, 512, 512, 8, 8, 512, 512, 512, 512])
    mq, mk, mv, mo, mi, mf, rq, rk, rv, rg = [W[:, sp[i]:sp[i + 1]] for i in range(10)]
    hs = [2 * g, 2 * g + 1]
    def hcols(M, h): return M[:, h * 128:(h + 1) * 128]
    def swp(M): return np.concatenate([M[:, 64:128], M[:, 0:64]], 1)
    fm = [hcols(mq, h) for h in hs] + [hcols(mk, h) for h in hs] + [hcols(rq, h) for h in hs] + [swp(hcols(rq, h)) for h in hs] \
        + [hcols(rk, h) for h in hs] + [swp(hcols(rk, h)) for h in hs]
    d["Wfm"] = np.ascontiguousarray(np.concatenate(fm, 1))
    gi = [mi[:, dd * 4 + h:dd * 4 + h + 1] for dd in range(2) for h in hs]
    gf = [mf[:, dd * 4 + h:dd * 4 + h + 1] for dd in range(2) for h in hs]
    tm = [hcols(mv, h) for h in hs] + [hcols(rv, h) for h in hs] + [hcols(mo, h) for h in hs] + [hcols(rg, h) for h in hs] + gi + gf
    d["Wtm"] = np.ascontiguousarray(np.concatenate(tm, 1))
    gb = I["ml_gate_b"][0]
    d["gbias"] = np.array([[gb[dd, 0, h] for dd in range(2) for h in hs] + [gb[dd, 1, h] for dd in range(2) for h in hs]], np.float32)
    d["rld"] = np.array([[I["ret_log_decay"][0][dd, h] for dd in range(2) for h in hs]], np.float32)
    d["mlg"] = np.ascontiguousarray(I["ml_norm_g"][0][hs].reshape(1, 256))
    d["retg"] = np.ascontiguousarray(I["ret_norm_g"][0][hs].reshape(1, 256))
    return d


NT2 = 33
GROUPS2 = [[0]] + [[1 + 4 * g + i for i in range(4)] for g in range(8)]


def host_consts_l2(half):
    c = common_consts()
    T = NT2 * 128
    inv = (10000.0 ** (-np.arange(16, dtype=np.float32) / 16)).astype(np.float32)
    t = np.arange(half * 4096, (half + 1) * 4096)
    rows = (t // 64).astype(np.float32); cols = (t % 64).astype(np.float32)
    ar = (rows[:, None] * inv[None, :]).astype(np.float32)
    ac = (cols[:, None] * inv[None, :]).astype(np.float32)
    cos64 = np.concatenate([np.cos(ar), np.cos(ar), np.cos(ac), np.cos(ac)], 1).astype(np.float32)
    sin64 = np.concatenate([-np.sin(ar), np.sin(ar), -np.sin(ac), np.sin(ac)], 1).astype(np.float32)
    cosT = np.ones((128, T), np.float32); sinT = np.zeros((128, T), np.float32)
    cosT[:, 128:] = np.concatenate([cos64, cos64], 1).T
    sinT[:, 128:] = np.concatenate([sin64, sin64], 1).T
    c["rc2"] = np.ascontiguousarray(cosT); c["rs2"] = np.ascontiguousarray(sinT)
    return c


def build_l2(cx, io, debug=None):
    nc = cx.nc
    S = cx.S
    cx.phase_begin("L2")
    xin = io["xin"]
    MG = io["MG"]; b_MG = io["b_MG"]
    idxm_d = io["idxm"]
    cT = io["cT"]; cctxT = io["cctxT"]
    wmod = io["wmod"]; bmod = io["bmod"]
    g2_0 = io["g2_0"]; g1_1 = io["g1_1"]
    w_out = io["w_out"]
    Wr = io["Wr"]; br = io["br"]
    w1 = io["w1"]; w3 = io["w3"]; w2 = io["w2"]
    Wq = io["Wq"]; Wk = io["Wk"]; Wv = io["Wv"]
    rc2_d = io["rc2"]; rs2_d = io["rs2"]
    hout = io["H2"]
    QT = io["QTs"]
    KT = io["KTo"]
    Vo = io["Vown"]
    H1 = cx.dscr("H1", [NT2, 128, D]); b_H1 = [Buf() for _ in range(NT2)]
    Vs = cx.dscr("Vs", [NT2, 128, D], BF16); b_Vs = [Buf() for _ in range(NT2)]
    UT = cx.dscr("UT", [NT2, 128, 8, 128], BF16); b_UT = [Buf() for _ in range(NT2)]

    def lc(name, shape, dt=F32):
        t = cx.sb(name + "_2s", shape, dt); b = Buf()
        S.dma("sp", t[:], io[name], writes=[b])
        return t, b
    idxm, b_idxm = lc("idxm", [128, NT2, 2], I32)
    ident_bf = lc("ident_bf", [128, 128], BF16)
    ident_f = lc("ident_f", [128, 128])
    ones_f = lc("ones_f", [128, 128])
    slt = lc("slt", [128, 128])
    blkstart = lc("blkstart", [128, 1])
    pcol = lc("pcol", [128, 1])
    thr = lc("thr", [128, 34])
    consts = {"thr": thr, "ident_bf": ident_bf, "ident_f": ident_f, "ones_f": ones_f, "slt": slt, "blkstart": blkstart, "pcol": pcol}
    moe = io["moe"]
    moe.c = consts
    moe.alloc_persistent()

    mods = cx.sb("mods", [128, 2, 6, D]); b_mods = Buf()
    cx.phase_begin("L2_mod")
    scb, b_scb = emit_silu_bcast(cx, [cT, cctxT], ones_f[0], ones_f[1])
    bmod_s, b_bmod = cx.load_bcast("bmod2", 6144, bmod)
    g2_s, b_g2 = cx.load_bcast("g2_0", D, g2_0)
    g1n_s, b_g1n = cx.load_bcast("g1_1", D, g1_1)
    wm_s = [cx.sb("wm_s%d" % i, [128, 8, 512]) for i in range(2)]; b_wm = [Buf(), Buf()]
    emit_mod(cx, scb, b_scb, 2, wmod, bmod_s, b_bmod, 6144,
             lambda w, cc: mods[:, w, cc // 2, (cc % 2) * 512:(cc % 2 + 1) * 512], b_mods, wm_s, b_wm)
    for w in range(2):
        for slot, gt, bg in [(2, g2_s, b_g2), (5, g1n_s, b_g1n)]:
            S.op("dve", lambda e, w=w, slot=slot, gt=gt: e.scalar_tensor_tensor(out=mods[:, w, slot, :], in0=mods[:, w, slot, :], scalar=1.0,
                                                                               in1=gt[:], op0=ALU.add, op1=ALU.mult),
                 reads=[b_mods, bg], writes=[b_mods])
    cx.phase_end()

    cx.phase_begin("L2_B")
    wo_s = cx.sb("wo_s", [128, 8, D], BF16); b_wo = Buf()
    for j in range(8):
        S.dma("pool", wo_s[:, j, :], w_out[j * 128:(j + 1) * 128, :], writes=[b_wo])
    nb = NormBufs(cx)
    ht = [cx.sb("ht%d" % i, [128, D]) for i in range(2)]; b_ht = [Buf(), Buf()]
    mt = [cx.sb("mt%d" % i, [128, D], BF16) for i in range(2)]; b_mt = [Buf(), Buf()]
    mT = [cx.sb("mT%d" % i, [128, 8, 128], BF16) for i in range(2)]; b_mT = [Buf(), Buf()]
    vt = [cx.sb("vt%d" % i, [128, D], BF16) for i in range(2)]; b_vt = [Buf(), Buf()]
    vT = [cx.sb("vT%d" % i, [128, 8, 128], BF16) for i in range(2)]; b_vT = [Buf(), Buf()]
    ytmp = cx.sb("ytmp", [128, D]); b_ytmp = Buf()
    for ti in range(NT2):
        p = ti % 2
        w = 1 if ti == 0 else 0
        S.dma("sp", ht[p][:], xin[ti * 128:(ti + 1) * 128, :], writes=[b_ht[p]])
        for r in range(2):
            S.dma("pool", mt[p][:, r * 512:(r + 1) * 512], MG, reads=[b_MG, b_idxm], writes=[b_mt[p]],
                  indirect={"in_offset": bass.IndirectOffsetOnAxis(ap=idxm[:, ti, r:r + 1], axis=0)})
        cmap = [(0, 0), (0, 1), (1, 0), (1, 1), (0, 2), (0, 3), (1, 2), (1, 3)]
        emit_transpose(cx, lambda j, p=p: mt[p][:, cmap[j][0] * 512 + cmap[j][1] * 128: cmap[j][0] * 512 + (cmap[j][1] + 1) * 128],
                       b_mt[p], 8, ident_bf[0], ident_bf[1], mT[p][:], b_mT[p])
        for half in range(2):
            pc, bc = cx.PB[half], cx.PBb[half]
            for j in range(8):
                S.op("pe", lambda e, j=j, p=p, pc=pc, half=half: e.matmul(pc[:], lhsT=mT[p][:, j, :], rhs=wo_s[:, j, half * 512:(half + 1) * 512],
                                                                          start=(j == 0), stop=(j == 7)),
                     reads=[b_mT[p], b_wo], writes=[bc])
            sl = slice(half * 512, (half + 1) * 512)
            S.op("dve", lambda e, pc=pc, w=w, sl=sl: e.tensor_tensor(out=ytmp[:, sl], in0=pc[:], in1=mods[:, w, 0, sl], op=ALU.mult),
                 reads=[bc, b_mods], writes=[b_ytmp])
            S.op("dve", lambda e, p=p, sl=sl: e.tensor_tensor(out=ht[p][:, sl], in0=ht[p][:, sl], in1=ytmp[:, sl], op=ALU.add),
                 reads=[b_ytmp, b_ht[p]], writes=[b_ht[p]])
        S.dma("sp", H1[ti], ht[p][:], reads=[b_ht[p]], writes=[b_H1[ti]])
        emit_adaln(cx, nb, ht[p][:], b_ht[p], mods[:, w, 2, :], mods[:, w, 1, :], b_mods, vt[p][:], b_vt[p])
        S.dma("sp", Vs[ti], vt[p][:], reads=[b_vt[p]], writes=[b_Vs[ti]])
        emit_transpose(cx, lambda j, p=p: vt[p][:, j * 128:(j + 1) * 128], b_vt[p], 8, ident_bf[0], ident_bf[1], vT[p][:], b_vT[p])
        moe.route_tile(ti, lambda j, p=p: vT[p][:, j, :], b_vT[p])
    cx.phase_end()

    cx.phase_begin("L2_C")
    moe.plan()
    vl = [cx.sb("vl%d" % i, [128, D], BF16) for i in range(2)]; b_vl = [Buf(), Buf()]
    for ti in range(NT2):
        p = ti % 2
        S.dma("sp", vl[p][:], Vs[ti], reads=[b_Vs[ti]], writes=[b_vl[p]])
        moe.dispatch_tile(ti, vl[p][:], b_vl[p], do_scatter=(debug != "plan"))
    if debug == "plan":
        dbg_dest = cx.dout("dbg_dest", [128, NT2 * 2], I32)
        dbg_idxw = cx.dout("dbg_idxw", [128, 128], I32)
        dbg_gate = cx.dout("dbg_gate", [128, NT2 * 2])
        dbg_oh = cx.dout("dbg_oh", [128, NT2 * 64])
        b_dbg = Buf()
        S.dma("sp", dbg_dest, moe.dest[:].rearrange("p t k -> p (t k)"), reads=moe.b_dest, writes=[b_dbg])
        S.dma("sp", dbg_idxw, moe.idxw[:], reads=[moe.b_idxw], writes=[b_dbg])
        S.dma("sp", dbg_gate, moe.gate[:].rearrange("p t k -> p (t k)"), reads=moe.b_gate, writes=[b_dbg])
        S.dma("sp", dbg_oh, moe.OH[:].rearrange("p t k e -> p (t k e)"), reads=moe.b_OH, writes=[b_dbg])
        cx.phase_end()
        return None
    moe.experts()
    cx.phase_end()

    cx.phase_begin("L2_D1")
    nb = NormBufs(cx, "d")
    h1t = [cx.sb("h1t%d" % i, [128, D]) for i in range(2)]; b_h1t = [Buf(), Buf()]
    y0 = [cx.sb("y0_%d" % i, [128, D]) for i in range(2)]; b_y0 = [Buf(), Buf()]
    y1 = [cx.sb("y1_%d" % i, [128, D]) for i in range(2)]; b_y1 = [Buf(), Buf()]
    ut = [cx.sb("ut%d" % i, [128, D], BF16) for i in range(2)]; b_ut = [Buf(), Buf()]
    uTt = [cx.sb("uTt%d" % i, [128, 8, 128], BF16) for i in range(2)]; b_uTt = [Buf(), Buf()]
    b_hout = Buf()
    for ti in range(NT2):
        p = ti % 2
        w = 1 if ti == 0 else 0
        S.dma("sp", h1t[p][:], H1[ti], reads=[b_H1[ti]], writes=[b_h1t[p]])
        moe.gather_tile(ti, y0[p][:], y1[p][:], b_y0[p], b_y1[p])
        S.op("dve", lambda e, p=p, ti=ti: e.tensor_scalar(out=y0[p][:], in0=y0[p][:], scalar1=moe.gate[:, ti, 0:1], scalar2=None, op0=ALU.mult),
             reads=[b_y0[p], moe.b_gate[ti]], writes=[b_y0[p]])
        S.op("dve", lambda e, p=p, ti=ti: e.scalar_tensor_tensor(out=y0[p][:], in0=y1[p][:], scalar=moe.gate[:, ti, 1:2], in1=y0[p][:],
                                                                op0=ALU.mult, op1=ALU.add),
             reads=[b_y0[p], b_y1[p], moe.b_gate[ti]], writes=[b_y0[p]])
        S.op("dve", lambda e, p=p, w=w: e.tensor_tensor(out=y0[p][:], in0=y0[p][:], in1=mods[:, w, 3, :], op=ALU.mult),
             reads=[b_y0[p], b_mods], writes=[b_y0[p]])
        S.op("dve", lambda e, p=p: e.tensor_tensor(out=h1t[p][:], in0=h1t[p][:], in1=y0[p][:], op=ALU.add),
             reads=[b_y0[p], b_h1t[p]], writes=[b_h1t[p]])
        S.dma("sp", hout[ti * 128:(ti + 1) * 128, :], h1t[p][:], reads=[b_h1t[p]], writes=[b_hout])
        emit_adaln(cx, nb, h1t[p][:], b_h1t[p], mods[:, w, 5, :], mods[:, w, 4, :], b_mods, ut[p][:], b_ut[p])
        emit_transpose(cx, lambda j, p=p: ut[p][:, j * 128:(j + 1) * 128], b_ut[p], 8, ident_bf[0], ident_bf[1], uTt[p][:], b_uTt[p])
        S.dma("sp", UT[ti], uTt[p][:], reads=[b_uTt[p]], writes=[b_UT[ti]])
    cx.phase_end()

    cx.phase_begin("L2_D2")
    Wq_s = cx.sb("Wq_s", [128, 8, 2048], BF16); Wk_s = cx.sb("Wk_s", [128, 8, 2048], BF16); Wv_s = cx.sb("Wv_s", [128, 8, D], BF16)
    b_Wq = Buf(); b_Wk = Buf(); b_Wv = Buf()
    for j in range(8):
        S.dma("pool", Wq_s[:, j, :], Wq[j * 128:(j + 1) * 128, :], writes=[b_Wq])
        S.dma("pool", Wk_s[:, j, :], Wk[j * 128:(j + 1) * 128, :], writes=[b_Wk])
        S.dma("pool", Wv_s[:, j, :], Wv[j * 128:(j + 1) * 128, :], writes=[b_Wv])
    uTg = [cx.sb("uTg%d" % i, [128, 4, 8, 128], BF16) for i in range(2)]; b_uTg = [Buf(), Buf()]
    rc = [cx.sb("rc%d" % i, [128, 512]) for i in range(2)]; rsn = [cx.sb("rsn%d" % i, [128, 512]) for i in range(2)]; b_rope = [Buf(), Buf()]
    t1 = cx.sb("t1", [128, 512]); t2 = cx.sb("t2", [128, 512]); b_t1 = Buf(); b_t2 = Buf()
    qkst = [cx.sb("qkst%d" % i, [128, 512], BF16) for i in range(4)]; b_qkst = [Buf() for _ in range(4)]
    vst = [cx.sb("vst%d" % i, [128, D], BF16) for i in range(2)]; b_vst = [Buf(), Buf()]
    b_QT = Buf(); b_KT = Buf(); b_Vo = Buf()
    si = 0; vi = 0; fi = 0
    for gi, tiles in enumerate(GROUPS2):
        p = gi % 2
        n = len(tiles) * 128
        t0 = tiles[0]
        S.dma("sp", uTg[p][:, 0:len(tiles)], UT[t0:t0 + len(tiles)].rearrange("c p j t -> p c j t"),
              reads=b_UT[t0:t0 + len(tiles)], writes=[b_uTg[p]])
        S.dma("sp", rc[p][:, 0:n], rc2_d[:, t0 * 128:t0 * 128 + n], writes=[b_rope[p]])
        S.dma("sp", rsn[p][:, 0:n], rs2_d[:, t0 * 128:t0 * 128 + n], writes=[b_rope[p]])
        for ci, ti in enumerate(tiles):
            vq = vi % 2; vi += 1
            for half in range(2):
                pc, bc = cx.PB[half], cx.PBb[half]
                for j in range(8):
                    S.op("pe", lambda e, j=j, ci=ci, pc=pc, half=half: e.matmul(pc[:], lhsT=uTg[p][:, ci, j, :], rhs=Wv_s[:, j, half * 512:(half + 1) * 512],
                                                                                start=(j == 0), stop=(j == 7)),
                         reads=[b_uTg[p], b_Wv], writes=[bc])
                S.op("act", lambda e, vq=vq, pc=pc, half=half: e.activation(out=vst[vq][:, half * 512:(half + 1) * 512], in_=pc[:], func=AF.Copy),
                     reads=[bc], writes=[b_vst[vq]])
            S.dma("sp", Vo[ti * 128:(ti + 1) * 128, :], vst[vq][:], reads=[b_vst[vq]], writes=[b_Vo])
        for qk in range(2):
            if qk == 0 and gi == 0:
                continue
            Ws, bW = (Wq_s, b_Wq) if qk == 0 else (Wk_s, b_Wk)
            sc_ = 0.125 if qk == 0 else 1.0
            for h in range(8):
                pa = 2 + fi % 4; fi += 1
                pb_ = 2 + fi % 4; fi += 1
                for (pp, cb) in [(pa, h), (pb_, 8 + h)]:
                    for j in range(8):
                        S.op("pe", lambda e, j=j, pp=pp, cb=cb: e.matmul(cx.PB[pp][:, 0:n], lhsT=Ws[:, j, cb * 128:(cb + 1) * 128],
                                                                         rhs=uTg[p][:, 0:len(tiles), j, :],
                                                                         start=(j == 0), stop=(j == 7)),
                             reads=[bW, b_uTg[p]], writes=[cx.PBb[pp]])
                S.op("dve", lambda e, pa=pa: e.scalar_tensor_tensor(out=t1[:, 0:n], in0=cx.PB[pa][:, 0:n], scalar=sc_, in1=rc[p][:, 0:n],
                                                                    op0=ALU.mult, op1=ALU.mult), reads=[cx.PBb[pa], b_rope[p]], writes=[b_t1])
                S.op("dve", lambda e, pb_=pb_: e.scalar_tensor_tensor(out=t2[:, 0:n], in0=cx.PB[pb_][:, 0:n], scalar=sc_, in1=rsn[p][:, 0:n],
                                                                      op0=ALU.mult, op1=ALU.mult), reads=[cx.PBb[pb_], b_rope[p]], writes=[b_t2])
                sq = si % 4; si += 1
                S.op("dve", lambda e, sq=sq: e.tensor_tensor(out=qkst[sq][:, 0:n], in0=t1[:, 0:n], in1=t2[:, 0:n], op=ALU.add),
                     reads=[b_t1, b_t2], writes=[b_qkst[sq]])
                if qk == 0:
                    q0 = (t0 - 1) * 128
                    S.dma("sp", QT[h, :, q0:q0 + n], qkst[sq][:, 0:n], reads=[b_qkst[sq]], writes=[b_QT])
                else:
                    S.dma("sp", KT[h, :, t0 * 128:t0 * 128 + n], qkst[sq][:, 0:n], reads=[b_qkst[sq]], writes=[b_KT])
    cx.phase_end()
    cx.phase_end()
    return b_hout, b_QT, b_KT, b_Vo


def l2_inputs(I, b, half, merged_full_b, consts):
    d = dict(consts)
    cs = slice(half * 128, (half + 1) * 128)
    ls = slice(half * 4096, (half + 1) * 4096)
    d["xin"] = np.ascontiguousarray(np.concatenate([I["ctx"][b][cs], I["x"][b][ls]], 0))
    if merged_full_b is not None:
        d["mergedIn"] = np.ascontiguousarray(np.concatenate([merged_full_b[0:256][cs], merged_full_b[256:][ls]], 0))
    d["cT"] = np.ascontiguousarray(I["c"][b].reshape(8, 128).T)
    d["cctxT"] = np.ascontiguousarray(I["c_ctx"].reshape(8, 128).T)
    d["wmod"] = np.ascontiguousarray(np.concatenate([I["w_mod"][0][:, 2048:6144], I["w_mod"][1][:, 0:2048]], 1))
    d["bmod"] = np.ascontiguousarray(np.concatenate([I["b_mod"][0][2048:6144], I["b_mod"][1][0:2048]])[None, :])
    d["g2_0"] = np.ascontiguousarray(I["norm2_g"][0][None, :])
    d["g1_1"] = np.ascontiguousarray(I["norm1_g"][1][None, :])
    d["w_out"] = I["w_out_even"][0]
    d["Wr"] = np.ascontiguousarray(np.concatenate([I["router_g_w"][0], I["router_e_w"][0]], 1))
    d["br"] = np.ascontiguousarray(np.concatenate([I["router_g_b"][0], I["router_e_b"][0]])[None, :])
    d["w1"] = I["w1"][0]; d["w3"] = I["w3"][0]; d["w2"] = I["w2"][0]
    W = I["w_in_odd"][0]
    perm = np.arange(64)
    perm = np.concatenate([perm[16:32], perm[0:16], perm[48:64], perm[32:48]])
    def swp(M):
        return M.reshape(D, 16, 64)[:, :, perm].reshape(D, 1024)
    d["Wq"] = np.ascontiguousarray(np.concatenate([W[:, 0:1024], swp(W[:, 0:1024])], 1))
    d["Wk"] = np.ascontiguousarray(np.concatenate([W[:, 1024:2048], swp(W[:, 1024:2048])], 1))
    d["Wv"] = np.ascontiguousarray(W[:, 2048:3072])
    return d


NT3 = 32
NK = 66
LAM_INIT = 0.8 - 0.6 * math.exp(-0.3 * 1)


def host_consts_l3():
    c = common_consts()
    c["ones_bf"] = np.ones((128, 128), np.float32).astype(ml_dtypes.bfloat16)
    return c


VCH = [(0, 1024), (1024, 1024), (2048, 1024), (3072, 1024), (4096, 128)]


def build_l3(cx, io, debug=None):
    nc = cx.nc
    S = cx.S
    cx.phase_begin("L3")
    QT = io["QTs"]; b_QTs = io["b_QTs"]
    KTg = io["KTg"]; b_KTg = io["b_KTg"]
    VG = io["VG"]; b_VG = io["b_VG"]
    H2 = io["H2"]; b_H2 = io["b_H2"]
    cT = io["cT"]
    wmod = io["wmod"]; bmod = io["bmod"]
    g2 = io["g2"]; gfin = io["gfin"]
    dalam = io["dalam"]; dagT = io["dagT"]
    w_out = io["w_out"]
    Wr = io["Wr"]; br = io["br"]
    w1 = io["w1"]; w3 = io["w3"]; w2 = io["w2"]
    out = io["out"]
    H3 = cx.dscr("H3", [NT3, 128, D]); b_H3 = [Buf() for _ in range(NT3)]
    Vs = cx.dscr("Vs3", [NT3, 128, D], BF16); b_Vs = [Buf() for _ in range(NT3)]

    def lc(name, shape, dt=F32):
        t = cx.sb(name + "_3s", shape, dt); b = Buf()
        S.dma("sp", t[:], io[name], writes=[b])
        return t, b
    ident_bf = lc("ident_bf", [128, 128], BF16)
    ident_f = lc("ident_f", [128, 128])
    ones_f = lc("ones_f", [128, 128])
    ones_bf = lc("ones_bf", [128, 128], BF16)
    slt = lc("slt", [128, 128])
    blkstart = lc("blkstart", [128, 1])
    pcol = lc("pcol", [128, 1])
    thr = lc("thr", [128, 34])
    consts = {"thr": thr, "ident_bf": ident_bf, "ident_f": ident_f, "ones_f": ones_f, "slt": slt, "blkstart": blkstart, "pcol": pcol}
    moe = io["moe"]
    moe.c = consts
    moe.alloc_persistent()

    mods = cx.sb("mods", [128, 4, D]); b_mods = Buf()
    gfin_s, b_gfin = cx.load_bcast("gfin3", D, gfin)
    cx.phase_begin("L3_mod")
    scb, b_scb = emit_silu_bcast(cx, [cT], ones_f[0], ones_f[1])
    bmod_s, b_bmod = cx.load_bcast("bmod3", 4096, bmod)
    g2_s, b_g2 = cx.load_bcast("g2_3", D, g2)
    wm_s = [cx.sb("wm_s%d" % i, [128, 8, 512]) for i in range(2)]; b_wm = [Buf(), Buf()]
    emit_mod(cx, scb, b_scb, 1, wmod, bmod_s, b_bmod, 4096,
             lambda w, cc: mods[:, cc // 2, (cc % 2) * 512:(cc % 2 + 1) * 512], b_mods, wm_s, b_wm)
    S.op("dve", lambda e: e.scalar_tensor_tensor(out=mods[:, 2, :], in0=mods[:, 2, :], scalar=1.0, in1=g2_s[:], op0=ALU.add, op1=ALU.mult),
         reads=[b_mods, b_g2], writes=[b_mods])
    cx.phase_end()

    cx.phase_begin("L3_attnouter")
    oT_all = cx.sb("oT_all", [128, 8, NT3 * 128], BF16); b_oT = [Buf() for _ in range(8)]
    lamw = cx.sb("lamw", [128, 256]); b_lamw = Buf()
    S.dma("sp", lamw[:], dalam.partition_broadcast(128), writes=[b_lamw])
    lamt = cx.sb("lamt", [128, 8]); b_lamt = Buf()
    prod = cx.sb("lprod", [128, 128]); b_prod = Buf()
    S.op("dve", lambda e: e.tensor_tensor(out=prod[:, 0:64], in0=lamw[:, 0:64], in1=lamw[:, 64:128], op=ALU.mult), reads=[b_lamw], writes=[b_prod])
    S.op("dve", lambda e: e.tensor_tensor(out=prod[:, 64:128], in0=lamw[:, 128:192], in1=lamw[:, 192:256], op=ALU.mult), reads=[b_lamw, b_prod], writes=[b_prod])
    S.op("dve", lambda e: e.tensor_reduce(out=lamt[:, 0:1], in_=prod[:, 0:64], axis=AX.X, op=ALU.add), reads=[b_prod], writes=[b_lamt])
    S.op("dve", lambda e: e.tensor_reduce(out=lamt[:, 1:2], in_=prod[:, 64:128], axis=AX.X, op=ALU.add), reads=[b_prod, b_lamt], writes=[b_lamt])
    S.op("act", lambda e: e.activation(out=lamt[:, 2:4], in_=lamt[:, 0:2], func=AF.Exp), reads=[b_lamt], writes=[b_lamt])
    S.op("dve", lambda e: e.tensor_tensor(out=lamt[:, 4:5], in0=lamt[:, 3:4], in1=lamt[:, 2:3], op=ALU.subtract), reads=[b_lamt], writes=[b_lamt])
    S.op("dve", lambda e: e.tensor_scalar(out=lamt[:, 4:5], in0=lamt[:, 4:5], scalar1=-LAM_INIT, scalar2=None, op0=ALU.add), reads=[b_lamt], writes=[b_lamt])
    gs = cx.sb("gs", [128, 8]); b_gs = Buf()
    S.dma("sp", gs[:], dagT, writes=[b_gs])
    S.op("dve", lambda e: e.tensor_scalar(out=gs[:], in0=gs[:], scalar1=1.0 - LAM_INIT, scalar2=None, op0=ALU.mult), reads=[b_gs], writes=[b_gs])

    cx.phase_begin("L3_attn")
    KTh = [cx.sb("KTh%d" % i, [128, NK * 128], BF16) for i in range(2)]; b_KTh = [Buf(), Buf()]
    Vh = [cx.sb("Vh%d" % i, [128, NK, 128], BF16) for i in range(2)]; b_Vh = [Buf(), Buf()]
    QTh1 = cx.sb("QTh", [128, 2, NT3 * 128], BF16); b_QTh1 = Buf()
    QTh = [QTh1, QTh1]; b_QTh = [b_QTh1, b_QTh1]
    S.op("dve", lambda e: e.memset(QTh1[:], 0.0), writes=[b_QTh1])
    Pt = [cx.sb("Pt%d" % i, [128, 512], BF16) for i in range(6)]; b_Pt = [Buf() for _ in range(6)]
    rec = cx.sb("rec", [128, 512]); b_rec = Buf()
    on1 = cx.sb("on1", [128, 512]); b_on1 = Buf()
    ot = cx.sb("ot", [128, 512]); b_ot = Buf()
    sq = cx.sb("sqt", [128, 512]); b_sq = Buf()
    rstd = cx.sb("rstdA", [128, 512]); b_rstdA = Buf()
    pi_ = 0
    si_ = 0
    nheads = 8 if debug != "attn1" else 1
    nqt = 8 if debug != "attn1" else 1
    for h in range(nheads):
        p = h % 2
        for r in range(2):
            S.dma("sp", KTh[p][:, r * 4224:(r + 1) * 4224], KTg[h, r * 128:(r + 1) * 128, :], reads=[b_KTg], writes=[b_KTh[p]])
            for (r0, n) in VCH:
                vr = 2 * r0 + r * n
                S.dma("sp", Vh[p][:, r * 33 + r0 // 128: r * 33 + (r0 + n) // 128, :],
                      VG[vr:vr + n, h * 128:(h + 1) * 128].rearrange("(k p) d -> p k d", p=128), reads=[b_VG], writes=[b_Vh[p]])
        for m_ in range(2):
            S.dma("sp", QTh[p][m_ * 64:(m_ + 1) * 64, m_, :], QT[h, m_ * 64:(m_ + 1) * 64, :], reads=[b_QTs], writes=[b_QTh[p]])
        steps = [(qt, m, kt) for qt in range(nqt) for m in range(2) for kt in range(NK)]
        LOOK = 2
        sbank = {}

        def issue_S(idx):
            nonlocal si_
            qt_, m_, kt_ = steps[idx]
            pst = si_ % 3; si_ += 1
            sbank[idx] = pst
            ms_ = slice(m_ * 64, (m_ + 1) * 64)
            qs_ = slice(qt_ * 512, (qt_ + 1) * 512)
            S.op("pe", lambda e: e.matmul(cx.PB[pst][:], lhsT=KTh[p][:, kt_ * 128:(kt_ + 1) * 128], rhs=QTh[p][:, m_, qs_], start=True, stop=True),
                 reads=[b_KTh[p], b_QTh[p]], writes=[cx.PBb[pst]])

        for i0 in range(min(LOOK, len(steps))):
            issue_S(i0)
        for idx, (qt, m, kt) in enumerate(steps):
            qs = slice(qt * 512, (qt + 1) * 512)
            gpar = (qt * 2 + m) % 2
            pO, bO = cx.PB[3 + 2 * gpar], cx.PBb[3 + 2 * gpar]
            pS, bS = cx.PB[4 + 2 * gpar], cx.PBb[4 + 2 * gpar]
            pst = sbank.pop(idx)
            pq = pi_ % 6; pi_ += 1
            S.op("act", lambda e, pq=pq, pst=pst: e.activation(out=Pt[pq][:], in_=cx.PB[pst][:], func=AF.Exp),
                 reads=[cx.PBb[pst]], writes=[b_Pt[pq]])
            if idx + LOOK < len(steps):
                issue_S(idx + LOOK)
            S.op("pe", lambda e, pq=pq, kt=kt: e.matmul(pO[:], lhsT=Vh[p][:, kt, :], rhs=Pt[pq][:], start=(kt == 0), stop=(kt == NK - 1)),
                 reads=[b_Vh[p], b_Pt[pq]], writes=[bO])
            S.op("pe", lambda e, pq=pq, kt=kt: e.matmul(pS[:], lhsT=ones_bf[0][:], rhs=Pt[pq][:], start=(kt == 0), stop=(kt == NK - 1)),
                 reads=[ones_bf[1], b_Pt[pq]], writes=[bS])
            if kt != NK - 1:
                continue
            S.op("dve", lambda e: e.reciprocal(out=rec[:], in_=pS[:]), reads=[bS], writes=[b_rec])
            if m == 0:
                S.op("dve", lambda e: e.tensor_tensor(out=on1[:], in0=pO[:], in1=rec[:], op=ALU.mult), reads=[bO, b_rec], writes=[b_on1])
                continue
            S.op("dve", lambda e: e.tensor_tensor(out=ot[:], in0=pO[:], in1=rec[:], op=ALU.mult), reads=[bO, b_rec], writes=[b_ot])
            S.op("dve", lambda e: e.scalar_tensor_tensor(out=ot[:], in0=ot[:], scalar=lamt[:, 4:5], in1=on1[:], op0=ALU.mult, op1=ALU.add),
                 reads=[b_ot, b_on1, b_lamt], writes=[b_ot])
            S.op("act", lambda e: e.activation(out=sq[:], in_=ot[:], func=AF.Square), reads=[b_ot], writes=[b_sq])
            pN, bN = cx.PT[:].bitcast(F32), cx.b_PT
            S.op("pe", lambda e: e.matmul(pN, lhsT=ones_f[0][:], rhs=sq[:], start=True, stop=True), reads=[ones_f[1], b_sq], writes=[bN])
            S.op("dve", lambda e: e.tensor_scalar(out=rstd[:], in0=pN, scalar1=1.0 / 128, scalar2=EPS, op0=ALU.mult, op1=ALU.add),
                 reads=[bN], writes=[b_rstdA])
            S.op("act", lambda e: e.activation(out=rstd[:], in_=rstd[:], func=AF.Sqrt), reads=[b_rstdA], writes=[b_rstdA])
            S.op("dve", lambda e: e.reciprocal(out=rstd[:], in_=rstd[:]), reads=[b_rstdA], writes=[b_rstdA])
            S.op("dve", lambda e, h=h, qs=qs: e.scalar_tensor_tensor(out=oT_all[:, h, qs], in0=ot[:], scalar=gs[:, h:h + 1], in1=rstd[:],
                                                                    op0=ALU.mult, op1=ALU.mult),
                 reads=[b_ot, b_gs, b_rstdA], writes=[b_oT[h]])

    if debug == "attn1":
        dbg = cx.dout("dbg_oT", [128, 512], BF16); b_dbg = Buf()
        S.dma("sp", dbg, oT_all[:, 0, 0:512], reads=b_oT, writes=[b_dbg])
        cx.phase_end()
        S.finish([b_dbg], "sp")
        return nc

    cx.phase_end()
    cx.phase_begin("L3_wout")
    wo_s = cx.sb("wo_s", [128, 8, D], BF16); b_wo = Buf()
    for j in range(8):
        S.dma("pool", wo_s[:, j, :], w_out[j * 128:(j + 1) * 128, :], writes=[b_wo])
    nb = NormBufs(cx)
    ht = [cx.sb("ht%d" % i, [128, D]) for i in range(2)]; b_ht = [Buf(), Buf()]
    vt = [cx.sb("vt%d" % i, [128, D], BF16) for i in range(2)]; b_vt = [Buf(), Buf()]
    vT = [cx.sb("vT%d" % i, [128, 8, 128], BF16) for i in range(2)]; b_vT = [Buf(), Buf()]
    ytmp = cx.sb("ytmp", [128, D]); b_ytmp = Buf()
    for ti in range(NT3):
        p = ti % 2
        S.dma("sp", ht[p][:], H2[(ti + 1) * 128:(ti + 2) * 128, :], reads=[b_H2], writes=[b_ht[p]])
        for half in range(2):
            pc, bc = cx.PB[half], cx.PBb[half]
            for j in range(8):
                S.op("pe", lambda e, j=j, pc=pc, half=half, ti=ti: e.matmul(pc[:], lhsT=oT_all[:, j, ti * 128:(ti + 1) * 128],
                                                                            rhs=wo_s[:, j, half * 512:(half + 1) * 512], start=(j == 0), stop=(j == 7)),
                     reads=b_oT + [b_wo], writes=[bc])
            sl = slice(half * 512, (half + 1) * 512)
            S.op("dve", lambda e, pc=pc, sl=sl: e.tensor_tensor(out=ytmp[:, sl], in0=pc[:], in1=mods[:, 0, sl], op=ALU.mult),
                 reads=[bc, b_mods], writes=[b_ytmp])
            S.op("dve", lambda e, p=p, sl=sl: e.tensor_tensor(out=ht[p][:, sl], in0=ht[p][:, sl], in1=ytmp[:, sl], op=ALU.add),
                 reads=[b_ytmp, b_ht[p]], writes=[b_ht[p]])
        S.dma("sp", H3[ti], ht[p][:], reads=[b_ht[p]], writes=[b_H3[ti]])
        emit_adaln(cx, nb, ht[p][:], b_ht[p], mods[:, 2, :], mods[:, 1, :], b_mods, vt[p][:], b_vt[p])
        S.dma("sp", Vs[ti], vt[p][:], reads=[b_vt[p]], writes=[b_Vs[ti]])
        emit_transpose(cx, lambda j, p=p: vt[p][:, j * 128:(j + 1) * 128], b_vt[p], 8, ident_bf[0], ident_bf[1], vT[p][:], b_vT[p])
        moe.route_tile(ti, lambda j, p=p: vT[p][:, j, :], b_vT[p])
    cx.phase_end()
    cx.phase_end()

    cx.phase_begin("L3_moe")
    moe.plan()
    vl = [cx.sb("vl%d" % i, [128, D], BF16) for i in range(2)]; b_vl = [Buf(), Buf()]
    for ti in range(NT3):
        p = ti % 2
        S.dma("sp", vl[p][:], Vs[ti], reads=[b_Vs[ti]], writes=[b_vl[p]])
        moe.dispatch_tile(ti, vl[p][:], b_vl[p])
    moe.experts()
    cx.phase_end()

    cx.phase_begin("L3_fin")
    nb = NormBufs(cx, "d")
    h1t = [cx.sb("h1t%d" % i, [128, D]) for i in range(2)]; b_h1t = [Buf(), Buf()]
    y0 = [cx.sb("y0_%d" % i, [128, D]) for i in range(2)]; b_y0 = [Buf(), Buf()]
    y1 = [cx.sb("y1_%d" % i, [128, D]) for i in range(2)]; b_y1 = [Buf(), Buf()]
    ot_ = [cx.sb("ofin%d" % i, [128, D]) for i in range(2)]; b_ofin = [Buf(), Buf()]
    b_out = Buf()
    for ti in range(NT3):
        p = ti % 2
        S.dma("sp", h1t[p][:], H3[ti], reads=[b_H3[ti]], writes=[b_h1t[p]])
        moe.gather_tile(ti, y0[p][:], y1[p][:], b_y0[p], b_y1[p])
        S.op("dve", lambda e, p=p, ti=ti: e.tensor_scalar(out=y0[p][:], in0=y0[p][:], scalar1=moe.gate[:, ti, 0:1], scalar2=None, op0=ALU.mult),
             reads=[b_y0[p], moe.b_gate[ti]], writes=[b_y0[p]])
        S.op("dve", lambda e, p=p, ti=ti: e.scalar_tensor_tensor(out=y0[p][:], in0=y1[p][:], scalar=moe.gate[:, ti, 1:2], in1=y0[p][:],
                                                                op0=ALU.mult, op1=ALU.add),
             reads=[b_y0[p], b_y1[p], moe.b_gate[ti]], writes=[b_y0[p]])
        S.op("dve", lambda e, p=p: e.tensor_tensor(out=y0[p][:], in0=y0[p][:], in1=mods[:, 3, :], op=ALU.mult),
             reads=[b_y0[p], b_mods], writes=[b_y0[p]])
        S.op("dve", lambda e, p=p: e.tensor_tensor(out=h1t[p][:], in0=h1t[p][:], in1=y0[p][:], op=ALU.add),
             reads=[b_y0[p], b_h1t[p]], writes=[b_h1t[p]])
        emit_adaln(cx, nb, h1t[p][:], b_h1t[p], gfin_s[:], None, b_gfin, ot_[p][:], b_ofin[p])
        S.dma("sp", out[ti * 128:(ti + 1) * 128, :], ot_[p][:], reads=[b_ofin[p]], writes=[b_out])
    cx.phase_end()
    cx.phase_end()
    return b_out


def l3_inputs(I, b, half, l2res_pair, consts):
    d = dict(consts)
    if l2res_pair is not None:
        d["QT"] = l2res_pair[half]["QT"]
        d["KT"] = np.ascontiguousarray(np.concatenate([l2res_pair[0]["KT"], l2res_pair[1]["KT"]], 2))
        d["Vf"] = np.ascontiguousarray(np.concatenate([l2res_pair[0]["Vo"], l2res_pair[1]["Vo"]], 0))
        d["hin"] = np.ascontiguousarray(l2res_pair[half]["hout"][128:])
    d["cT"] = np.ascontiguousarray(I["c"][b].reshape(8, 128).T)
    d["wmod"] = np.ascontiguousarray(I["w_mod"][1][:, 2048:6144])
    d["bmod"] = np.ascontiguousarray(I["b_mod"][1][None, 2048:6144])
    d["g2"] = np.ascontiguousarray(I["norm2_g"][1][None, :])
    d["gfin"] = np.ascontiguousarray(I["final_norm_g"][None, :])
    d["dalam"] = np.ascontiguousarray(I["da_lambda"][0].reshape(1, 256))
    d["dagT"] = np.ascontiguousarray(I["da_norm_g"][0].T)
    d["w_out"] = I["w_out_odd"][0]
    d["Wr"] = np.ascontiguousarray(np.concatenate([I["router_g_w"][1], I["router_e_w"][1]], 1))
    d["br"] = np.ascontiguousarray(np.concatenate([I["router_g_b"][1], I["router_e_b"][1]])[None, :])
    d["w1"] = I["w1"][1]; d["w3"] = I["w3"][1]; d["w2"] = I["w2"][1]
    return d


PAIRS = [[0, 1], [2, 3], [4, 5], [6, 7]]
MCH = [(0, 2048), (2048, 2048), (4096, 2048), (6144, 2048), (8192, 256)]

L1_SPECS = [("xin", [NT * 128, D], F32), ("cT", [128, 8], F32), ("cctxT", [128, 8], F32), ("wmod", [D, 2048], F32), ("bmod", [1, 2048], F32),
            ("g1", [1, D], F32), ("Wfm", [D, 1536], F32), ("Wtm", [D, 1032], F32), ("gbias", [1, 8], F32), ("rld", [1, 4], F32),
            ("mlg", [1, 256], F32), ("retg", [1, 256], F32), ("ident_bf", [128, 128], BF16), ("ident_f", [128, 128], F32),
            ("mask_f32", [128, 2, 128], F32), ("mask_bf", [128, 2, 128], BF16), ("ones_f", [128, 128], F32), ("poscol", [128, 4], F32),
            ("ropecos", [128, NT * 128], F32), ("ropesin", [128, NT * 128], F32)]
L2_SPECS = [("xin", [NT2 * 128, D], F32), ("idxm", [128, NT2, 2], I32), ("cT", [128, 8], F32), ("cctxT", [128, 8], F32),
            ("wmod", [D, 6144], F32), ("bmod", [1, 6144], F32), ("g2_0", [1, D], F32), ("g1_1", [1, D], F32), ("w_out", [D, D], F32),
            ("Wr", [D, 36], F32), ("br", [1, 36], F32), ("w1", [32, D, 512], F32), ("w3", [32, D, 512], F32), ("w2", [32, 512, D], F32),
            ("Wq", [D, 2048], F32), ("Wk", [D, 2048], F32), ("Wv", [D, D], F32), ("rc2", [128, NT2 * 128], F32), ("rs2", [128, NT2 * 128], F32),
            ("ident_bf", [128, 128], BF16), ("ident_f", [128, 128], F32), ("ones_f", [128, 128], F32), ("slt", [128, 128], F32),
            ("blkstart", [128, 1], F32), ("pcol", [128, 1], F32), ("thr", [128, 34], F32)]
L3_SPECS = [("cT", [128, 8], F32), ("wmod", [D, 4096], F32), ("bmod", [1, 4096], F32), ("g2", [1, D], F32), ("gfin", [1, D], F32),
            ("dalam", [1, 256], F32), ("dagT", [128, 8], F32), ("w_out", [D, D], F32), ("Wr", [D, 36], F32), ("br", [1, 36], F32),
            ("w1", [32, D, 512], F32), ("w3", [32, D, 512], F32), ("w2", [32, 512, D], F32),
            ("ident_bf", [128, 128], BF16), ("ident_f", [128, 128], F32), ("ones_f", [128, 128], F32), ("ones_bf", [128, 128], BF16),
            ("slt", [128, 128], F32), ("blkstart", [128, 1], F32), ("pcol", [128, 1], F32), ("thr", [128, 34], F32)]


def build_fused(nc):
    cx = Ctx(nc)
    S = cx.S
    io1 = {n: cx.din("l1_" + n, sh, dt) for (n, sh, dt) in L1_SPECS}
    io2 = {n: cx.din("l2_" + n, sh, dt) for (n, sh, dt) in L2_SPECS}
    io3 = {n: cx.din("l3_" + n, sh, dt) for (n, sh, dt) in L3_SPECS}
    out = cx.dout("out", [NT3 * 128, D])

    moeA = MoE(cx, NT2, io2["Wr"], io2["br"], io2["w1"], io2["w3"], io2["w2"], tag="A")
    moeB = MoE(cx, NT3, io3["Wr"], io3["br"], io3["w1"], io3["w3"], io3["w2"], tag="B")
    io1["after_weights"] = moeA.precast
    io2["moe"] = moeA
    io3["moe"] = moeB

    mergedX = cx.dscr("mergedX", [NT * 128, 512], BF16)
    io1["merged"] = mergedX
    b_merged = build_l1(cx, io1)

    MG = cx.dscr("MG", [2 * NT * 128, 512], BF16); b_MG = Buf()
    for (r0, n) in MCH:
        S.collective("AllGather", [mergedX[r0:r0 + n, :]], [MG[2 * r0:2 * r0 + 2 * n, :]], PAIRS, reads=[b_merged], writes=[b_MG])

    H2 = cx.dscr("H2", [NT2 * 128, D])
    QTs = cx.dscr("QTs", [8, 128, 4096], BF16)
    KTo = cx.dscr("KTo", [8, 128, NT2 * 128], BF16)
    Vown = cx.dscr("Vown", [NT2 * 128, D], BF16)
    io2.update({"MG": MG, "b_MG": b_MG, "H2": H2, "QTs": QTs, "KTo": KTo, "Vown": Vown})
    b_H2, b_QTs, b_KTo, b_Vown = build_l2(cx, io2)

    KTg = cx.dscr("KTg", [8, 256, NT2 * 128], BF16); b_KTg = Buf()
    VG = cx.dscr("VG", [2 * NT2 * 128, D], BF16); b_VG = Buf()
    for h in range(8):
        S.collective("AllGather", [KTo[h]], [KTg[h]], PAIRS, reads=[b_KTo], writes=[b_KTg])
    for (r0, n) in VCH:
        S.collective("AllGather", [Vown[r0:r0 + n, :]], [VG[2 * r0:2 * r0 + 2 * n, :]], PAIRS, reads=[b_Vown], writes=[b_VG])

    moeB.precast()
    io3.update({"QTs": QTs, "b_QTs": b_QTs, "KTg": KTg, "b_KTg": b_KTg, "VG": VG, "b_VG": b_VG, "H2": H2, "b_H2": b_H2, "out": out})
    b_out = build_l3(cx, io3)
    S.finish([b_out], "sp")
    print("fused instructions:", S.n_inst)
    return nc


def fused_inputs(I, core, c1, c2, c3):
    b, g = core // 2, core % 2
    d = {}
    for k, v in l1_inputs(I, b, g, c1).items():
        d["l1_" + k] = v
    d2 = l2_inputs(I, b, g, None, c2[g])
    for k, v in d2.items():
        d["l2_" + k] = v
    trow = np.concatenate([np.arange(g * 128, (g + 1) * 128), 256 + np.arange(g * 4096, (g + 1) * 4096)])
    r0 = np.minimum((trow // 2048) * 2048, 8192)
    n = np.where(r0 < 8192, 2048, 256)
    idx = np.stack([2 * r0 + r * n + (trow - r0) for r in range(2)], 1).astype(np.int32)
    d["l2_idxm"] = np.ascontiguousarray(idx.reshape(NT2, 128, 2).transpose(1, 0, 2))
    d3 = l3_inputs(I, b, g, None, c3)
    for k, v in d3.items():
        d["l3_" + k] = v
    return d


from concourse.bass_utils import run_bass_kernel_spmd

_NC_CACHE = {}


def kernel(**inputs):
    I = {k: np.asarray(v) for k, v in inputs.items()}
    cores = list(range(8))
    if "fused" not in _NC_CACHE:
        nc = bass.Bass("TRN2", target_bir_lowering=False)
        build_fused(nc)
        _NC_CACHE["fused"] = nc
    nc = _NC_CACHE["fused"]
    c1 = host_consts_l1(); c2 = [host_consts_l2(0), host_consts_l2(1)]; c3 = host_consts_l3()
    names = set(["l1_" + n for n, _, _ in L1_SPECS] + ["l2_" + n for n, _, _ in L2_SPECS] + ["l3_" + n for n, _, _ in L3_SPECS])
    in_maps = []
    for core in cores:
        m = fused_inputs(I, core, c1, c2, c3)
        in_maps.append({k: v for k, v in m.items() if k in names})
    res = run_bass_kernel_spmd(nc, in_maps, core_ids=cores).results
    out = np.empty((4, 8192, 1024), np.float32)
    for core in cores:
        b, half = core // 2, core % 2
        out[b, half * 4096:(half + 1) * 4096] = res[core]["out"]
    return out
```
